# Optimizing a Trainium2 kernel written in Bass

```python
import jax, jax.numpy as jnp
from jax import lax
import numpy as np

D_MODEL = 1024
BATCH = 16
SEQ = 2048
DEPTH = 1

N_MEM = 256
HEAD_DIM = 64
RWKV_HEADS = 6
RWKV_WIDTH = RWKV_HEADS * HEAD_DIM
DECAY_LORA = 32
ICLR_LORA = 32
GATE_LORA = 64
RWKV_IN = 3 * RWKV_WIDTH + GATE_LORA + 2 * DECAY_LORA + 2 * ICLR_LORA
ATT_HEADS = 6
ATT_WIDTH = ATT_HEADS * HEAD_DIM
DILATED_BRANCHES = ((128, 1), (512, 4), (2048, 16))
MEM_HEADS = 4
MEM_WIDTH = MEM_HEADS * HEAD_DIM
MIX_WIDTH = RWKV_WIDTH + ATT_WIDTH + MEM_WIDTH
IN_WIDTH = RWKV_IN + 3 * ATT_WIDTH + MEM_WIDTH
N_EXPERTS = 32
TOP_K = 4
D_EXPERT = D_MODEL
SWIGLU_LIMIT = 7.0
SWIGLU_ALPHA = 1.702
MOE_BLOCK = 128
LN_EPS = 1e-5
RWKV_GN_EPS = 64e-5
NEG_INF = -1e30
DEEPNORM_ALPHA = (2 * DEPTH) ** 0.25
DEEPNORM_BETA = (8 * DEPTH) ** -0.25

kernel_name = "hybrid_rwkv7_dilated_alibi_memxattn_moe_deepnorm"


def layer_norm(x, g, b):
    xf = x.astype(jnp.float32)
    mu = jnp.mean(xf, -1, keepdims=True)
    var = jnp.mean(jnp.square(xf - mu), -1, keepdims=True)
    return ((xf - mu) * lax.rsqrt(var + LN_EPS) * g + b).astype(x.dtype)


def rwkv7_bidirectional(p, mu, w0, w_up, a0, a_up, g_up, k_k, k_a, r_k, gn_g, gn_b):
    B, S, _ = p.shape
    H, N, C = RWKV_HEADS, HEAD_DIM, RWKV_WIDTH
    f32 = jnp.float32
    prev = jnp.pad(p[:, :-1], ((0, 0), (1, 0), (0, 0)))
    nxt = jnp.pad(p[:, 1:], ((0, 0), (0, 1), (0, 0)))
    p = (p + mu * (0.5 * (prev + nxt) - p)).astype(f32)
    r, k, v = p[..., :C], p[..., C:2 * C], p[..., 2 * C:3 * C]
    o = 3 * C
    g_lo = p[..., o:o + GATE_LORA]
    o += GATE_LORA
    w_lo = p[..., o:o + 2 * DECAY_LORA].reshape(B, S, 2, DECAY_LORA)
    o += 2 * DECAY_LORA
    a_lo = p[..., o:].reshape(B, S, 2, ICLR_LORA)
    w_logit = w0 + jnp.einsum('bsdr,drc->bsdc', jnp.tanh(w_lo), w_up)
    decay = jnp.exp(-jnp.exp(-jax.nn.softplus(-w_logit) - 0.5))
    a = jax.nn.sigmoid(a0 + jnp.einsum('bsdr,drc->bsdc', a_lo, a_up))
    g = jax.nn.sigmoid(g_lo) @ g_up
    kk = (k * k_k).reshape(B, S, H, N)
    kk = kk / jnp.maximum(jnp.sqrt(jnp.sum(kk * kk, -1, keepdims=True)), 1e-12)
    k_dir = (k[:, :, None, :] * (1.0 + (a - 1.0) * k_a)).reshape(B, S, 2, H, N)
    r_h = r.reshape(B, S, H, N)
    v_h = v.reshape(B, S, H, N)

    def both(t):
        return jnp.stack([t, t], axis=2)

    def time_major(t):
        t = jnp.stack([t[:, :, 0], jnp.flip(t[:, :, 1], axis=1)], axis=0)
        return jnp.transpose(t, (2, 0, 1, 3, 4))

    kk2 = both(kk)
    xs = (time_major(both(r_h)), time_major(decay.reshape(B, S, 2, H, N)), time_major(k_dir),
          time_major(both(v_h)), time_major(kk2), time_major(kk2 * a.reshape(B, S, 2, H, N)))

    def step(state, inp):
        r_t, w_t, k_t, v_t, kk_t, b_t = inp
        sa = jnp.einsum('dbhij,dbhj->dbhi', state, kk_t)
        state = state * w_t[..., None, :] - sa[..., :, None] * b_t[..., None, :] + v_t[..., :, None] * k_t[..., None, :]
        return state, jnp.einsum('dbhij,dbhj->dbhi', state, r_t)

    _, ys = lax.scan(step, jnp.zeros((2, B, H, N, N), f32), xs)
    y = jnp.transpose(ys[:, 0] + jnp.flip(ys[:, 1], axis=0), (1, 0, 2, 3))
    mean = jnp.mean(y, -1, keepdims=True)
    var = jnp.mean(jnp.square(y - mean), -1, keepdims=True)
    y = ((y - mean) * lax.rsqrt(var + RWKV_GN_EPS)).reshape(B, S, C) * gn_g + gn_b
    bonus = jnp.sum(r_h[:, :, None] * k_dir * r_k, axis=(2, 4))[..., None] * v_h
    return (y + bonus.reshape(B, S, C)) * g


def dilated_branch(q, k, v, slopes, window, dil):
    B, H, S, N = q.shape
    half = window // (2 * dil)
    L = S // dil
    nb = -(-L // half)
    pad = nb * half - L

    def by_residue(t):
        return jnp.transpose(t.reshape(B, H, L, dil, N), (0, 1, 3, 2, 4))

    qb = jnp.pad(by_residue(q), ((0, 0), (0, 0), (0, 0), (0, pad), (0, 0))).reshape(B, H, dil, nb, half, N)

    def windows(t):
        t = jnp.pad(by_residue(t), ((0, 0), (0, 0), (0, 0), (half, pad + half), (0, 0))).reshape(B, H, dil, nb + 2, half, N)
        return jnp.concatenate([t[:, :, :, :-2], t[:, :, :, 1:-1], t[:, :, :, 2:]], axis=4)

    kw, vw = windows(k), windows(v)
    s = jnp.einsum('bhrnqd,bhrnkd->bhrnqk', qb, kw).astype(jnp.float32)
    qi = jnp.arange(half)[:, None]
    kj = jnp.arange(3 * half)[None, :]
    delta = kj - half - qi
    lk = jnp.arange(nb)[:, None, None] * half - half + kj[None]
    valid = (jnp.abs(delta) <= half)[None] & (lk >= 0) & (lk < L)
    dist = (jnp.abs(delta) * dil).astype(jnp.float32)
    s = jnp.where(valid, s - slopes[None, :, None, None, None, None] * dist, NEG_INF)
    m = jnp.max(s, -1, keepdims=True)
    pr = jnp.exp(s - m)
    den = jnp.sum(pr, -1, keepdims=True)
    o = jnp.einsum('bhrnqk,bhrnkd->bhrnqd', pr, vw) / den
    lse = (m + jnp.log(den))[..., 0]

    def back(t):
        t = t.reshape((B, H, dil, nb * half) + t.shape[5:])[:, :, :, :L]
        t = jnp.moveaxis(t, 2, 3)
        return t.reshape((B, H, S) + t.shape[4:])

    return back(o), back(lse)


def dilated_mixture_attention(q, k, v):
    slopes = jnp.exp2(-8.0 * jnp.arange(1, ATT_HEADS + 1, dtype=jnp.float32) / ATT_HEADS)
    outs, lses = [], []
    for window, dil in DILATED_BRANCHES:
        o_b, lse_b = dilated_branch(q, k, v, slopes, window, dil)
        outs.append(o_b)
        lses.append(lse_b)
    wts = jax.nn.softmax(jnp.stack(lses), axis=0)
    return jnp.sum(wts[..., None] * jnp.stack(outs), axis=0)


def parallel_mixer(h, mem, w_in, mu, w0, w_up, a0, a_up, g_up, k_k, k_a, r_k, gn_g, gn_b, w_mem_kv, w_out):
    B, S, _ = h.shape
    f32 = jnp.float32
    p = h @ w_in
    y_rwkv = rwkv7_bidirectional(p[..., :RWKV_IN], mu, w0, w_up, a0, a_up, g_up, k_k, k_a, r_k, gn_g, gn_b)
    o = RWKV_IN

    def heads_first(t, n):
        return jnp.transpose(t.reshape(B, S, n, HEAD_DIM), (0, 2, 1, 3)).astype(f32)

    q = heads_first(p[..., o:o + ATT_WIDTH], ATT_HEADS) * HEAD_DIM ** -0.5
    k = heads_first(p[..., o + ATT_WIDTH:o + 2 * ATT_WIDTH], ATT_HEADS)
    v = heads_first(p[..., o + 2 * ATT_WIDTH:o + 3 * ATT_WIDTH], ATT_HEADS)
    y_att = jnp.transpose(dilated_mixture_attention(q, k, v), (0, 2, 1, 3)).reshape(B, S, ATT_WIDTH)
    o += 3 * ATT_WIDTH
    q_m = p[..., o:].reshape(B, S, MEM_HEADS, HEAD_DIM).astype(f32)
    kv_m = (mem @ w_mem_kv).reshape(B, N_MEM, 2, MEM_HEADS, HEAD_DIM).astype(f32)
    s_m = jnp.einsum('bshd,bmhd->bhsm', q_m, kv_m[:, :, 0]) * HEAD_DIM ** -0.5
    y_mem = jnp.einsum('bhsm,bmhd->bshd', jax.nn.softmax(s_m, axis=-1), kv_m[:, :, 1]).reshape(B, S, MEM_WIDTH)
    y = jnp.concatenate([y_rwkv, y_att, y_mem], axis=-1).astype(h.dtype)
    return y @ w_out


def moe(h, w_router, b_router, w_gate_up, b_gate_up, w_down, b_down):
    B, S, D = h.shape
    T = B * S
    n_assign = T * TOP_K
    xt = h.reshape(T, D)
    logits = (xt @ w_router).astype(jnp.float32) + b_router
    top_v, top_e = lax.top_k(logits, TOP_K)
    gates = jax.nn.softmax(top_v, axis=-1)
    flat_e = top_e.reshape(-1)
    order = jnp.argsort(flat_e)
    sorted_e = flat_e[order]
    counts = jnp.bincount(flat_e, length=N_EXPERTS)
    padded = (counts + MOE_BLOCK - 1) // MOE_BLOCK * MOE_BLOCK
    starts = jnp.cumsum(counts) - counts
    pends = jnp.cumsum(padded)
    pstarts = pends - padded
    dest_sorted = pstarts[sorted_e] + jnp.arange(n_assign) - starts[sorted_e]
    dest = jnp.zeros((n_assign,), jnp.int32).at[order].set(dest_sorted.astype(jnp.int32))
    n_rows = n_assign + N_EXPERTS * MOE_BLOCK
    n_blocks = n_rows // MOE_BLOCK
    row_token = jnp.full((n_rows,), T, jnp.int32).at[dest].set(jnp.arange(n_assign, dtype=jnp.int32) // TOP_K)
    x_rows = jnp.concatenate([xt, jnp.zeros((1, D), xt.dtype)], axis=0)[row_token].reshape(n_blocks, MOE_BLOCK, D)
    block_e = jnp.minimum(jnp.searchsorted(pends, jnp.arange(n_blocks) * MOE_BLOCK, side='right'), N_EXPERTS - 1)

    def expert_block(args):
        xb, e = args
        gu = xb @ w_gate_up[e] + b_gate_up[e]
        gate = jnp.minimum(gu[:, :D_EXPERT], SWIGLU_LIMIT)
        up = jnp.clip(gu[:, D_EXPERT:], -SWIGLU_LIMIT, SWIGLU_LIMIT)
        act = (up + 1.0) * gate * jax.nn.sigmoid(SWIGLU_ALPHA * gate)
        return act @ w_down[e] + b_down[e]

    y_rows = lax.map(expert_block, (x_rows, block_e)).reshape(n_rows, D)
    y = jnp.einsum('tkd,tk->td', y_rows[dest].reshape(T, TOP_K, D), gates.astype(y_rows.dtype))
    return y.reshape(B, S, D)


def setup_inputs(seed: int = 0) -> dict:
    key = jax.random.key(seed)
    ks = jax.random.split(key, 32)
    f32 = jnp.float32
    L, C, D, E, F = DEPTH, RWKV_WIDTH, D_MODEL, N_EXPERTS, D_EXPERT

    def nrm(i, shape, scale):
        return scale * jax.random.normal(ks[i], shape, f32)

    ramp = (jnp.arange(C, dtype=f32) / (C - 1)) ** 0.9
    return {
        'x': nrm(0, (BATCH, SEQ, D), 1.0),
        'mem': nrm(1, (BATCH, N_MEM, D), 1.0),
        'ln_in_g': 1.0 + nrm(2, (D,), 0.02),
        'ln_in_b': nrm(3, (D,), 0.02),
        'w_in': nrm(4, (L, D, IN_WIDTH), D ** -0.5),
        'mu_shift': jax.random.uniform(ks[5], (L, RWKV_IN), f32),
        'w0': -5.5 + 5.0 * ramp + nrm(6, (L, 2, C), 0.1),
        'w_up': nrm(7, (L, 2, DECAY_LORA, C), 0.5 * DECAY_LORA ** -0.5),
        'a0': nrm(8, (L, 2, C), 0.1),
        'a_up': nrm(9, (L, 2, ICLR_LORA, C), 0.5 * ICLR_LORA ** -0.5),
        'g_up': nrm(10, (L, GATE_LORA, C), GATE_LORA ** -0.5),
        'k_k': 0.85 + nrm(11, (L, C), 0.05),
        'k_a': 1.0 + nrm(12, (L, C), 0.05),
        'r_k': nrm(13, (L, RWKV_HEADS, HEAD_DIM), 0.1),
        'gn_g': 1.0 + nrm(14, (L, C), 0.02),
        'gn_b': nrm(15, (L, C), 0.02),
        'w_mem_kv': nrm(16, (L, D, 2 * MEM_WIDTH), D ** -0.5),
        'w_out': nrm(17, (L, MIX_WIDTH, D), DEEPNORM_BETA * MIX_WIDTH ** -0.5),
        'ln1_g': 1.0 + nrm(18, (L, D), 0.02),
        'ln1_b': nrm(19, (L, D), 0.02),
        'w_router': nrm(20, (L, D, E), D ** -0.5),
        'b_router': nrm(21, (L, E), 0.01),
        'w_gate_up': nrm(22, (L, E, D, 2 * F), D ** -0.5),
        'b_gate_up': nrm(23, (L, E, 2 * F), 0.01),
        'w_down': nrm(24, (L, E, F, D), DEEPNORM_BETA * F ** -0.5),
        'b_down': nrm(25, (L, E, D), 0.01),
        'ln2_g': 1.0 + nrm(26, (L, D), 0.02),
        'ln2_b': nrm(27, (L, D), 0.02),
    }


def reference(x, mem, ln_in_g, ln_in_b, w_in, mu_shift, w0, w_up, a0, a_up, g_up, k_k, k_a, r_k, gn_g, gn_b,
              w_mem_kv, w_out, ln1_g, ln1_b, w_router, b_router, w_gate_up, b_gate_up, w_down, b_down, ln2_g, ln2_b):
    h = layer_norm(x, ln_in_g, ln_in_b)
    for l in range(DEPTH):
        mix = parallel_mixer(h, mem, w_in[l], mu_shift[l], w0[l], w_up[l], a0[l], a_up[l], g_up[l], k_k[l], k_a[l],
                             r_k[l], gn_g[l], gn_b[l], w_mem_kv[l], w_out[l])
        h = layer_norm(DEEPNORM_ALPHA * h + mix, ln1_g[l], ln1_b[l])
        ffn = moe(h, w_router[l], b_router[l], w_gate_up[l], b_gate_up[l], w_down[l], b_down[l])
        h = layer_norm(DEEPNORM_ALPHA * h + ffn, ln2_g[l], ln2_b[l])
    return h
```

```python
import numpy as np
from contextlib import ExitStack
import concourse.bass as bass
import concourse.mybir as mybir
from concourse.bass_utils import run_bass_kernel_spmd

F32 = mybir.dt.float32
BF16 = mybir.dt.bfloat16
U32 = mybir.dt.uint32
AF = mybir.ActivationFunctionType
ALU = mybir.AluOpType
AX = mybir.AxisListType

D = 1024
S = 2048
NB = 2
NT = S // 128
C = 384
H = 6
RWKV_IN = 1344
IN_W = 2752
NE = 32
ALPHA = 2.0 ** 0.25
LN_EPS = 1e-5
GN_EPS = 64e-5
LDS = float(np.exp(-0.5))
STRIP_W = 3200
STRIP_C = 1536
CAP = 640
BIGF = 1.0e6
import os
SKIP = os.environ.get('KSKIP', '')
SPARSE = os.environ.get('KDENSE', '') == ''


class Buf:
    __slots__ = ("name", "w", "rs")

    def __init__(self, name):
        self.name = name
        self.w = None
        self.rs = []


class Ev:
    __slots__ = ("sem", "val", "eng")

    def __init__(self, eng):
        self.sem = None
        self.val = None
        self.eng = eng


class V:
    __slots__ = ("ap", "bufs")

    def __init__(self, ap, bufs):
        self.ap = ap
        self.bufs = bufs if isinstance(bufs, (list, tuple)) else [bufs]

    def __getitem__(self, k):
        return V(self.ap[k], self.bufs)

    def re(self, pat, **kw):
        return V(self.ap.rearrange(pat, **kw), self.bufs)

    def bc(self, shape):
        return V(self.ap.to_broadcast(list(shape)), self.bufs)

    def un(self, axis):
        return V(self.ap.unsqueeze(axis), self.bufs)

    def bitcast(self, dt):
        return V(self.ap.bitcast(dt), self.bufs)

    def on(self, bufs):
        return V(self.ap, bufs)


def _ap(x):
    return x.ap if isinstance(x, V) else x


class Sched:
    ENG = ("pe", "act", "dve", "pool", "sp")
    NQ = 16

    def __init__(self, nc, es):
        self.nc = nc
        self.es = es
        self.eng = dict(pe=nc.tensor, act=nc.scalar, dve=nc.vector, pool=nc.gpsimd, sp=nc.sync)
        self.sem = {k: es.enter_context(nc.semaphore("s_" + k)) for k in self.ENG}
        self.cnt = {k: 0 for k in self.ENG}
        self.dsem = {q: [es.enter_context(nc.semaphore("d_%s%d" % (q, i))) for i in range(self.NQ)]
                     for q in ("sp", "pool")}
        self.dcnt = {q: 0 for q in self.dsem}
        self.seen = {k: {} for k in self.ENG}
        self.pending = {k: [] for k in self.ENG}
        self.all_dma = []
        self.ninst = 0

    def _wait(self, e, ev):
        assert ev.val is not None, "dependency on an unresolved (non-inc) event"
        key = id(ev.sem)
        if self.seen[e].get(key, 0) >= ev.val:
            return
        self.eng[e].wait_ge(ev.sem, ev.val)
        self.seen[e][key] = ev.val

    def _deps(self, e, reads, writes, is_dma):
        for v in reads:
            for b in v.bufs:
                if b.w is not None:
                    if b.w.eng == e and e == "pe" and not is_dma:
                        continue
                    self._wait(e, b.w)
        for v in writes:
            for b in v.bufs:
                if b.w is not None and (is_dma or b.w.eng != e or e != "pe"):
                    self._wait(e, b.w)
                for r in b.rs:
                    if is_dma or r.eng != e or e != "pe":
                        self._wait(e, r)

    def _record(self, ev, reads, writes):
        for v in reads:
            for b in v.bufs:
                if ev.eng is not None:
                    b.rs = [r for r in b.rs if r.eng != ev.eng]
                b.rs.append(ev)
        for v in writes:
            for b in v.bufs:
                b.w = ev
                b.rs = []

    def op(self, e, fn, reads, writes, inc=True):
        reads = [r for r in reads if isinstance(r, V)]
        writes = [w for w in writes if isinstance(w, V)]
        self._deps(e, reads, writes, False)
        ins = fn(self.eng[e])
        self.ninst += 1
        ev = Ev(e)
        if inc:
            self.cnt[e] += 1
            ins.then_inc(self.sem[e], 1)
            ev.sem = self.sem[e]
            ev.val = self.cnt[e]
            for p in self.pending[e]:
                p.sem = ev.sem
                p.val = ev.val
            self.pending[e] = []
        else:
            self.pending[e].append(ev)
        self._record(ev, reads, writes)
        return ev

    def dma(self, q, out, in_, **kw):
        self._deps(q, [in_], [out], True)
        n = self.dcnt[q]
        self.dcnt[q] += 1
        sem = self.dsem[q][n % self.NQ]
        val = 16 * (n // self.NQ + 1)
        if n >= self.NQ:
            pe_ = Ev(None); pe_.sem = sem; pe_.val = val - 16
            self._wait(q, pe_)
        ins = self.eng[q].dma_start(out=_ap(out), in_=_ap(in_), **kw)
        ins.then_inc(sem, 16)
        self.ninst += 1
        ev = Ev(None)
        ev.sem = sem
        ev.val = val
        self._record(ev, [in_], [out])
        self.all_dma.append(ev)
        return ev

    def _idma(self, out, out_off, in_, in_off, reads, writes, bound):
        q = "pool"
        self._deps(q, reads, writes, True)
        n = self.dcnt[q]
        self.dcnt[q] += 1
        sem = self.dsem[q][n % self.NQ]
        val = 16 * (n // self.NQ + 1)
        if n >= self.NQ:
            pe_ = Ev(None); pe_.sem = sem; pe_.val = val - 16
            self._wait(q, pe_)
        ins = self.eng[q].indirect_dma_start(out=_ap(out), out_offset=out_off, in_=_ap(in_), in_offset=in_off)
        ins.then_inc(sem, 16)
        self.ninst += 1
        ev = Ev(None)
        ev.sem = sem
        ev.val = val
        self._record(ev, reads, writes)
        return ev

    def idma_scatter(self, dram, idx, src, bound):
        off = bass.IndirectOffsetOnAxis(ap=_ap(idx), axis=0)
        return self._idma(dram, off, src, None, [src, idx], [V(dram.ap, [Buf("scatter")])], bound)

    def idma_gather(self, dst, dram, idx, bound):
        off = bass.IndirectOffsetOnAxis(ap=_ap(idx), axis=0)
        return self._idma(dst, None, dram, off, [idx], [dst], bound)

    def barrier(self):
        for e in self.ENG:
            for o in self.ENG:
                if self.cnt[o] > 0:
                    assert not self.pending[o]
                    ev = Ev(o)
                    ev.sem = self.sem[o]
                    ev.val = self.cnt[o]
                    self._wait(e, ev)
            for q in self.dsem:
                n = self.dcnt[q]
                for i in range(min(n, self.NQ)):
                    ev = Ev(None)
                    ev.sem = self.dsem[q][i]
                    ev.val = 16 * ((n - 1 - i) // self.NQ + 1)
                    self._wait(e, ev)

    def _pe_rows(self, lhsT):
        a = _ap(lhsT)
        lo = a.base_partition()
        rows = (lo, lo + a.shape[0])
        prev = getattr(self, "_last_pe", None)
        if prev is not None:
            pins, pev, prow = prev
            if rows[0] >= prow[1] or prow[0] >= rows[1]:
                if pev.val is None:
                    self.cnt["pe"] += 1
                    pins.then_inc(self.sem["pe"], 1)
                    for p in self.pending["pe"]:
                        p.sem = self.sem["pe"]
                        p.val = self.cnt["pe"]
                    self.pending["pe"] = []
                self._wait("pe", pev)
        return rows

    def _pe_op(self, fn, lhsT, reads, writes, inc):
        rows = self._pe_rows(lhsT)
        box = []

        def f(e):
            i = fn(e)
            box.append(i)
            return i
        ev = self.op("pe", f, reads, writes, inc=inc)
        self._last_pe = (box[0], ev, rows)
        return ev

    def mm(self, out, lhsT, rhs, start=True, stop=True, inc=False):
        return self._pe_op(lambda e: e.matmul(_ap(out), _ap(lhsT), _ap(rhs), start=start, stop=stop),
                           lhsT, [lhsT, rhs], [out], inc)

    def tr(self, out, in_, ident, inc=False):
        return self._pe_op(lambda e: e.transpose(_ap(out), _ap(in_), _ap(ident)), in_, [in_, ident], [out], inc)

    def act(self, out, in_, func, bias=None, scale=None, accum_out=None):
        kw = {}
        if bias is not None:
            kw["bias"] = _ap(bias)
        if scale is not None:
            kw["scale"] = _ap(scale)
        if accum_out is not None:
            kw["accum_out"] = _ap(accum_out)
        return self.op("act", lambda e: e.activation(_ap(out), _ap(in_), func, **kw),
                       [in_, bias, scale], [out, accum_out])

    def ts(self, e, out, in0, s1, op0, s2=None, op1=None, accum_out=None):
        kw = {}
        if op1 is not None:
            kw["op1"] = op1
        if accum_out is not None:
            kw["accum_out"] = _ap(accum_out)
        return self.op(e, lambda g: g.tensor_scalar(_ap(out), _ap(in0), _ap(s1), _ap(s2), op0, **kw),
                       [in0, s1, s2], [out, accum_out])

    def tt(self, e, out, in0, in1, op):
        return self.op(e, lambda g: g.tensor_tensor(_ap(out), _ap(in0), _ap(in1), op), [in0, in1], [out])

    def stt(self, out, in0, scalar, in1, op0, op1):
        return self.op("dve", lambda g: g.scalar_tensor_tensor(_ap(out), _ap(in0), _ap(scalar), _ap(in1), op0, op1),
                       [in0, scalar, in1], [out])

    def cp(self, e, out, in_):
        if e == "act":
            return self.op("act", lambda g: g.copy(_ap(out), _ap(in_)), [in_], [out])
        return self.op(e, lambda g: g.tensor_copy(_ap(out), _ap(in_)), [in_], [out])

    def red(self, out, in_, op, axis=AX.X):
        return self.op("dve", lambda g: g.tensor_reduce(_ap(out), _ap(in_), axis, op), [in_], [out])

    def memset(self, e, out, val):
        return self.op(e, lambda g: g.memset(_ap(out), val), [], [out])


class Alloc:
    N = [0]

    def __init__(self, nc, es):
        self.nc = nc
        self.es = es

    def sb(self, shape, dt, name=None, nbuf=1):
        Alloc.N[0] += 1
        name = "%s_%d" % (name or "t", Alloc.N[0])
        t = self.es.enter_context(self.nc.sbuf_tensor(name, list(shape), dt))
        return V(t[:], [Buf(name)])

    def dram(self, name, shape, dt, kind="Internal"):
        t = self.nc.dram_tensor(name, list(shape), dt, kind=kind)
        return V(t.ap(), [Buf(name)])


def build_program(debug=(), moe=True, stages=('rwkv', 'att', 'mem', 'out')):
    nc = bass.Bass("TRN2", target_bir_lowering=False)
    es = ExitStack()
    S_ = Sched(nc, es)
    G = Alloc(nc, es)
    dbg = {}

    def din(name, shape, dt=F32):
        return V(nc.dram_tensor(name, list(shape), dt, kind="ExternalInput").ap(), [Buf(name)])

    x_d = din("x", [NB * S, D])
    mem_d = din("mem", [NB * 256, D])
    ln_in_g = din("ln_in_g", [D]); ln_in_b = din("ln_in_b", [D])
    w_in = din("w_in", [D, IN_W])
    mu_d = din("mu_shift", [RWKV_IN])
    w0_d = din("w0", [2 * C]); w_up_d = din("w_up", [2, 32, C])
    a0_d = din("a0", [2 * C]); a_up_d = din("a_up", [2, 32, C])
    g_up_d = din("g_up", [64, C])
    k_k_d = din("k_k", [C]); k_a_d = din("k_a", [C]); r_k_d = din("r_k", [C])
    gn_g_d = din("gn_g", [C]); gn_b_d = din("gn_b", [C])
    w_mem_kv = din("w_mem_kv", [D, 512])
    w_out = din("w_out", [D, D])
    ln1_g = din("ln1_g", [D]); ln1_b = din("ln1_b", [D])
    w_router = din("w_router", [D, NE]); b_router = din("b_router", [NE])
    if moe:
        w_gate_up = din("w_gate_up", [NE, D, 2 * D]); b_gate_up = din("b_gate_up", [NE, 2 * D])
        w_down = din("w_down", [NE, D, D]); b_down = din("b_down", [NE, D])
    ln2_g = din("ln2_g", [D]); ln2_b = din("ln2_b", [D])
    c_ident = din("c_ident", [128, 128])
    c_strip = din("c_strip", [H, 128, STRIP_W])
    c_masks = din("c_masks", [128, 5 * 64 + 2])
    c_tri = din("c_tri", [2, 128, 128])
    c_utri = din("c_utri", [128, 128])
    c_erow = din("c_erow", [128, NE])

    out_d = V(nc.dram_tensor("out", [NB * S, D], F32, kind="ExternalOutput").ap(), [Buf("out")])

    h0_d = [G.dram("h0_%d" % b, [S, D], F32) for b in range(NB)]
    h1_d = [G.dram("h1_%d" % g, [1024, D], F32) for g in range(4)]
    h1T_d = [G.dram("h1T_%d" % g, [128, 8, 1024], BF16) for g in range(4)]

    banks = []
    for i in range(8):
        t = es.enter_context(nc.psum_tensor("ps%d" % i, [128, 512], F32))
        banks.append(V(t[:], [Buf("ps%d" % i)]))
    bank_i = {"gen": 0, "acc": 0}
    bank_grp = {"gen": [2, 3, 4, 5, 6, 7], "acc": [0, 1]}

    def psum(grp="gen"):
        l = bank_grp[grp]
        b = banks[l[bank_i[grp] % len(l)]]
        bank_i[grp] += 1
        return b

    ident32 = G.sb([128, 128], F32, "ident32")
    identb = G.sb([128, 128], BF16, "identb")
    S_.dma("sp", ident32, c_ident)
    S_.cp("dve", identb, ident32)
    Gall = G.sb([128, NB * NT, NE], F32, "Gall")
    ones_b = G.sb([128, 128], BF16, "ones_b")
    S_.memset("dve", ones_b, 1.0)
    zeros_b = G.sb([128, 512], BF16, "zeros_b")
    S_.memset("dve", zeros_b, 0.0)
    carry = G.sb([128, NE], F32, "carry")
    S_.memset("dve", carry, 0.0)
    idx_all = G.sb([128, NB * NT, 4], U32, "idx_all")
    gk_all = G.sb([128, NB * NT, 4], F32, "gk_all")
    utri32 = G.sb([128, 128], F32, "utri32")
    S_.dma("sp", utri32, c_utri)
    utri_b = G.sb([128, 128], BF16, "utri_b")
    S_.cp("dve", utri_b, utri32)
    erow = G.sb([128, NE], F32, "erow")
    S_.dma("sp", erow, c_erow)
    x_rows = G.dram("x_rows", [NE * CAP + 128, D], BF16)
    y_rows = G.dram("y_rows", [NE * CAP + 128, D], F32)
    zeros32 = G.sb([128, D], F32, "zeros32")
    S_.memset("pool", zeros32, 0.0)
    S_.dma("sp", V(y_rows.ap[NE * CAP:NE * CAP + 128, :], [Buf("ydump")]), zeros32)
    zb = zeros32.bitcast(BF16).re("p (n d) -> p n d", n=2)
    xr = x_rows.re("(n p) d -> p n d", p=128)
    nrow = (NE * CAP + 128) // 128
    for n0 in range(0, nrow, 2):
        nn = min(2, nrow - n0)
        S_.dma("sp", V(xr.ap[:, n0:n0 + nn, :], [Buf("xfill")]), zb[:, 0:nn, :])

    def bcast_load(dst, src1d, q="sp"):
        S_.dma(q, dst, V(src1d.ap.partition_broadcast(128), src1d.bufs))

    def layernorm(es2, xin, gam, bet, out, tmp_pool):
        st = tmp_pool["st"]; mv = tmp_pool["mv"]; rs = tmp_pool["rs"]
        for i in range(2):
            S_.op("dve", lambda g, i=i: g.bn_stats(_ap(st[:, i, :]), _ap(xin[:, i * 512:(i + 1) * 512])),
                  [xin], [st])
        S_.op("dve", lambda g: g.bn_aggr(_ap(mv), _ap(st)), [st], [mv])
        S_.ts("dve", rs, mv[:, 1:2], LN_EPS, ALU.add)
        S_.act(rs, rs, AF.Sqrt)
        S_.op("dve", lambda g: g.reciprocal(_ap(rs), _ap(rs)), [rs], [rs])
        S_.ts("dve", out, xin, mv[:, 0:1], ALU.subtract, rs[:, 0:1], ALU.mult)
        S_.tt("pool", out, out, gam, ALU.mult)
        S_.tt("pool", out, out, bet, ALU.add)

    def stage_s1(b, hT):
        with ExitStack() as es1:
            A1 = Alloc(nc, es1)
            gam = A1.sb([128, D], F32, "gam"); bet = A1.sb([128, D], F32, "bet")
            bcast_load(gam, ln_in_g); bcast_load(bet, ln_in_b)
            xt = [A1.sb([128, D], F32, "xt") for _ in range(2)]
            ht = [A1.sb([128, D], F32, "ht") for _ in range(2)]
            hb = [A1.sb([128, D], BF16, "hb") for _ in range(2)]
            tp = [dict(st=A1.sb([128, 2, 6], F32, "st"), mv=A1.sb([128, 2], F32, "mv"),
                       rs=A1.sb([128, 1], F32, "rs")) for _ in range(2)]
            for t in range(NT):
                i = t % 2
                S_.dma("sp", xt[i], x_d[b * S + t * 128: b * S + (t + 1) * 128, :])
                layernorm(es1, xt[i], gam, bet, ht[i], tp[i])
                S_.dma("sp", h0_d[b][t * 128:(t + 1) * 128, :], ht[i])
                S_.cp("act", hb[i], ht[i])
                ps = psum().bitcast(BF16)
                for k in range(8):
                    S_.tr(ps[:, k * 128:(k + 1) * 128], hb[i][:, k * 128:(k + 1) * 128], identb, inc=(k == 7))
                S_.cp("act", hT[:, :, t * 128:(t + 1) * 128], ps.re("p (k t) -> p k t", k=8))

    def attn_core(nh, KTv, QTv, Vv, ytok, n_st, s_range, strips=None):
        with ExitStack() as esc:
            Ac = Alloc(nc, esc)
            Eb = [Ac.sb([128, 512], BF16, "E") for _ in range(3)]
            Pb = [Ac.sb([128, 512], BF16, "P") for _ in range(3)]
            rec = Ac.sb([128, 4], F32, "rec")
            it = 0
            for h in range(nh):
                c, p0 = h // 2, 64 * (h % 2)
                for tq in range(4):
                    s_lo, s_hi, jvalid = s_range(tq)
                    acc = psum("acc")
                    accv = acc[:, 0:260].re("p (j d) -> p j d", j=4)
                    S_.mm(acc[:, 0:260], zeros_b[:, 0:128], zeros_b[:, 0:260], start=True, stop=False)
                    for st_ in range(s_lo, s_hi + 1):
                        sc = psum()
                        S_.mm(sc, KTv[p0:p0 + 64, c, st_ * 128:(st_ + 1) * 128],
                              QTv[p0:p0 + 64, c, tq * 512:(tq + 1) * 512], inc=True)
                        E = Eb[it % 3]; P = Pb[it % 3]; it += 1
                        S_.act(E, sc, AF.Exp, scale=0.125)
                        if strips is not None:
                            off = tq * 512 - st_ * 128 + STRIP_C
                            S_.tt("dve" if it % 3 else "pool", P, E, strips[:, h, off:off + 512], ALU.mult)
                        else:
                            P = E
                        for j in range(4):
                            ok, last = jvalid(tq, j, st_)
                            if not ok:
                                continue
                            S_.mm(accv[:, j, :], P[:, j * 128:(j + 1) * 128], Vv[:, st_, h, :],
                                  start=False, stop=last, inc=True)
                    S_.op("dve", lambda g: g.reciprocal(_ap(rec), _ap(accv[:, :, 64])), [accv], [rec])
                    for j in range(4):
                        S_.ts("dve", ytok[:, 4 * tq + j, h * 64:(h + 1) * 64], accv[:, j, 0:64], rec[:, j:j + 1], ALU.mult)

    def proj_fm(dst, Wv, hT, nchunk, col0):
        n = 0
        for c in range(nchunk):
            for tg in range(4):
                ps = psum()
                for k in range(8):
                    S_.mm(ps, Wv[:, k, col0 + c * 128: col0 + (c + 1) * 128],
                          hT[:, k, tg * 512:(tg + 1) * 512], start=(k == 0), stop=(k == 7), inc=(k == 7))
                S_.cp("act" if n % 2 else "dve", dst[:, c, tg * 512:(tg + 1) * 512], ps)
                n += 1

    def ytok_to_yT(ytok, yT, nchunk):
        for t in range(NT):
            ps = psum().bitcast(BF16)
            for cc in range(nchunk):
                S_.tr(ps[:, cc * 128:(cc + 1) * 128], ytok[:, t, cc * 128:(cc + 1) * 128], identb, inc=(cc == nchunk - 1))
            S_.cp("act" if t % 2 else "dve", yT[:, 0:nchunk, t * 128:(t + 1) * 128],
                  ps[:, 0:nchunk * 128].re("p (k t) -> p k t", k=nchunk))

    def stage_att(b, hT, yT_a):
        with ExitStack() as es2:
            A2 = Alloc(nc, es2)
            Wqkv = A2.sb([128, 8, 3 * C], BF16, "Wqkv")
            S_.dma("pool", Wqkv, w_in[:, RWKV_IN:RWKV_IN + 3 * C].re("(k p) c -> p k c", p=128))
            strips = A2.sb([128, H, STRIP_W], BF16, "strips")
            for h in range(H):
                S_.dma("pool", strips[:, h, :], c_strip[h])
            QT = A2.sb([128, 3, S], BF16, "QT")
            KT = A2.sb([128, 3, S], BF16, "KT")
            Vt = A2.sb([128, NT, H, 65], BF16, "Vt")
            ytok = A2.sb([128, NT, C], BF16, "ytok")
            S_.memset("pool", Vt[:, :, :, 64:65], 1.0)
            proj_fm(QT, Wqkv, hT, 3, 0)
            proj_fm(KT, Wqkv, hT, 3, C)
            for t in range(NT):
                ps = psum()
                for k in range(8):
                    S_.mm(ps[:, 0:C], hT[:, k, t * 128:(t + 1) * 128], Wqkv[:, k, 2 * C:3 * C],
                          start=(k == 0), stop=(k == 7), inc=(k == 7))
                S_.cp("act" if t % 2 else "dve", Vt[:, t, :, 0:64], ps[:, 0:C].re("p (h d) -> p h d", h=H))

            def s_range(tq):
                s_hi = min(NT - 1, 4 * tq + 3 + 8)

                def jvalid(tq, j, st_):
                    tt_ = 4 * tq + j
                    return abs(tt_ - st_) <= 8, (st_ == s_hi and j == 3)
                return max(0, 4 * tq - 8), s_hi, jvalid
            attn_core(H, KT, QT, Vt, ytok, NT, s_range, strips)
            ytok_to_yT(ytok, yT_a, 3)

    def stage_mem(b, hT, yT_m):
        with ExitStack() as es3:
            A3 = Alloc(nc, es3)
            Wkv = A3.sb([128, 8, 512], BF16, "Wkv")
            S_.dma("pool", Wkv, w_mem_kv.re("(k p) c -> p k c", p=128))
            Wqm = A3.sb([128, 8, 256], BF16, "Wqm")
            S_.dma("pool", Wqm, w_in[:, RWKV_IN + 3 * C:IN_W].re("(k p) c -> p k c", p=128))
            memb = A3.sb([128, 2, D], BF16, "memb")
            S_.dma("pool", memb, mem_d[b * 256:(b + 1) * 256, :].re("(m p) d -> p m d", p=128))
            memT = A3.sb([128, 8, 256], BF16, "memT")
            for m in range(2):
                ps = psum().bitcast(BF16)
                for k in range(8):
                    S_.tr(ps[:, k * 128:(k + 1) * 128], memb[:, m, k * 128:(k + 1) * 128], identb, inc=(k == 7))
                S_.cp("act", memT[:, :, m * 128:(m + 1) * 128], ps.re("p (k t) -> p k t", k=8))
            KmT = A3.sb([128, 2, 256], BF16, "KmT")
            for c in range(2):
                ps = psum()
                for k in range(8):
                    S_.mm(ps[:, 0:256], Wkv[:, k, c * 128:(c + 1) * 128], memT[:, k, :],
                          start=(k == 0), stop=(k == 7), inc=(k == 7))
                S_.cp("dve", KmT[:, c, :], ps[:, 0:256])
            Vm = A3.sb([128, 2, 4, 65], BF16, "Vm")
            S_.memset("pool", Vm[:, :, :, 64:65], 1.0)
            for m in range(2):
                ps = psum()
                for k in range(8):
                    S_.mm(ps[:, 0:256], memT[:, k, m * 128:(m + 1) * 128], Wkv[:, k, 256:512],
                          start=(k == 0), stop=(k == 7), inc=(k == 7))
                S_.cp("dve", Vm[:, m, :, 0:64], ps[:, 0:256].re("p (h d) -> p h d", h=4))
            QmT = A3.sb([128, 2, S], BF16, "QmT")
            proj_fm(QmT, Wqm, hT, 2, 0)
            ytok = A3.sb([128, NT, 256], BF16, "ytokm")

            def s_range(tq):
                return 0, 1, (lambda tq, j, st_: (True, st_ == 1 and j == 3))
            attn_core(4, KmT, QmT, Vm, ytok, 2, s_range, None)
            ytok_to_yT(ytok, yT_m, 2)

    def stage_rwkv(b, hT, yT_r):
        with ExitStack() as es4:
            A4 = Alloc(nc, es4)
            f32t = lambda name, n=C: A4.sb([128, n], F32, name)
            Wr = A4.sb([128, 16, RWKV_IN], BF16, "Wr")
            with ExitStack() as esw:
                Aw = Alloc(nc, esw)
                mub = Aw.sb([128, RWKV_IN], F32, "mub"); bcast_load(mub, mu_d)
                omb = Aw.sb([128, RWKV_IN], F32, "omb"); hmb = Aw.sb([128, RWKV_IN], F32, "hmb")
                S_.ts("dve", omb, mub, -1.0, ALU.mult, 1.0, ALU.add)
                S_.ts("pool", hmb, mub, 0.5, ALU.mult)
                stg = [Aw.sb([128, 8, 336], F32, "stg") for _ in range(2)]
                for pc in range(4):
                    cs = slice(pc * 336, (pc + 1) * 336)
                    S_.dma("sp", stg[pc % 2], w_in[:, cs].re("(k p) c -> p k c", p=128))
                    S_.tt("dve", Wr[:, 0:8, cs], stg[pc % 2], omb[:, cs].un(1).bc([128, 8, 336]), ALU.mult)
                    S_.tt("pool", Wr[:, 8:16, cs], stg[pc % 2], hmb[:, cs].un(1).bc([128, 8, 336]), ALU.mult)
                S_.barrier()
            kkb = f32t("kkb"); bcast_load(kkb, k_k_d)
            kab = f32t("kab"); bcast_load(kab, k_a_d)
            omkab = f32t("omkab"); S_.ts("dve", omkab, kab, -1.0, ALU.mult, 1.0, ALU.add)
            rkb = f32t("rkb"); bcast_load(rkb, r_k_d)
            gngb = f32t("gngb"); bcast_load(gngb, gn_g_d)
            gnbb = f32t("gnbb"); bcast_load(gnbb, gn_b_d)
            w0b = f32t("w0b", 2 * C); bcast_load(w0b, w0_d)
            a0b = f32t("a0b", 2 * C); bcast_load(a0b, a0_d)
            gup = A4.sb([128, C], BF16, "gup"); S_.dma("pool", gup[0:64, :], g_up_d)
            wupbd = A4.sb([128, 2 * C], BF16, "wupbd"); S_.memset("pool", wupbd, 0.0)
            S_.dma("pool", wupbd[64:96, 0:C], w_up_d[0]); S_.dma("pool", wupbd[96:128, C:2 * C], w_up_d[1])
            aupbd = A4.sb([128, 2 * C], BF16, "aupbd"); S_.memset("pool", aupbd, 0.0)
            S_.dma("pool", aupbd[0:32, 0:C], a_up_d[0]); S_.dma("pool", aupbd[32:64, C:2 * C], a_up_d[1])
            Yacc = A4.sb([128, NT, C], F32, "Yacc")
            T32 = A4.sb([128, 3, 64], F32, "T32"); Tb = A4.sb([128, 3, 64], BF16, "Tb"); Ttmp = A4.sb([128, 3, 64], F32, "Ttmp")
            hs = A4.sb([128, 8, 128], BF16, "hs")
            r32 = f32t("r32"); k32 = f32t("k32"); v32 = f32t("v32")
            sgw = f32t("sgw"); a32 = f32t("a32"); a32b = f32t("a32b")
            ecw = f32t("ecw"); encw = f32t("encw")
            kk = f32t("kk"); sq = f32t("sq"); kd = f32t("kd"); bb = f32t("bb")
            ss = A4.sb([128, 6], F32, "ss"); rn = A4.sb([128, 6], F32, "rn")
            lor = A4.sb([128, 3, 128], BF16, "lor")
            tok4 = A4.sb([128, 4, C], BF16, "tok4")
            Pp = [A4.sb([128, 6, 64], BF16, "Pp") for _ in range(2)]
            PTp = [A4.sb([128, 6, 64], BF16, "PTp") for _ in range(2)]
            Zs = A4.sb([128, C], BF16, "Zs"); Us = A4.sb([128, C], BF16, "Us")
            fs1 = A4.sb([128, 6], F32, "fs1"); fs2 = A4.sb([128, 6], F32, "fs2"); fs3 = A4.sb([128, 6], F32, "fs3")
            yob = A4.sb([128, C], BF16, "yob")
            sets = []
            for i in range(2):
                sets.append(dict(
                    FM=A4.sb([128, 4, 3, 128], BF16, "FM"), Vb=A4.sb([128, C], BF16, "Vb"),
                    XT=[A4.sb([128, 6, 64], BF16, "XT") for _ in range(2)],
                    LkT=A4.sb([128, 6, 64], BF16, "LkT"), RbT=A4.sb([128, 6, 64], BF16, "RbT"),
                    RkT=A4.sb([128, 6, 64], BF16, "RkT"), WCe=A4.sb([128, 3, 2], F32, "WCe"),
                    Bt=A4.sb([128, C], BF16, "Bt"), Kt=A4.sb([128, C], BF16, "Kt"),
                    g32=f32t("g32"), bv=f32t("bv")))

            def v6(x):
                return x.re("p (h j) -> p h j", h=6)
            HORD = (0, 2, 4, 1, 3, 5)

            def prep(t, d, st):
                lo = t * 128
                if 0 < t < NT - 1:
                    S_.tt("pool", hs, hT[:, :, lo - 1:lo + 127], hT[:, :, lo + 1:lo + 129], ALU.add)
                elif t == 0:
                    S_.tt("pool", hs[:, :, 1:128], hT[:, :, 0:127], hT[:, :, 2:129], ALU.add)
                    S_.cp("pool", hs[:, :, 0:1], hT[:, :, 1:2])
                else:
                    S_.tt("pool", hs[:, :, 0:127], hT[:, :, lo - 1:lo + 126], hT[:, :, lo + 1:lo + 128], ALU.add)
                    S_.cp("pool", hs[:, :, 127:128], hT[:, :, lo + 126:lo + 127])

                def lhs(kc):
                    return hT[:, kc, lo:lo + 128] if kc < 8 else hs[:, kc - 8, :]
                for part, dst in ((0, r32), (1, k32), (2, v32)):
                    ps = psum()
                    for kc in range(16):
                        S_.mm(ps[:, 0:C], lhs(kc), Wr[:, kc, part * C:(part + 1) * C], start=(kc == 0), stop=(kc == 15), inc=(kc == 15))
                    S_.cp("act", dst, ps[:, 0:C])
                S_.cp("pool", st["Vb"], v32)
                ps = psum()
                for kc in range(16):
                    S_.mm(ps[:, 0:128], Wr[:, kc, 3 * C:3 * C + 128], lhs(kc), start=(kc == 0), stop=(kc == 15), inc=(kc == 15))
                for kc in range(16):
                    S_.mm(ps[0:64, 128:256], Wr[:, kc, 3 * C + 128:3 * C + 192], lhs(kc), start=(kc == 0), stop=(kc == 15), inc=(kc == 15))
                S_.act(lor[0:64, 0, :], ps[0:64, 0:128], AF.Sigmoid)
                S_.act(lor[64:128, 1, :], ps[64:128, 0:128], AF.Tanh)
                S_.cp("act", lor[0:64, 2, :], ps[0:64, 128:256])
                psg = psum()
                S_.mm(psg[:, 0:C], lor[0:64, 0, :], gup[0:64, :], inc=True)
                S_.cp("act", st["g32"], psg[:, 0:C])
                psw = psum()
                S_.mm(psw[:, 0:C], lor[64:128, 1, :], wupbd[64:128, d * C:(d + 1) * C], inc=True)
                S_.tt("dve", sgw, psw[:, 0:C], w0b[:, d * C:(d + 1) * C], ALU.add)
                S_.act(sgw, sgw, AF.Sigmoid)
                psa = psum()
                S_.mm(psa[:, 0:C], lor[0:64, 2, :], aupbd[0:64, d * C:(d + 1) * C], inc=True)
                S_.tt("dve", a32, psa[:, 0:C], a0b[:, d * C:(d + 1) * C], ALU.add)
                S_.act(a32, a32, AF.Sigmoid)
                pscs = psum()
                S_.mm(pscs[:, 0:C], tri32[:, d, :], sgw, inc=True)
                pswc = psum()
                for c in range(3):
                    S_.mm(pswc[:, c * 2:(c + 1) * 2], sgw[:, c * 128:(c + 1) * 128], BLK, inc=(c == 2))
                S_.act(st["WCe"].re("p c q -> p (c q)"), pswc[:, 0:6], AF.Exp, scale=-LDS)
                S_.act(ecw, pscs[:, 0:C], AF.Exp, scale=-LDS)
                S_.act(encw, pscs[:, 0:C], AF.Exp, scale=LDS)
                S_.act(sgw, sgw, AF.Exp, scale=LDS)
                S_.tt("dve", sgw, sgw, ecw, ALU.mult)
                S_.tt("pool", kk, k32, kkb, ALU.mult)
                S_.tt("pool", sq, kk, kk, ALU.mult)
                S_.red(ss, v6(sq), ALU.add)
                S_.act(rn, ss, AF.Sqrt)
                S_.ts("dve", rn, rn, 1e-12, ALU.max)
                S_.op("dve", lambda g: g.reciprocal(_ap(rn), _ap(rn)), [rn], [rn])
                S_.tt("dve", v6(kk), v6(kk), rn.un(2).bc([128, 6, 64]), ALU.mult)
                S_.tt("pool", kd, a32, kab, ALU.mult)
                S_.tt("pool", kd, kd, omkab, ALU.add)
                S_.tt("pool", kd, kd, k32, ALU.mult)
                S_.tt("dve", bb, kk, a32, ALU.mult)
                S_.tt("dve", tok4[:, 0, :], kk, sgw, ALU.mult)
                S_.stt(tok4[:, 1, :], bb, -1.0, encw, ALU.mult, ALU.mult)
                S_.tt("pool", tok4[:, 2, :], kd, encw, ALU.mult)
                S_.tt("pool", tok4[:, 3, :], r32, ecw, ALU.mult)
                S_.cp("act", st["Bt"], tok4[:, 1, :])
                S_.cp("act", st["Kt"], tok4[:, 2, :])
                if d == 1:
                    psa2 = psum()
                    S_.mm(psa2[:, 0:C], lor[0:64, 2, :], aupbd[0:64, 0:C], inc=True)
                    S_.tt("dve", a32b, psa2[:, 0:C], a0b[:, 0:C], ALU.add)
                    S_.act(a32b, a32b, AF.Sigmoid)
                    S_.tt("pool", a32b, a32b, a32, ALU.add)
                    S_.tt("pool", a32b, a32b, kab, ALU.mult)
                    S_.stt(a32b, omkab, 2.0, a32b, ALU.mult, ALU.add)
                    S_.tt("pool", a32b, a32b, k32, ALU.mult)
                    S_.tt("pool", a32b, a32b, rkb, ALU.mult)
                    S_.tt("pool", a32b, a32b, r32, ALU.mult)
                    S_.red(fs3, v6(a32b), ALU.add)
                    S_.tt("dve", v6(st["bv"]), v6(v32), fs3.un(2).bc([128, 6, 64]), ALU.mult)
                FM = st["FM"]
                for half in range(2):
                    ps = psum().bitcast(BF16)
                    n = 0
                    for o in (2 * half, 2 * half + 1):
                        for c in range(3):
                            S_.tr(ps[:, n * 128:(n + 1) * 128], tok4[:, o, c * 128:(c + 1) * 128], identb, inc=(n == 5))
                            n += 1
                    S_.cp("act" if half else "dve", FM[:, 2 * half:2 * half + 2, :, :].re("p o c t -> p (o c) t"),
                          ps[:, 0:768].re("p (n t) -> p n t", n=6))
                KKi, Bi, Ki, Ri = 0, 1, 2, 3
                st["XTf"] = st["XT"][1]
                if "cc" in SKIP:
                    return

                def cc_mm(ps, li, ri):
                    n = 0
                    for q in range(2):
                        q0 = 64 * q
                        for h in HORD:
                            c, p0 = h // 2, 64 * (h % 2)
                            n += 1
                            S_.mm(ps[q0:q0 + 64, h * 64:(h + 1) * 64], FM[p0:p0 + 64, li, c, q0:q0 + 64],
                                  FM[p0:p0 + 64, ri, c, q0:q0 + 64], inc=(n == 12))

                def mbc(m):
                    return m.un(1).bc([128, 6, 64])
                XT = st["XT"]
                ps = psum(); cc_mm(ps, Bi, KKi)
                S_.tt("dve", PTp[0], v6(ps[:, 0:C]), mbc(MS[d]), ALU.mult)
                S_.tt("pool", XT[0], PTp[0], mbc(MID), ALU.add)
                ps = psum(); cc_mm(ps, KKi, Bi)
                S_.tt("dve", Pp[0], v6(ps[:, 0:C]), mbc(MS[1 - d]), ALU.mult)
                ps = psum(); cc_mm(ps, Ki, KKi)
                S_.tt("dve", st["LkT"], v6(ps[:, 0:C]), mbc(MS[d]), ALU.mult)
                ps = psum(); cc_mm(ps, Bi, Ri)
                S_.tt("dve", st["RbT"], v6(ps[:, 0:C]), mbc(MI[d]), ALU.mult)
                ps = psum(); cc_mm(ps, Ki, Ri)
                S_.tt("dve", st["RkT"], v6(ps[:, 0:C]), mbc(MI[d]), ALU.mult)
                if "chain" in SKIP:
                    return
                cur = 0
                for lvl in range(1, 6):
                    nxt = 1 - cur

                    def sq_mm(ps, lt, rt):
                        n = 0
                        for q in range(2):
                            q0 = 64 * q
                            for h in range(H):
                                n += 1
                                S_.mm(ps[q0:q0 + 64, h * 64:(h + 1) * 64], lt[q0:q0 + 64, h, :], rt[q0:q0 + 64, h, :], inc=(n == 12))
                    psP = psum(); sq_mm(psP, PTp[cur], Pp[cur])
                    if lvl < 5:
                        psPT = psum(); sq_mm(psPT, Pp[cur], PTp[cur])
                    S_.cp("act", Pp[nxt], v6(psP[:, 0:C]))
                    if lvl < 5:
                        S_.cp("dve", PTp[nxt], v6(psPT[:, 0:C]))
                    psX = psum(); sq_mm(psX, Pp[nxt], XT[(lvl - 1) % 2])
                    S_.tt("dve", XT[lvl % 2], v6(psX[:, 0:C]), XT[(lvl - 1) % 2], ALU.add)
                    cur = nxt
                st["XTf"] = XT[5 % 2]

            def seq_chunk(t, q, d, st):
                q0 = 64 * q
                FM = st["FM"]; Vb = st["Vb"]; XT = st["XTf"]
                hc = lambda h: slice(h * 64, (h + 1) * 64)
                psZ = psum()
                S_.mm(psZ[q0:q0 + 64, 0:C], zeros_b[:, 0:64], zeros_b[:, 0:C], start=True, stop=False)
                for h in HORD:
                    c, p0 = h // 2, 64 * (h % 2)
                    S_.mm(psZ[q0:q0 + 64, hc(h)], FM[p0:p0 + 64, 0, c, q0:q0 + 64], Tb[p0:p0 + 64, c, :], start=False, stop=False)
                for h in range(H):
                    S_.mm(psZ[q0:q0 + 64, hc(h)], st["LkT"][q0:q0 + 64, h, :], Vb[q0:q0 + 64, hc(h)], start=False, stop=(h == H - 1), inc=(h == H - 1))
                S_.cp("act", Zs[q0:q0 + 64, :], psZ[q0:q0 + 64, 0:C])
                psU = psum()
                for h in range(H):
                    S_.mm(psU[q0:q0 + 64, hc(h)], XT[q0:q0 + 64, h, :], Zs[q0:q0 + 64, hc(h)], inc=(h == H - 1))
                S_.cp("dve", Us[q0:q0 + 64, :], psU[q0:q0 + 64, 0:C])
                psY = psum()
                S_.mm(psY[q0:q0 + 64, 0:C], zeros_b[:, 0:64], zeros_b[:, 0:C], start=True, stop=False)
                for h in HORD:
                    c, p0 = h // 2, 64 * (h % 2)
                    S_.mm(psY[q0:q0 + 64, hc(h)], FM[p0:p0 + 64, 3, c, q0:q0 + 64], Tb[p0:p0 + 64, c, :], start=False, stop=False)
                for h in range(H):
                    S_.mm(psY[q0:q0 + 64, hc(h)], st["RbT"][q0:q0 + 64, h, :], Us[q0:q0 + 64, hc(h)], start=False, stop=False)
                for h in range(H):
                    S_.mm(psY[q0:q0 + 64, hc(h)], st["RkT"][q0:q0 + 64, h, :], Vb[q0:q0 + 64, hc(h)], start=False, stop=(h == H - 1), inc=(h == H - 1))
                psT = psum()
                for h in range(H):
                    c, p0 = h // 2, 64 * (h % 2)
                    S_.mm(psT[p0:p0 + 64, c * 64:(c + 1) * 64], st["Bt"][q0:q0 + 64, hc(h)], Us[q0:q0 + 64, hc(h)], start=True, stop=False)
                    S_.mm(psT[p0:p0 + 64, c * 64:(c + 1) * 64], st["Kt"][q0:q0 + 64, hc(h)], Vb[q0:q0 + 64, hc(h)], start=False, stop=True, inc=(h == H - 1))
                if d == 0:
                    S_.cp("act", Yacc[q0:q0 + 64, t, :], psY[q0:q0 + 64, 0:C])
                else:
                    S_.tt("pool" if False else "dve", Yacc[q0:q0 + 64, t, :], psY[q0:q0 + 64, 0:C], Yacc[q0:q0 + 64, t, :], ALU.add)
                S_.tt("dve", Ttmp, psT[:, 0:192].re("p (c i) -> p c i", c=3), T32, ALU.add)
                S_.tt("dve", T32, Ttmp, st["WCe"][:, :, q:q + 1].bc([128, 3, 64]), ALU.mult)
                S_.cp("act", Tb, T32)

            def finalize(t, st):
                Y = Yacc[:, t, :]
                S_.red(fs1, v6(Y), ALU.add)
                S_.tt("pool", sq, Y, Y, ALU.mult)
                S_.red(fs2, v6(sq), ALU.add)
                S_.ts("dve", fs1, fs1, 1.0 / 64, ALU.mult)
                S_.tt("dve", fs3, fs1, fs1, ALU.mult)
                S_.stt(fs2, fs2, 1.0 / 64, fs3, ALU.mult, ALU.subtract)
                S_.ts("dve", fs2, fs2, GN_EPS, ALU.add)
                S_.act(fs2, fs2, AF.Sqrt)
                S_.op("dve", lambda g: g.reciprocal(_ap(fs2), _ap(fs2)), [fs2], [fs2])
                S_.tt("dve", v6(sq), v6(Y), fs1.un(2).bc([128, 6, 64]), ALU.subtract)
                S_.tt("dve", v6(sq), v6(sq), fs2.un(2).bc([128, 6, 64]), ALU.mult)
                S_.tt("pool", sq, sq, gngb, ALU.mult)
                S_.tt("pool", sq, sq, gnbb, ALU.add)
                S_.tt("pool", sq, sq, st["bv"], ALU.add)
                S_.tt("pool", yob, sq, st["g32"], ALU.mult)
                ps = psum().bitcast(BF16)
                for cc in range(3):
                    S_.tr(ps[:, cc * 128:(cc + 1) * 128], yob[:, cc * 128:(cc + 1) * 128], identb, inc=(cc == 2))
                S_.cp("act", yT_r[:, 0:3, t * 128:(t + 1) * 128], ps[:, 0:384].re("p (k t) -> p k t", k=3))

            for d in (0, 1):
                S_.memset("dve", T32, 0.0)
                S_.memset("pool", Tb, 0.0)
                order = list(range(NT)) if d == 0 else list(range(NT - 1, -1, -1))
                prep(order[0], d, sets[0])
                for i, t in enumerate(order):
                    st = sets[i % 2]
                    if i + 1 < NT:
                        prep(order[i + 1], d, sets[(i + 1) % 2])
                    if "seq" in SKIP:
                        continue
                    for q in ((0, 1) if d == 0 else (1, 0)):
                        seq_chunk(t, q, d, st)
                    if d == 1:
                        finalize(t, st)

    def stage_out(b, yTs):
        with ExitStack() as es5:
            A5 = Alloc(nc, es5)
            Wo = A5.sb([128, 8, D], BF16, "Wo")
            S_.dma("pool", Wo, w_out.re("(k p) c -> p k c", p=128))
            gam = A5.sb([128, D], F32, "gam1"); bet = A5.sb([128, D], F32, "bet1")
            bcast_load(gam, ln1_g); bcast_load(bet, ln1_b)
            Wr32 = A5.sb([128, 8, NE], F32, "Wr32")
            S_.dma("sp", Wr32, w_router.re("(k p) e -> p k e", p=128))
            brb = A5.sb([128, NE], F32, "brb"); bcast_load(brb, b_router)
            h0t = [A5.sb([128, D], F32, "h0t") for _ in range(2)]
            zt = [A5.sb([128, D], F32, "zt") for _ in range(2)]
            h1t = [A5.sb([128, D], F32, "h1t") for _ in range(2)]
            h1T32 = [A5.sb([128, 8, 128], F32, "h1T32") for _ in range(2)]
            h1Tb = [A5.sb([128, 8, 128], BF16, "h1Tb") for _ in range(2)]
            hhi = [A5.sb([128, D], BF16, "hhi") for _ in range(2)]
            hlo = [A5.sb([128, D], BF16, "hlo") for _ in range(2)]
            tp = [dict(st=A5.sb([128, 2, 6], F32, "st"), mv=A5.sb([128, 2], F32, "mv"),
                       rs=A5.sb([128, 1], F32, "rs")) for _ in range(2)]
            lg = A5.sb([128, NE], F32, "lg"); mx8 = A5.sb([128, 8], F32, "mx8"); nmx = A5.sb([128, 1], F32, "nmx")
            ex = A5.sb([128, NE], F32, "ex"); mk = A5.sb([128, NE], F32, "mk"); sm = A5.sb([128, 1], F32, "sm")
            rt = dict(mkb=A5.sb([128, NE], BF16, "mkb"), pos=A5.sb([128, NE], F32, "pos"), vv=A5.sb([128, NE], F32, "vv"),
                      nd=A5.sb([128, NE], F32, "nd"), mx=A5.sb([128, 8], F32, "mxr"), sel=A5.sb([128, NE], F32, "sel"),
                      idf=A5.sb([128, 4], F32, "idf"))
            for t in range(NT):
                i = t % 2
                gt = b * NT + t
                grp, tin = gt // 8, gt % 8
                S_.dma("sp", h0t[i], h0_d[b][t * 128:(t + 1) * 128, :])
                for half in range(2):
                    ps = psum()
                    for k in range(8):
                        S_.mm(ps, yTs[k][:, t * 128:(t + 1) * 128], Wo[:, k, half * 512:(half + 1) * 512],
                              start=(k == 0), stop=(k == 7), inc=(k == 7))
                    S_.stt(zt[i][:, half * 512:(half + 1) * 512], h0t[i][:, half * 512:(half + 1) * 512], ALPHA, ps, ALU.mult, ALU.add)
                layernorm(es5, zt[i], gam, bet, h1t[i], tp[i])
                S_.dma("sp", h1_d[grp][tin * 128:(tin + 1) * 128, :], h1t[i])
                S_.cp("act", hhi[i], h1t[i])
                S_.tt("dve", hlo[i], h1t[i], hhi[i], ALU.subtract)
                for half in range(2):
                    ps = psum()
                    for k in range(4):
                        kk_ = half * 4 + k
                        S_.mm(ps[:, k * 128:(k + 1) * 128], hhi[i][:, kk_ * 128:(kk_ + 1) * 128], identb, start=True, stop=False)
                        S_.mm(ps[:, k * 128:(k + 1) * 128], hlo[i][:, kk_ * 128:(kk_ + 1) * 128], identb, start=False, stop=True, inc=(k == 3))
                    S_.cp("act", h1T32[i][:, half * 4:half * 4 + 4, :], ps.re("p (k t) -> p k t", k=4))
                    S_.cp("dve", h1Tb[i][:, half * 4:half * 4 + 4, :], h1T32[i][:, half * 4:half * 4 + 4, :])
                if "h1Tdma" not in SKIP:
                    S_.dma("sp", h1T_d[grp][:, :, tin * 128:(tin + 1) * 128], h1Tb[i])
                if "router" in SKIP:
                    continue
                ps = psum()
                for k in range(8):
                    S_.mm(ps[:, 0:NE], h1T32[i][:, k, :], Wr32[:, k, :], start=(k == 0), stop=(k == 7), inc=(k == 7))
                S_.tt("dve", lg, ps[:, 0:NE], brb, ALU.add)
                if "top" in SKIP:
                    continue
                S_.op("dve", lambda g: g.max(_ap(mx8), _ap(lg)), [lg], [mx8])
                S_.ts("dve", mk, lg, mx8[:, 3:4], ALU.is_ge)
                S_.ts("dve", nmx, mx8[:, 0:1], -1.0, ALU.mult)
                S_.act(ex, lg, AF.Exp, bias=nmx[:, 0:1])
                S_.tt("dve", ex, ex, mk, ALU.mult)
                S_.red(sm, ex, ALU.add)
                S_.op("dve", lambda g: g.reciprocal(_ap(sm), _ap(sm)), [sm], [sm])
                S_.ts("dve", Gall[:, gt, :], ex, sm[:, 0:1], ALU.mult)
                if SPARSE:
                    route_tile(gt, mk, hhi[i], rt)

    def stage_moe():
        with ExitStack() as es6:
            A6 = Alloc(nc, es6)
            gam = A6.sb([128, D], F32, "gam2"); bet = A6.sb([128, D], F32, "bet2")
            bcast_load(gam, ln2_g); bcast_load(bet, ln2_b)
            bguT = A6.sb([128, 16, NE], F32, "bguT")
            with ExitStack() as esb:
                Ab = Alloc(nc, esb)
                bgu_sb = Ab.sb([NE, 2 * D], F32, "bgu_sb")
                bgu_hi = Ab.sb([NE, 2 * D], BF16, "bgu_hi"); bgu_lo = Ab.sb([NE, 2 * D], BF16, "bgu_lo")
                S_.dma("sp", bgu_sb, b_gate_up)
                S_.cp("act", bgu_hi, bgu_sb)
                S_.tt("dve", bgu_lo, bgu_sb, bgu_hi, ALU.subtract)
                for fc in range(16):
                    ps = psum()
                    S_.mm(ps[:, 0:NE], bgu_hi[0:NE, fc * 128:(fc + 1) * 128], identb[0:NE, 0:NE], start=True, stop=False)
                    S_.mm(ps[:, 0:NE], bgu_lo[0:NE, fc * 128:(fc + 1) * 128], identb[0:NE, 0:NE], start=False, stop=True, inc=True)
                    S_.cp("act", bguT[:, fc, :], ps[:, 0:NE])
                S_.barrier()
            bd_sb = A6.sb([NE, D], F32, "bd_sb")
            S_.dma("sp", bd_sb, b_down)
            h1T = A6.sb([128, 8, 1024], BF16, "h1T")
            acc = A6.sb([128, 8, D], F32, "acc")
            wgu = [A6.sb([128, 8, 2, 256], BF16, "wgu") for _ in range(6)]
            wd = [A6.sb([128, 8, D], BF16, "wd") for _ in range(2)]
            actT = A6.sb([128, 8, 1024], BF16, "actT")
            g1 = [A6.sb([128, 512], F32, "g1") for _ in range(2)]
            s1 = [A6.sb([128, 512], F32, "s1") for _ in range(2)]
            u1 = [A6.sb([128, 512], F32, "u1") for _ in range(2)]
            GT = A6.sb([NE, 128], F32, "GT")
            Ghi = A6.sb([128, NE], BF16, "Ghi"); Glo = A6.sb([128, NE], BF16, "Glo")
            h1t = A6.sb([128, D], F32, "h1t2"); zt = A6.sb([128, D], F32, "zt2"); ot = A6.sb([128, D], F32, "ot2")
            tp = dict(st=A6.sb([128, 2, 6], F32, "st"), mv=A6.sb([128, 2], F32, "mv"), rs=A6.sb([128, 1], F32, "rs"))
            tmpd = [A6.sb([128, 512], F32, "tmpd") for _ in range(2)]
            npiece = 0
            it = 0
            nd = 0
            for g in range(4):
                S_.dma("sp", h1T, h1T_d[g])
                for tl in range(8):
                    gt = g * 8 + tl
                    ps = psum()
                    S_.cp("act", Ghi, Gall[:, gt, :])
                    S_.tt("dve", Glo, Gall[:, gt, :], Ghi, ALU.subtract)
                    S_.mm(ps[0:NE, 0:128], Ghi, identb, start=True, stop=False)
                    S_.mm(ps[0:NE, 0:128], Glo, identb, start=False, stop=True, inc=True)
                    S_.cp("act", GT, ps[0:NE, 0:128])
                    for half in range(2):
                        ps = psum()
                        S_.mm(ps, GT[0:NE, :], bd_sb[0:NE, half * 512:(half + 1) * 512], inc=True)
                        S_.cp("act" if half else "dve", acc[:, tl, half * 512:(half + 1) * 512], ps)
                for e in range(NE):
                    wde = wd[e % 2]
                    if "moe_w" not in SKIP:
                        S_.dma("pool", wde, w_down[e].re("(k p) c -> p k c", p=128))
                    src = w_gate_up[e].re("(k p) (u f) -> p k u f", p=128, u=2)
                    for p in range(4):
                        wp = wgu[npiece % 6]; npiece += 1
                        for u_ in range(2):
                            if "moe_w" not in SKIP:
                                S_.dma("pool", wp[:, :, u_, :], src[:, :, u_, p * 256:(p + 1) * 256])
                        if "moe_gu" in SKIP:
                            continue
                        for sg in range(2):
                            for f2 in range(2):
                                fc = 2 * p + f2
                                i = it % 2; it += 1
                                psg = psum(); psu = psum()
                                for k in range(8):
                                    S_.mm(psg, wp[:, k, 0, f2 * 128:(f2 + 1) * 128], h1T[:, k, sg * 512:(sg + 1) * 512],
                                          start=(k == 0), stop=(k == 7), inc=(k == 7))
                                for k in range(8):
                                    S_.mm(psu, wp[:, k, 1, f2 * 128:(f2 + 1) * 128], h1T[:, k, sg * 512:(sg + 1) * 512],
                                          start=(k == 0), stop=(k == 7), inc=(k == 7))
                                S_.ts("dve", g1[i], psg, bguT[:, fc, e:e + 1], ALU.add, 7.0, ALU.min)
                                S_.act(s1[i], g1[i], AF.Sigmoid, scale=1.702)
                                S_.act(u1[i], psu, AF.Identity, bias=bguT[:, 8 + fc, e:e + 1])
                                S_.ts("pool", u1[i], u1[i], 7.0, ALU.min, -7.0, ALU.max)
                                S_.tt("pool", g1[i], g1[i], s1[i], ALU.mult)
                                S_.stt(actT[:, fc, sg * 512:(sg + 1) * 512], u1[i], 1.0, g1[i], ALU.add, ALU.mult)
                    for tl in range(8):
                        gt = g * 8 + tl
                        if "moe_down" in SKIP:
                            continue
                        for half in range(2):
                            ps = psum()
                            for fc in range(8):
                                S_.mm(ps, actT[:, fc, tl * 128:(tl + 1) * 128], wde[:, fc, half * 512:(half + 1) * 512],
                                      start=(fc == 0), stop=(fc == 7), inc=(fc == 7))
                            a_ = acc[:, tl, half * 512:(half + 1) * 512]
                            td = tmpd[nd % 2]; nd += 1
                            S_.ts("dve", td, ps, Gall[:, gt, e:e + 1], ALU.mult)
                            S_.tt("pool", a_, a_, td, ALU.add)
                for tl in range(8):
                    gt = g * 8 + tl
                    S_.dma("sp", h1t, h1_d[g][tl * 128:(tl + 1) * 128, :])
                    S_.stt(zt, h1t, ALPHA, acc[:, tl, :], ALU.mult, ALU.add)
                    layernorm(es6, zt, gam, bet, ot, tp)
                    S_.dma("sp", out_d[gt * 128:(gt + 1) * 128, :], ot)

    def route_tile(gt, mk, hb_tile, rt):
        mkb = rt["mkb"]; pos = rt["pos"]; vv = rt["vv"]; nd = rt["nd"]; mx = rt["mx"]; sel = rt["sel"]; idf = rt["idf"]
        S_.cp("dve", mkb, mk)
        ps = psum()
        S_.mm(ps[:, 0:NE], utri_b, mkb, inc=True)
        ps2 = psum()
        S_.mm(ps2[:, 0:NE], ones_b, mkb, inc=True)
        S_.tt("dve", pos, ps[:, 0:NE], carry, ALU.add)
        S_.tt("dve", carry, ps2[:, 0:NE], carry, ALU.add)
        S_.ts("dve", vv, pos, float(CAP), ALU.is_lt)
        S_.tt("dve", vv, vv, mk, ALU.mult)
        S_.tt("dve", pos, pos, erow, ALU.add)
        S_.ts("dve", nd, pos, -1.0, ALU.mult, BIGF, ALU.add)
        S_.tt("dve", nd, nd, vv, ALU.mult)
        S_.ts("dve", nd, nd, -BIGF, ALU.add)
        S_.op("dve", lambda g: g.max(_ap(mx), _ap(nd)), [nd], [mx])
        S_.ts("dve", idf, mx[:, 0:4], -1.0, ALU.mult, float(NE * CAP), ALU.min)
        S_.cp("dve", idx_all[:, gt, :], idf)
        for k in range(4):
            S_.ts("dve", sel, nd, mx[:, k:k + 1], ALU.is_equal)
            S_.tt("dve", sel, sel, Gall[:, gt, :], ALU.mult)
            S_.red(gk_all[:, gt, k:k + 1], sel, ALU.add)
        S_.ts("dve", idf, mx[:, 0:4], -0.5 * BIGF, ALU.is_gt)
        S_.tt("dve", gk_all[:, gt, :], gk_all[:, gt, :], idf, ALU.mult)
        for k in range(4):
            S_.idma_scatter(x_rows, idx_all[:, gt, k:k + 1], hb_tile, NE * CAP - 1)

    def stage_moe_sparse():
        S_.barrier()
        with ExitStack() as es6:
            A6 = Alloc(nc, es6)
            bguT = A6.sb([128, 16, NE], F32, "bguT")
            with ExitStack() as esb:
                Ab = Alloc(nc, esb)
                bgu_sb = Ab.sb([NE, 2 * D], F32, "bgu_sb")
                bgu_hi = Ab.sb([NE, 2 * D], BF16, "bgu_hi"); bgu_lo = Ab.sb([NE, 2 * D], BF16, "bgu_lo")
                S_.dma("sp", bgu_sb, b_gate_up)
                S_.cp("act", bgu_hi, bgu_sb)
                S_.tt("dve", bgu_lo, bgu_sb, bgu_hi, ALU.subtract)
                for fc in range(16):
                    ps = psum()
                    S_.mm(ps[:, 0:NE], bgu_hi[0:NE, fc * 128:(fc + 1) * 128], identb[0:NE, 0:NE], start=True, stop=False)
                    S_.mm(ps[:, 0:NE], bgu_lo[0:NE, fc * 128:(fc + 1) * 128], identb[0:NE, 0:NE], start=False, stop=True, inc=True)
                    S_.cp("act", bguT[:, fc, :], ps[:, 0:NE])
                S_.barrier()
            with ExitStack() as ese:
                Ae = Alloc(nc, ese)
                NS = CAP // 128
                xe = [Ae.sb([128, NS, D], BF16, "xe") for _ in range(2)]
                xT = [Ae.sb([128, 8, CAP], BF16, "xT") for _ in range(2)]
                wgu = [Ae.sb([128, 8, 2, 256], BF16, "wgu") for _ in range(6)]
                wd = [Ae.sb([128, 8, D], BF16, "wd") for _ in range(2)]
                actT = Ae.sb([128, 8, CAP], BF16, "actT")
                g1 = [Ae.sb([128, 512], F32, "g1") for _ in range(2)]
                s1 = [Ae.sb([128, 512], F32, "s1") for _ in range(2)]
                u1 = [Ae.sb([128, 512], F32, "u1") for _ in range(2)]
                yo = [Ae.sb([128, D], F32, "yo") for _ in range(2)]
                blocks = [(0, 512)] + ([(512, CAP - 512)] if CAP > 512 else [])
                issued = [0]

                def issue_weights(upto):
                    while issued[0] <= min(upto, NE * 4 - 1):
                        P = issued[0]; issued[0] += 1
                        e, p = P // 4, P % 4
                        if p == 0:
                            S_.dma("pool", wd[e % 2], w_down[e].re("(k p) c -> p k c", p=128))
                        src = w_gate_up[e].re("(k p) (u f) -> p k u f", p=128, u=2)
                        for u_ in range(2):
                            S_.dma("pool", wgu[P % 6][:, :, u_, :], src[:, :, u_, p * 256:(p + 1) * 256])

                def load_x(e):
                    S_.dma("sp", xe[e % 2], x_rows[e * CAP:(e + 1) * CAP, :].re("(n p) d -> p n d", p=128))
                    for n in range(NS):
                        ps = psum().bitcast(BF16)
                        for k in range(8):
                            S_.tr(ps[:, k * 128:(k + 1) * 128], xe[e % 2][:, n, k * 128:(k + 1) * 128], identb, inc=(k == 7))
                        S_.cp("act" if n % 2 else "dve", xT[e % 2][:, :, n * 128:(n + 1) * 128], ps.re("p (k t) -> p k t", k=8))
                it = 0
                load_x(0)
                for e in range(NE):
                    if e + 1 < NE:
                        load_x(e + 1)
                    xTe = xT[e % 2]
                    for p in range(4):
                        issue_weights(e * 4 + p + 3)
                        wp = wgu[(e * 4 + p) % 6]
                        for (c0, cn) in blocks:
                            for f2 in range(2):
                                fc = 2 * p + f2
                                i = it % 2; it += 1
                                psg = psum(); psu = psum()
                                for k in range(8):
                                    S_.mm(psg[:, 0:cn], wp[:, k, 0, f2 * 128:(f2 + 1) * 128], xTe[:, k, c0:c0 + cn],
                                          start=(k == 0), stop=(k == 7), inc=(k == 7))
                                for k in range(8):
                                    S_.mm(psu[:, 0:cn], wp[:, k, 1, f2 * 128:(f2 + 1) * 128], xTe[:, k, c0:c0 + cn],
                                          start=(k == 0), stop=(k == 7), inc=(k == 7))
                                gg = g1[i][:, 0:cn]; sg_ = s1[i][:, 0:cn]; uu = u1[i][:, 0:cn]
                                S_.ts("dve", gg, psg[:, 0:cn], bguT[:, fc, e:e + 1], ALU.add, 7.0, ALU.min)
                                S_.act(sg_, gg, AF.Sigmoid, scale=1.702)
                                S_.act(uu, psu[:, 0:cn], AF.Identity, bias=bguT[:, 8 + fc, e:e + 1])
                                S_.ts("pool", uu, uu, 7.0, ALU.min, -7.0, ALU.max)
                                S_.tt("pool", gg, gg, sg_, ALU.mult)
                                S_.stt(actT[:, fc, c0:c0 + cn], uu, 1.0, gg, ALU.add, ALU.mult)
                    wde = wd[e % 2]
                    for n in range(NS):
                        yt_ = yo[n % 2]
                        for half in range(2):
                            ps = psum()
                            for fc in range(8):
                                S_.mm(ps, actT[:, fc, n * 128:(n + 1) * 128], wde[:, fc, half * 512:(half + 1) * 512],
                                      start=(fc == 0), stop=(fc == 7), inc=(fc == 7))
                            S_.cp("act" if half else "dve", yt_[:, half * 512:(half + 1) * 512], ps)
                        r0 = e * CAP + n * 128
                        S_.dma("sp", V(y_rows.ap[r0:r0 + 128, :], [Buf("yrow")]), yt_)
            S_.barrier()
            with ExitStack() as esc:
                Ac = Alloc(nc, esc)
                gam = Ac.sb([128, D], F32, "gam2"); bet = Ac.sb([128, D], F32, "bet2")
                bcast_load(gam, ln2_g); bcast_load(bet, ln2_b)
                bd_sb = Ac.sb([NE, D], F32, "bd_sb")
                S_.dma("sp", bd_sb, b_down)
                GT = Ac.sb([NE, 128], F32, "GT")
                Ghi = Ac.sb([128, NE], BF16, "Ghi"); Glo = Ac.sb([128, NE], BF16, "Glo")
                yk = [Ac.sb([128, D], F32, "yk") for _ in range(4)]
                h1t = Ac.sb([128, D], F32, "h1t2"); zt = Ac.sb([128, D], F32, "zt2"); ot = Ac.sb([128, D], F32, "ot2")
                tmpc = Ac.sb([128, D], F32, "tmpc")
                tp = dict(st=Ac.sb([128, 2, 6], F32, "st"), mv=Ac.sb([128, 2], F32, "mv"), rs=Ac.sb([128, 1], F32, "rs"))
                for gt in range(NB * NT):
                    g, tl = gt // 8, gt % 8
                    for k in range(4):
                        S_.idma_gather(yk[k], y_rows, idx_all[:, gt, k:k + 1], NE * CAP - 1)
                    S_.dma("sp", h1t, h1_d[g][tl * 128:(tl + 1) * 128, :])
                    S_.cp("act", Ghi, Gall[:, gt, :])
                    S_.tt("dve", Glo, Gall[:, gt, :], Ghi, ALU.subtract)
                    ps = psum()
                    S_.mm(ps[0:NE, 0:128], Ghi, identb, start=True, stop=False)
                    S_.mm(ps[0:NE, 0:128], Glo, identb, start=False, stop=True, inc=True)
                    S_.cp("act", GT, ps[0:NE, 0:128])
                    for half in range(2):
                        ps = psum()
                        S_.mm(ps, GT[0:NE, :], bd_sb[0:NE, half * 512:(half + 1) * 512], inc=True)
                        S_.stt(zt[:, half * 512:(half + 1) * 512], h1t[:, half * 512:(half + 1) * 512], ALPHA, ps, ALU.mult, ALU.add)
                    for k in range(4):
                        S_.ts("dve" if k % 2 else "pool", tmpc, yk[k], gk_all[:, gt, k:k + 1], ALU.mult)
                        S_.tt("pool" if k % 2 else "dve", zt, zt, tmpc, ALU.add)
                    layernorm(esc, zt, gam, bet, ot, tp)
                    S_.dma("sp", out_d[gt * 128:(gt + 1) * 128, :], ot)

    msk = G.sb([128, 5 * 64 + 2], F32, "msk")
    S_.dma("sp", msk, c_masks)
    tri32 = G.sb([128, 2, 128], F32, "tri32")
    S_.dma("sp", tri32, c_tri.re("d p t -> p d t"))
    MS = [msk[:, 0:64], msk[:, 128:192]]
    MI = [msk[:, 64:128], msk[:, 192:256]]
    MID = msk[:, 256:320]
    BLK = msk[:, 320:322]

    def dbg_out(name, src, shape, dt):
        if name in debug:
            dbg[name] = V(nc.dram_tensor("dbg_" + name, list(shape), dt, kind="ExternalOutput").ap(), [Buf("dbg_" + name)])
            S_.dma("sp", dbg[name], src)

    for b in range(NB):
        with ExitStack() as esA:
            A = Alloc(nc, esA)
            hT = A.sb([128, 8, S], BF16, "hT")
            stage_s1(b, hT)
            S_.barrier()
            if b == 0:
                dbg_out("hT", hT, [128, 8, S], BF16)
            yT_r = A.sb([128, 3, S], BF16, "yT_r")
            if "rwkv" in stages:
                stage_rwkv(b, hT, yT_r)
                S_.barrier()
            yT_a = A.sb([128, 3, S], BF16, "yT_a")
            if "att" in stages:
                stage_att(b, hT, yT_a)
                S_.barrier()
            yT_m = A.sb([128, 2, S], BF16, "yT_m")
            if "mem" in stages:
                stage_mem(b, hT, yT_m)
                S_.barrier()
            if b == 0:
                dbg_out("yT_r", yT_r, [128, 3, S], BF16)
                dbg_out("yT_a", yT_a, [128, 3, S], BF16)
                dbg_out("yT_m", yT_m, [128, 2, S], BF16)
            if "out" in stages:
                stage_out(b, [yT_r[:, 0], yT_r[:, 1], yT_r[:, 2], yT_a[:, 0], yT_a[:, 1], yT_a[:, 2], yT_m[:, 0], yT_m[:, 1]])
                S_.barrier()
    if "h1" in debug:
        dbg["h1"] = V(nc.dram_tensor("dbg_h1", [1024, D], F32, kind="ExternalOutput").ap(), [Buf("dbg_h1")])
        S_.dma("sp", dbg["h1"], h1_d[0])
        dbg["G"] = V(nc.dram_tensor("dbg_G", [128, NB * NT, NE], F32, kind="ExternalOutput").ap(), [Buf("dbg_G")])
        S_.dma("sp", dbg["G"], Gall)

    if moe:
        if SPARSE:
            stage_moe_sparse()
        else:
            stage_moe()
    S_.barrier()
    es.close()
    return nc, S_


def host_constants():
    ident = np.eye(128, dtype=np.float32)
    slopes = np.exp2(-8.0 * np.arange(1, H + 1, dtype=np.float32) / H).astype(np.float32)
    x = np.arange(STRIP_W)[None, :]
    p = np.arange(128)[:, None]
    delta = x - p - STRIP_C
    ad = np.abs(delta)
    mult = ((ad <= 64).astype(np.float32) + ((delta % 4 == 0) & (ad <= 256)).astype(np.float32)
            + ((delta % 16 == 0) & (ad <= 1024)).astype(np.float32))
    strip = np.stack([mult * np.exp(-(slopes[h] * ad.astype(np.float32))) for h in range(H)]).astype(np.float32)
    masks = np.zeros((128, 5 * 64 + 2), np.float32)
    si = np.arange(64)[:, None]; ti = np.arange(64)[None, :]
    for q in range(2):
        r = slice(64 * q, 64 * q + 64)
        masks[r, 0:64] = (si < ti); masks[r, 64:128] = (si <= ti)
        masks[r, 128:192] = (si > ti); masks[r, 192:256] = (si >= ti)
        masks[r, 256:320] = (si == ti)
        masks[r, 320 + q] = 1.0
    tri = np.zeros((2, 128, 128), np.float32)
    for q in range(2):
        r = slice(64 * q, 64 * q + 64)
        tri[0, r, r] = (si <= ti); tri[1, r, r] = (si >= ti)
    utri = (np.arange(128)[:, None] < np.arange(128)[None, :]).astype(np.float32)
    erow = np.tile((np.arange(NE, dtype=np.float32) * CAP)[None, :], (128, 1))
    return dict(c_ident=ident, c_strip=strip, c_masks=masks, c_tri=tri, c_utri=utri, c_erow=erow)


def make_in_maps(inputs):
    consts = host_constants()
    maps = []
    sq = lambda a: np.ascontiguousarray(a)
    for i in range(8):
        m = dict(consts)
        m["x"] = sq(inputs["x"][2 * i:2 * i + 2].reshape(NB * S, D))
        m["mem"] = sq(inputs["mem"][2 * i:2 * i + 2].reshape(NB * 256, D))
        for k in ("ln_in_g", "ln_in_b"):
            m[k] = sq(inputs[k])
        m["w_in"] = sq(inputs["w_in"][0])
        m["mu_shift"] = sq(inputs["mu_shift"][0])
        m["w0"] = sq(inputs["w0"][0].reshape(-1)); m["w_up"] = sq(inputs["w_up"][0])
        m["a0"] = sq(inputs["a0"][0].reshape(-1)); m["a_up"] = sq(inputs["a_up"][0])
        m["g_up"] = sq(inputs["g_up"][0])
        for k in ("k_k", "k_a", "gn_g", "gn_b", "ln1_g", "ln1_b", "ln2_g", "ln2_b", "b_router",
                  "w_mem_kv", "w_out", "w_router", "w_gate_up", "b_gate_up", "w_down", "b_down"):
            m[k] = sq(inputs[k][0])
        m["r_k"] = sq(inputs["r_k"][0].reshape(-1))
        maps.append(m)
    return maps


def kernel(**inputs):
    inputs = {k: np.asarray(v) for k, v in inputs.items()}
    nc, _ = build_program()
    maps = make_in_maps(inputs)
    res = run_bass_kernel_spmd(nc, maps, core_ids=list(range(8)))
    out = np.stack([r["out"].reshape(NB, S, D) for r in res.results]).reshape(16, S, D)
    return out.astype(np.float32)
```

```python
import numpy as np
from contextlib import ExitStack
import concourse.bass as bass
import concourse.mybir as mybir
from concourse.bass_utils import run_bass_kernel_spmd

F32 = mybir.dt.float32
BF16 = mybir.dt.bfloat16
U32 = mybir.dt.uint32
AF = mybir.ActivationFunctionType
ALU = mybir.AluOpType
AX = mybir.AxisListType

D = 1024
S = 2048
NB = 2
NT = S // 128
C = 384
H = 6
RWKV_IN = 1344
IN_W = 2752
NE = 32
ALPHA = 2.0 ** 0.25
LN_EPS = 1e-5
GN_EPS = 64e-5
LDS = float(np.exp(-0.5))
STRIP_W = 3200
STRIP_C = 1536
CAP = 640
BIGF = 1.0e6
import os
SKIP = os.environ.get('KSKIP', '')
SPARSE = os.environ.get('KDENSE', '') == ''


class Buf:
    __slots__ = ("name", "w", "rs")

    def __init__(self, name):
        self.name = name
        self.w = None
        self.rs = []


class Ev:
    __slots__ = ("sem", "val", "eng")

    def __init__(self, eng):
        self.sem = None
        self.val = None
        self.eng = eng


class V:
    __slots__ = ("ap", "bufs")

    def __init__(self, ap, bufs):
        self.ap = ap
        self.bufs = bufs if isinstance(bufs, (list, tuple)) else [bufs]

    def __getitem__(self, k):
        return V(self.ap[k], self.bufs)

    def re(self, pat, **kw):
        return V(self.ap.rearrange(pat, **kw), self.bufs)

    def bc(self, shape):
        return V(self.ap.to_broadcast(list(shape)), self.bufs)

    def un(self, axis):
        return V(self.ap.unsqueeze(axis), self.bufs)

    def bitcast(self, dt):
        return V(self.ap.bitcast(dt), self.bufs)

    def on(self, bufs):
        return V(self.ap, bufs)


def _ap(x):
    return x.ap if isinstance(x, V) else x


class Sched:
    ENG = ("pe", "act", "dve", "pool", "sp")
    NQ = 16

    def __init__(self, nc, es):
        self.nc = nc
        self.es = es
        self.eng = dict(pe=nc.tensor, act=nc.scalar, dve=nc.vector, pool=nc.gpsimd, sp=nc.sync)
        self.sem = {k: es.enter_context(nc.semaphore("s_" + k)) for k in self.ENG}
        self.cnt = {k: 0 for k in self.ENG}
        self.dsem = {q: [es.enter_context(nc.semaphore("d_%s%d" % (q, i))) for i in range(self.NQ)]
                     for q in ("sp", "pool")}
        self.dcnt = {q: 0 for q in self.dsem}
        self.seen = {k: {} for k in self.ENG}
        self.pending = {k: [] for k in self.ENG}
        self.all_dma = []
        self.ninst = 0

    def _wait(self, e, ev):
        assert ev.val is not None, "dependency on an unresolved (non-inc) event"
        key = id(ev.sem)
        if self.seen[e].get(key, 0) >= ev.val:
            return
        self.eng[e].wait_ge(ev.sem, ev.val)
        self.seen[e][key] = ev.val

    def _deps(self, e, reads, writes, is_dma):
        for v in reads:
            for b in v.bufs:
                if b.w is not None:
                    if b.w.eng == e and e == "pe" and not is_dma:
                        continue
                    self._wait(e, b.w)
        for v in writes:
            for b in v.bufs:
                if b.w is not None and (is_dma or b.w.eng != e or e != "pe"):
                    self._wait(e, b.w)
                for r in b.rs:
                    if is_dma or r.eng != e or e != "pe":
                        self._wait(e, r)

    def _record(self, ev, reads, writes):
        for v in reads:
            for b in v.bufs:
                if ev.eng is not None:
                    b.rs = [r for r in b.rs if r.eng != ev.eng]
                b.rs.append(ev)
        for v in writes:
            for b in v.bufs:
                b.w = ev
                b.rs = []

    def op(self, e, fn, reads, writes, inc=True):
        reads = [r for r in reads if isinstance(r, V)]
        writes = [w for w in writes if isinstance(w, V)]
        self._deps(e, reads, writes, False)
        ins = fn(self.eng[e])
        self.ninst += 1
        ev = Ev(e)
        if inc:
            self.cnt[e] += 1
            ins.then_inc(self.sem[e], 1)
            ev.sem = self.sem[e]
            ev.val = self.cnt[e]
            for p in self.pending[e]:
                p.sem = ev.sem
                p.val = ev.val
            self.pending[e] = []
        else:
            self.pending[e].append(ev)
        self._record(ev, reads, writes)
        return ev

    def dma(self, q, out, in_, **kw):
        self._deps(q, [in_], [out], True)
        n = self.dcnt[q]
        self.dcnt[q] += 1
        sem = self.dsem[q][n % self.NQ]
        val = 16 * (n // self.NQ + 1)
        if n >= self.NQ:
            pe_ = Ev(None); pe_.sem = sem; pe_.val = val - 16
            self._wait(q, pe_)
        ins = self.eng[q].dma_start(out=_ap(out), in_=_ap(in_), **kw)
        ins.then_inc(sem, 16)
        self.ninst += 1
        ev = Ev(None)
        ev.sem = sem
        ev.val = val
        self._record(ev, [in_], [out])
        self.all_dma.append(ev)
        return ev

    def _idma(self, out, out_off, in_, in_off, reads, writes, bound):
        q = "pool"
        self._deps(q, reads, writes, True)
        n = self.dcnt[q]
        self.dcnt[q] += 1
        sem = self.dsem[q][n % self.NQ]
        val = 16 * (n // self.NQ + 1)
        if n >= self.NQ:
            pe_ = Ev(None); pe_.sem = sem; pe_.val = val - 16
            self._wait(q, pe_)
        ins = self.eng[q].indirect_dma_start(out=_ap(out), out_offset=out_off, in_=_ap(in_), in_offset=in_off)
        ins.then_inc(sem, 16)
        self.ninst += 1
        ev = Ev(None)
        ev.sem = sem
        ev.val = val
        self._record(ev, reads, writes)
        return ev

    def idma_scatter(self, dram, idx, src, bound):
        off = bass.IndirectOffsetOnAxis(ap=_ap(idx), axis=0)
        return self._idma(dram, off, src, None, [src, idx], [V(dram.ap, [Buf("scatter")])], bound)

    def idma_gather(self, dst, dram, idx, bound):
        off = bass.IndirectOffsetOnAxis(ap=_ap(idx), axis=0)
        return self._idma(dst, None, dram, off, [idx], [dst], bound)

    def barrier(self):
        for e in self.ENG:
            for o in self.ENG:
                if self.cnt[o] > 0:
                    assert not self.pending[o]
                    ev = Ev(o)
                    ev.sem = self.sem[o]
                    ev.val = self.cnt[o]
                    self._wait(e, ev)
            for q in self.dsem:
                n = self.dcnt[q]
                for i in range(min(n, self.NQ)):
                    ev = Ev(None)
                    ev.sem = self.dsem[q][i]
                    ev.val = 16 * ((n - 1 - i) // self.NQ + 1)
                    self._wait(e, ev)

    def _pe_rows(self, lhsT):
        a = _ap(lhsT)
        lo = a.base_partition()
        rows = (lo, lo + a.shape[0])
        prev = getattr(self, "_last_pe", None)
        if prev is not None:
            pins, pev, prow = prev
            if rows[0] >= prow[1] or prow[0] >= rows[1]:
                if pev.val is None:
                    self.cnt["pe"] += 1
                    pins.then_inc(self.sem["pe"], 1)
                    for p in self.pending["pe"]:
                        p.sem = self.sem["pe"]
                        p.val = self.cnt["pe"]
                    self.pending["pe"] = []
                self._wait("pe", pev)
        return rows

    def _pe_op(self, fn, lhsT, reads, writes, inc):
        rows = self._pe_rows(lhsT)
        box = []

        def f(e):
            i = fn(e)
            box.append(i)
            return i
        ev = self.op("pe", f, reads, writes, inc=inc)
        self._last_pe = (box[0], ev, rows)
        return ev

    def mm(self, out, lhsT, rhs, start=True, stop=True, inc=False):
        return self._pe_op(lambda e: e.matmul(_ap(out), _ap(lhsT), _ap(rhs), start=start, stop=stop),
                           lhsT, [lhsT, rhs], [out], inc)

    def tr(self, out, in_, ident, inc=False):
        return self._pe_op(lambda e: e.transpose(_ap(out), _ap(in_), _ap(ident)), in_, [in_, ident], [out], inc)

    def act(self, out, in_, func, bias=None, scale=None, accum_out=None):
        kw = {}
        if bias is not None:
            kw["bias"] = _ap(bias)
        if scale is not None:
            kw["scale"] = _ap(scale)
        if accum_out is not None:
            kw["accum_out"] = _ap(accum_out)
        return self.op("act", lambda e: e.activation(_ap(out), _ap(in_), func, **kw),
                       [in_, bias, scale], [out, accum_out])

    def ts(self, e, out, in0, s1, op0, s2=None, op1=None, accum_out=None):
        kw = {}
        if op1 is not None:
            kw["op1"] = op1
        if accum_out is not None:
            kw["accum_out"] = _ap(accum_out)
        return self.op(e, lambda g: g.tensor_scalar(_ap(out), _ap(in0), _ap(s1), _ap(s2), op0, **kw),
                       [in0, s1, s2], [out, accum_out])

    def tt(self, e, out, in0, in1, op):
        return self.op(e, lambda g: g.tensor_tensor(_ap(out), _ap(in0), _ap(in1), op), [in0, in1], [out])

    def stt(self, out, in0, scalar, in1, op0, op1):
        return self.op("dve", lambda g: g.scalar_tensor_tensor(_ap(out), _ap(in0), _ap(scalar), _ap(in1), op0, op1),
                       [in0, scalar, in1], [out])

    def cp(self, e, out, in_):
        if e == "act":
            return self.op("act", lambda g: g.copy(_ap(out), _ap(in_)), [in_], [out])
        return self.op(e, lambda g: g.tensor_copy(_ap(out), _ap(in_)), [in_], [out])

    def red(self, out, in_, op, axis=AX.X):
        return self.op("dve", lambda g: g.tensor_reduce(_ap(out), _ap(in_), axis, op), [in_], [out])

    def memset(self, e, out, val):
        return self.op(e, lambda g: g.memset(_ap(out), val), [], [out])


class Alloc:
    N = [0]

    def __init__(self, nc, es):
        self.nc = nc
        self.es = es

    def sb(self, shape, dt, name=None, nbuf=1):
        Alloc.N[0] += 1
        name = "%s_%d" % (name or "t", Alloc.N[0])
        t = self.es.enter_context(self.nc.sbuf_tensor(name, list(shape), dt))
        return V(t[:], [Buf(name)])

    def dram(self, name, shape, dt, kind="Internal"):
        t = self.nc.dram_tensor(name, list(shape), dt, kind=kind)
        return V(t.ap(), [Buf(name)])


def build_program(debug=(), moe=True, stages=('rwkv', 'att', 'mem', 'out')):
    nc = bass.Bass("TRN2", target_bir_lowering=False)
    es = ExitStack()
    S_ = Sched(nc, es)
    G = Alloc(nc, es)
    dbg = {}

    def din(name, shape, dt=F32):
        return V(nc.dram_tensor(name, list(shape), dt, kind="ExternalInput").ap(), [Buf(name)])

    x_d = din("x", [NB * S, D])
    mem_d = din("mem", [NB * 256, D])
    ln_in_g = din("ln_in_g", [D]); ln_in_b = din("ln_in_b", [D])
    w_in = din("w_in", [D, IN_W])
    mu_d = din("mu_shift", [RWKV_IN])
    w0_d = din("w0", [2 * C]); w_up_d = din("w_up", [2, 32, C])
    a0_d = din("a0", [2 * C]); a_up_d = din("a_up", [2, 32, C])
    g_up_d = din("g_up", [64, C])
    k_k_d = din("k_k", [C]); k_a_d = din("k_a", [C]); r_k_d = din("r_k", [C])
    gn_g_d = din("gn_g", [C]); gn_b_d = din("gn_b", [C])
    w_mem_kv = din("w_mem_kv", [D, 512])
    w_out = din("w_out", [D, D])
    ln1_g = din("ln1_g", [D]); ln1_b = din("ln1_b", [D])
    w_router = din("w_router", [D, NE]); b_router = din("b_router", [NE])
    if moe:
        w_gate_up = din("w_gate_up", [NE, D, 2 * D]); b_gate_up = din("b_gate_up", [NE, 2 * D])
        w_down = din("w_down", [NE, D, D]); b_down = din("b_down", [NE, D])
    ln2_g = din("ln2_g", [D]); ln2_b = din("ln2_b", [D])
    c_ident = din("c_ident", [128, 128])
    c_strip = din("c_strip", [H, 128, STRIP_W])
    c_masks = din("c_masks", [128, 5 * 64 + 2])
    c_tri = din("c_tri", [2, 128, 128])
    c_utri = din("c_utri", [128, 128])
    c_erow = din("c_erow", [128, NE])

    out_d = V(nc.dram_tensor("out", [NB * S, D], F32, kind="ExternalOutput").ap(), [Buf("out")])

    h0_d = [G.dram("h0_%d" % b, [S, D], F32) for b in range(NB)]
    h1_d = [G.dram("h1_%d" % g, [1024, D], F32) for g in range(4)]
    h1T_d = [G.dram("h1T_%d" % g, [128, 8, 1024], BF16) for g in range(4)]

    banks = []
    for i in range(8):
        t = es.enter_context(nc.psum_tensor("ps%d" % i, [128, 512], F32))
        banks.append(V(t[:], [Buf("ps%d" % i)]))
    bank_i = {"gen": 0, "acc": 0}
    bank_grp = {"gen": [2, 3, 4, 5, 6, 7], "acc": [0, 1]}

    def psum(grp="gen"):
        l = bank_grp[grp]
        b = banks[l[bank_i[grp] % len(l)]]
        bank_i[grp] += 1
        return b

    ident32 = G.sb([128, 128], F32, "ident32")
    identb = G.sb([128, 128], BF16, "identb")
    S_.dma("sp", ident32, c_ident)
    S_.cp("dve", identb, ident32)
    Gall = G.sb([128, NB * NT, NE], F32, "Gall")
    ones_b = G.sb([128, 128], BF16, "ones_b")
    S_.memset("dve", ones_b, 1.0)
    zeros_b = G.sb([128, 512], BF16, "zeros_b")
    S_.memset("dve", zeros_b, 0.0)
    carry = G.sb([128, NE], F32, "carry")
    S_.memset("dve", carry, 0.0)
    idx_all = G.sb([128, NB * NT, 4], U32, "idx_all")
    gk_all = G.sb([128, NB * NT, 4], F32, "gk_all")
    utri32 = G.sb([128, 128], F32, "utri32")
    S_.dma("sp", utri32, c_utri)
    utri_b = G.sb([128, 128], BF16, "utri_b")
    S_.cp("dve", utri_b, utri32)
    erow = G.sb([128, NE], F32, "erow")
    S_.dma("sp", erow, c_erow)
    x_rows = G.dram("x_rows", [NE * CAP + 128, D], BF16)
    y_rows = G.dram("y_rows", [NE * CAP + 128, D], F32)
    zeros32 = G.sb([128, D], F32, "zeros32")
    S_.memset("pool", zeros32, 0.0)
    S_.dma("sp", V(y_rows.ap[NE * CAP:NE * CAP + 128, :], [Buf("ydump")]), zeros32)
    zb = zeros32.bitcast(BF16).re("p (n d) -> p n d", n=2)
    xr = x_rows.re("(n p) d -> p n d", p=128)
    nrow = (NE * CAP + 128) // 128
    for n0 in range(0, nrow, 2):
        nn = min(2, nrow - n0)
        S_.dma("sp", V(xr.ap[:, n0:n0 + nn, :], [Buf("xfill")]), zb[:, 0:nn, :])

    def bcast_load(dst, src1d, q="sp"):
        S_.dma(q, dst, V(src1d.ap.partition_broadcast(128), src1d.bufs))

    def layernorm(es2, xin, gam, bet, out, tmp_pool, eng2="pool"):
        st = tmp_pool["st"]; mv = tmp_pool["mv"]; rs = tmp_pool["rs"]
        for i in range(2):
            S_.op("dve", lambda g, i=i: g.bn_stats(_ap(st[:, i, :]), _ap(xin[:, i * 512:(i + 1) * 512])),
                  [xin], [st])
        S_.op("dve", lambda g: g.bn_aggr(_ap(mv), _ap(st)), [st], [mv])
        S_.ts("dve", rs, mv[:, 1:2], LN_EPS, ALU.add)
        S_.act(rs, rs, AF.Sqrt)
        S_.op("dve", lambda g: g.reciprocal(_ap(rs), _ap(rs)), [rs], [rs])
        S_.ts("dve", out, xin, mv[:, 0:1], ALU.subtract, rs[:, 0:1], ALU.mult)
        S_.tt(eng2, out, out, gam, ALU.mult)
        S_.tt(eng2, out, out, bet, ALU.add)

    def stage_s1(b, hT):
        with ExitStack() as es1:
            A1 = Alloc(nc, es1)
            gam = A1.sb([128, D], F32, "gam"); bet = A1.sb([128, D], F32, "bet")
            bcast_load(gam, ln_in_g); bcast_load(bet, ln_in_b)
            xt = [A1.sb([128, D], F32, "xt") for _ in range(2)]
            ht = [A1.sb([128, D], F32, "ht") for _ in range(2)]
            hb = [A1.sb([128, D], BF16, "hb") for _ in range(2)]
            tp = [dict(st=A1.sb([128, 2, 6], F32, "st"), mv=A1.sb([128, 2], F32, "mv"),
                       rs=A1.sb([128, 1], F32, "rs")) for _ in range(2)]
            for t in range(NT):
                i = t % 2
                S_.dma("sp", xt[i], x_d[b * S + t * 128: b * S + (t + 1) * 128, :])
                layernorm(es1, xt[i], gam, bet, ht[i], tp[i])
                S_.dma("sp", h0_d[b][t * 128:(t + 1) * 128, :], ht[i])
                S_.cp("act", hb[i], ht[i])
                ps = psum().bitcast(BF16)
                for k in range(8):
                    S_.tr(ps[:, k * 128:(k + 1) * 128], hb[i][:, k * 128:(k + 1) * 128], identb, inc=(k == 7))
                S_.cp("act", hT[:, :, t * 128:(t + 1) * 128], ps.re("p (k t) -> p k t", k=8))

    def attn_core(nh, KTv, QTv, Vv, ytok, n_st, s_range, strips=None):
        with ExitStack() as esc:
            Ac = Alloc(nc, esc)
            Eb = [Ac.sb([128, 512], BF16, "E") for _ in range(3)]
            Pb = [Ac.sb([128, 512], BF16, "P") for _ in range(3)]
            rec = Ac.sb([128, 4], F32, "rec")
            it = 0
            for h in range(nh):
                c, p0 = h // 2, 64 * (h % 2)
                for tq in range(4):
                    s_lo, s_hi, jvalid = s_range(tq)
                    acc = psum("acc")
                    accv = acc[:, 0:260].re("p (j d) -> p j d", j=4)
                    S_.mm(acc[:, 0:260], zeros_b[:, 0:128], zeros_b[:, 0:260], start=True, stop=False)
                    for st_ in range(s_lo, s_hi + 1):
                        sc = psum()
                        S_.mm(sc, KTv[p0:p0 + 64, c, st_ * 128:(st_ + 1) * 128],
                              QTv[p0:p0 + 64, c, tq * 512:(tq + 1) * 512], inc=True)
                        E = Eb[it % 3]; P = Pb[it % 3]; it += 1
                        S_.act(E, sc, AF.Exp, scale=0.125)
                        if strips is not None:
                            off = tq * 512 - st_ * 128 + STRIP_C
                            S_.tt("dve" if it % 3 else "pool", P, E, strips[:, h, off:off + 512], ALU.mult)
                        else:
                            P = E
                        for j in range(4):
                            ok, last = jvalid(tq, j, st_)
                            if not ok:
                                continue
                            S_.mm(accv[:, j, :], P[:, j * 128:(j + 1) * 128], Vv[:, st_, h, :],
                                  start=False, stop=last, inc=True)
                    S_.op("dve", lambda g: g.reciprocal(_ap(rec), _ap(accv[:, :, 64])), [accv], [rec])
                    for j in range(4):
                        S_.ts("dve", ytok[:, 4 * tq + j, h * 64:(h + 1) * 64], accv[:, j, 0:64], rec[:, j:j + 1], ALU.mult)

    def proj_fm(dst, Wv, hT, nchunk, col0):
        n = 0
        for c in range(nchunk):
            for tg in range(4):
                ps = psum()
                for k in range(8):
                    S_.mm(ps, Wv[:, k, col0 + c * 128: col0 + (c + 1) * 128],
                          hT[:, k, tg * 512:(tg + 1) * 512], start=(k == 0), stop=(k == 7), inc=(k == 7))
                S_.cp("act" if n % 2 else "dve", dst[:, c, tg * 512:(tg + 1) * 512], ps)
                n += 1

    def ytok_to_yT(ytok, yT, nchunk):
        for t in range(NT):
            ps = psum().bitcast(BF16)
            for cc in range(nchunk):
                S_.tr(ps[:, cc * 128:(cc + 1) * 128], ytok[:, t, cc * 128:(cc + 1) * 128], identb, inc=(cc == nchunk - 1))
            S_.cp("act" if t % 2 else "dve", yT[:, 0:nchunk, t * 128:(t + 1) * 128],
                  ps[:, 0:nchunk * 128].re("p (k t) -> p k t", k=nchunk))

    def stage_att(b, hT, yT_a):
        with ExitStack() as es2:
            A2 = Alloc(nc, es2)
            Wqkv = A2.sb([128, 8, 3 * C], BF16, "Wqkv")
            S_.dma("pool", Wqkv, w_in[:, RWKV_IN:RWKV_IN + 3 * C].re("(k p) c -> p k c", p=128))
            strips = A2.sb([128, H, STRIP_W], BF16, "strips")
            for h in range(H):
                S_.dma("pool", strips[:, h, :], c_strip[h])
            QT = A2.sb([128, 3, S], BF16, "QT")
            KT = A2.sb([128, 3, S], BF16, "KT")
            Vt = A2.sb([128, NT, H, 65], BF16, "Vt")
            ytok = A2.sb([128, NT, C], BF16, "ytok")
            S_.memset("pool", Vt[:, :, :, 64:65], 1.0)
            proj_fm(QT, Wqkv, hT, 3, 0)
            proj_fm(KT, Wqkv, hT, 3, C)
            for t in range(NT):
                ps = psum()
                for k in range(8):
                    S_.mm(ps[:, 0:C], hT[:, k, t * 128:(t + 1) * 128], Wqkv[:, k, 2 * C:3 * C],
                          start=(k == 0), stop=(k == 7), inc=(k == 7))
                S_.cp("act" if t % 2 else "dve", Vt[:, t, :, 0:64], ps[:, 0:C].re("p (h d) -> p h d", h=H))

            def s_range(tq):
                s_hi = min(NT - 1, 4 * tq + 3 + 8)

                def jvalid(tq, j, st_):
                    tt_ = 4 * tq + j
                    return abs(tt_ - st_) <= 8, (st_ == s_hi and j == 3)
                return max(0, 4 * tq - 8), s_hi, jvalid
            attn_core(H, KT, QT, Vt, ytok, NT, s_range, strips)
            ytok_to_yT(ytok, yT_a, 3)

    def stage_mem(b, hT, yT_m):
        with ExitStack() as es3:
            A3 = Alloc(nc, es3)
            Wkv = A3.sb([128, 8, 512], BF16, "Wkv")
            S_.dma("pool", Wkv, w_mem_kv.re("(k p) c -> p k c", p=128))
            Wqm = A3.sb([128, 8, 256], BF16, "Wqm")
            S_.dma("pool", Wqm, w_in[:, RWKV_IN + 3 * C:IN_W].re("(k p) c -> p k c", p=128))
            memb = A3.sb([128, 2, D], BF16, "memb")
            S_.dma("pool", memb, mem_d[b * 256:(b + 1) * 256, :].re("(m p) d -> p m d", p=128))
            memT = A3.sb([128, 8, 256], BF16, "memT")
            for m in range(2):
                ps = psum().bitcast(BF16)
                for k in range(8):
                    S_.tr(ps[:, k * 128:(k + 1) * 128], memb[:, m, k * 128:(k + 1) * 128], identb, inc=(k == 7))
                S_.cp("act", memT[:, :, m * 128:(m + 1) * 128], ps.re("p (k t) -> p k t", k=8))
            KmT = A3.sb([128, 2, 256], BF16, "KmT")
            for c in range(2):
                ps = psum()
                for k in range(8):
                    S_.mm(ps[:, 0:256], Wkv[:, k, c * 128:(c + 1) * 128], memT[:, k, :],
                          start=(k == 0), stop=(k == 7), inc=(k == 7))
                S_.cp("dve", KmT[:, c, :], ps[:, 0:256])
            Vm = A3.sb([128, 2, 4, 65], BF16, "Vm")
            S_.memset("pool", Vm[:, :, :, 64:65], 1.0)
            for m in range(2):
                ps = psum()
                for k in range(8):
                    S_.mm(ps[:, 0:256], memT[:, k, m * 128:(m + 1) * 128], Wkv[:, k, 256:512],
                          start=(k == 0), stop=(k == 7), inc=(k == 7))
                S_.cp("dve", Vm[:, m, :, 0:64], ps[:, 0:256].re("p (h d) -> p h d", h=4))
            QmT = A3.sb([128, 2, S], BF16, "QmT")
            proj_fm(QmT, Wqm, hT, 2, 0)
            ytok = A3.sb([128, NT, 256], BF16, "ytokm")

            def s_range(tq):
                return 0, 1, (lambda tq, j, st_: (True, st_ == 1 and j == 3))
            attn_core(4, KmT, QmT, Vm, ytok, 2, s_range, None)
            ytok_to_yT(ytok, yT_m, 2)

    def stage_rwkv(b, hT, yT_r):
        with ExitStack() as es4:
            A4 = Alloc(nc, es4)
            f32t = lambda name, n=C: A4.sb([128, n], F32, name)
            Wr = A4.sb([128, 16, RWKV_IN], BF16, "Wr")
            with ExitStack() as esw:
                Aw = Alloc(nc, esw)
                mub = Aw.sb([128, RWKV_IN], F32, "mub"); bcast_load(mub, mu_d)
                omb = Aw.sb([128, RWKV_IN], F32, "omb"); hmb = Aw.sb([128, RWKV_IN], F32, "hmb")
                S_.ts("dve", omb, mub, -1.0, ALU.mult, 1.0, ALU.add)
                S_.ts("pool", hmb, mub, 0.5, ALU.mult)
                stg = [Aw.sb([128, 8, 336], F32, "stg") for _ in range(2)]
                for pc in range(4):
                    cs = slice(pc * 336, (pc + 1) * 336)
                    S_.dma("sp", stg[pc % 2], w_in[:, cs].re("(k p) c -> p k c", p=128))
                    S_.tt("dve", Wr[:, 0:8, cs], stg[pc % 2], omb[:, cs].un(1).bc([128, 8, 336]), ALU.mult)
                    S_.tt("pool", Wr[:, 8:16, cs], stg[pc % 2], hmb[:, cs].un(1).bc([128, 8, 336]), ALU.mult)
                S_.barrier()
            kkb = f32t("kkb"); bcast_load(kkb, k_k_d)
            kab = f32t("kab"); bcast_load(kab, k_a_d)
            omkab = f32t("omkab"); S_.ts("dve", omkab, kab, -1.0, ALU.mult, 1.0, ALU.add)
            rkb = f32t("rkb"); bcast_load(rkb, r_k_d)
            gngb = f32t("gngb"); bcast_load(gngb, gn_g_d)
            gnbb = f32t("gnbb"); bcast_load(gnbb, gn_b_d)
            w0b = f32t("w0b", 2 * C); bcast_load(w0b, w0_d)
            a0b = f32t("a0b", 2 * C); bcast_load(a0b, a0_d)
            gup = A4.sb([128, C], BF16, "gup"); S_.dma("pool", gup[0:64, :], g_up_d)
            wupbd = A4.sb([128, 2 * C], BF16, "wupbd"); S_.memset("pool", wupbd, 0.0)
            S_.dma("pool", wupbd[64:96, 0:C], w_up_d[0]); S_.dma("pool", wupbd[96:128, C:2 * C], w_up_d[1])
            aupbd = A4.sb([128, 2 * C], BF16, "aupbd"); S_.memset("pool", aupbd, 0.0)
            S_.dma("pool", aupbd[0:32, 0:C], a_up_d[0]); S_.dma("pool", aupbd[32:64, C:2 * C], a_up_d[1])
            Yacc = A4.sb([128, NT, C], F32, "Yacc")
            T32 = A4.sb([128, 3, 64], F32, "T32"); Tb = A4.sb([128, 3, 64], BF16, "Tb"); Ttmp = A4.sb([128, 3, 64], F32, "Ttmp")
            hs = A4.sb([128, 8, 128], BF16, "hs")
            r32 = f32t("r32"); k32 = f32t("k32"); v32 = f32t("v32")
            sgw = f32t("sgw"); a32 = f32t("a32"); a32b = f32t("a32b")
            ecw = f32t("ecw"); encw = f32t("encw")
            kk = f32t("kk"); sq = f32t("sq"); kd = f32t("kd"); bb = f32t("bb")
            ss = A4.sb([128, 6], F32, "ss"); rn = A4.sb([128, 6], F32, "rn")
            lor = A4.sb([128, 3, 128], BF16, "lor")
            tok4 = A4.sb([128, 4, C], BF16, "tok4")
            Pp = [A4.sb([128, 6, 64], BF16, "Pp") for _ in range(2)]
            PTp = [A4.sb([128, 6, 64], BF16, "PTp") for _ in range(2)]
            Zs = A4.sb([128, C], BF16, "Zs"); Us = A4.sb([128, C], BF16, "Us")
            fs1 = A4.sb([128, 6], F32, "fs1"); fs2 = A4.sb([128, 6], F32, "fs2"); fs3 = A4.sb([128, 6], F32, "fs3")
            yob = A4.sb([128, C], BF16, "yob")
            sets = []
            for i in range(2):
                sets.append(dict(
                    FM=A4.sb([128, 4, 3, 128], BF16, "FM"), Vb=A4.sb([128, C], BF16, "Vb"),
                    XT=[A4.sb([128, 6, 64], BF16, "XT") for _ in range(2)],
                    LkT=A4.sb([128, 6, 64], BF16, "LkT"), RbT=A4.sb([128, 6, 64], BF16, "RbT"),
                    RkT=A4.sb([128, 6, 64], BF16, "RkT"), WCe=A4.sb([128, 3, 2], F32, "WCe"),
                    Bt=A4.sb([128, C], BF16, "Bt"), Kt=A4.sb([128, C], BF16, "Kt"),
                    g32=f32t("g32"), bv=f32t("bv")))

            def v6(x):
                return x.re("p (h j) -> p h j", h=6)
            HORD = (0, 2, 4, 1, 3, 5)

            def prep(t, d, st):
                lo = t * 128
                if 0 < t < NT - 1:
                    S_.tt("pool", hs, hT[:, :, lo - 1:lo + 127], hT[:, :, lo + 1:lo + 129], ALU.add)
                elif t == 0:
                    S_.tt("pool", hs[:, :, 1:128], hT[:, :, 0:127], hT[:, :, 2:129], ALU.add)
                    S_.cp("pool", hs[:, :, 0:1], hT[:, :, 1:2])
                else:
                    S_.tt("pool", hs[:, :, 0:127], hT[:, :, lo - 1:lo + 126], hT[:, :, lo + 1:lo + 128], ALU.add)
                    S_.cp("pool", hs[:, :, 127:128], hT[:, :, lo + 126:lo + 127])

                def lhs(kc):
                    return hT[:, kc, lo:lo + 128] if kc < 8 else hs[:, kc - 8, :]
                for part, dst in ((0, r32), (1, k32), (2, v32)):
                    ps = psum()
                    for kc in range(16):
                        S_.mm(ps[:, 0:C], lhs(kc), Wr[:, kc, part * C:(part + 1) * C], start=(kc == 0), stop=(kc == 15), inc=(kc == 15))
                    S_.cp("act", dst, ps[:, 0:C])
                S_.cp("pool", st["Vb"], v32)
                ps = psum()
                for kc in range(16):
                    S_.mm(ps[:, 0:128], Wr[:, kc, 3 * C:3 * C + 128], lhs(kc), start=(kc == 0), stop=(kc == 15), inc=(kc == 15))
                for kc in range(16):
                    S_.mm(ps[0:64, 128:256], Wr[:, kc, 3 * C + 128:3 * C + 192], lhs(kc), start=(kc == 0), stop=(kc == 15), inc=(kc == 15))
                S_.act(lor[0:64, 0, :], ps[0:64, 0:128], AF.Sigmoid)
                S_.act(lor[64:128, 1, :], ps[64:128, 0:128], AF.Tanh)
                S_.cp("act", lor[0:64, 2, :], ps[0:64, 128:256])
                psg = psum()
                S_.mm(psg[:, 0:C], lor[0:64, 0, :], gup[0:64, :], inc=True)
                S_.cp("act", st["g32"], psg[:, 0:C])
                psw = psum()
                S_.mm(psw[:, 0:C], lor[64:128, 1, :], wupbd[64:128, d * C:(d + 1) * C], inc=True)
                S_.tt("dve", sgw, psw[:, 0:C], w0b[:, d * C:(d + 1) * C], ALU.add)
                S_.act(sgw, sgw, AF.Sigmoid)
                psa = psum()
                S_.mm(psa[:, 0:C], lor[0:64, 2, :], aupbd[0:64, d * C:(d + 1) * C], inc=True)
                S_.tt("dve", a32, psa[:, 0:C], a0b[:, d * C:(d + 1) * C], ALU.add)
                S_.act(a32, a32, AF.Sigmoid)
                pscs = psum()
                S_.mm(pscs[:, 0:C], tri32[:, d, :], sgw, inc=True)
                pswc = psum()
                for c in range(3):
                    S_.mm(pswc[:, c * 2:(c + 1) * 2], sgw[:, c * 128:(c + 1) * 128], BLK, inc=(c == 2))
                S_.act(st["WCe"].re("p c q -> p (c q)"), pswc[:, 0:6], AF.Exp, scale=-LDS)
                S_.act(ecw, pscs[:, 0:C], AF.Exp, scale=-LDS)
                S_.act(encw, pscs[:, 0:C], AF.Exp, scale=LDS)
                S_.act(sgw, sgw, AF.Exp, scale=LDS)
                S_.tt("dve", sgw, sgw, ecw, ALU.mult)
                S_.tt("pool", kk, k32, kkb, ALU.mult)
                S_.tt("pool", sq, kk, kk, ALU.mult)
                S_.red(ss, v6(sq), ALU.add)
                S_.act(rn, ss, AF.Sqrt)
                S_.ts("dve", rn, rn, 1e-12, ALU.max)
                S_.op("dve", lambda g: g.reciprocal(_ap(rn), _ap(rn)), [rn], [rn])
                S_.tt("dve", v6(kk), v6(kk), rn.un(2).bc([128, 6, 64]), ALU.mult)
                S_.tt("pool", kd, a32, kab, ALU.mult)
                S_.tt("pool", kd, kd, omkab, ALU.add)
                S_.tt("pool", kd, kd, k32, ALU.mult)
                S_.tt("dve", bb, kk, a32, ALU.mult)
                S_.tt("dve", tok4[:, 0, :], kk, sgw, ALU.mult)
                S_.stt(tok4[:, 1, :], bb, -1.0, encw, ALU.mult, ALU.mult)
                S_.tt("pool", tok4[:, 2, :], kd, encw, ALU.mult)
                S_.tt("pool", tok4[:, 3, :], r32, ecw, ALU.mult)
                S_.cp("act", st["Bt"], tok4[:, 1, :])
                S_.cp("act", st["Kt"], tok4[:, 2, :])
                if d == 1:
                    psa2 = psum()
                    S_.mm(psa2[:, 0:C], lor[0:64, 2, :], aupbd[0:64, 0:C], inc=True)
                    S_.tt("dve", a32b, psa2[:, 0:C], a0b[:, 0:C], ALU.add)
                    S_.act(a32b, a32b, AF.Sigmoid)
                    S_.tt("pool", a32b, a32b, a32, ALU.add)
                    S_.tt("pool", a32b, a32b, kab, ALU.mult)
                    S_.stt(a32b, omkab, 2.0, a32b, ALU.mult, ALU.add)
                    S_.tt("pool", a32b, a32b, k32, ALU.mult)
                    S_.tt("pool", a32b, a32b, rkb, ALU.mult)
                    S_.tt("pool", a32b, a32b, r32, ALU.mult)
                    S_.red(fs3, v6(a32b), ALU.add)
                    S_.tt("dve", v6(st["bv"]), v6(v32), fs3.un(2).bc([128, 6, 64]), ALU.mult)
                FM = st["FM"]
                for half in range(2):
                    ps = psum().bitcast(BF16)
                    n = 0
                    for o in (2 * half, 2 * half + 1):
                        for c in range(3):
                            S_.tr(ps[:, n * 128:(n + 1) * 128], tok4[:, o, c * 128:(c + 1) * 128], identb, inc=(n == 5))
                            n += 1
                    S_.cp("act" if half else "dve", FM[:, 2 * half:2 * half + 2, :, :].re("p o c t -> p (o c) t"),
                          ps[:, 0:768].re("p (n t) -> p n t", n=6))
                KKi, Bi, Ki, Ri = 0, 1, 2, 3
                st["XTf"] = st["XT"][1]
                if "cc" in SKIP:
                    return

                def cc_mm(ps, li, ri):
                    n = 0
                    for q in range(2):
                        q0 = 64 * q
                        for h in HORD:
                            c, p0 = h // 2, 64 * (h % 2)
                            n += 1
                            S_.mm(ps[q0:q0 + 64, h * 64:(h + 1) * 64], FM[p0:p0 + 64, li, c, q0:q0 + 64],
                                  FM[p0:p0 + 64, ri, c, q0:q0 + 64], inc=(n == 12))

                def mbc(m):
                    return m.un(1).bc([128, 6, 64])
                XT = st["XT"]
                ps = psum(); cc_mm(ps, Bi, KKi)
                S_.tt("dve", PTp[0], v6(ps[:, 0:C]), mbc(MS[d]), ALU.mult)
                S_.tt("pool", XT[0], PTp[0], mbc(MID), ALU.add)
                ps = psum(); cc_mm(ps, KKi, Bi)
                S_.tt("dve", Pp[0], v6(ps[:, 0:C]), mbc(MS[1 - d]), ALU.mult)
                ps = psum(); cc_mm(ps, Ki, KKi)
                S_.tt("dve", st["LkT"], v6(ps[:, 0:C]), mbc(MS[d]), ALU.mult)
                ps = psum(); cc_mm(ps, Bi, Ri)
                S_.tt("dve", st["RbT"], v6(ps[:, 0:C]), mbc(MI[d]), ALU.mult)
                ps = psum(); cc_mm(ps, Ki, Ri)
                S_.tt("dve", st["RkT"], v6(ps[:, 0:C]), mbc(MI[d]), ALU.mult)
                if "chain" in SKIP:
                    return
                cur = 0
                for lvl in range(1, 6):
                    nxt = 1 - cur

                    def sq_mm(ps, lt, rt):
                        n = 0
                        for q in range(2):
                            q0 = 64 * q
                            for h in range(H):
                                n += 1
                                S_.mm(ps[q0:q0 + 64, h * 64:(h + 1) * 64], lt[q0:q0 + 64, h, :], rt[q0:q0 + 64, h, :], inc=(n == 12))
                    psP = psum(); sq_mm(psP, PTp[cur], Pp[cur])
                    if lvl < 5:
                        psPT = psum(); sq_mm(psPT, Pp[cur], PTp[cur])
                    S_.cp("act", Pp[nxt], v6(psP[:, 0:C]))
                    if lvl < 5:
                        S_.cp("dve", PTp[nxt], v6(psPT[:, 0:C]))
                    psX = psum(); sq_mm(psX, Pp[nxt], XT[(lvl - 1) % 2])
                    S_.tt("dve", XT[lvl % 2], v6(psX[:, 0:C]), XT[(lvl - 1) % 2], ALU.add)
                    cur = nxt
                st["XTf"] = XT[5 % 2]

            def seq_chunk(t, q, d, st):
                q0 = 64 * q
                FM = st["FM"]; Vb = st["Vb"]; XT = st["XTf"]
                hc = lambda h: slice(h * 64, (h + 1) * 64)
                psZ = psum()
                S_.mm(psZ[q0:q0 + 64, 0:C], zeros_b[:, 0:64], zeros_b[:, 0:C], start=True, stop=False)
                for h in HORD:
                    c, p0 = h // 2, 64 * (h % 2)
                    S_.mm(psZ[q0:q0 + 64, hc(h)], FM[p0:p0 + 64, 0, c, q0:q0 + 64], Tb[p0:p0 + 64, c, :], start=False, stop=False)
                for h in range(H):
                    S_.mm(psZ[q0:q0 + 64, hc(h)], st["LkT"][q0:q0 + 64, h, :], Vb[q0:q0 + 64, hc(h)], start=False, stop=(h == H - 1), inc=(h == H - 1))
                S_.cp("act", Zs[q0:q0 + 64, :], psZ[q0:q0 + 64, 0:C])
                psU = psum()
                for h in range(H):
                    S_.mm(psU[q0:q0 + 64, hc(h)], XT[q0:q0 + 64, h, :], Zs[q0:q0 + 64, hc(h)], inc=(h == H - 1))
                S_.cp("dve", Us[q0:q0 + 64, :], psU[q0:q0 + 64, 0:C])
                psY = psum()
                S_.mm(psY[q0:q0 + 64, 0:C], zeros_b[:, 0:64], zeros_b[:, 0:C], start=True, stop=False)
                for h in HORD:
                    c, p0 = h // 2, 64 * (h % 2)
                    S_.mm(psY[q0:q0 + 64, hc(h)], FM[p0:p0 + 64, 3, c, q0:q0 + 64], Tb[p0:p0 + 64, c, :], start=False, stop=False)
                for h in range(H):
                    S_.mm(psY[q0:q0 + 64, hc(h)], st["RbT"][q0:q0 + 64, h, :], Us[q0:q0 + 64, hc(h)], start=False, stop=False)
                for h in range(H):
                    S_.mm(psY[q0:q0 + 64, hc(h)], st["RkT"][q0:q0 + 64, h, :], Vb[q0:q0 + 64, hc(h)], start=False, stop=(h == H - 1), inc=(h == H - 1))
                psT = psum()
                for h in range(H):
                    c, p0 = h // 2, 64 * (h % 2)
                    S_.mm(psT[p0:p0 + 64, c * 64:(c + 1) * 64], st["Bt"][q0:q0 + 64, hc(h)], Us[q0:q0 + 64, hc(h)], start=True, stop=False)
                    S_.mm(psT[p0:p0 + 64, c * 64:(c + 1) * 64], st["Kt"][q0:q0 + 64, hc(h)], Vb[q0:q0 + 64, hc(h)], start=False, stop=True, inc=(h == H - 1))
                if d == 0:
                    S_.cp("act", Yacc[q0:q0 + 64, t, :], psY[q0:q0 + 64, 0:C])
                else:
                    S_.tt("pool" if False else "dve", Yacc[q0:q0 + 64, t, :], psY[q0:q0 + 64, 0:C], Yacc[q0:q0 + 64, t, :], ALU.add)
                S_.tt("dve", Ttmp, psT[:, 0:192].re("p (c i) -> p c i", c=3), T32, ALU.add)
                S_.tt("dve", T32, Ttmp, st["WCe"][:, :, q:q + 1].bc([128, 3, 64]), ALU.mult)
                S_.cp("act", Tb, T32)

            def finalize(t, st):
                Y = Yacc[:, t, :]
                S_.red(fs1, v6(Y), ALU.add)
                S_.tt("pool", sq, Y, Y, ALU.mult)
                S_.red(fs2, v6(sq), ALU.add)
                S_.ts("dve", fs1, fs1, 1.0 / 64, ALU.mult)
                S_.tt("dve", fs3, fs1, fs1, ALU.mult)
                S_.stt(fs2, fs2, 1.0 / 64, fs3, ALU.mult, ALU.subtract)
                S_.ts("dve", fs2, fs2, GN_EPS, ALU.add)
                S_.act(fs2, fs2, AF.Sqrt)
                S_.op("dve", lambda g: g.reciprocal(_ap(fs2), _ap(fs2)), [fs2], [fs2])
                S_.tt("dve", v6(sq), v6(Y), fs1.un(2).bc([128, 6, 64]), ALU.subtract)
                S_.tt("dve", v6(sq), v6(sq), fs2.un(2).bc([128, 6, 64]), ALU.mult)
                S_.tt("pool", sq, sq, gngb, ALU.mult)
                S_.tt("pool", sq, sq, gnbb, ALU.add)
                S_.tt("pool", sq, sq, st["bv"], ALU.add)
                S_.tt("pool", yob, sq, st["g32"], ALU.mult)
                ps = psum().bitcast(BF16)
                for cc in range(3):
                    S_.tr(ps[:, cc * 128:(cc + 1) * 128], yob[:, cc * 128:(cc + 1) * 128], identb, inc=(cc == 2))
                S_.cp("act", yT_r[:, 0:3, t * 128:(t + 1) * 128], ps[:, 0:384].re("p (k t) -> p k t", k=3))

            for d in (0, 1):
                S_.memset("dve", T32, 0.0)
                S_.memset("pool", Tb, 0.0)
                order = list(range(NT)) if d == 0 else list(range(NT - 1, -1, -1))
                prep(order[0], d, sets[0])
                for i, t in enumerate(order):
                    st = sets[i % 2]
                    if i + 1 < NT:
                        prep(order[i + 1], d, sets[(i + 1) % 2])
                    if "seq" in SKIP:
                        continue
                    for q in ((0, 1) if d == 0 else (1, 0)):
                        seq_chunk(t, q, d, st)
                    if d == 1:
                        finalize(t, st)

    def stage_out(b, yTs):
        with ExitStack() as es5:
            A5 = Alloc(nc, es5)
            Wo = A5.sb([128, 8, D], BF16, "Wo")
            S_.dma("pool", Wo, w_out.re("(k p) c -> p k c", p=128))
            gam = A5.sb([128, D], F32, "gam1"); bet = A5.sb([128, D], F32, "bet1")
            bcast_load(gam, ln1_g); bcast_load(bet, ln1_b)
            Wr32 = A5.sb([128, 8, NE], F32, "Wr32")
            S_.dma("sp", Wr32, w_router.re("(k p) e -> p k e", p=128))
            brb = A5.sb([128, NE], F32, "brb"); bcast_load(brb, b_router)
            h0t = [A5.sb([128, D], F32, "h0t") for _ in range(2)]
            zt = [A5.sb([128, D], F32, "zt") for _ in range(2)]
            h1t = [A5.sb([128, D], F32, "h1t") for _ in range(2)]
            h1T32 = [A5.sb([128, 8, 128], F32, "h1T32") for _ in range(2)]
            h1Tb = [A5.sb([128, 8, 128], BF16, "h1Tb") for _ in range(2)]
            hhi = [A5.sb([128, D], BF16, "hhi") for _ in range(2)]
            hlo = [A5.sb([128, D], BF16, "hlo") for _ in range(2)]
            tp = [dict(st=A5.sb([128, 2, 6], F32, "st"), mv=A5.sb([128, 2], F32, "mv"),
                       rs=A5.sb([128, 1], F32, "rs")) for _ in range(2)]
            lg = A5.sb([128, NE], F32, "lg"); mx8 = A5.sb([128, 8], F32, "mx8"); nmx = A5.sb([128, 1], F32, "nmx")
            ex = A5.sb([128, NE], F32, "ex"); mk = A5.sb([128, NE], F32, "mk"); sm = A5.sb([128, 1], F32, "sm")
            rt = dict(mkb=A5.sb([128, NE], BF16, "mkb"), pos=A5.sb([128, NE], F32, "pos"), vv=A5.sb([128, NE], F32, "vv"),
                      nd=A5.sb([128, NE], F32, "nd"), mx=A5.sb([128, 8], F32, "mxr"), sel=A5.sb([128, NE], F32, "sel"),
                      idf=A5.sb([128, 4], F32, "idf"))
            for t in range(NT):
                i = t % 2
                gt = b * NT + t
                grp, tin = gt // 8, gt % 8
                S_.dma("sp", h0t[i], h0_d[b][t * 128:(t + 1) * 128, :])
                for half in range(2):
                    ps = psum()
                    for k in range(8):
                        S_.mm(ps, yTs[k][:, t * 128:(t + 1) * 128], Wo[:, k, half * 512:(half + 1) * 512],
                              start=(k == 0), stop=(k == 7), inc=(k == 7))
                    S_.stt(zt[i][:, half * 512:(half + 1) * 512], h0t[i][:, half * 512:(half + 1) * 512], ALPHA, ps, ALU.mult, ALU.add)
                layernorm(es5, zt[i], gam, bet, h1t[i], tp[i])
                S_.dma("sp", h1_d[grp][tin * 128:(tin + 1) * 128, :], h1t[i])
                S_.cp("act", hhi[i], h1t[i])
                S_.tt("dve", hlo[i], h1t[i], hhi[i], ALU.subtract)
                for half in range(2):
                    ps = psum()
                    for k in range(4):
                        kk_ = half * 4 + k
                        S_.mm(ps[:, k * 128:(k + 1) * 128], hhi[i][:, kk_ * 128:(kk_ + 1) * 128], identb, start=True, stop=False)
                        S_.mm(ps[:, k * 128:(k + 1) * 128], hlo[i][:, kk_ * 128:(kk_ + 1) * 128], identb, start=False, stop=True, inc=(k == 3))
                    S_.cp("act", h1T32[i][:, half * 4:half * 4 + 4, :], ps.re("p (k t) -> p k t", k=4))
                    S_.cp("dve", h1Tb[i][:, half * 4:half * 4 + 4, :], h1T32[i][:, half * 4:half * 4 + 4, :])
                if "h1Tdma" not in SKIP:
                    S_.dma("sp", h1T_d[grp][:, :, tin * 128:(tin + 1) * 128], h1Tb[i])
                if "router" in SKIP:
                    continue
                ps = psum()
                for k in range(8):
                    S_.mm(ps[:, 0:NE], h1T32[i][:, k, :], Wr32[:, k, :], start=(k == 0), stop=(k == 7), inc=(k == 7))
                S_.tt("dve", lg, ps[:, 0:NE], brb, ALU.add)
                if "top" in SKIP:
                    continue
                S_.op("dve", lambda g: g.max(_ap(mx8), _ap(lg)), [lg], [mx8])
                S_.ts("dve", mk, lg, mx8[:, 3:4], ALU.is_ge)
                S_.ts("dve", nmx, mx8[:, 0:1], -1.0, ALU.mult)
                S_.act(ex, lg, AF.Exp, bias=nmx[:, 0:1])
                S_.tt("dve", ex, ex, mk, ALU.mult)
                S_.red(sm, ex, ALU.add)
                S_.op("dve", lambda g: g.reciprocal(_ap(sm), _ap(sm)), [sm], [sm])
                S_.ts("dve", Gall[:, gt, :], ex, sm[:, 0:1], ALU.mult)
                if SPARSE:
                    route_tile(gt, mk, hhi[i], rt)

    def stage_moe():
        with ExitStack() as es6:
            A6 = Alloc(nc, es6)
            gam = A6.sb([128, D], F32, "gam2"); bet = A6.sb([128, D], F32, "bet2")
            bcast_load(gam, ln2_g); bcast_load(bet, ln2_b)
            bguT = A6.sb([128, 16, NE], F32, "bguT")
            with ExitStack() as esb:
                Ab = Alloc(nc, esb)
                bgu_sb = Ab.sb([NE, 2 * D], F32, "bgu_sb")
                bgu_hi = Ab.sb([NE, 2 * D], BF16, "bgu_hi"); bgu_lo = Ab.sb([NE, 2 * D], BF16, "bgu_lo")
                S_.dma("sp", bgu_sb, b_gate_up)
                S_.cp("act", bgu_hi, bgu_sb)
                S_.tt("dve", bgu_lo, bgu_sb, bgu_hi, ALU.subtract)
                for fc in range(16):
                    ps = psum()
                    S_.mm(ps[:, 0:NE], bgu_hi[0:NE, fc * 128:(fc + 1) * 128], identb[0:NE, 0:NE], start=True, stop=False)
                    S_.mm(ps[:, 0:NE], bgu_lo[0:NE, fc * 128:(fc + 1) * 128], identb[0:NE, 0:NE], start=False, stop=True, inc=True)
                    S_.cp("act", bguT[:, fc, :], ps[:, 0:NE])
                S_.barrier()
            bd_sb = A6.sb([NE, D], F32, "bd_sb")
            S_.dma("sp", bd_sb, b_down)
            h1T = A6.sb([128, 8, 1024], BF16, "h1T")
            acc = A6.sb([128, 8, D], F32, "acc")
            wgu = [A6.sb([128, 8, 2, 256], BF16, "wgu") for _ in range(6)]
            wd = [A6.sb([128, 8, D], BF16, "wd") for _ in range(2)]
            actT = A6.sb([128, 8, 1024], BF16, "actT")
            g1 = [A6.sb([128, 512], F32, "g1") for _ in range(2)]
            s1 = [A6.sb([128, 512], F32, "s1") for _ in range(2)]
            u1 = [A6.sb([128, 512], F32, "u1") for _ in range(2)]
            GT = A6.sb([NE, 128], F32, "GT")
            Ghi = A6.sb([128, NE], BF16, "Ghi"); Glo = A6.sb([128, NE], BF16, "Glo")
            h1t = A6.sb([128, D], F32, "h1t2"); zt = A6.sb([128, D], F32, "zt2"); ot = A6.sb([128, D], F32, "ot2")
            tp = dict(st=A6.sb([128, 2, 6], F32, "st"), mv=A6.sb([128, 2], F32, "mv"), rs=A6.sb([128, 1], F32, "rs"))
            tmpd = [A6.sb([128, 512], F32, "tmpd") for _ in range(2)]
            npiece = 0
            it = 0
            nd = 0
            for g in range(4):
                S_.dma("sp", h1T, h1T_d[g])
                for tl in range(8):
                    gt = g * 8 + tl
                    ps = psum()
                    S_.cp("act", Ghi, Gall[:, gt, :])
                    S_.tt("dve", Glo, Gall[:, gt, :], Ghi, ALU.subtract)
                    S_.mm(ps[0:NE, 0:128], Ghi, identb, start=True, stop=False)
                    S_.mm(ps[0:NE, 0:128], Glo, identb, start=False, stop=True, inc=True)
                    S_.cp("act", GT, ps[0:NE, 0:128])
                    for half in range(2):
                        ps = psum()
                        S_.mm(ps, GT[0:NE, :], bd_sb[0:NE, half * 512:(half + 1) * 512], inc=True)
                        S_.cp("act" if half else "dve", acc[:, tl, half * 512:(half + 1) * 512], ps)
                for e in range(NE):
                    wde = wd[e % 2]
                    if "moe_w" not in SKIP:
                        S_.dma("pool", wde, w_down[e].re("(k p) c -> p k c", p=128))
                    src = w_gate_up[e].re("(k p) (u f) -> p k u f", p=128, u=2)
                    for p in range(4):
                        wp = wgu[npiece % 6]; npiece += 1
                        for u_ in range(2):
                            if "moe_w" not in SKIP:
                                S_.dma("pool", wp[:, :, u_, :], src[:, :, u_, p * 256:(p + 1) * 256])
                        if "moe_gu" in SKIP:
                            continue
                        for sg in range(2):
                            for f2 in range(2):
                                fc = 2 * p + f2
                                i = it % 2; it += 1
                                psg = psum(); psu = psum()
                                for k in range(8):
                                    S_.mm(psg, wp[:, k, 0, f2 * 128:(f2 + 1) * 128], h1T[:, k, sg * 512:(sg + 1) * 512],
                                          start=(k == 0), stop=(k == 7), inc=(k == 7))
                                for k in range(8):
                                    S_.mm(psu, wp[:, k, 1, f2 * 128:(f2 + 1) * 128], h1T[:, k, sg * 512:(sg + 1) * 512],
                                          start=(k == 0), stop=(k == 7), inc=(k == 7))
                                S_.ts("dve", g1[i], psg, bguT[:, fc, e:e + 1], ALU.add, 7.0, ALU.min)
                                S_.act(s1[i], g1[i], AF.Sigmoid, scale=1.702)
                                S_.act(u1[i], psu, AF.Identity, bias=bguT[:, 8 + fc, e:e + 1])
                                S_.ts("pool", u1[i], u1[i], 7.0, ALU.min, -7.0, ALU.max)
                                S_.tt("pool", g1[i], g1[i], s1[i], ALU.mult)
                                S_.stt(actT[:, fc, sg * 512:(sg + 1) * 512], u1[i], 1.0, g1[i], ALU.add, ALU.mult)
                    for tl in range(8):
                        gt = g * 8 + tl
                        if "moe_down" in SKIP:
                            continue
                        for half in range(2):
                            ps = psum()
                            for fc in range(8):
                                S_.mm(ps, actT[:, fc, tl * 128:(tl + 1) * 128], wde[:, fc, half * 512:(half + 1) * 512],
                                      start=(fc == 0), stop=(fc == 7), inc=(fc == 7))
                            a_ = acc[:, tl, half * 512:(half + 1) * 512]
                            td = tmpd[nd % 2]; nd += 1
                            S_.ts("dve", td, ps, Gall[:, gt, e:e + 1], ALU.mult)
                            S_.tt("pool", a_, a_, td, ALU.add)
                for tl in range(8):
                    gt = g * 8 + tl
                    S_.dma("sp", h1t, h1_d[g][tl * 128:(tl + 1) * 128, :])
                    S_.stt(zt, h1t, ALPHA, acc[:, tl, :], ALU.mult, ALU.add)
                    layernorm(es6, zt, gam, bet, ot, tp)
                    S_.dma("sp", out_d[gt * 128:(gt + 1) * 128, :], ot)

    def route_tile(gt, mk, hb_tile, rt):
        mkb = rt["mkb"]; pos = rt["pos"]; vv = rt["vv"]; nd = rt["nd"]; mx = rt["mx"]; sel = rt["sel"]; idf = rt["idf"]
        S_.cp("dve", mkb, mk)
        ps = psum()
        S_.mm(ps[:, 0:NE], utri_b, mkb, inc=True)
        ps2 = psum()
        S_.mm(ps2[:, 0:NE], ones_b, mkb, inc=True)
        S_.tt("dve", pos, ps[:, 0:NE], carry, ALU.add)
        S_.tt("dve", carry, ps2[:, 0:NE], carry, ALU.add)
        S_.ts("dve", vv, pos, float(CAP), ALU.is_lt)
        S_.tt("dve", vv, vv, mk, ALU.mult)
        S_.tt("dve", pos, pos, erow, ALU.add)
        S_.ts("dve", nd, pos, -1.0, ALU.mult, BIGF, ALU.add)
        S_.tt("dve", nd, nd, vv, ALU.mult)
        S_.ts("dve", nd, nd, -BIGF, ALU.add)
        S_.op("dve", lambda g: g.max(_ap(mx), _ap(nd)), [nd], [mx])
        S_.ts("dve", idf, mx[:, 0:4], -1.0, ALU.mult, float(NE * CAP), ALU.min)
        S_.cp("dve", idx_all[:, gt, :], idf)
        for k in range(4):
            S_.ts("dve", sel, nd, mx[:, k:k + 1], ALU.is_equal)
            S_.tt("dve", sel, sel, Gall[:, gt, :], ALU.mult)
            S_.red(gk_all[:, gt, k:k + 1], sel, ALU.add)
        S_.ts("dve", idf, mx[:, 0:4], -0.5 * BIGF, ALU.is_gt)
        S_.tt("dve", gk_all[:, gt, :], gk_all[:, gt, :], idf, ALU.mult)
        for k in range(4):
            S_.idma_scatter(x_rows, idx_all[:, gt, k:k + 1], hb_tile, NE * CAP - 1)

    def stage_moe_sparse():
        S_.barrier()
        with ExitStack() as es6:
            A6 = Alloc(nc, es6)
            bguT = A6.sb([128, 16, NE], F32, "bguT")
            with ExitStack() as esb:
                Ab = Alloc(nc, esb)
                bgu_sb = Ab.sb([NE, 2 * D], F32, "bgu_sb")
                bgu_hi = Ab.sb([NE, 2 * D], BF16, "bgu_hi"); bgu_lo = Ab.sb([NE, 2 * D], BF16, "bgu_lo")
                S_.dma("sp", bgu_sb, b_gate_up)
                S_.cp("act", bgu_hi, bgu_sb)
                S_.tt("dve", bgu_lo, bgu_sb, bgu_hi, ALU.subtract)
                for fc in range(16):
                    ps = psum()
                    S_.mm(ps[:, 0:NE], bgu_hi[0:NE, fc * 128:(fc + 1) * 128], identb[0:NE, 0:NE], start=True, stop=False)
                    S_.mm(ps[:, 0:NE], bgu_lo[0:NE, fc * 128:(fc + 1) * 128], identb[0:NE, 0:NE], start=False, stop=True, inc=True)
                    S_.cp("act", bguT[:, fc, :], ps[:, 0:NE])
                S_.barrier()
            with ExitStack() as ese:
                Ae = Alloc(nc, ese)
                NS = CAP // 128
                xe = [Ae.sb([128, NS, D], BF16, "xe") for _ in range(2)]
                xT = [Ae.sb([128, 8, CAP], BF16, "xT") for _ in range(2)]
                wgu = [Ae.sb([128, 8, 2 * D], BF16, "wgu") for _ in range(2)]
                wd = [Ae.sb([128, 8, D], BF16, "wd") for _ in range(2)]
                actT = Ae.sb([128, 8, CAP], BF16, "actT")
                g1 = [Ae.sb([128, 512], F32, "g1") for _ in range(2)]
                s1 = [Ae.sb([128, 512], F32, "s1") for _ in range(2)]
                u1 = [Ae.sb([128, 512], F32, "u1") for _ in range(2)]
                yo = [Ae.sb([128, D], F32, "yo") for _ in range(2)]
                blocks = [(0, 512)] + ([(512, CAP - 512)] if CAP > 512 else [])
                issued = [0]

                def issue_weights(upto):
                    while issued[0] <= min(upto, NE - 1):
                        e = issued[0]; issued[0] += 1
                        S_.dma("pool", wgu[e % 2], w_gate_up[e].re("(k p) f -> p k f", p=128))
                        S_.dma("pool", wd[e % 2], w_down[e].re("(k p) c -> p k c", p=128))

                def load_x(e):
                    S_.dma("sp", xe[e % 2], x_rows[e * CAP:(e + 1) * CAP, :].re("(n p) d -> p n d", p=128))
                    for n in range(NS):
                        ps = psum().bitcast(BF16)
                        for k in range(8):
                            S_.tr(ps[:, k * 128:(k + 1) * 128], xe[e % 2][:, n, k * 128:(k + 1) * 128], identb, inc=(k == 7))
                        S_.cp("act" if n % 2 else "dve", xT[e % 2][:, :, n * 128:(n + 1) * 128], ps.re("p (k t) -> p k t", k=8))
                it = 0
                load_x(0)
                for e in range(NE):
                    if e + 1 < NE:
                        load_x(e + 1)
                    xTe = xT[e % 2]
                    issue_weights(e + 1)
                    wge = wgu[e % 2]
                    for p in range(4):
                        for (c0, cn) in blocks:
                            for f2 in range(2):
                                fc = 2 * p + f2
                                i = it % 2; it += 1
                                psg = psum(); psu = psum()
                                for k in range(8):
                                    S_.mm(psg[:, 0:cn], wge[:, k, fc * 128:(fc + 1) * 128], xTe[:, k, c0:c0 + cn],
                                          start=(k == 0), stop=(k == 7), inc=(k == 7))
                                for k in range(8):
                                    S_.mm(psu[:, 0:cn], wge[:, k, D + fc * 128:D + (fc + 1) * 128], xTe[:, k, c0:c0 + cn],
                                          start=(k == 0), stop=(k == 7), inc=(k == 7))
                                gg = g1[i][:, 0:cn]; sg_ = s1[i][:, 0:cn]; uu = u1[i][:, 0:cn]
                                S_.ts("dve", gg, psg[:, 0:cn], bguT[:, fc, e:e + 1], ALU.add, 7.0, ALU.min)
                                S_.act(sg_, gg, AF.Sigmoid, scale=1.702)
                                S_.act(uu, psu[:, 0:cn], AF.Identity, bias=bguT[:, 8 + fc, e:e + 1])
                                S_.ts("pool", uu, uu, 7.0, ALU.min, -7.0, ALU.max)
                                S_.tt("pool", gg, gg, sg_, ALU.mult)
                                S_.stt(actT[:, fc, c0:c0 + cn], uu, 1.0, gg, ALU.add, ALU.mult)
                    wde = wd[e % 2]
                    for n in range(NS):
                        yt_ = yo[n % 2]
                        for half in range(2):
                            ps = psum()
                            for fc in range(8):
                                S_.mm(ps, actT[:, fc, n * 128:(n + 1) * 128], wde[:, fc, half * 512:(half + 1) * 512],
                                      start=(fc == 0), stop=(fc == 7), inc=(fc == 7))
                            S_.cp("act" if half else "dve", yt_[:, half * 512:(half + 1) * 512], ps)
                        r0 = e * CAP + n * 128
                        S_.dma("sp", V(y_rows.ap[r0:r0 + 128, :], [Buf("yrow")]), yt_)
            S_.barrier()
            with ExitStack() as esc:
                Ac = Alloc(nc, esc)
                gam = Ac.sb([128, D], F32, "gam2"); bet = Ac.sb([128, D], F32, "bet2")
                bcast_load(gam, ln2_g); bcast_load(bet, ln2_b)
                bd_sb = Ac.sb([NE, D], F32, "bd_sb")
                S_.dma("sp", bd_sb, b_down)
                GT = Ac.sb([NE, 128], F32, "GT")
                Ghi = Ac.sb([128, NE], BF16, "Ghi"); Glo = Ac.sb([128, NE], BF16, "Glo")
                yk = [[Ac.sb([128, D], F32, "yk") for _ in range(4)] for _ in range(2)]
                h1t = [Ac.sb([128, D], F32, "h1t2") for _ in range(2)]
                zt = Ac.sb([128, D], F32, "zt2"); ot = Ac.sb([128, D], F32, "ot2")
                tmpc = [Ac.sb([128, D], F32, "tmpc") for _ in range(2)]
                tp = dict(st=Ac.sb([128, 2, 6], F32, "st"), mv=Ac.sb([128, 2], F32, "mv"), rs=Ac.sb([128, 1], F32, "rs"))

                def fetch(gt):
                    for k in range(4):
                        S_.idma_gather(yk[gt % 2][k], y_rows, idx_all[:, gt, k:k + 1], NE * CAP - 1)
                    S_.dma("sp", h1t[gt % 2], h1_d[gt // 8][(gt % 8) * 128:(gt % 8 + 1) * 128, :])
                fetch(0)
                for gt in range(NB * NT):
                    if gt + 1 < NB * NT:
                        fetch(gt + 1)
                    S_.cp("act", Ghi, Gall[:, gt, :])
                    S_.tt("dve", Glo, Gall[:, gt, :], Ghi, ALU.subtract)
                    ps = psum()
                    S_.mm(ps[0:NE, 0:128], Ghi, identb, start=True, stop=False)
                    S_.mm(ps[0:NE, 0:128], Glo, identb, start=False, stop=True, inc=True)
                    S_.cp("act", GT, ps[0:NE, 0:128])
                    for half in range(2):
                        ps = psum()
                        S_.mm(ps, GT[0:NE, :], bd_sb[0:NE, half * 512:(half + 1) * 512], inc=True)
                        S_.stt(zt[:, half * 512:(half + 1) * 512], h1t[gt % 2][:, half * 512:(half + 1) * 512], ALPHA, ps, ALU.mult, ALU.add)
                    for k in range(4):
                        S_.act(tmpc[k % 2], yk[gt % 2][k], AF.Copy, scale=gk_all[:, gt, k:k + 1])
                        S_.tt("dve", zt, zt, tmpc[k % 2], ALU.add)
                    layernorm(esc, zt, gam, bet, ot, tp, eng2="dve")
                    S_.dma("sp", out_d[gt * 128:(gt + 1) * 128, :], ot)

    msk = G.sb([128, 5 * 64 + 2], F32, "msk")
    S_.dma("sp", msk, c_masks)
    tri32 = G.sb([128, 2, 128], F32, "tri32")
    S_.dma("sp", tri32, c_tri.re("d p t -> p d t"))
    MS = [msk[:, 0:64], msk[:, 128:192]]
    MI = [msk[:, 64:128], msk[:, 192:256]]
    MID = msk[:, 256:320]
    BLK = msk[:, 320:322]

    def dbg_out(name, src, shape, dt):
        if name in debug:
            dbg[name] = V(nc.dram_tensor("dbg_" + name, list(shape), dt, kind="ExternalOutput").ap(), [Buf("dbg_" + name)])
            S_.dma("sp", dbg[name], src)

    for b in range(NB):
        with ExitStack() as esA:
            A = Alloc(nc, esA)
            hT = A.sb([128, 8, S], BF16, "hT")
            stage_s1(b, hT)
            S_.barrier()
            if b == 0:
                dbg_out("hT", hT, [128, 8, S], BF16)
            yT_r = A.sb([128, 3, S], BF16, "yT_r")
            if "rwkv" in stages:
                stage_rwkv(b, hT, yT_r)
                S_.barrier()
            yT_a = A.sb([128, 3, S], BF16, "yT_a")
            if "att" in stages:
                stage_att(b, hT, yT_a)
                S_.barrier()
            yT_m = A.sb([128, 2, S], BF16, "yT_m")
            if "mem" in stages:
                stage_mem(b, hT, yT_m)
                S_.barrier()
            if b == 0:
                dbg_out("yT_r", yT_r, [128, 3, S], BF16)
                dbg_out("yT_a", yT_a, [128, 3, S], BF16)
                dbg_out("yT_m", yT_m, [128, 2, S], BF16)
            if "out" in stages:
                stage_out(b, [yT_r[:, 0], yT_r[:, 1], yT_r[:, 2], yT_a[:, 0], yT_a[:, 1], yT_a[:, 2], yT_m[:, 0], yT_m[:, 1]])
                S_.barrier()
    if "h1" in debug:
        dbg["h1"] = V(nc.dram_tensor("dbg_h1", [1024, D], F32, kind="ExternalOutput").ap(), [Buf("dbg_h1")])
        S_.dma("sp", dbg["h1"], h1_d[0])
        dbg["G"] = V(nc.dram_tensor("dbg_G", [128, NB * NT, NE], F32, kind="ExternalOutput").ap(), [Buf("dbg_G")])
        S_.dma("sp", dbg["G"], Gall)

    if moe:
        if SPARSE:
            stage_moe_sparse()
        else:
            stage_moe()
    S_.barrier()
    es.close()
    return nc, S_


def host_constants():
    ident = np.eye(128, dtype=np.float32)
    slopes = np.exp2(-8.0 * np.arange(1, H + 1, dtype=np.float32) / H).astype(np.float32)
    x = np.arange(STRIP_W)[None, :]
    p = np.arange(128)[:, None]
    delta = x - p - STRIP_C
    ad = np.abs(delta)
    mult = ((ad <= 64).astype(np.float32) + ((delta % 4 == 0) & (ad <= 256)).astype(np.float32)
            + ((delta % 16 == 0) & (ad <= 1024)).astype(np.float32))
    strip = np.stack([mult * np.exp(-(slopes[h] * ad.astype(np.float32))) for h in range(H)]).astype(np.float32)
    masks = np.zeros((128, 5 * 64 + 2), np.float32)
    si = np.arange(64)[:, None]; ti = np.arange(64)[None, :]
    for q in range(2):
        r = slice(64 * q, 64 * q + 64)
        masks[r, 0:64] = (si < ti); masks[r, 64:128] = (si <= ti)
        masks[r, 128:192] = (si > ti); masks[r, 192:256] = (si >= ti)
        masks[r, 256:320] = (si == ti)
        masks[r, 320 + q] = 1.0
    tri = np.zeros((2, 128, 128), np.float32)
    for q in range(2):
        r = slice(64 * q, 64 * q + 64)
        tri[0, r, r] = (si <= ti); tri[1, r, r] = (si >= ti)
    utri = (np.arange(128)[:, None] < np.arange(128)[None, :]).astype(np.float32)
    erow = np.tile((np.arange(NE, dtype=np.float32) * CAP)[None, :], (128, 1))
    return dict(c_ident=ident, c_strip=strip, c_masks=masks, c_tri=tri, c_utri=utri, c_erow=erow)


def make_in_maps(inputs):
    consts = host_constants()
    maps = []
    sq = lambda a: np.ascontiguousarray(a)
    for i in range(8):
        m = dict(consts)
        m["x"] = sq(inputs["x"][2 * i:2 * i + 2].reshape(NB * S, D))
        m["mem"] = sq(inputs["mem"][2 * i:2 * i + 2].reshape(NB * 256, D))
        for k in ("ln_in_g", "ln_in_b"):
            m[k] = sq(inputs[k])
        m["w_in"] = sq(inputs["w_in"][0])
        m["mu_shift"] = sq(inputs["mu_shift"][0])
        m["w0"] = sq(inputs["w0"][0].reshape(-1)); m["w_up"] = sq(inputs["w_up"][0])
        m["a0"] = sq(inputs["a0"][0].reshape(-1)); m["a_up"] = sq(inputs["a_up"][0])
        m["g_up"] = sq(inputs["g_up"][0])
        for k in ("k_k", "k_a", "gn_g", "gn_b", "ln1_g", "ln1_b", "ln2_g", "ln2_b", "b_router",
                  "w_mem_kv", "w_out", "w_router", "w_gate_up", "b_gate_up", "w_down", "b_down"):
            m[k] = sq(inputs[k][0])
        m["r_k"] = sq(inputs["r_k"][0].reshape(-1))
        maps.append(m)
    return maps


def kernel(**inputs):
    inputs = {k: np.asarray(v) for k, v in inputs.items()}
    nc, _ = build_program()
    maps = make_in_maps(inputs)
    res = run_bass_kernel_spmd(nc, maps, core_ids=list(range(8)))
    out = np.stack([r["out"].reshape(NB, S, D) for r in res.results]).reshape(16, S, D)
    return out.astype(np.float32)
```

```python
import numpy as np
from contextlib import ExitStack
import concourse.bass as bass
import concourse.mybir as mybir
from concourse.bass_utils import run_bass_kernel_spmd

F32 = mybir.dt.float32
BF16 = mybir.dt.bfloat16
U32 = mybir.dt.uint32
AF = mybir.ActivationFunctionType
ALU = mybir.AluOpType
AX = mybir.AxisListType

D = 1024
S = 2048
NB = 2
NT = S // 128
C = 384
H = 6
RWKV_IN = 1344
IN_W = 2752
NE = 32
ALPHA = 2.0 ** 0.25
LN_EPS = 1e-5
GN_EPS = 64e-5
LDS = float(np.exp(-0.5))
STRIP_W = 3200
STRIP_C = 1536
CAP = 640
BIGF = 1.0e6
import os
SKIP = os.environ.get('KSKIP', '')
SPARSE = os.environ.get('KDENSE', '') == ''


class Buf:
    __slots__ = ("name", "w", "rs")

    def __init__(self, name):
        self.name = name
        self.w = None
        self.rs = []


class Ev:
    __slots__ = ("sem", "val", "eng")

    def __init__(self, eng):
        self.sem = None
        self.val = None
        self.eng = eng


class V:
    __slots__ = ("ap", "bufs")

    def __init__(self, ap, bufs):
        self.ap = ap
        self.bufs = bufs if isinstance(bufs, (list, tuple)) else [bufs]

    def __getitem__(self, k):
        return V(self.ap[k], self.bufs)

    def re(self, pat, **kw):
        return V(self.ap.rearrange(pat, **kw), self.bufs)

    def bc(self, shape):
        return V(self.ap.to_broadcast(list(shape)), self.bufs)

    def un(self, axis):
        return V(self.ap.unsqueeze(axis), self.bufs)

    def bitcast(self, dt):
        return V(self.ap.bitcast(dt), self.bufs)

    def on(self, bufs):
        return V(self.ap, bufs)


def _ap(x):
    return x.ap if isinstance(x, V) else x


class Sched:
    ENG = ("pe", "act", "dve", "pool", "sp")
    NQ = 16

    def __init__(self, nc, es):
        self.nc = nc
        self.es = es
        self.eng = dict(pe=nc.tensor, act=nc.scalar, dve=nc.vector, pool=nc.gpsimd, sp=nc.sync)
        self.sem = {k: es.enter_context(nc.semaphore("s_" + k)) for k in self.ENG}
        self.cnt = {k: 0 for k in self.ENG}
        self.dsem = {q: [es.enter_context(nc.semaphore("d_%s%d" % (q, i))) for i in range(self.NQ)]
                     for q in ("sp", "pool")}
        self.dcnt = {q: 0 for q in self.dsem}
        self.seen = {k: {} for k in self.ENG}
        self.pending = {k: [] for k in self.ENG}
        self.all_dma = []
        self.ninst = 0

    def _wait(self, e, ev):
        assert ev.val is not None, "dependency on an unresolved (non-inc) event"
        key = id(ev.sem)
        if self.seen[e].get(key, 0) >= ev.val:
            return
        self.eng[e].wait_ge(ev.sem, ev.val)
        self.seen[e][key] = ev.val

    def _deps(self, e, reads, writes, is_dma):
        for v in reads:
            for b in v.bufs:
                if b.w is not None:
                    if b.w.eng == e and e == "pe" and not is_dma:
                        continue
                    self._wait(e, b.w)
        for v in writes:
            for b in v.bufs:
                if b.w is not None and (is_dma or b.w.eng != e or e != "pe"):
                    self._wait(e, b.w)
                for r in b.rs:
                    if is_dma or r.eng != e or e != "pe":
                        self._wait(e, r)

    def _record(self, ev, reads, writes):
        for v in reads:
            for b in v.bufs:
                if ev.eng is not None:
                    b.rs = [r for r in b.rs if r.eng != ev.eng]
                b.rs.append(ev)
        for v in writes:
            for b in v.bufs:
                b.w = ev
                b.rs = []

    def op(self, e, fn, reads, writes, inc=True):
        reads = [r for r in reads if isinstance(r, V)]
        writes = [w for w in writes if isinstance(w, V)]
        self._deps(e, reads, writes, False)
        ins = fn(self.eng[e])
        self.ninst += 1
        ev = Ev(e)
        if inc:
            self.cnt[e] += 1
            ins.then_inc(self.sem[e], 1)
            ev.sem = self.sem[e]
            ev.val = self.cnt[e]
            for p in self.pending[e]:
                p.sem = ev.sem
                p.val = ev.val
            self.pending[e] = []
        else:
            self.pending[e].append(ev)
        self._record(ev, reads, writes)
        return ev

    def dma(self, q, out, in_, **kw):
        self._deps(q, [in_], [out], True)
        n = self.dcnt[q]
        self.dcnt[q] += 1
        sem = self.dsem[q][n % self.NQ]
        val = 16 * (n // self.NQ + 1)
        if n >= self.NQ:
            pe_ = Ev(None); pe_.sem = sem; pe_.val = val - 16
            self._wait(q, pe_)
        ins = self.eng[q].dma_start(out=_ap(out), in_=_ap(in_), **kw)
        ins.then_inc(sem, 16)
        self.ninst += 1
        ev = Ev(None)
        ev.sem = sem
        ev.val = val
        self._record(ev, [in_], [out])
        self.all_dma.append(ev)
        return ev

    def _idma(self, out, out_off, in_, in_off, reads, writes, bound):
        q = "pool"
        self._deps(q, reads, writes, True)
        n = self.dcnt[q]
        self.dcnt[q] += 1
        sem = self.dsem[q][n % self.NQ]
        val = 16 * (n // self.NQ + 1)
        if n >= self.NQ:
            pe_ = Ev(None); pe_.sem = sem; pe_.val = val - 16
            self._wait(q, pe_)
        ins = self.eng[q].indirect_dma_start(out=_ap(out), out_offset=out_off, in_=_ap(in_), in_offset=in_off)
        ins.then_inc(sem, 16)
        self.ninst += 1
        ev = Ev(None)
        ev.sem = sem
        ev.val = val
        self._record(ev, reads, writes)
        return ev

    def idma_scatter(self, dram, idx, src, bound):
        off = bass.IndirectOffsetOnAxis(ap=_ap(idx), axis=0)
        return self._idma(dram, off, src, None, [src, idx], [V(dram.ap, [Buf("scatter")])], bound)

    def idma_gather(self, dst, dram, idx, bound):
        off = bass.IndirectOffsetOnAxis(ap=_ap(idx), axis=0)
        return self._idma(dst, None, dram, off, [idx], [dst], bound)

    def barrier(self):
        for e in self.ENG:
            for o in self.ENG:
                if self.cnt[o] > 0:
                    assert not self.pending[o]
                    ev = Ev(o)
                    ev.sem = self.sem[o]
                    ev.val = self.cnt[o]
                    self._wait(e, ev)
            for q in self.dsem:
                n = self.dcnt[q]
                for i in range(min(n, self.NQ)):
                    ev = Ev(None)
                    ev.sem = self.dsem[q][i]
                    ev.val = 16 * ((n - 1 - i) // self.NQ + 1)
                    self._wait(e, ev)

    def _pe_rows(self, lhsT):
        a = _ap(lhsT)
        lo = a.base_partition()
        rows = (lo, lo + a.shape[0])
        prev = getattr(self, "_last_pe", None)
        if prev is not None:
            pins, pev, prow = prev
            if rows[0] >= prow[1] or prow[0] >= rows[1]:
                if pev.val is None:
                    self.cnt["pe"] += 1
                    pins.then_inc(self.sem["pe"], 1)
                    for p in self.pending["pe"]:
                        p.sem = self.sem["pe"]
                        p.val = self.cnt["pe"]
                    self.pending["pe"] = []
                self._wait("pe", pev)
        return rows

    def _pe_op(self, fn, lhsT, reads, writes, inc):
        rows = self._pe_rows(lhsT)
        box = []

        def f(e):
            i = fn(e)
            box.append(i)
            return i
        ev = self.op("pe", f, reads, writes, inc=inc)
        self._last_pe = (box[0], ev, rows)
        return ev

    def mm(self, out, lhsT, rhs, start=True, stop=True, inc=False):
        return self._pe_op(lambda e: e.matmul(_ap(out), _ap(lhsT), _ap(rhs), start=start, stop=stop),
                           lhsT, [lhsT, rhs], [out], inc)

    def tr(self, out, in_, ident, inc=False):
        return self._pe_op(lambda e: e.transpose(_ap(out), _ap(in_), _ap(ident)), in_, [in_, ident], [out], inc)

    def act(self, out, in_, func, bias=None, scale=None, accum_out=None):
        kw = {}
        if bias is not None:
            kw["bias"] = _ap(bias)
        if scale is not None:
            kw["scale"] = _ap(scale)
        if accum_out is not None:
            kw["accum_out"] = _ap(accum_out)
        return self.op("act", lambda e: e.activation(_ap(out), _ap(in_), func, **kw),
                       [in_, bias, scale], [out, accum_out])

    def ts(self, e, out, in0, s1, op0, s2=None, op1=None, accum_out=None):
        kw = {}
        if op1 is not None:
            kw["op1"] = op1
        if accum_out is not None:
            kw["accum_out"] = _ap(accum_out)
        return self.op(e, lambda g: g.tensor_scalar(_ap(out), _ap(in0), _ap(s1), _ap(s2), op0, **kw),
                       [in0, s1, s2], [out, accum_out])

    def tt(self, e, out, in0, in1, op):
        return self.op(e, lambda g: g.tensor_tensor(_ap(out), _ap(in0), _ap(in1), op), [in0, in1], [out])

    def stt(self, out, in0, scalar, in1, op0, op1):
        return self.op("dve", lambda g: g.scalar_tensor_tensor(_ap(out), _ap(in0), _ap(scalar), _ap(in1), op0, op1),
                       [in0, scalar, in1], [out])

    def cp(self, e, out, in_):
        if e == "act":
            return self.op("act", lambda g: g.copy(_ap(out), _ap(in_)), [in_], [out])
        return self.op(e, lambda g: g.tensor_copy(_ap(out), _ap(in_)), [in_], [out])

    def red(self, out, in_, op, axis=AX.X):
        return self.op("dve", lambda g: g.tensor_reduce(_ap(out), _ap(in_), axis, op), [in_], [out])

    def memset(self, e, out, val):
        return self.op(e, lambda g: g.memset(_ap(out), val), [], [out])


class Alloc:
    N = [0]

    def __init__(self, nc, es):
        self.nc = nc
        self.es = es

    def sb(self, shape, dt, name=None, nbuf=1):
        Alloc.N[0] += 1
        name = "%s_%d" % (name or "t", Alloc.N[0])
        t = self.es.enter_context(self.nc.sbuf_tensor(name, list(shape), dt))
        return V(t[:], [Buf(name)])

    def dram(self, name, shape, dt, kind="Internal"):
        t = self.nc.dram_tensor(name, list(shape), dt, kind=kind)
        return V(t.ap(), [Buf(name)])


def build_program(debug=(), moe=True, stages=('rwkv', 'att', 'mem', 'out')):
    nc = bass.Bass("TRN2", target_bir_lowering=False)
    es = ExitStack()
    S_ = Sched(nc, es)
    G = Alloc(nc, es)
    dbg = {}

    def din(name, shape, dt=F32):
        return V(nc.dram_tensor(name, list(shape), dt, kind="ExternalInput").ap(), [Buf(name)])

    x_d = din("x", [NB * S, D])
    mem_d = din("mem", [NB * 256, D])
    ln_in_g = din("ln_in_g", [D]); ln_in_b = din("ln_in_b", [D])
    w_in = din("w_in", [D, IN_W])
    mu_d = din("mu_shift", [RWKV_IN])
    w0_d = din("w0", [2 * C]); w_up_d = din("w_up", [2, 32, C])
    a0_d = din("a0", [2 * C]); a_up_d = din("a_up", [2, 32, C])
    g_up_d = din("g_up", [64, C])
    k_k_d = din("k_k", [C]); k_a_d = din("k_a", [C]); r_k_d = din("r_k", [C])
    gn_g_d = din("gn_g", [C]); gn_b_d = din("gn_b", [C])
    w_mem_kv = din("w_mem_kv", [D, 512])
    w_out = din("w_out", [D, D])
    ln1_g = din("ln1_g", [D]); ln1_b = din("ln1_b", [D])
    w_router = din("w_router", [D, NE]); b_router = din("b_router", [NE])
    if moe:
        w_gate_up = din("w_gate_up", [NE, D, 2 * D]); b_gate_up = din("b_gate_up", [NE, 2 * D])
        w_down = din("w_down", [NE, D, D]); b_down = din("b_down", [NE, D])
    ln2_g = din("ln2_g", [D]); ln2_b = din("ln2_b", [D])
    c_ident = din("c_ident", [128, 128])
    c_strip = din("c_strip", [H, 128, STRIP_W])
    c_masks = din("c_masks", [128, 5 * 64 + 2])
    c_tri = din("c_tri", [2, 128, 128])
    c_utri = din("c_utri", [128, 128])
    c_erow = din("c_erow", [128, NE])

    out_d = V(nc.dram_tensor("out", [NB * S, D], F32, kind="ExternalOutput").ap(), [Buf("out")])

    h0_d = [G.dram("h0_%d" % b, [S, D], F32) for b in range(NB)]
    h1_d = [G.dram("h1_%d" % g, [1024, D], F32) for g in range(4)]
    h1T_d = [G.dram("h1T_%d" % g, [128, 8, 1024], BF16) for g in range(4)]

    banks = []
    for i in range(8):
        t = es.enter_context(nc.psum_tensor("ps%d" % i, [128, 512], F32))
        banks.append(V(t[:], [Buf("ps%d" % i)]))
    bank_i = {"gen": 0, "acc": 0}
    bank_grp = {"gen": [2, 3, 4, 5, 6, 7], "acc": [0, 1]}

    def psum(grp="gen"):
        l = bank_grp[grp]
        b = banks[l[bank_i[grp] % len(l)]]
        bank_i[grp] += 1
        return b

    ident32 = G.sb([128, 128], F32, "ident32")
    identb = G.sb([128, 128], BF16, "identb")
    S_.dma("sp", ident32, c_ident)
    S_.cp("dve", identb, ident32)
    Gall = G.sb([128, NB * NT, NE], F32, "Gall")
    ones_b = G.sb([128, 128], BF16, "ones_b")
    S_.memset("dve", ones_b, 1.0)
    zeros_b = G.sb([128, 512], BF16, "zeros_b")
    S_.memset("dve", zeros_b, 0.0)
    carry = G.sb([128, NE], F32, "carry")
    S_.memset("dve", carry, 0.0)
    idx_all = G.sb([128, NB * NT, 4], U32, "idx_all")
    gk_all = G.sb([128, NB * NT, 4], F32, "gk_all")
    utri32 = G.sb([128, 128], F32, "utri32")
    S_.dma("sp", utri32, c_utri)
    utri_b = G.sb([128, 128], BF16, "utri_b")
    S_.cp("dve", utri_b, utri32)
    erow = G.sb([128, NE], F32, "erow")
    S_.dma("sp", erow, c_erow)
    x_rows = G.dram("x_rows", [NE * CAP + 128, D], BF16)
    y_rows = G.dram("y_rows", [NE * CAP + 128, D], F32)
    zeros32 = G.sb([128, D], F32, "zeros32")
    S_.memset("pool", zeros32, 0.0)
    S_.dma("sp", V(y_rows.ap[NE * CAP:NE * CAP + 128, :], [Buf("ydump")]), zeros32)
    zb = zeros32.bitcast(BF16).re("p (n d) -> p n d", n=2)
    xr = x_rows.re("(n p) d -> p n d", p=128)
    nrow = (NE * CAP + 128) // 128
    for n0 in range(0, nrow, 2):
        nn = min(2, nrow - n0)
        S_.dma("sp", V(xr.ap[:, n0:n0 + nn, :], [Buf("xfill")]), zb[:, 0:nn, :])

    def bcast_load(dst, src1d, q="sp"):
        S_.dma(q, dst, V(src1d.ap.partition_broadcast(128), src1d.bufs))

    def layernorm(es2, xin, gam, bet, out, tmp_pool, eng2="pool"):
        st = tmp_pool["st"]; mv = tmp_pool["mv"]; rs = tmp_pool["rs"]
        for i in range(2):
            S_.op("dve", lambda g, i=i: g.bn_stats(_ap(st[:, i, :]), _ap(xin[:, i * 512:(i + 1) * 512])),
                  [xin], [st])
        S_.op("dve", lambda g: g.bn_aggr(_ap(mv), _ap(st)), [st], [mv])
        S_.ts("dve", rs, mv[:, 1:2], LN_EPS, ALU.add)
        S_.act(rs, rs, AF.Sqrt)
        S_.op("dve", lambda g: g.reciprocal(_ap(rs), _ap(rs)), [rs], [rs])
        S_.ts("dve", out, xin, mv[:, 0:1], ALU.subtract, rs[:, 0:1], ALU.mult)
        S_.tt(eng2, out, out, gam, ALU.mult)
        S_.tt(eng2, out, out, bet, ALU.add)

    def stage_s1(b, hT):
        with ExitStack() as es1:
            A1 = Alloc(nc, es1)
            gam = A1.sb([128, D], F32, "gam"); bet = A1.sb([128, D], F32, "bet")
            bcast_load(gam, ln_in_g); bcast_load(bet, ln_in_b)
            xt = [A1.sb([128, D], F32, "xt") for _ in range(2)]
            ht = [A1.sb([128, D], F32, "ht") for _ in range(2)]
            hb = [A1.sb([128, D], BF16, "hb") for _ in range(2)]
            tp = [dict(st=A1.sb([128, 2, 6], F32, "st"), mv=A1.sb([128, 2], F32, "mv"),
                       rs=A1.sb([128, 1], F32, "rs")) for _ in range(2)]
            for t in range(NT):
                i = t % 2
                S_.dma("sp", xt[i], x_d[b * S + t * 128: b * S + (t + 1) * 128, :])
                layernorm(es1, xt[i], gam, bet, ht[i], tp[i])
                S_.dma("sp", h0_d[b][t * 128:(t + 1) * 128, :], ht[i])
                S_.cp("act", hb[i], ht[i])
                ps = psum().bitcast(BF16)
                for k in range(8):
                    S_.tr(ps[:, k * 128:(k + 1) * 128], hb[i][:, k * 128:(k + 1) * 128], identb, inc=(k == 7))
                S_.cp("act", hT[:, :, t * 128:(t + 1) * 128], ps.re("p (k t) -> p k t", k=8))

    def attn_core(nh, KTv, QTv, Vv, ytok, n_st, s_range, strips=None):
        with ExitStack() as esc:
            Ac = Alloc(nc, esc)
            Eb = [Ac.sb([128, 512], BF16, "E") for _ in range(3)]
            Pb = [Ac.sb([128, 512], BF16, "P") for _ in range(3)]
            rec = Ac.sb([128, 4], F32, "rec")
            it = 0
            for h in range(nh):
                c, p0 = h // 2, 64 * (h % 2)
                for tq in range(4):
                    s_lo, s_hi, jvalid = s_range(tq)
                    acc = psum("acc")
                    accv = acc[:, 0:260].re("p (j d) -> p j d", j=4)
                    S_.mm(acc[:, 0:260], zeros_b[:, 0:128], zeros_b[:, 0:260], start=True, stop=False)
                    for st_ in range(s_lo, s_hi + 1):
                        sc = psum()
                        S_.mm(sc, KTv[p0:p0 + 64, c, st_ * 128:(st_ + 1) * 128],
                              QTv[p0:p0 + 64, c, tq * 512:(tq + 1) * 512], inc=True)
                        E = Eb[it % 3]; P = Pb[it % 3]; it += 1
                        S_.act(E, sc, AF.Exp, scale=0.125)
                        if strips is not None:
                            off = tq * 512 - st_ * 128 + STRIP_C
                            S_.tt("dve" if it % 3 else "pool", P, E, strips[:, h, off:off + 512], ALU.mult)
                        else:
                            P = E
                        for j in range(4):
                            ok, last = jvalid(tq, j, st_)
                            if not ok:
                                continue
                            S_.mm(accv[:, j, :], P[:, j * 128:(j + 1) * 128], Vv[:, st_, h, :],
                                  start=False, stop=last, inc=True)
                    S_.op("dve", lambda g: g.reciprocal(_ap(rec), _ap(accv[:, :, 64])), [accv], [rec])
                    for j in range(4):
                        S_.ts("dve", ytok[:, 4 * tq + j, h * 64:(h + 1) * 64], accv[:, j, 0:64], rec[:, j:j + 1], ALU.mult)

    def proj_fm(dst, Wv, hT, nchunk, col0):
        n = 0
        for c in range(nchunk):
            for tg in range(4):
                ps = psum()
                for k in range(8):
                    S_.mm(ps, Wv[:, k, col0 + c * 128: col0 + (c + 1) * 128],
                          hT[:, k, tg * 512:(tg + 1) * 512], start=(k == 0), stop=(k == 7), inc=(k == 7))
                S_.cp("act" if n % 2 else "dve", dst[:, c, tg * 512:(tg + 1) * 512], ps)
                n += 1

    def ytok_to_yT(ytok, yT, nchunk):
        for t in range(NT):
            ps = psum().bitcast(BF16)
            for cc in range(nchunk):
                S_.tr(ps[:, cc * 128:(cc + 1) * 128], ytok[:, t, cc * 128:(cc + 1) * 128], identb, inc=(cc == nchunk - 1))
            S_.cp("act" if t % 2 else "dve", yT[:, 0:nchunk, t * 128:(t + 1) * 128],
                  ps[:, 0:nchunk * 128].re("p (k t) -> p k t", k=nchunk))

    def stage_att(b, hT, yT_a):
        with ExitStack() as es2:
            A2 = Alloc(nc, es2)
            Wqkv = A2.sb([128, 8, 3 * C], BF16, "Wqkv")
            S_.dma("pool", Wqkv, w_in[:, RWKV_IN:RWKV_IN + 3 * C].re("(k p) c -> p k c", p=128))
            strips = A2.sb([128, H, STRIP_W], BF16, "strips")
            for h in range(H):
                S_.dma("pool", strips[:, h, :], c_strip[h])
            QT = A2.sb([128, 3, S], BF16, "QT")
            KT = A2.sb([128, 3, S], BF16, "KT")
            Vt = A2.sb([128, NT, H, 65], BF16, "Vt")
            ytok = A2.sb([128, NT, C], BF16, "ytok")
            S_.memset("pool", Vt[:, :, :, 64:65], 1.0)
            proj_fm(QT, Wqkv, hT, 3, 0)
            proj_fm(KT, Wqkv, hT, 3, C)
            for t in range(NT):
                ps = psum()
                for k in range(8):
                    S_.mm(ps[:, 0:C], hT[:, k, t * 128:(t + 1) * 128], Wqkv[:, k, 2 * C:3 * C],
                          start=(k == 0), stop=(k == 7), inc=(k == 7))
                S_.cp("act" if t % 2 else "dve", Vt[:, t, :, 0:64], ps[:, 0:C].re("p (h d) -> p h d", h=H))

            def s_range(tq):
                s_hi = min(NT - 1, 4 * tq + 3 + 8)

                def jvalid(tq, j, st_):
                    tt_ = 4 * tq + j
                    return abs(tt_ - st_) <= 8, (st_ == s_hi and j == 3)
                return max(0, 4 * tq - 8), s_hi, jvalid
            attn_core(H, KT, QT, Vt, ytok, NT, s_range, strips)
            ytok_to_yT(ytok, yT_a, 3)

    def stage_mem(b, hT, yT_m):
        with ExitStack() as es3:
            A3 = Alloc(nc, es3)
            Wkv = A3.sb([128, 8, 512], BF16, "Wkv")
            S_.dma("pool", Wkv, w_mem_kv.re("(k p) c -> p k c", p=128))
            Wqm = A3.sb([128, 8, 256], BF16, "Wqm")
            S_.dma("pool", Wqm, w_in[:, RWKV_IN + 3 * C:IN_W].re("(k p) c -> p k c", p=128))
            memb = A3.sb([128, 2, D], BF16, "memb")
            S_.dma("pool", memb, mem_d[b * 256:(b + 1) * 256, :].re("(m p) d -> p m d", p=128))
            memT = A3.sb([128, 8, 256], BF16, "memT")
            for m in range(2):
                ps = psum().bitcast(BF16)
                for k in range(8):
                    S_.tr(ps[:, k * 128:(k + 1) * 128], memb[:, m, k * 128:(k + 1) * 128], identb, inc=(k == 7))
                S_.cp("act", memT[:, :, m * 128:(m + 1) * 128], ps.re("p (k t) -> p k t", k=8))
            KmT = A3.sb([128, 2, 256], BF16, "KmT")
            for c in range(2):
                ps = psum()
                for k in range(8):
                    S_.mm(ps[:, 0:256], Wkv[:, k, c * 128:(c + 1) * 128], memT[:, k, :],
                          start=(k == 0), stop=(k == 7), inc=(k == 7))
                S_.cp("dve", KmT[:, c, :], ps[:, 0:256])
            Vm = A3.sb([128, 2, 4, 65], BF16, "Vm")
            S_.memset("pool", Vm[:, :, :, 64:65], 1.0)
            for m in range(2):
                ps = psum()
                for k in range(8):
                    S_.mm(ps[:, 0:256], memT[:, k, m * 128:(m + 1) * 128], Wkv[:, k, 256:512],
                          start=(k == 0), stop=(k == 7), inc=(k == 7))
                S_.cp("dve", Vm[:, m, :, 0:64], ps[:, 0:256].re("p (h d) -> p h d", h=4))
            QmT = A3.sb([128, 2, S], BF16, "QmT")
            proj_fm(QmT, Wqm, hT, 2, 0)
            ytok = A3.sb([128, NT, 256], BF16, "ytokm")

            def s_range(tq):
                return 0, 1, (lambda tq, j, st_: (True, st_ == 1 and j == 3))
            attn_core(4, KmT, QmT, Vm, ytok, 2, s_range, None)
            ytok_to_yT(ytok, yT_m, 2)

    def stage_rwkv(b, hT, yT_r):
        with ExitStack() as es4:
            A4 = Alloc(nc, es4)
            f32t = lambda name, n=C: A4.sb([128, n], F32, name)
            Wr = A4.sb([128, 16, RWKV_IN], BF16, "Wr")
            with ExitStack() as esw:
                Aw = Alloc(nc, esw)
                mub = Aw.sb([128, RWKV_IN], F32, "mub"); bcast_load(mub, mu_d)
                omb = Aw.sb([128, RWKV_IN], F32, "omb"); hmb = Aw.sb([128, RWKV_IN], F32, "hmb")
                S_.ts("dve", omb, mub, -1.0, ALU.mult, 1.0, ALU.add)
                S_.ts("pool", hmb, mub, 0.5, ALU.mult)
                stg = [Aw.sb([128, 8, 336], F32, "stg") for _ in range(2)]
                for pc in range(4):
                    cs = slice(pc * 336, (pc + 1) * 336)
                    S_.dma("sp", stg[pc % 2], w_in[:, cs].re("(k p) c -> p k c", p=128))
                    S_.tt("dve", Wr[:, 0:8, cs], stg[pc % 2], omb[:, cs].un(1).bc([128, 8, 336]), ALU.mult)
                    S_.tt("pool", Wr[:, 8:16, cs], stg[pc % 2], hmb[:, cs].un(1).bc([128, 8, 336]), ALU.mult)
                S_.barrier()
            kkb = f32t("kkb"); bcast_load(kkb, k_k_d)
            kab = f32t("kab"); bcast_load(kab, k_a_d)
            omkab = f32t("omkab"); S_.ts("dve", omkab, kab, -1.0, ALU.mult, 1.0, ALU.add)
            rkb = f32t("rkb"); bcast_load(rkb, r_k_d)
            gngb = f32t("gngb"); bcast_load(gngb, gn_g_d)
            gnbb = f32t("gnbb"); bcast_load(gnbb, gn_b_d)
            w0b = f32t("w0b", 2 * C); bcast_load(w0b, w0_d)
            a0b = f32t("a0b", 2 * C); bcast_load(a0b, a0_d)
            gup = A4.sb([128, C], BF16, "gup"); S_.dma("pool", gup[0:64, :], g_up_d)
            wupbd = A4.sb([128, 2 * C], BF16, "wupbd"); S_.memset("pool", wupbd, 0.0)
            S_.dma("pool", wupbd[64:96, 0:C], w_up_d[0]); S_.dma("pool", wupbd[96:128, C:2 * C], w_up_d[1])
            aupbd = A4.sb([128, 2 * C], BF16, "aupbd"); S_.memset("pool", aupbd, 0.0)
            S_.dma("pool", aupbd[0:32, 0:C], a_up_d[0]); S_.dma("pool", aupbd[32:64, C:2 * C], a_up_d[1])
            Yacc = A4.sb([128, NT, C], F32, "Yacc")
            T32 = A4.sb([128, 3, 64], F32, "T32"); Tb = A4.sb([128, 3, 64], BF16, "Tb"); Ttmp = A4.sb([128, 3, 64], F32, "Ttmp")
            hs = A4.sb([128, 8, 128], BF16, "hs")
            r32 = f32t("r32"); k32 = f32t("k32"); v32 = f32t("v32")
            sgw = f32t("sgw"); a32 = f32t("a32"); a32b = f32t("a32b")
            ecw = f32t("ecw"); encw = f32t("encw")
            kk = f32t("kk"); sq = f32t("sq"); kd = f32t("kd"); bb = f32t("bb")
            ss = A4.sb([128, 6], F32, "ss"); rn = A4.sb([128, 6], F32, "rn")
            lor = A4.sb([128, 3, 128], BF16, "lor")
            tok4 = A4.sb([128, 4, C], BF16, "tok4")
            Pp = [A4.sb([128, 6, 64], BF16, "Pp") for _ in range(2)]
            PTp = [A4.sb([128, 6, 64], BF16, "PTp") for _ in range(2)]
            Zs = A4.sb([128, C], BF16, "Zs"); Us = A4.sb([128, C], BF16, "Us")
            fs1 = A4.sb([128, 6], F32, "fs1"); fs2 = A4.sb([128, 6], F32, "fs2"); fs3 = A4.sb([128, 6], F32, "fs3")
            yob = A4.sb([128, C], BF16, "yob")
            sets = []
            for i in range(2):
                sets.append(dict(
                    FM=A4.sb([128, 4, 3, 128], BF16, "FM"), Vb=A4.sb([128, C], BF16, "Vb"),
                    XT=[A4.sb([128, 6, 64], BF16, "XT") for _ in range(2)],
                    LkT=A4.sb([128, 6, 64], BF16, "LkT"), RbT=A4.sb([128, 6, 64], BF16, "RbT"),
                    RkT=A4.sb([128, 6, 64], BF16, "RkT"), WCe=A4.sb([128, 3, 2], F32, "WCe"),
                    Bt=A4.sb([128, C], BF16, "Bt"), Kt=A4.sb([128, C], BF16, "Kt"),
                    g32=f32t("g32"), bv=f32t("bv")))

            def v6(x):
                return x.re("p (h j) -> p h j", h=6)
            HORD = (0, 2, 4, 1, 3, 5)

            def prep(t, d, st):
                lo = t * 128
                if 0 < t < NT - 1:
                    S_.tt("pool", hs, hT[:, :, lo - 1:lo + 127], hT[:, :, lo + 1:lo + 129], ALU.add)
                elif t == 0:
                    S_.tt("pool", hs[:, :, 1:128], hT[:, :, 0:127], hT[:, :, 2:129], ALU.add)
                    S_.cp("pool", hs[:, :, 0:1], hT[:, :, 1:2])
                else:
                    S_.tt("pool", hs[:, :, 0:127], hT[:, :, lo - 1:lo + 126], hT[:, :, lo + 1:lo + 128], ALU.add)
                    S_.cp("pool", hs[:, :, 127:128], hT[:, :, lo + 126:lo + 127])

                def lhs(kc):
                    return hT[:, kc, lo:lo + 128] if kc < 8 else hs[:, kc - 8, :]
                for part, dst in ((0, r32), (1, k32), (2, v32)):
                    ps = psum()
                    for kc in range(16):
                        S_.mm(ps[:, 0:C], lhs(kc), Wr[:, kc, part * C:(part + 1) * C], start=(kc == 0), stop=(kc == 15), inc=(kc == 15))
                    S_.cp("act", dst, ps[:, 0:C])
                S_.cp("pool", st["Vb"], v32)
                yield
                ps = psum()
                for kc in range(16):
                    S_.mm(ps[:, 0:128], Wr[:, kc, 3 * C:3 * C + 128], lhs(kc), start=(kc == 0), stop=(kc == 15), inc=(kc == 15))
                for kc in range(16):
                    S_.mm(ps[0:64, 128:256], Wr[:, kc, 3 * C + 128:3 * C + 192], lhs(kc), start=(kc == 0), stop=(kc == 15), inc=(kc == 15))
                S_.act(lor[0:64, 0, :], ps[0:64, 0:128], AF.Sigmoid)
                S_.act(lor[64:128, 1, :], ps[64:128, 0:128], AF.Tanh)
                S_.cp("act", lor[0:64, 2, :], ps[0:64, 128:256])
                yield
                psg = psum()
                S_.mm(psg[:, 0:C], lor[0:64, 0, :], gup[0:64, :], inc=True)
                S_.cp("act", st["g32"], psg[:, 0:C])
                psw = psum()
                S_.mm(psw[:, 0:C], lor[64:128, 1, :], wupbd[64:128, d * C:(d + 1) * C], inc=True)
                S_.tt("dve", sgw, psw[:, 0:C], w0b[:, d * C:(d + 1) * C], ALU.add)
                S_.act(sgw, sgw, AF.Sigmoid)
                psa = psum()
                S_.mm(psa[:, 0:C], lor[0:64, 2, :], aupbd[0:64, d * C:(d + 1) * C], inc=True)
                S_.tt("dve", a32, psa[:, 0:C], a0b[:, d * C:(d + 1) * C], ALU.add)
                S_.act(a32, a32, AF.Sigmoid)
                pscs = psum()
                S_.mm(pscs[:, 0:C], tri32[:, d, :], sgw, inc=True)
                pswc = psum()
                for c in range(3):
                    S_.mm(pswc[:, c * 2:(c + 1) * 2], sgw[:, c * 128:(c + 1) * 128], BLK, inc=(c == 2))
                S_.act(st["WCe"].re("p c q -> p (c q)"), pswc[:, 0:6], AF.Exp, scale=-LDS)
                S_.act(ecw, pscs[:, 0:C], AF.Exp, scale=-LDS)
                S_.act(encw, pscs[:, 0:C], AF.Exp, scale=LDS)
                S_.act(sgw, sgw, AF.Exp, scale=LDS)
                S_.tt("dve", sgw, sgw, ecw, ALU.mult)
                yield
                S_.tt("pool", kk, k32, kkb, ALU.mult)
                S_.tt("pool", sq, kk, kk, ALU.mult)
                S_.red(ss, v6(sq), ALU.add)
                S_.act(rn, ss, AF.Sqrt)
                S_.ts("dve", rn, rn, 1e-12, ALU.max)
                S_.op("dve", lambda g: g.reciprocal(_ap(rn), _ap(rn)), [rn], [rn])
                S_.tt("dve", v6(kk), v6(kk), rn.un(2).bc([128, 6, 64]), ALU.mult)
                S_.tt("pool", kd, a32, kab, ALU.mult)
                S_.tt("pool", kd, kd, omkab, ALU.add)
                S_.tt("pool", kd, kd, k32, ALU.mult)
                S_.tt("dve", bb, kk, a32, ALU.mult)
                S_.tt("dve", tok4[:, 0, :], kk, sgw, ALU.mult)
                S_.stt(tok4[:, 1, :], bb, -1.0, encw, ALU.mult, ALU.mult)
                S_.tt("pool", tok4[:, 2, :], kd, encw, ALU.mult)
                S_.tt("pool", tok4[:, 3, :], r32, ecw, ALU.mult)
                S_.cp("act", st["Bt"], tok4[:, 1, :])
                S_.cp("act", st["Kt"], tok4[:, 2, :])
                if d == 1:
                    psa2 = psum()
                    S_.mm(psa2[:, 0:C], lor[0:64, 2, :], aupbd[0:64, 0:C], inc=True)
                    S_.tt("dve", a32b, psa2[:, 0:C], a0b[:, 0:C], ALU.add)
                    S_.act(a32b, a32b, AF.Sigmoid)
                    S_.tt("pool", a32b, a32b, a32, ALU.add)
                    S_.tt("pool", a32b, a32b, kab, ALU.mult)
                    S_.stt(a32b, omkab, 2.0, a32b, ALU.mult, ALU.add)
                    S_.tt("pool", a32b, a32b, k32, ALU.mult)
                    S_.tt("pool", a32b, a32b, rkb, ALU.mult)
                    S_.tt("pool", a32b, a32b, r32, ALU.mult)
                    S_.red(fs3, v6(a32b), ALU.add)
                    S_.tt("dve", v6(st["bv"]), v6(v32), fs3.un(2).bc([128, 6, 64]), ALU.mult)
                yield
                FM = st["FM"]
                for half in range(2):
                    ps = psum().bitcast(BF16)
                    n = 0
                    for o in (2 * half, 2 * half + 1):
                        for c in range(3):
                            S_.tr(ps[:, n * 128:(n + 1) * 128], tok4[:, o, c * 128:(c + 1) * 128], identb, inc=(n == 5))
                            n += 1
                    S_.cp("act" if half else "dve", FM[:, 2 * half:2 * half + 2, :, :].re("p o c t -> p (o c) t"),
                          ps[:, 0:768].re("p (n t) -> p n t", n=6))
                KKi, Bi, Ki, Ri = 0, 1, 2, 3
                st["XTf"] = st["XT"][1]
                yield

                def cc_mm(ps, li, ri):
                    n = 0
                    for q in range(2):
                        q0 = 64 * q
                        for h in HORD:
                            c, p0 = h // 2, 64 * (h % 2)
                            n += 1
                            S_.mm(ps[q0:q0 + 64, h * 64:(h + 1) * 64], FM[p0:p0 + 64, li, c, q0:q0 + 64],
                                  FM[p0:p0 + 64, ri, c, q0:q0 + 64], inc=(n == 12))

                def mbc(m):
                    return m.un(1).bc([128, 6, 64])
                XT = st["XT"]
                ps = psum(); cc_mm(ps, Bi, KKi)
                S_.tt("dve", PTp[0], v6(ps[:, 0:C]), mbc(MS[d]), ALU.mult)
                S_.tt("pool", XT[0], PTp[0], mbc(MID), ALU.add)
                yield
                ps = psum(); cc_mm(ps, KKi, Bi)
                S_.tt("dve", Pp[0], v6(ps[:, 0:C]), mbc(MS[1 - d]), ALU.mult)
                yield
                ps = psum(); cc_mm(ps, Ki, KKi)
                S_.tt("dve", st["LkT"], v6(ps[:, 0:C]), mbc(MS[d]), ALU.mult)
                yield
                ps = psum(); cc_mm(ps, Bi, Ri)
                S_.tt("dve", st["RbT"], v6(ps[:, 0:C]), mbc(MI[d]), ALU.mult)
                yield
                ps = psum(); cc_mm(ps, Ki, Ri)
                S_.tt("dve", st["RkT"], v6(ps[:, 0:C]), mbc(MI[d]), ALU.mult)
                cur = 0
                for lvl in range(1, 6):
                    nxt = 1 - cur

                    def sq_mm(ps, lt, rt):
                        n = 0
                        for q in range(2):
                            q0 = 64 * q
                            for h in range(H):
                                n += 1
                                S_.mm(ps[q0:q0 + 64, h * 64:(h + 1) * 64], lt[q0:q0 + 64, h, :], rt[q0:q0 + 64, h, :], inc=(n == 12))
                    yield
                    psP = psum(); sq_mm(psP, PTp[cur], Pp[cur])
                    if lvl < 5:
                        psPT = psum(); sq_mm(psPT, Pp[cur], PTp[cur])
                    S_.cp("act", Pp[nxt], v6(psP[:, 0:C]))
                    if lvl < 5:
                        S_.cp("dve", PTp[nxt], v6(psPT[:, 0:C]))
                    yield
                    psX = psum(); sq_mm(psX, Pp[nxt], XT[(lvl - 1) % 2])
                    S_.tt("dve", XT[lvl % 2], v6(psX[:, 0:C]), XT[(lvl - 1) % 2], ALU.add)
                    cur = nxt
                st["XTf"] = XT[5 % 2]

            def seq_chunk(t, q, d, st):
                q0 = 64 * q
                FM = st["FM"]; Vb = st["Vb"]; XT = st["XTf"]
                hc = lambda h: slice(h * 64, (h + 1) * 64)
                psZ = psum()
                S_.mm(psZ[q0:q0 + 64, 0:C], zeros_b[:, 0:64], zeros_b[:, 0:C], start=True, stop=False)
                for h in HORD:
                    c, p0 = h // 2, 64 * (h % 2)
                    S_.mm(psZ[q0:q0 + 64, hc(h)], FM[p0:p0 + 64, 0, c, q0:q0 + 64], Tb[p0:p0 + 64, c, :], start=False, stop=False)
                for h in range(H):
                    S_.mm(psZ[q0:q0 + 64, hc(h)], st["LkT"][q0:q0 + 64, h, :], Vb[q0:q0 + 64, hc(h)], start=False, stop=(h == H - 1), inc=(h == H - 1))
                S_.cp("act", Zs[q0:q0 + 64, :], psZ[q0:q0 + 64, 0:C])
                yield
                psU = psum()
                for h in range(H):
                    S_.mm(psU[q0:q0 + 64, hc(h)], XT[q0:q0 + 64, h, :], Zs[q0:q0 + 64, hc(h)], inc=(h == H - 1))
                S_.cp("dve", Us[q0:q0 + 64, :], psU[q0:q0 + 64, 0:C])
                yield
                psY = psum()
                S_.mm(psY[q0:q0 + 64, 0:C], zeros_b[:, 0:64], zeros_b[:, 0:C], start=True, stop=False)
                for h in HORD:
                    c, p0 = h // 2, 64 * (h % 2)
                    S_.mm(psY[q0:q0 + 64, hc(h)], FM[p0:p0 + 64, 3, c, q0:q0 + 64], Tb[p0:p0 + 64, c, :], start=False, stop=False)
                for h in range(H):
                    S_.mm(psY[q0:q0 + 64, hc(h)], st["RbT"][q0:q0 + 64, h, :], Us[q0:q0 + 64, hc(h)], start=False, stop=False)
                for h in range(H):
                    S_.mm(psY[q0:q0 + 64, hc(h)], st["RkT"][q0:q0 + 64, h, :], Vb[q0:q0 + 64, hc(h)], start=False, stop=(h == H - 1), inc=(h == H - 1))
                psT = psum()
                for h in range(H):
                    c, p0 = h // 2, 64 * (h % 2)
                    S_.mm(psT[p0:p0 + 64, c * 64:(c + 1) * 64], st["Bt"][q0:q0 + 64, hc(h)], Us[q0:q0 + 64, hc(h)], start=True, stop=False)
                    S_.mm(psT[p0:p0 + 64, c * 64:(c + 1) * 64], st["Kt"][q0:q0 + 64, hc(h)], Vb[q0:q0 + 64, hc(h)], start=False, stop=True, inc=(h == H - 1))
                if d == 0:
                    S_.cp("act", Yacc[q0:q0 + 64, t, :], psY[q0:q0 + 64, 0:C])
                else:
                    S_.tt("pool" if False else "dve", Yacc[q0:q0 + 64, t, :], psY[q0:q0 + 64, 0:C], Yacc[q0:q0 + 64, t, :], ALU.add)
                S_.tt("dve", Ttmp, psT[:, 0:192].re("p (c i) -> p c i", c=3), T32, ALU.add)
                S_.tt("dve", T32, Ttmp, st["WCe"][:, :, q:q + 1].bc([128, 3, 64]), ALU.mult)
                S_.cp("act", Tb, T32)
                yield

            def finalize(t, st):
                Y = Yacc[:, t, :]
                S_.red(fs1, v6(Y), ALU.add)
                S_.tt("pool", sq, Y, Y, ALU.mult)
                S_.red(fs2, v6(sq), ALU.add)
                S_.ts("dve", fs1, fs1, 1.0 / 64, ALU.mult)
                S_.tt("dve", fs3, fs1, fs1, ALU.mult)
                S_.stt(fs2, fs2, 1.0 / 64, fs3, ALU.mult, ALU.subtract)
                S_.ts("dve", fs2, fs2, GN_EPS, ALU.add)
                S_.act(fs2, fs2, AF.Sqrt)
                S_.op("dve", lambda g: g.reciprocal(_ap(fs2), _ap(fs2)), [fs2], [fs2])
                S_.tt("dve", v6(sq), v6(Y), fs1.un(2).bc([128, 6, 64]), ALU.subtract)
                S_.tt("dve", v6(sq), v6(sq), fs2.un(2).bc([128, 6, 64]), ALU.mult)
                S_.tt("pool", sq, sq, gngb, ALU.mult)
                S_.tt("pool", sq, sq, gnbb, ALU.add)
                S_.tt("pool", sq, sq, st["bv"], ALU.add)
                S_.tt("pool", yob, sq, st["g32"], ALU.mult)
                ps = psum().bitcast(BF16)
                for cc in range(3):
                    S_.tr(ps[:, cc * 128:(cc + 1) * 128], yob[:, cc * 128:(cc + 1) * 128], identb, inc=(cc == 2))
                S_.cp("act", yT_r[:, 0:3, t * 128:(t + 1) * 128], ps[:, 0:384].re("p (k t) -> p k t", k=3))

            def seqfin(t, d, st):
                for q in ((0, 1) if d == 0 else (1, 0)):
                    yield from seq_chunk(t, q, d, st)
                if d == 1:
                    finalize(t, st)
                yield

            def run_interleaved(gens):
                gens = list(gens)
                while gens:
                    for g_ in list(gens):
                        try:
                            next(g_)
                        except StopIteration:
                            gens.remove(g_)

            for d in (0, 1):
                S_.memset("dve", T32, 0.0)
                S_.memset("pool", Tb, 0.0)
                order = list(range(NT)) if d == 0 else list(range(NT - 1, -1, -1))
                run_interleaved([prep(order[0], d, sets[0])])
                for i, t in enumerate(order):
                    st = sets[i % 2]
                    gens = [seqfin(t, d, st)]
                    if i + 1 < NT:
                        gens.append(prep(order[i + 1], d, sets[(i + 1) % 2]))
                    run_interleaved(gens)

    def stage_out(b, yTs):
        with ExitStack() as es5:
            A5 = Alloc(nc, es5)
            Wo = A5.sb([128, 8, D], BF16, "Wo")
            S_.dma("pool", Wo, w_out.re("(k p) c -> p k c", p=128))
            gam = A5.sb([128, D], F32, "gam1"); bet = A5.sb([128, D], F32, "bet1")
            bcast_load(gam, ln1_g); bcast_load(bet, ln1_b)
            Wr32 = A5.sb([128, 8, NE], F32, "Wr32")
            S_.dma("sp", Wr32, w_router.re("(k p) e -> p k e", p=128))
            brb = A5.sb([128, NE], F32, "brb"); bcast_load(brb, b_router)
            h0t = [A5.sb([128, D], F32, "h0t") for _ in range(2)]
            zt = [A5.sb([128, D], F32, "zt") for _ in range(2)]
            h1t = [A5.sb([128, D], F32, "h1t") for _ in range(2)]
            h1T32 = [A5.sb([128, 8, 128], F32, "h1T32") for _ in range(2)]
            h1Tb = [A5.sb([128, 8, 128], BF16, "h1Tb") for _ in range(2)]
            hhi = [A5.sb([128, D], BF16, "hhi") for _ in range(2)]
            hlo = [A5.sb([128, D], BF16, "hlo") for _ in range(2)]
            tp = [dict(st=A5.sb([128, 2, 6], F32, "st"), mv=A5.sb([128, 2], F32, "mv"),
                       rs=A5.sb([128, 1], F32, "rs")) for _ in range(2)]
            lg = A5.sb([128, NE], F32, "lg"); mx8 = A5.sb([128, 8], F32, "mx8"); nmx = A5.sb([128, 1], F32, "nmx")
            ex = A5.sb([128, NE], F32, "ex"); mk = A5.sb([128, NE], F32, "mk"); sm = A5.sb([128, 1], F32, "sm")
            rt = dict(mkb=A5.sb([128, NE], BF16, "mkb"), pos=A5.sb([128, NE], F32, "pos"), vv=A5.sb([128, NE], F32, "vv"),
                      nd=A5.sb([128, NE], F32, "nd"), mx=A5.sb([128, 8], F32, "mxr"), sel=A5.sb([128, NE], F32, "sel"),
                      idf=A5.sb([128, 4], F32, "idf"))
            for t in range(NT):
                i = t % 2
                gt = b * NT + t
                grp, tin = gt // 8, gt % 8
                S_.dma("sp", h0t[i], h0_d[b][t * 128:(t + 1) * 128, :])
                for half in range(2):
                    ps = psum()
                    for k in range(8):
                        S_.mm(ps, yTs[k][:, t * 128:(t + 1) * 128], Wo[:, k, half * 512:(half + 1) * 512],
                              start=(k == 0), stop=(k == 7), inc=(k == 7))
                    S_.stt(zt[i][:, half * 512:(half + 1) * 512], h0t[i][:, half * 512:(half + 1) * 512], ALPHA, ps, ALU.mult, ALU.add)
                layernorm(es5, zt[i], gam, bet, h1t[i], tp[i])
                S_.dma("sp", h1_d[grp][tin * 128:(tin + 1) * 128, :], h1t[i])
                S_.cp("act", hhi[i], h1t[i])
                S_.tt("dve", hlo[i], h1t[i], hhi[i], ALU.subtract)
                for half in range(2):
                    ps = psum()
                    for k in range(4):
                        kk_ = half * 4 + k
                        S_.mm(ps[:, k * 128:(k + 1) * 128], hhi[i][:, kk_ * 128:(kk_ + 1) * 128], identb, start=True, stop=False)
                        S_.mm(ps[:, k * 128:(k + 1) * 128], hlo[i][:, kk_ * 128:(kk_ + 1) * 128], identb, start=False, stop=True, inc=(k == 3))
                    S_.cp("act", h1T32[i][:, half * 4:half * 4 + 4, :], ps.re("p (k t) -> p k t", k=4))
                    S_.cp("dve", h1Tb[i][:, half * 4:half * 4 + 4, :], h1T32[i][:, half * 4:half * 4 + 4, :])
                if "h1Tdma" not in SKIP:
                    S_.dma("sp", h1T_d[grp][:, :, tin * 128:(tin + 1) * 128], h1Tb[i])
                if "router" in SKIP:
                    continue
                ps = psum()
                for k in range(8):
                    S_.mm(ps[:, 0:NE], h1T32[i][:, k, :], Wr32[:, k, :], start=(k == 0), stop=(k == 7), inc=(k == 7))
                S_.tt("dve", lg, ps[:, 0:NE], brb, ALU.add)
                if "top" in SKIP:
                    continue
                S_.op("dve", lambda g: g.max(_ap(mx8), _ap(lg)), [lg], [mx8])
                S_.ts("dve", mk, lg, mx8[:, 3:4], ALU.is_ge)
                S_.ts("dve", nmx, mx8[:, 0:1], -1.0, ALU.mult)
                S_.act(ex, lg, AF.Exp, bias=nmx[:, 0:1])
                S_.tt("dve", ex, ex, mk, ALU.mult)
                S_.red(sm, ex, ALU.add)
                S_.op("dve", lambda g: g.reciprocal(_ap(sm), _ap(sm)), [sm], [sm])
                S_.ts("dve", Gall[:, gt, :], ex, sm[:, 0:1], ALU.mult)
                if SPARSE:
                    route_tile(gt, mk, hhi[i], rt)

    def stage_moe():
        with ExitStack() as es6:
            A6 = Alloc(nc, es6)
            gam = A6.sb([128, D], F32, "gam2"); bet = A6.sb([128, D], F32, "bet2")
            bcast_load(gam, ln2_g); bcast_load(bet, ln2_b)
            bguT = A6.sb([128, 16, NE], F32, "bguT")
            with ExitStack() as esb:
                Ab = Alloc(nc, esb)
                bgu_sb = Ab.sb([NE, 2 * D], F32, "bgu_sb")
                bgu_hi = Ab.sb([NE, 2 * D], BF16, "bgu_hi"); bgu_lo = Ab.sb([NE, 2 * D], BF16, "bgu_lo")
                S_.dma("sp", bgu_sb, b_gate_up)
                S_.cp("act", bgu_hi, bgu_sb)
                S_.tt("dve", bgu_lo, bgu_sb, bgu_hi, ALU.subtract)
                for fc in range(16):
                    ps = psum()
                    S_.mm(ps[:, 0:NE], bgu_hi[0:NE, fc * 128:(fc + 1) * 128], identb[0:NE, 0:NE], start=True, stop=False)
                    S_.mm(ps[:, 0:NE], bgu_lo[0:NE, fc * 128:(fc + 1) * 128], identb[0:NE, 0:NE], start=False, stop=True, inc=True)
                    S_.cp("act", bguT[:, fc, :], ps[:, 0:NE])
                S_.barrier()
            bd_sb = A6.sb([NE, D], F32, "bd_sb")
            S_.dma("sp", bd_sb, b_down)
            h1T = A6.sb([128, 8, 1024], BF16, "h1T")
            acc = A6.sb([128, 8, D], F32, "acc")
            wgu = [A6.sb([128, 8, 2, 256], BF16, "wgu") for _ in range(6)]
            wd = [A6.sb([128, 8, D], BF16, "wd") for _ in range(2)]
            actT = A6.sb([128, 8, 1024], BF16, "actT")
            g1 = [A6.sb([128, 512], F32, "g1") for _ in range(2)]
            s1 = [A6.sb([128, 512], F32, "s1") for _ in range(2)]
            u1 = [A6.sb([128, 512], F32, "u1") for _ in range(2)]
            GT = A6.sb([NE, 128], F32, "GT")
            Ghi = A6.sb([128, NE], BF16, "Ghi"); Glo = A6.sb([128, NE], BF16, "Glo")
            h1t = A6.sb([128, D], F32, "h1t2"); zt = A6.sb([128, D], F32, "zt2"); ot = A6.sb([128, D], F32, "ot2")
            tp = dict(st=A6.sb([128, 2, 6], F32, "st"), mv=A6.sb([128, 2], F32, "mv"), rs=A6.sb([128, 1], F32, "rs"))
            tmpd = [A6.sb([128, 512], F32, "tmpd") for _ in range(2)]
            npiece = 0
            it = 0
            nd = 0
            for g in range(4):
                S_.dma("sp", h1T, h1T_d[g])
                for tl in range(8):
                    gt = g * 8 + tl
                    ps = psum()
                    S_.cp("act", Ghi, Gall[:, gt, :])
                    S_.tt("dve", Glo, Gall[:, gt, :], Ghi, ALU.subtract)
                    S_.mm(ps[0:NE, 0:128], Ghi, identb, start=True, stop=False)
                    S_.mm(ps[0:NE, 0:128], Glo, identb, start=False, stop=True, inc=True)
                    S_.cp("act", GT, ps[0:NE, 0:128])
                    for half in range(2):
                        ps = psum()
                        S_.mm(ps, GT[0:NE, :], bd_sb[0:NE, half * 512:(half + 1) * 512], inc=True)
                        S_.cp("act" if half else "dve", acc[:, tl, half * 512:(half + 1) * 512], ps)
                for e in range(NE):
                    wde = wd[e % 2]
                    if "moe_w" not in SKIP:
                        S_.dma("pool", wde, w_down[e].re("(k p) c -> p k c", p=128))
                    src = w_gate_up[e].re("(k p) (u f) -> p k u f", p=128, u=2)
                    for p in range(4):
                        wp = wgu[npiece % 6]; npiece += 1
                        for u_ in range(2):
                            if "moe_w" not in SKIP:
                                S_.dma("pool", wp[:, :, u_, :], src[:, :, u_, p * 256:(p + 1) * 256])
                        if "moe_gu" in SKIP:
                            continue
                        for sg in range(2):
                            for f2 in range(2):
                                fc = 2 * p + f2
                                i = it % 2; it += 1
                                psg = psum(); psu = psum()
                                for k in range(8):
                                    S_.mm(psg, wp[:, k, 0, f2 * 128:(f2 + 1) * 128], h1T[:, k, sg * 512:(sg + 1) * 512],
                                          start=(k == 0), stop=(k == 7), inc=(k == 7))
                                for k in range(8):
                                    S_.mm(psu, wp[:, k, 1, f2 * 128:(f2 + 1) * 128], h1T[:, k, sg * 512:(sg + 1) * 512],
                                          start=(k == 0), stop=(k == 7), inc=(k == 7))
                                S_.ts("dve", g1[i], psg, bguT[:, fc, e:e + 1], ALU.add, 7.0, ALU.min)
                                S_.act(s1[i], g1[i], AF.Sigmoid, scale=1.702)
                                S_.act(u1[i], psu, AF.Identity, bias=bguT[:, 8 + fc, e:e + 1])
                                S_.ts("pool", u1[i], u1[i], 7.0, ALU.min, -7.0, ALU.max)
                                S_.tt("pool", g1[i], g1[i], s1[i], ALU.mult)
                                S_.stt(actT[:, fc, sg * 512:(sg + 1) * 512], u1[i], 1.0, g1[i], ALU.add, ALU.mult)
                    for tl in range(8):
                        gt = g * 8 + tl
                        if "moe_down" in SKIP:
                            continue
                        for half in range(2):
                            ps = psum()
                            for fc in range(8):
                                S_.mm(ps, actT[:, fc, tl * 128:(tl + 1) * 128], wde[:, fc, half * 512:(half + 1) * 512],
                                      start=(fc == 0), stop=(fc == 7), inc=(fc == 7))
                            a_ = acc[:, tl, half * 512:(half + 1) * 512]
                            td = tmpd[nd % 2]; nd += 1
                            S_.ts("dve", td, ps, Gall[:, gt, e:e + 1], ALU.mult)
                            S_.tt("pool", a_, a_, td, ALU.add)
                for tl in range(8):
                    gt = g * 8 + tl
                    S_.dma("sp", h1t, h1_d[g][tl * 128:(tl + 1) * 128, :])
                    S_.stt(zt, h1t, ALPHA, acc[:, tl, :], ALU.mult, ALU.add)
                    layernorm(es6, zt, gam, bet, ot, tp)
                    S_.dma("sp", out_d[gt * 128:(gt + 1) * 128, :], ot)

    def route_tile(gt, mk, hb_tile, rt):
        mkb = rt["mkb"]; pos = rt["pos"]; vv = rt["vv"]; nd = rt["nd"]; mx = rt["mx"]; sel = rt["sel"]; idf = rt["idf"]
        S_.cp("dve", mkb, mk)
        ps = psum()
        S_.mm(ps[:, 0:NE], utri_b, mkb, inc=True)
        ps2 = psum()
        S_.mm(ps2[:, 0:NE], ones_b, mkb, inc=True)
        S_.tt("dve", pos, ps[:, 0:NE], carry, ALU.add)
        S_.tt("dve", carry, ps2[:, 0:NE], carry, ALU.add)
        S_.ts("dve", vv, pos, float(CAP), ALU.is_lt)
        S_.tt("dve", vv, vv, mk, ALU.mult)
        S_.tt("dve", pos, pos, erow, ALU.add)
        S_.ts("dve", nd, pos, -1.0, ALU.mult, BIGF, ALU.add)
        S_.tt("dve", nd, nd, vv, ALU.mult)
        S_.ts("dve", nd, nd, -BIGF, ALU.add)
        S_.op("dve", lambda g: g.max(_ap(mx), _ap(nd)), [nd], [mx])
        S_.ts("dve", idf, mx[:, 0:4], -1.0, ALU.mult, float(NE * CAP), ALU.min)
        S_.cp("dve", idx_all[:, gt, :], idf)
        for k in range(4):
            S_.ts("dve", sel, nd, mx[:, k:k + 1], ALU.is_equal)
            S_.tt("dve", sel, sel, Gall[:, gt, :], ALU.mult)
            S_.red(gk_all[:, gt, k:k + 1], sel, ALU.add)
        S_.ts("dve", idf, mx[:, 0:4], -0.5 * BIGF, ALU.is_gt)
        S_.tt("dve", gk_all[:, gt, :], gk_all[:, gt, :], idf, ALU.mult)
        for k in range(4):
            S_.idma_scatter(x_rows, idx_all[:, gt, k:k + 1], hb_tile, NE * CAP - 1)

    def stage_moe_sparse():
        S_.barrier()
        with ExitStack() as es6:
            A6 = Alloc(nc, es6)
            bguT = A6.sb([128, 16, NE], F32, "bguT")
            with ExitStack() as esb:
                Ab = Alloc(nc, esb)
                bgu_sb = Ab.sb([NE, 2 * D], F32, "bgu_sb")
                bgu_hi = Ab.sb([NE, 2 * D], BF16, "bgu_hi"); bgu_lo = Ab.sb([NE, 2 * D], BF16, "bgu_lo")
                S_.dma("sp", bgu_sb, b_gate_up)
                S_.cp("act", bgu_hi, bgu_sb)
                S_.tt("dve", bgu_lo, bgu_sb, bgu_hi, ALU.subtract)
                for fc in range(16):
                    ps = psum()
                    S_.mm(ps[:, 0:NE], bgu_hi[0:NE, fc * 128:(fc + 1) * 128], identb[0:NE, 0:NE], start=True, stop=False)
                    S_.mm(ps[:, 0:NE], bgu_lo[0:NE, fc * 128:(fc + 1) * 128], identb[0:NE, 0:NE], start=False, stop=True, inc=True)
                    S_.cp("act", bguT[:, fc, :], ps[:, 0:NE])
                S_.barrier()
            with ExitStack() as ese:
                Ae = Alloc(nc, ese)
                NS = CAP // 128
                xe = [Ae.sb([128, NS, D], BF16, "xe") for _ in range(2)]
                xT = [Ae.sb([128, 8, CAP], BF16, "xT") for _ in range(2)]
                wgu = [Ae.sb([128, 8, 2 * D], BF16, "wgu") for _ in range(2)]
                wd = [Ae.sb([128, 8, D], BF16, "wd") for _ in range(2)]
                actT = Ae.sb([128, 8, CAP], BF16, "actT")
                g1 = [Ae.sb([128, 512], F32, "g1") for _ in range(2)]
                s1 = [Ae.sb([128, 512], F32, "s1") for _ in range(2)]
                u1 = [Ae.sb([128, 512], F32, "u1") for _ in range(2)]
                yo = [Ae.sb([128, D], F32, "yo") for _ in range(2)]
                blocks = [(0, 512)] + ([(512, CAP - 512)] if CAP > 512 else [])
                issued = [0]

                wds = [Ae.sb([128, 2, D], F32, "wds") for _ in range(2)]

                def wd_src(e, j):
                    return w_down[e].re("(k p) c -> p k c", p=128)[:, 2 * j:2 * j + 2, :]

                def wd_dma(e, j):
                    S_.dma("sp", wds[j % 2], wd_src(e, j))

                def wd_cast(e, j):
                    S_.cp("act", wd[e % 2][:, 2 * j:2 * j + 2, :], wds[j % 2])

                def issue_weights(upto):
                    while issued[0] <= min(upto, NE - 1):
                        e = issued[0]; issued[0] += 1
                        S_.dma("pool", wgu[e % 2], w_gate_up[e].re("(k p) f -> p k f", p=128))
                        if e == 0:
                            for j in range(4):
                                wd_dma(0, j)
                                wd_cast(0, j)
                        else:
                            wd_dma(e, 0)
                            wd_dma(e, 1)

                def load_x(e):
                    S_.dma("sp", xe[e % 2], x_rows[e * CAP:(e + 1) * CAP, :].re("(n p) d -> p n d", p=128))
                    for n in range(NS):
                        ps = psum().bitcast(BF16)
                        for k in range(8):
                            S_.tr(ps[:, k * 128:(k + 1) * 128], xe[e % 2][:, n, k * 128:(k + 1) * 128], identb, inc=(k == 7))
                        S_.cp("act" if n % 2 else "dve", xT[e % 2][:, :, n * 128:(n + 1) * 128], ps.re("p (k t) -> p k t", k=8))
                it = 0
                load_x(0)
                for e in range(NE):
                    if e + 1 < NE:
                        load_x(e + 1)
                    xTe = xT[e % 2]
                    issue_weights(e + 1)
                    wge = wgu[e % 2]
                    for p in range(4):
                        if e + 1 < NE:
                            wd_cast(e + 1, p)
                            if p + 2 < 4:
                                wd_dma(e + 1, p + 2)
                        for (c0, cn) in blocks:
                            for f2 in range(2):
                                fc = 2 * p + f2
                                i = it % 2; it += 1
                                psg = psum(); psu = psum()
                                for k in range(8):
                                    S_.mm(psg[:, 0:cn], wge[:, k, fc * 128:(fc + 1) * 128], xTe[:, k, c0:c0 + cn],
                                          start=(k == 0), stop=(k == 7), inc=(k == 7))
                                for k in range(8):
                                    S_.mm(psu[:, 0:cn], wge[:, k, D + fc * 128:D + (fc + 1) * 128], xTe[:, k, c0:c0 + cn],
                                          start=(k == 0), stop=(k == 7), inc=(k == 7))
                                gg = g1[i][:, 0:cn]; sg_ = s1[i][:, 0:cn]; uu = u1[i][:, 0:cn]
                                S_.ts("dve", gg, psg[:, 0:cn], bguT[:, fc, e:e + 1], ALU.add, 7.0, ALU.min)
                                S_.act(sg_, gg, AF.Sigmoid, scale=1.702)
                                S_.act(uu, psu[:, 0:cn], AF.Identity, bias=bguT[:, 8 + fc, e:e + 1])
                                S_.ts("pool", uu, uu, 7.0, ALU.min, -7.0, ALU.max)
                                S_.tt("pool", gg, gg, sg_, ALU.mult)
                                S_.stt(actT[:, fc, c0:c0 + cn], uu, 1.0, gg, ALU.add, ALU.mult)
                    wde = wd[e % 2]
                    for n in range(NS):
                        yt_ = yo[n % 2]
                        for half in range(2):
                            ps = psum()
                            for fc in range(8):
                                S_.mm(ps, actT[:, fc, n * 128:(n + 1) * 128], wde[:, fc, half * 512:(half + 1) * 512],
                                      start=(fc == 0), stop=(fc == 7), inc=(fc == 7))
                            S_.cp("act" if half else "dve", yt_[:, half * 512:(half + 1) * 512], ps)
                        r0 = e * CAP + n * 128
                        S_.dma("sp", V(y_rows.ap[r0:r0 + 128, :], [Buf("yrow")]), yt_)
            S_.barrier()
            with ExitStack() as esc:
                Ac = Alloc(nc, esc)
                gam = Ac.sb([128, D], F32, "gam2"); bet = Ac.sb([128, D], F32, "bet2")
                bcast_load(gam, ln2_g); bcast_load(bet, ln2_b)
                bd_sb = Ac.sb([NE, D], F32, "bd_sb")
                S_.dma("sp", bd_sb, b_down)
                GT = Ac.sb([NE, 128], F32, "GT")
                Ghi = Ac.sb([128, NE], BF16, "Ghi"); Glo = Ac.sb([128, NE], BF16, "Glo")
                yk = [[Ac.sb([128, D], F32, "yk") for _ in range(4)] for _ in range(2)]
                h1t = [Ac.sb([128, D], F32, "h1t2") for _ in range(2)]
                zt = Ac.sb([128, D], F32, "zt2"); ot = Ac.sb([128, D], F32, "ot2")
                tmpc = [Ac.sb([128, D], F32, "tmpc") for _ in range(2)]
                tp = dict(st=Ac.sb([128, 2, 6], F32, "st"), mv=Ac.sb([128, 2], F32, "mv"), rs=Ac.sb([128, 1], F32, "rs"))

                def fetch(gt):
                    for k in range(4):
                        S_.idma_gather(yk[gt % 2][k], y_rows, idx_all[:, gt, k:k + 1], NE * CAP - 1)
                    S_.dma("sp", h1t[gt % 2], h1_d[gt // 8][(gt % 8) * 128:(gt % 8 + 1) * 128, :])
                fetch(0)
                for gt in range(NB * NT):
                    if gt + 1 < NB * NT:
                        fetch(gt + 1)
                    S_.cp("act", Ghi, Gall[:, gt, :])
                    S_.tt("dve", Glo, Gall[:, gt, :], Ghi, ALU.subtract)
                    ps = psum()
                    S_.mm(ps[0:NE, 0:128], Ghi, identb, start=True, stop=False)
                    S_.mm(ps[0:NE, 0:128], Glo, identb, start=False, stop=True, inc=True)
                    S_.cp("act", GT, ps[0:NE, 0:128])
                    for half in range(2):
                        ps = psum()
                        S_.mm(ps, GT[0:NE, :], bd_sb[0:NE, half * 512:(half + 1) * 512], inc=True)
                        S_.stt(zt[:, half * 512:(half + 1) * 512], h1t[gt % 2][:, half * 512:(half + 1) * 512], ALPHA, ps, ALU.mult, ALU.add)
                    for k in range(4):
                        S_.act(tmpc[k % 2], yk[gt % 2][k], AF.Copy, scale=gk_all[:, gt, k:k + 1])
                        S_.tt("dve", zt, zt, tmpc[k % 2], ALU.add)
                    layernorm(esc, zt, gam, bet, ot, tp, eng2="dve")
                    S_.dma("sp", out_d[gt * 128:(gt + 1) * 128, :], ot)

    msk = G.sb([128, 5 * 64 + 2], F32, "msk")
    S_.dma("sp", msk, c_masks)
    tri32 = G.sb([128, 2, 128], F32, "tri32")
    S_.dma("sp", tri32, c_tri.re("d p t -> p d t"))
    MS = [msk[:, 0:64], msk[:, 128:192]]
    MI = [msk[:, 64:128], msk[:, 192:256]]
    MID = msk[:, 256:320]
    BLK = msk[:, 320:322]

    def dbg_out(name, src, shape, dt):
        if name in debug:
            dbg[name] = V(nc.dram_tensor("dbg_" + name, list(shape), dt, kind="ExternalOutput").ap(), [Buf("dbg_" + name)])
            S_.dma("sp", dbg[name], src)

    for b in range(NB):
        with ExitStack() as esA:
            A = Alloc(nc, esA)
            hT = A.sb([128, 8, S], BF16, "hT")
            stage_s1(b, hT)
            S_.barrier()
            if b == 0:
                dbg_out("hT", hT, [128, 8, S], BF16)
            yT_r = A.sb([128, 3, S], BF16, "yT_r")
            if "rwkv" in stages:
                stage_rwkv(b, hT, yT_r)
                S_.barrier()
            yT_a = A.sb([128, 3, S], BF16, "yT_a")
            if "att" in stages:
                stage_att(b, hT, yT_a)
                S_.barrier()
            yT_m = A.sb([128, 2, S], BF16, "yT_m")
            if "mem" in stages:
                stage_mem(b, hT, yT_m)
                S_.barrier()
            if b == 0:
                dbg_out("yT_r", yT_r, [128, 3, S], BF16)
                dbg_out("yT_a", yT_a, [128, 3, S], BF16)
                dbg_out("yT_m", yT_m, [128, 2, S], BF16)
            if "out" in stages:
                stage_out(b, [yT_r[:, 0], yT_r[:, 1], yT_r[:, 2], yT_a[:, 0], yT_a[:, 1], yT_a[:, 2], yT_m[:, 0], yT_m[:, 1]])
                S_.barrier()
    if "h1" in debug:
        dbg["h1"] = V(nc.dram_tensor("dbg_h1", [1024, D], F32, kind="ExternalOutput").ap(), [Buf("dbg_h1")])
        S_.dma("sp", dbg["h1"], h1_d[0])
        dbg["G"] = V(nc.dram_tensor("dbg_G", [128, NB * NT, NE], F32, kind="ExternalOutput").ap(), [Buf("dbg_G")])
        S_.dma("sp", dbg["G"], Gall)

    if moe:
        if SPARSE:
            stage_moe_sparse()
        else:
            stage_moe()
    S_.barrier()
    es.close()
    return nc, S_


def host_constants():
    ident = np.eye(128, dtype=np.float32)
    slopes = np.exp2(-8.0 * np.arange(1, H + 1, dtype=np.float32) / H).astype(np.float32)
    x = np.arange(STRIP_W)[None, :]
    p = np.arange(128)[:, None]
    delta = x - p - STRIP_C
    ad = np.abs(delta)
    mult = ((ad <= 64).astype(np.float32) + ((delta % 4 == 0) & (ad <= 256)).astype(np.float32)
            + ((delta % 16 == 0) & (ad <= 1024)).astype(np.float32))
    strip = np.stack([mult * np.exp(-(slopes[h] * ad.astype(np.float32))) for h in range(H)]).astype(np.float32)
    masks = np.zeros((128, 5 * 64 + 2), np.float32)
    si = np.arange(64)[:, None]; ti = np.arange(64)[None, :]
    for q in range(2):
        r = slice(64 * q, 64 * q + 64)
        masks[r, 0:64] = (si < ti); masks[r, 64:128] = (si <= ti)
        masks[r, 128:192] = (si > ti); masks[r, 192:256] = (si >= ti)
        masks[r, 256:320] = (si == ti)
        masks[r, 320 + q] = 1.0
    tri = np.zeros((2, 128, 128), np.float32)
    for q in range(2):
        r = slice(64 * q, 64 * q + 64)
        tri[0, r, r] = (si <= ti); tri[1, r, r] = (si >= ti)
    utri = (np.arange(128)[:, None] < np.arange(128)[None, :]).astype(np.float32)
    erow = np.tile((np.arange(NE, dtype=np.float32) * CAP)[None, :], (128, 1))
    return dict(c_ident=ident, c_strip=strip, c_masks=masks, c_tri=tri, c_utri=utri, c_erow=erow)


def make_in_maps(inputs):
    consts = host_constants()
    maps = []
    sq = lambda a: np.ascontiguousarray(a)
    for i in range(8):
        m = dict(consts)
        m["x"] = sq(inputs["x"][2 * i:2 * i + 2].reshape(NB * S, D))
        m["mem"] = sq(inputs["mem"][2 * i:2 * i + 2].reshape(NB * 256, D))
        for k in ("ln_in_g", "ln_in_b"):
            m[k] = sq(inputs[k])
        m["w_in"] = sq(inputs["w_in"][0])
        m["mu_shift"] = sq(inputs["mu_shift"][0])
        m["w0"] = sq(inputs["w0"][0].reshape(-1)); m["w_up"] = sq(inputs["w_up"][0])
        m["a0"] = sq(inputs["a0"][0].reshape(-1)); m["a_up"] = sq(inputs["a_up"][0])
        m["g_up"] = sq(inputs["g_up"][0])
        for k in ("k_k", "k_a", "gn_g", "gn_b", "ln1_g", "ln1_b", "ln2_g", "ln2_b", "b_router",
                  "w_mem_kv", "w_out", "w_router", "w_gate_up", "b_gate_up", "w_down", "b_down"):
            m[k] = sq(inputs[k][0])
        m["r_k"] = sq(inputs["r_k"][0].reshape(-1))
        maps.append(m)
    return maps


def kernel(**inputs):
    inputs = {k: np.asarray(v) for k, v in inputs.items()}
    nc, _ = build_program()
    maps = make_in_maps(inputs)
    res = run_bass_kernel_spmd(nc, maps, core_ids=list(range(8)))
    out = np.stack([r["out"].reshape(NB, S, D) for r in res.results]).reshape(16, S, D)
    return out.astype(np.float32)
```

```python
import numpy as np
from contextlib import ExitStack
import concourse.bass as bass
import concourse.mybir as mybir
from concourse.bass_utils import run_bass_kernel_spmd

F32 = mybir.dt.float32
BF16 = mybir.dt.bfloat16
U32 = mybir.dt.uint32
AF = mybir.ActivationFunctionType
ALU = mybir.AluOpType
AX = mybir.AxisListType

D = 1024
S = 2048
NB = 2
NT = S // 128
C = 384
H = 6
RWKV_IN = 1344
IN_W = 2752
NE = 32
ALPHA = 2.0 ** 0.25
LN_EPS = 1e-5
GN_EPS = 64e-5
LDS = float(np.exp(-0.5))
STRIP_W = 3200
STRIP_C = 1536
CAP = 640
BIGF = 1.0e6
import os
SKIP = os.environ.get('KSKIP', '')
SPARSE = os.environ.get('KDENSE', '') == ''


class Buf:
    __slots__ = ("name", "w", "rs")

    def __init__(self, name):
        self.name = name
        self.w = None
        self.rs = []


class Ev:
    __slots__ = ("sem", "val", "eng")

    def __init__(self, eng):
        self.sem = None
        self.val = None
        self.eng = eng


class V:
    __slots__ = ("ap", "bufs")

    def __init__(self, ap, bufs):
        self.ap = ap
        self.bufs = bufs if isinstance(bufs, (list, tuple)) else [bufs]

    def __getitem__(self, k):
        return V(self.ap[k], self.bufs)

    def re(self, pat, **kw):
        return V(self.ap.rearrange(pat, **kw), self.bufs)

    def bc(self, shape):
        return V(self.ap.to_broadcast(list(shape)), self.bufs)

    def un(self, axis):
        return V(self.ap.unsqueeze(axis), self.bufs)

    def bitcast(self, dt):
        return V(self.ap.bitcast(dt), self.bufs)

    def on(self, bufs):
        return V(self.ap, bufs)


def _ap(x):
    return x.ap if isinstance(x, V) else x


class Sched:
    ENG = ("pe", "act", "dve", "pool", "sp")
    NQ = 16

    def __init__(self, nc, es):
        self.nc = nc
        self.es = es
        self.eng = dict(pe=nc.tensor, act=nc.scalar, dve=nc.vector, pool=nc.gpsimd, sp=nc.sync)
        self.sem = {k: es.enter_context(nc.semaphore("s_" + k)) for k in self.ENG}
        self.cnt = {k: 0 for k in self.ENG}
        self.dsem = {q: [es.enter_context(nc.semaphore("d_%s%d" % (q, i))) for i in range(self.NQ)]
                     for q in ("sp", "pool")}
        self.dcnt = {q: 0 for q in self.dsem}
        self.seen = {k: {} for k in self.ENG}
        self.pending = {k: [] for k in self.ENG}
        self.all_dma = []
        self.ninst = 0

    def _wait(self, e, ev):
        assert ev.val is not None, "dependency on an unresolved (non-inc) event"
        key = id(ev.sem)
        if self.seen[e].get(key, 0) >= ev.val:
            return
        self.eng[e].wait_ge(ev.sem, ev.val)
        self.seen[e][key] = ev.val

    def _deps(self, e, reads, writes, is_dma):
        for v in reads:
            for b in v.bufs:
                if b.w is not None:
                    if b.w.eng == e and e == "pe" and not is_dma:
                        continue
                    self._wait(e, b.w)
        for v in writes:
            for b in v.bufs:
                if b.w is not None and (is_dma or b.w.eng != e or e != "pe"):
                    self._wait(e, b.w)
                for r in b.rs:
                    if is_dma or r.eng != e or e != "pe":
                        self._wait(e, r)

    def _record(self, ev, reads, writes):
        for v in reads:
            for b in v.bufs:
                if ev.eng is not None:
                    b.rs = [r for r in b.rs if r.eng != ev.eng]
                b.rs.append(ev)
        for v in writes:
            for b in v.bufs:
                b.w = ev
                b.rs = []

    def op(self, e, fn, reads, writes, inc=True):
        reads = [r for r in reads if isinstance(r, V)]
        writes = [w for w in writes if isinstance(w, V)]
        self._deps(e, reads, writes, False)
        ins = fn(self.eng[e])
        self.ninst += 1
        ev = Ev(e)
        if inc:
            self.cnt[e] += 1
            ins.then_inc(self.sem[e], 1)
            ev.sem = self.sem[e]
            ev.val = self.cnt[e]
            for p in self.pending[e]:
                p.sem = ev.sem
                p.val = ev.val
            self.pending[e] = []
        else:
            self.pending[e].append(ev)
        self._record(ev, reads, writes)
        return ev

    def dma(self, q, out, in_, **kw):
        self._deps(q, [in_], [out], True)
        n = self.dcnt[q]
        self.dcnt[q] += 1
        sem = self.dsem[q][n % self.NQ]
        val = 16 * (n // self.NQ + 1)
        if n >= self.NQ:
            pe_ = Ev(None); pe_.sem = sem; pe_.val = val - 16
            self._wait(q, pe_)
        ins = self.eng[q].dma_start(out=_ap(out), in_=_ap(in_), **kw)
        ins.then_inc(sem, 16)
        self.ninst += 1
        ev = Ev(None)
        ev.sem = sem
        ev.val = val
        self._record(ev, [in_], [out])
        self.all_dma.append(ev)
        return ev

    def _idma(self, out, out_off, in_, in_off, reads, writes, bound):
        q = "pool"
        self._deps(q, reads, writes, True)
        n = self.dcnt[q]
        self.dcnt[q] += 1
        sem = self.dsem[q][n % self.NQ]
        val = 16 * (n // self.NQ + 1)
        if n >= self.NQ:
            pe_ = Ev(None); pe_.sem = sem; pe_.val = val - 16
            self._wait(q, pe_)
        ins = self.eng[q].indirect_dma_start(out=_ap(out), out_offset=out_off, in_=_ap(in_), in_offset=in_off)
        ins.then_inc(sem, 16)
        self.ninst += 1
        ev = Ev(None)
        ev.sem = sem
        ev.val = val
        self._record(ev, reads, writes)
        return ev

    def idma_scatter(self, dram, idx, src, bound):
        off = bass.IndirectOffsetOnAxis(ap=_ap(idx), axis=0)
        return self._idma(dram, off, src, None, [src, idx], [V(dram.ap, [Buf("scatter")])], bound)

    def idma_gather(self, dst, dram, idx, bound):
        off = bass.IndirectOffsetOnAxis(ap=_ap(idx), axis=0)
        return self._idma(dst, None, dram, off, [idx], [dst], bound)

    def barrier(self):
        for e in self.ENG:
            for o in self.ENG:
                if self.cnt[o] > 0:
                    assert not self.pending[o]
                    ev = Ev(o)
                    ev.sem = self.sem[o]
                    ev.val = self.cnt[o]
                    self._wait(e, ev)
            for q in self.dsem:
                n = self.dcnt[q]
                for i in range(min(n, self.NQ)):
                    ev = Ev(None)
                    ev.sem = self.dsem[q][i]
                    ev.val = 16 * ((n - 1 - i) // self.NQ + 1)
                    self._wait(e, ev)

    def _pe_rows(self, lhsT):
        a = _ap(lhsT)
        lo = a.base_partition()
        rows = (lo, lo + a.shape[0])
        prev = getattr(self, "_last_pe", None)
        if prev is not None:
            pins, pev, prow = prev
            if rows[0] >= prow[1] or prow[0] >= rows[1]:
                if pev.val is None:
                    self.cnt["pe"] += 1
                    pins.then_inc(self.sem["pe"], 1)
                    for p in self.pending["pe"]:
                        p.sem = self.sem["pe"]
                        p.val = self.cnt["pe"]
                    self.pending["pe"] = []
                self._wait("pe", pev)
        return rows

    def _pe_op(self, fn, lhsT, reads, writes, inc):
        rows = self._pe_rows(lhsT)
        box = []

        def f(e):
            i = fn(e)
            box.append(i)
            return i
        ev = self.op("pe", f, reads, writes, inc=inc)
        self._last_pe = (box[0], ev, rows)
        return ev

    def mm(self, out, lhsT, rhs, start=True, stop=True, inc=False):
        return self._pe_op(lambda e: e.matmul(_ap(out), _ap(lhsT), _ap(rhs), start=start, stop=stop),
                           lhsT, [lhsT, rhs], [out], inc)

    def tr(self, out, in_, ident, inc=False):
        return self._pe_op(lambda e: e.transpose(_ap(out), _ap(in_), _ap(ident)), in_, [in_, ident], [out], inc)

    def act(self, out, in_, func, bias=None, scale=None, accum_out=None):
        kw = {}
        if bias is not None:
            kw["bias"] = _ap(bias)
        if scale is not None:
            kw["scale"] = _ap(scale)
        if accum_out is not None:
            kw["accum_out"] = _ap(accum_out)
        return self.op("act", lambda e: e.activation(_ap(out), _ap(in_), func, **kw),
                       [in_, bias, scale], [out, accum_out])

    def ts(self, e, out, in0, s1, op0, s2=None, op1=None, accum_out=None):
        kw = {}
        if op1 is not None:
            kw["op1"] = op1
        if accum_out is not None:
            kw["accum_out"] = _ap(accum_out)
        return self.op(e, lambda g: g.tensor_scalar(_ap(out), _ap(in0), _ap(s1), _ap(s2), op0, **kw),
                       [in0, s1, s2], [out, accum_out])

    def tt(self, e, out, in0, in1, op):
        return self.op(e, lambda g: g.tensor_tensor(_ap(out), _ap(in0), _ap(in1), op), [in0, in1], [out])

    def stt(self, out, in0, scalar, in1, op0, op1):
        return self.op("dve", lambda g: g.scalar_tensor_tensor(_ap(out), _ap(in0), _ap(scalar), _ap(in1), op0, op1),
                       [in0, scalar, in1], [out])

    def cp(self, e, out, in_):
        if e == "act":
            return self.op("act", lambda g: g.copy(_ap(out), _ap(in_)), [in_], [out])
        return self.op(e, lambda g: g.tensor_copy(_ap(out), _ap(in_)), [in_], [out])

    def red(self, out, in_, op, axis=AX.X):
        return self.op("dve", lambda g: g.tensor_reduce(_ap(out), _ap(in_), axis, op), [in_], [out])

    def memset(self, e, out, val):
        return self.op(e, lambda g: g.memset(_ap(out), val), [], [out])


class Alloc:
    N = [0]

    def __init__(self, nc, es):
        self.nc = nc
        self.es = es

    def sb(self, shape, dt, name=None, nbuf=1):
        Alloc.N[0] += 1
        name = "%s_%d" % (name or "t", Alloc.N[0])
        t = self.es.enter_context(self.nc.sbuf_tensor(name, list(shape), dt))
        return V(t[:], [Buf(name)])

    def dram(self, name, shape, dt, kind="Internal"):
        t = self.nc.dram_tensor(name, list(shape), dt, kind=kind)
        return V(t.ap(), [Buf(name)])


def build_program(debug=(), moe=True, stages=('rwkv', 'att', 'mem', 'out')):
    nc = bass.Bass("TRN2", target_bir_lowering=False)
    es = ExitStack()
    S_ = Sched(nc, es)
    G = Alloc(nc, es)
    dbg = {}

    def din(name, shape, dt=F32):
        return V(nc.dram_tensor(name, list(shape), dt, kind="ExternalInput").ap(), [Buf(name)])

    x_d = din("x", [NB * S, D])
    mem_d = din("mem", [NB * 256, D])
    ln_in_g = din("ln_in_g", [D]); ln_in_b = din("ln_in_b", [D])
    w_in = din("w_in", [D, IN_W])
    mu_d = din("mu_shift", [RWKV_IN])
    w0_d = din("w0", [2 * C]); w_up_d = din("w_up", [2, 32, C])
    a0_d = din("a0", [2 * C]); a_up_d = din("a_up", [2, 32, C])
    g_up_d = din("g_up", [64, C])
    k_k_d = din("k_k", [C]); k_a_d = din("k_a", [C]); r_k_d = din("r_k", [C])
    gn_g_d = din("gn_g", [C]); gn_b_d = din("gn_b", [C])
    w_mem_kv = din("w_mem_kv", [D, 512])
    w_out = din("w_out", [D, D])
    ln1_g = din("ln1_g", [D]); ln1_b = din("ln1_b", [D])
    w_router = din("w_router", [D, NE]); b_router = din("b_router", [NE])
    if moe:
        w_gate_up = din("w_gate_up", [NE, D, 2 * D]); b_gate_up = din("b_gate_up", [NE, 2 * D])
        w_down = din("w_down", [NE, D, D]); b_down = din("b_down", [NE, D])
    ln2_g = din("ln2_g", [D]); ln2_b = din("ln2_b", [D])
    c_ident = din("c_ident", [128, 128])
    c_strip = din("c_strip", [H, 128, STRIP_W])
    c_masks = din("c_masks", [128, 5 * 64 + 2])
    c_tri = din("c_tri", [2, 128, 128])
    c_utri = din("c_utri", [128, 128])
    c_erow = din("c_erow", [128, NE])

    out_d = V(nc.dram_tensor("out", [NB * S, D], F32, kind="ExternalOutput").ap(), [Buf("out")])

    h0_d = [G.dram("h0_%d" % b, [S, D], F32) for b in range(NB)]
    h1_d = [G.dram("h1_%d" % g, [1024, D], F32) for g in range(4)]
    h1T_d = [G.dram("h1T_%d" % g, [128, 8, 1024], BF16) for g in range(4)]

    banks = []
    for i in range(8):
        t = es.enter_context(nc.psum_tensor("ps%d" % i, [128, 512], F32))
        banks.append(V(t[:], [Buf("ps%d" % i)]))
    bank_i = {"gen": 0, "acc": 0}
    bank_grp = {"gen": [2, 3, 4, 5, 6, 7], "acc": [0, 1]}

    def psum(grp="gen"):
        l = bank_grp[grp]
        b = banks[l[bank_i[grp] % len(l)]]
        bank_i[grp] += 1
        return b

    ident32 = G.sb([128, 128], F32, "ident32")
    identb = G.sb([128, 128], BF16, "identb")
    S_.dma("sp", ident32, c_ident)
    S_.cp("dve", identb, ident32)
    Gall = G.sb([128, NB * NT, NE], F32, "Gall")
    ones_b = G.sb([128, 128], BF16, "ones_b")
    S_.memset("dve", ones_b, 1.0)
    zeros_b = G.sb([128, 512], BF16, "zeros_b")
    S_.memset("dve", zeros_b, 0.0)
    carry = G.sb([128, NE], F32, "carry")
    S_.memset("dve", carry, 0.0)
    idx_all = G.sb([128, NB * NT, 4], U32, "idx_all")
    gk_all = G.sb([128, NB * NT, 4], F32, "gk_all")
    utri32 = G.sb([128, 128], F32, "utri32")
    S_.dma("sp", utri32, c_utri)
    utri_b = G.sb([128, 128], BF16, "utri_b")
    S_.cp("dve", utri_b, utri32)
    erow = G.sb([128, NE], F32, "erow")
    S_.dma("sp", erow, c_erow)
    x_rows = G.dram("x_rows", [NE * CAP + 128, D], BF16)
    y_rows = G.dram("y_rows", [NE * CAP + 128, D], F32)
    zeros32 = G.sb([128, D], F32, "zeros32")
    S_.memset("pool", zeros32, 0.0)
    S_.dma("sp", V(y_rows.ap[NE * CAP:NE * CAP + 128, :], [Buf("ydump")]), zeros32)
    zb = zeros32.bitcast(BF16).re("p (n d) -> p n d", n=2)
    xr = x_rows.re("(n p) d -> p n d", p=128)
    nrow = (NE * CAP + 128) // 128
    for n0 in range(0, nrow, 2):
        nn = min(2, nrow - n0)
        S_.dma("sp", V(xr.ap[:, n0:n0 + nn, :], [Buf("xfill")]), zb[:, 0:nn, :])

    def bcast_load(dst, src1d, q="sp"):
        S_.dma(q, dst, V(src1d.ap.partition_broadcast(128), src1d.bufs))

    def layernorm(es2, xin, gam, bet, out, tmp_pool, eng2="pool"):
        st = tmp_pool["st"]; mv = tmp_pool["mv"]; rs = tmp_pool["rs"]
        for i in range(2):
            S_.op("dve", lambda g, i=i: g.bn_stats(_ap(st[:, i, :]), _ap(xin[:, i * 512:(i + 1) * 512])),
                  [xin], [st])
        S_.op("dve", lambda g: g.bn_aggr(_ap(mv), _ap(st)), [st], [mv])
        S_.ts("dve", rs, mv[:, 1:2], LN_EPS, ALU.add)
        S_.act(rs, rs, AF.Sqrt)
        S_.op("dve", lambda g: g.reciprocal(_ap(rs), _ap(rs)), [rs], [rs])
        S_.ts("dve", out, xin, mv[:, 0:1], ALU.subtract, rs[:, 0:1], ALU.mult)
        S_.tt(eng2, out, out, gam, ALU.mult)
        S_.tt(eng2, out, out, bet, ALU.add)

    def stage_s1(b, hT):
        with ExitStack() as es1:
            A1 = Alloc(nc, es1)
            gam = A1.sb([128, D], F32, "gam"); bet = A1.sb([128, D], F32, "bet")
            bcast_load(gam, ln_in_g); bcast_load(bet, ln_in_b)
            xt = [A1.sb([128, D], F32, "xt") for _ in range(2)]
            ht = [A1.sb([128, D], F32, "ht") for _ in range(2)]
            hb = [A1.sb([128, D], BF16, "hb") for _ in range(2)]
            tp = [dict(st=A1.sb([128, 2, 6], F32, "st"), mv=A1.sb([128, 2], F32, "mv"),
                       rs=A1.sb([128, 1], F32, "rs")) for _ in range(2)]
            for t in range(NT):
                i = t % 2
                S_.dma("sp", xt[i], x_d[b * S + t * 128: b * S + (t + 1) * 128, :])
                layernorm(es1, xt[i], gam, bet, ht[i], tp[i])
                S_.dma("sp", h0_d[b][t * 128:(t + 1) * 128, :], ht[i])
                S_.cp("act", hb[i], ht[i])
                ps = psum().bitcast(BF16)
                for k in range(8):
                    S_.tr(ps[:, k * 128:(k + 1) * 128], hb[i][:, k * 128:(k + 1) * 128], identb, inc=(k == 7))
                S_.cp("act", hT[:, :, t * 128:(t + 1) * 128], ps.re("p (k t) -> p k t", k=8))

    def attn_core(nh, KTv, QTv, Vv, ytok, n_st, s_range, strips=None):
        with ExitStack() as esc:
            Ac = Alloc(nc, esc)
            Eb = [Ac.sb([128, 512], BF16, "E") for _ in range(3)]
            Pb = [Ac.sb([128, 512], BF16, "P") for _ in range(3)]
            rec = Ac.sb([128, 4], F32, "rec")
            it = 0
            for h in range(nh):
                c, p0 = h // 2, 64 * (h % 2)
                for tq in range(4):
                    s_lo, s_hi, jvalid = s_range(tq)
                    acc = psum("acc")
                    accv = acc[:, 0:260].re("p (j d) -> p j d", j=4)
                    S_.mm(acc[:, 0:260], zeros_b[:, 0:128], zeros_b[:, 0:260], start=True, stop=False)
                    sts = list(range(s_lo, s_hi + 1))

                    def score(st_):
                        sc = psum()
                        S_.mm(sc, KTv[p0:p0 + 64, c, st_ * 128:(st_ + 1) * 128],
                              QTv[p0:p0 + 64, c, tq * 512:(tq + 1) * 512], inc=True)
                        return sc
                    sc_next = score(sts[0])
                    for n_, st_ in enumerate(sts):
                        sc = sc_next
                        if n_ + 1 < len(sts):
                            sc_next = score(sts[n_ + 1])
                        E = Eb[it % 3]; P = Pb[it % 3]; it += 1
                        S_.act(E, sc, AF.Exp, scale=0.125)
                        if strips is not None:
                            off = tq * 512 - st_ * 128 + STRIP_C
                            S_.tt("dve" if it % 3 else "pool", P, E, strips[:, h, off:off + 512], ALU.mult)
                        else:
                            P = E
                        for j in range(4):
                            ok, last = jvalid(tq, j, st_)
                            if not ok:
                                continue
                            S_.mm(accv[:, j, :], P[:, j * 128:(j + 1) * 128], Vv[:, st_, h, :],
                                  start=False, stop=last, inc=True)
                    S_.op("dve", lambda g: g.reciprocal(_ap(rec), _ap(accv[:, :, 64])), [accv], [rec])
                    for j in range(4):
                        S_.ts("dve", ytok[:, 4 * tq + j, h * 64:(h + 1) * 64], accv[:, j, 0:64], rec[:, j:j + 1], ALU.mult)

    def proj_fm(dst, Wv, hT, nchunk, col0):
        n = 0
        for c in range(nchunk):
            for tg in range(4):
                ps = psum()
                for k in range(8):
                    S_.mm(ps, Wv[:, k, col0 + c * 128: col0 + (c + 1) * 128],
                          hT[:, k, tg * 512:(tg + 1) * 512], start=(k == 0), stop=(k == 7), inc=(k == 7))
                S_.cp("act" if n % 2 else "dve", dst[:, c, tg * 512:(tg + 1) * 512], ps)
                n += 1

    def ytok_to_yT(ytok, yT, nchunk):
        for t in range(NT):
            ps = psum().bitcast(BF16)
            for cc in range(nchunk):
                S_.tr(ps[:, cc * 128:(cc + 1) * 128], ytok[:, t, cc * 128:(cc + 1) * 128], identb, inc=(cc == nchunk - 1))
            S_.cp("act" if t % 2 else "dve", yT[:, 0:nchunk, t * 128:(t + 1) * 128],
                  ps[:, 0:nchunk * 128].re("p (k t) -> p k t", k=nchunk))

    def stage_att(b, hT, yT_a):
        with ExitStack() as es2:
            A2 = Alloc(nc, es2)
            Wqkv = A2.sb([128, 8, 3 * C], BF16, "Wqkv")
            S_.dma("pool", Wqkv, w_in[:, RWKV_IN:RWKV_IN + 3 * C].re("(k p) c -> p k c", p=128))
            strips = A2.sb([128, H, STRIP_W], BF16, "strips")
            for h in range(H):
                S_.dma("pool", strips[:, h, :], c_strip[h])
            QT = A2.sb([128, 3, S], BF16, "QT")
            KT = A2.sb([128, 3, S], BF16, "KT")
            Vt = A2.sb([128, NT, H, 65], BF16, "Vt")
            ytok = A2.sb([128, NT, C], BF16, "ytok")
            S_.memset("pool", Vt[:, :, :, 64:65], 1.0)
            proj_fm(QT, Wqkv, hT, 3, 0)
            proj_fm(KT, Wqkv, hT, 3, C)
            for t in range(NT):
                ps = psum()
                for k in range(8):
                    S_.mm(ps[:, 0:C], hT[:, k, t * 128:(t + 1) * 128], Wqkv[:, k, 2 * C:3 * C],
                          start=(k == 0), stop=(k == 7), inc=(k == 7))
                S_.cp("act" if t % 2 else "dve", Vt[:, t, :, 0:64], ps[:, 0:C].re("p (h d) -> p h d", h=H))

            def s_range(tq):
                s_hi = min(NT - 1, 4 * tq + 3 + 8)

                def jvalid(tq, j, st_):
                    tt_ = 4 * tq + j
                    return abs(tt_ - st_) <= 8, (st_ == s_hi and j == 3)
                return max(0, 4 * tq - 8), s_hi, jvalid
            attn_core(H, KT, QT, Vt, ytok, NT, s_range, strips)
            ytok_to_yT(ytok, yT_a, 3)

    def stage_mem(b, hT, yT_m):
        with ExitStack() as es3:
            A3 = Alloc(nc, es3)
            Wkv = A3.sb([128, 8, 512], BF16, "Wkv")
            S_.dma("pool", Wkv, w_mem_kv.re("(k p) c -> p k c", p=128))
            Wqm = A3.sb([128, 8, 256], BF16, "Wqm")
            S_.dma("pool", Wqm, w_in[:, RWKV_IN + 3 * C:IN_W].re("(k p) c -> p k c", p=128))
            memb = A3.sb([128, 2, D], BF16, "memb")
            S_.dma("pool", memb, mem_d[b * 256:(b + 1) * 256, :].re("(m p) d -> p m d", p=128))
            memT = A3.sb([128, 8, 256], BF16, "memT")
            for m in range(2):
                ps = psum().bitcast(BF16)
                for k in range(8):
                    S_.tr(ps[:, k * 128:(k + 1) * 128], memb[:, m, k * 128:(k + 1) * 128], identb, inc=(k == 7))
                S_.cp("act", memT[:, :, m * 128:(m + 1) * 128], ps.re("p (k t) -> p k t", k=8))
            KmT = A3.sb([128, 2, 256], BF16, "KmT")
            for c in range(2):
                ps = psum()
                for k in range(8):
                    S_.mm(ps[:, 0:256], Wkv[:, k, c * 128:(c + 1) * 128], memT[:, k, :],
                          start=(k == 0), stop=(k == 7), inc=(k == 7))
                S_.cp("dve", KmT[:, c, :], ps[:, 0:256])
            Vm = A3.sb([128, 2, 4, 65], BF16, "Vm")
            S_.memset("pool", Vm[:, :, :, 64:65], 1.0)
            for m in range(2):
                ps = psum()
                for k in range(8):
                    S_.mm(ps[:, 0:256], memT[:, k, m * 128:(m + 1) * 128], Wkv[:, k, 256:512],
                          start=(k == 0), stop=(k == 7), inc=(k == 7))
                S_.cp("dve", Vm[:, m, :, 0:64], ps[:, 0:256].re("p (h d) -> p h d", h=4))
            QmT = A3.sb([128, 2, S], BF16, "QmT")
            proj_fm(QmT, Wqm, hT, 2, 0)
            ytok = A3.sb([128, NT, 256], BF16, "ytokm")

            def s_range(tq):
                return 0, 1, (lambda tq, j, st_: (True, st_ == 1 and j == 3))
            attn_core(4, KmT, QmT, Vm, ytok, 2, s_range, None)
            ytok_to_yT(ytok, yT_m, 2)

    def stage_rwkv(b, hT, yT_r):
        with ExitStack() as es4:
            A4 = Alloc(nc, es4)
            f32t = lambda name, n=C: A4.sb([128, n], F32, name)
            Wr = A4.sb([128, 16, RWKV_IN], BF16, "Wr")
            with ExitStack() as esw:
                Aw = Alloc(nc, esw)
                mub = Aw.sb([128, RWKV_IN], F32, "mub"); bcast_load(mub, mu_d)
                omb = Aw.sb([128, RWKV_IN], F32, "omb"); hmb = Aw.sb([128, RWKV_IN], F32, "hmb")
                S_.ts("dve", omb, mub, -1.0, ALU.mult, 1.0, ALU.add)
                S_.ts("pool", hmb, mub, 0.5, ALU.mult)
                stg = [Aw.sb([128, 8, 336], F32, "stg") for _ in range(2)]
                for pc in range(4):
                    cs = slice(pc * 336, (pc + 1) * 336)
                    S_.dma("sp", stg[pc % 2], w_in[:, cs].re("(k p) c -> p k c", p=128))
                    S_.tt("dve", Wr[:, 0:8, cs], stg[pc % 2], omb[:, cs].un(1).bc([128, 8, 336]), ALU.mult)
                    S_.tt("pool", Wr[:, 8:16, cs], stg[pc % 2], hmb[:, cs].un(1).bc([128, 8, 336]), ALU.mult)
                S_.barrier()
            kkb = f32t("kkb"); bcast_load(kkb, k_k_d)
            kab = f32t("kab"); bcast_load(kab, k_a_d)
            omkab = f32t("omkab"); S_.ts("dve", omkab, kab, -1.0, ALU.mult, 1.0, ALU.add)
            rkb = f32t("rkb"); bcast_load(rkb, r_k_d)
            gngb = f32t("gngb"); bcast_load(gngb, gn_g_d)
            gnbb = f32t("gnbb"); bcast_load(gnbb, gn_b_d)
            w0b = f32t("w0b", 2 * C); bcast_load(w0b, w0_d)
            a0b = f32t("a0b", 2 * C); bcast_load(a0b, a0_d)
            gup = A4.sb([128, C], BF16, "gup"); S_.dma("pool", gup[0:64, :], g_up_d)
            wupbd = A4.sb([128, 2 * C], BF16, "wupbd"); S_.memset("pool", wupbd, 0.0)
            S_.dma("pool", wupbd[64:96, 0:C], w_up_d[0]); S_.dma("pool", wupbd[96:128, C:2 * C], w_up_d[1])
            aupbd = A4.sb([128, 2 * C], BF16, "aupbd"); S_.memset("pool", aupbd, 0.0)
            S_.dma("pool", aupbd[0:32, 0:C], a_up_d[0]); S_.dma("pool", aupbd[32:64, C:2 * C], a_up_d[1])
            Yacc = A4.sb([128, NT, C], F32, "Yacc")
            T32 = A4.sb([128, 3, 64], F32, "T32"); Tb = A4.sb([128, 3, 64], BF16, "Tb"); Ttmp = A4.sb([128, 3, 64], F32, "Ttmp")
            hs = A4.sb([128, 8, 128], BF16, "hs")
            r32 = f32t("r32"); k32 = f32t("k32"); v32 = f32t("v32")
            sgw = f32t("sgw"); a32 = f32t("a32"); a32b = f32t("a32b")
            ecw = f32t("ecw"); encw = f32t("encw")
            kk = f32t("kk"); sq = f32t("sq"); kd = f32t("kd"); bb = f32t("bb")
            ss = A4.sb([128, 6], F32, "ss"); rn = A4.sb([128, 6], F32, "rn")
            lor = A4.sb([128, 3, 128], BF16, "lor")
            tok4 = A4.sb([128, 4, C], BF16, "tok4")
            Pp = [A4.sb([128, 6, 64], BF16, "Pp") for _ in range(2)]
            PTp = [A4.sb([128, 6, 64], BF16, "PTp") for _ in range(2)]
            Zs = A4.sb([128, C], BF16, "Zs"); Us = A4.sb([128, C], BF16, "Us")
            fs1 = A4.sb([128, 6], F32, "fs1"); fs2 = A4.sb([128, 6], F32, "fs2"); fs3 = A4.sb([128, 6], F32, "fs3")
            yob = A4.sb([128, C], BF16, "yob")
            sets = []
            for i in range(2):
                sets.append(dict(
                    FM=A4.sb([128, 4, 3, 128], BF16, "FM"), Vb=A4.sb([128, C], BF16, "Vb"),
                    XT=[A4.sb([128, 6, 64], BF16, "XT") for _ in range(2)],
                    LkT=A4.sb([128, 6, 64], BF16, "LkT"), RbT=A4.sb([128, 6, 64], BF16, "RbT"),
                    RkT=A4.sb([128, 6, 64], BF16, "RkT"), WCe=A4.sb([128, 3, 2], F32, "WCe"),
                    Bt=A4.sb([128, C], BF16, "Bt"), Kt=A4.sb([128, C], BF16, "Kt"),
                    g32=f32t("g32"), bv=f32t("bv")))

            def v6(x):
                return x.re("p (h j) -> p h j", h=6)
            HORD = (0, 2, 4, 1, 3, 5)

            def prep(t, d, st):
                lo = t * 128
                if 0 < t < NT - 1:
                    S_.tt("pool", hs, hT[:, :, lo - 1:lo + 127], hT[:, :, lo + 1:lo + 129], ALU.add)
                elif t == 0:
                    S_.tt("pool", hs[:, :, 1:128], hT[:, :, 0:127], hT[:, :, 2:129], ALU.add)
                    S_.cp("pool", hs[:, :, 0:1], hT[:, :, 1:2])
                else:
                    S_.tt("pool", hs[:, :, 0:127], hT[:, :, lo - 1:lo + 126], hT[:, :, lo + 1:lo + 128], ALU.add)
                    S_.cp("pool", hs[:, :, 127:128], hT[:, :, lo + 126:lo + 127])

                def lhs(kc):
                    return hT[:, kc, lo:lo + 128] if kc < 8 else hs[:, kc - 8, :]
                for part, dst in ((0, r32), (1, k32), (2, v32)):
                    ps = psum()
                    for kc in range(16):
                        S_.mm(ps[:, 0:C], lhs(kc), Wr[:, kc, part * C:(part + 1) * C], start=(kc == 0), stop=(kc == 15), inc=(kc == 15))
                    S_.cp("act", dst, ps[:, 0:C])
                S_.cp("pool", st["Vb"], v32)
                yield
                ps = psum()
                for kc in range(16):
                    S_.mm(ps[:, 0:128], Wr[:, kc, 3 * C:3 * C + 128], lhs(kc), start=(kc == 0), stop=(kc == 15), inc=(kc == 15))
                for kc in range(16):
                    S_.mm(ps[0:64, 128:256], Wr[:, kc, 3 * C + 128:3 * C + 192], lhs(kc), start=(kc == 0), stop=(kc == 15), inc=(kc == 15))
                S_.act(lor[0:64, 0, :], ps[0:64, 0:128], AF.Sigmoid)
                S_.act(lor[64:128, 1, :], ps[64:128, 0:128], AF.Tanh)
                S_.cp("act", lor[0:64, 2, :], ps[0:64, 128:256])
                yield
                psg = psum()
                S_.mm(psg[:, 0:C], lor[0:64, 0, :], gup[0:64, :], inc=True)
                S_.cp("act", st["g32"], psg[:, 0:C])
                psw = psum()
                S_.mm(psw[:, 0:C], lor[64:128, 1, :], wupbd[64:128, d * C:(d + 1) * C], inc=True)
                S_.tt("dve", sgw, psw[:, 0:C], w0b[:, d * C:(d + 1) * C], ALU.add)
                S_.act(sgw, sgw, AF.Sigmoid)
                psa = psum()
                S_.mm(psa[:, 0:C], lor[0:64, 2, :], aupbd[0:64, d * C:(d + 1) * C], inc=True)
                S_.tt("dve", a32, psa[:, 0:C], a0b[:, d * C:(d + 1) * C], ALU.add)
                S_.act(a32, a32, AF.Sigmoid)
                pscs = psum()
                S_.mm(pscs[:, 0:C], tri32[:, d, :], sgw, inc=True)
                pswc = psum()
                for c in range(3):
                    S_.mm(pswc[:, c * 2:(c + 1) * 2], sgw[:, c * 128:(c + 1) * 128], BLK, inc=(c == 2))
                S_.act(st["WCe"].re("p c q -> p (c q)"), pswc[:, 0:6], AF.Exp, scale=-LDS)
                S_.act(ecw, pscs[:, 0:C], AF.Exp, scale=-LDS)
                S_.act(encw, pscs[:, 0:C], AF.Exp, scale=LDS)
                S_.act(sgw, sgw, AF.Exp, scale=LDS)
                S_.tt("dve", sgw, sgw, ecw, ALU.mult)
                yield
                S_.tt("pool", kk, k32, kkb, ALU.mult)
                S_.tt("pool", sq, kk, kk, ALU.mult)
                S_.red(ss, v6(sq), ALU.add)
                S_.act(rn, ss, AF.Sqrt)
                S_.ts("dve", rn, rn, 1e-12, ALU.max)
                S_.op("dve", lambda g: g.reciprocal(_ap(rn), _ap(rn)), [rn], [rn])
                S_.tt("dve", v6(kk), v6(kk), rn.un(2).bc([128, 6, 64]), ALU.mult)
                S_.tt("pool", kd, a32, kab, ALU.mult)
                S_.tt("pool", kd, kd, omkab, ALU.add)
                S_.tt("pool", kd, kd, k32, ALU.mult)
                S_.tt("dve", bb, kk, a32, ALU.mult)
                S_.tt("dve", tok4[:, 0, :], kk, sgw, ALU.mult)
                S_.stt(tok4[:, 1, :], bb, -1.0, encw, ALU.mult, ALU.mult)
                S_.tt("pool", tok4[:, 2, :], kd, encw, ALU.mult)
                S_.tt("pool", tok4[:, 3, :], r32, ecw, ALU.mult)
                S_.cp("act", st["Bt"], tok4[:, 1, :])
                S_.cp("act", st["Kt"], tok4[:, 2, :])
                if d == 1:
                    psa2 = psum()
                    S_.mm(psa2[:, 0:C], lor[0:64, 2, :], aupbd[0:64, 0:C], inc=True)
                    S_.tt("dve", a32b, psa2[:, 0:C], a0b[:, 0:C], ALU.add)
                    S_.act(a32b, a32b, AF.Sigmoid)
                    S_.tt("pool", a32b, a32b, a32, ALU.add)
                    S_.tt("pool", a32b, a32b, kab, ALU.mult)
                    S_.stt(a32b, omkab, 2.0, a32b, ALU.mult, ALU.add)
                    S_.tt("pool", a32b, a32b, k32, ALU.mult)
                    S_.tt("pool", a32b, a32b, rkb, ALU.mult)
                    S_.tt("pool", a32b, a32b, r32, ALU.mult)
                    S_.red(fs3, v6(a32b), ALU.add)
                    S_.tt("dve", v6(st["bv"]), v6(v32), fs3.un(2).bc([128, 6, 64]), ALU.mult)
                yield
                FM = st["FM"]
                for half in range(2):
                    ps = psum().bitcast(BF16)
                    n = 0
                    for o in (2 * half, 2 * half + 1):
                        for c in range(3):
                            S_.tr(ps[:, n * 128:(n + 1) * 128], tok4[:, o, c * 128:(c + 1) * 128], identb, inc=(n == 5))
                            n += 1
                    S_.cp("act" if half else "dve", FM[:, 2 * half:2 * half + 2, :, :].re("p o c t -> p (o c) t"),
                          ps[:, 0:768].re("p (n t) -> p n t", n=6))
                KKi, Bi, Ki, Ri = 0, 1, 2, 3
                st["XTf"] = st["XT"][1]
                yield

                def cc_mm(ps, li, ri):
                    n = 0
                    for q in range(2):
                        q0 = 64 * q
                        for h in HORD:
                            c, p0 = h // 2, 64 * (h % 2)
                            n += 1
                            S_.mm(ps[q0:q0 + 64, h * 64:(h + 1) * 64], FM[p0:p0 + 64, li, c, q0:q0 + 64],
                                  FM[p0:p0 + 64, ri, c, q0:q0 + 64], inc=(n == 12))

                def mbc(m):
                    return m.un(1).bc([128, 6, 64])
                XT = st["XT"]
                ps = psum(); cc_mm(ps, Bi, KKi)
                S_.tt("dve", PTp[0], v6(ps[:, 0:C]), mbc(MS[d]), ALU.mult)
                S_.tt("pool", XT[0], PTp[0], mbc(MID), ALU.add)
                yield
                ps = psum(); cc_mm(ps, KKi, Bi)
                S_.tt("dve", Pp[0], v6(ps[:, 0:C]), mbc(MS[1 - d]), ALU.mult)
                yield
                ps = psum(); cc_mm(ps, Ki, KKi)
                S_.tt("dve", st["LkT"], v6(ps[:, 0:C]), mbc(MS[d]), ALU.mult)
                yield
                ps = psum(); cc_mm(ps, Bi, Ri)
                S_.tt("dve", st["RbT"], v6(ps[:, 0:C]), mbc(MI[d]), ALU.mult)
                yield
                ps = psum(); cc_mm(ps, Ki, Ri)
                S_.tt("dve", st["RkT"], v6(ps[:, 0:C]), mbc(MI[d]), ALU.mult)
                cur = 0
                for lvl in range(1, 6):
                    nxt = 1 - cur

                    def sq_mm(ps, lt, rt):
                        n = 0
                        for q in range(2):
                            q0 = 64 * q
                            for h in range(H):
                                n += 1
                                S_.mm(ps[q0:q0 + 64, h * 64:(h + 1) * 64], lt[q0:q0 + 64, h, :], rt[q0:q0 + 64, h, :], inc=(n == 12))
                    yield
                    psP = psum(); sq_mm(psP, PTp[cur], Pp[cur])
                    if lvl < 5:
                        psPT = psum(); sq_mm(psPT, Pp[cur], PTp[cur])
                    S_.cp("act", Pp[nxt], v6(psP[:, 0:C]))
                    if lvl < 5:
                        S_.cp("dve", PTp[nxt], v6(psPT[:, 0:C]))
                    yield
                    psX = psum(); sq_mm(psX, Pp[nxt], XT[(lvl - 1) % 2])
                    S_.tt("dve", XT[lvl % 2], v6(psX[:, 0:C]), XT[(lvl - 1) % 2], ALU.add)
                    cur = nxt
                st["XTf"] = XT[5 % 2]

            def seq_chunk(t, q, d, st):
                q0 = 64 * q
                FM = st["FM"]; Vb = st["Vb"]; XT = st["XTf"]
                hc = lambda h: slice(h * 64, (h + 1) * 64)
                psZ = psum()
                S_.mm(psZ[q0:q0 + 64, 0:C], zeros_b[:, 0:64], zeros_b[:, 0:C], start=True, stop=False)
                for h in HORD:
                    c, p0 = h // 2, 64 * (h % 2)
                    S_.mm(psZ[q0:q0 + 64, hc(h)], FM[p0:p0 + 64, 0, c, q0:q0 + 64], Tb[p0:p0 + 64, c, :], start=False, stop=False)
                for h in range(H):
                    S_.mm(psZ[q0:q0 + 64, hc(h)], st["LkT"][q0:q0 + 64, h, :], Vb[q0:q0 + 64, hc(h)], start=False, stop=(h == H - 1), inc=(h == H - 1))
                S_.cp("act", Zs[q0:q0 + 64, :], psZ[q0:q0 + 64, 0:C])
                yield
                psU = psum()
                for h in range(H):
                    S_.mm(psU[q0:q0 + 64, hc(h)], XT[q0:q0 + 64, h, :], Zs[q0:q0 + 64, hc(h)], inc=(h == H - 1))
                S_.cp("dve", Us[q0:q0 + 64, :], psU[q0:q0 + 64, 0:C])
                yield
                psY = psum()
                S_.mm(psY[q0:q0 + 64, 0:C], zeros_b[:, 0:64], zeros_b[:, 0:C], start=True, stop=False)
                for h in HORD:
                    c, p0 = h // 2, 64 * (h % 2)
                    S_.mm(psY[q0:q0 + 64, hc(h)], FM[p0:p0 + 64, 3, c, q0:q0 + 64], Tb[p0:p0 + 64, c, :], start=False, stop=False)
                for h in range(H):
                    S_.mm(psY[q0:q0 + 64, hc(h)], st["RbT"][q0:q0 + 64, h, :], Us[q0:q0 + 64, hc(h)], start=False, stop=False)
                for h in range(H):
                    S_.mm(psY[q0:q0 + 64, hc(h)], st["RkT"][q0:q0 + 64, h, :], Vb[q0:q0 + 64, hc(h)], start=False, stop=(h == H - 1), inc=(h == H - 1))
                psT = psum()
                for h in range(H):
                    c, p0 = h // 2, 64 * (h % 2)
                    S_.mm(psT[p0:p0 + 64, c * 64:(c + 1) * 64], st["Bt"][q0:q0 + 64, hc(h)], Us[q0:q0 + 64, hc(h)], start=True, stop=False)
                    S_.mm(psT[p0:p0 + 64, c * 64:(c + 1) * 64], st["Kt"][q0:q0 + 64, hc(h)], Vb[q0:q0 + 64, hc(h)], start=False, stop=True, inc=(h == H - 1))
                if d == 0:
                    S_.cp("act", Yacc[q0:q0 + 64, t, :], psY[q0:q0 + 64, 0:C])
                else:
                    S_.tt("pool" if False else "dve", Yacc[q0:q0 + 64, t, :], psY[q0:q0 + 64, 0:C], Yacc[q0:q0 + 64, t, :], ALU.add)
                S_.tt("dve", Ttmp, psT[:, 0:192].re("p (c i) -> p c i", c=3), T32, ALU.add)
                S_.tt("dve", T32, Ttmp, st["WCe"][:, :, q:q + 1].bc([128, 3, 64]), ALU.mult)
                S_.cp("act", Tb, T32)
                yield

            def finalize(t, st):
                Y = Yacc[:, t, :]
                S_.red(fs1, v6(Y), ALU.add)
                S_.tt("pool", sq, Y, Y, ALU.mult)
                S_.red(fs2, v6(sq), ALU.add)
                S_.ts("dve", fs1, fs1, 1.0 / 64, ALU.mult)
                S_.tt("dve", fs3, fs1, fs1, ALU.mult)
                S_.stt(fs2, fs2, 1.0 / 64, fs3, ALU.mult, ALU.subtract)
                S_.ts("dve", fs2, fs2, GN_EPS, ALU.add)
                S_.act(fs2, fs2, AF.Sqrt)
                S_.op("dve", lambda g: g.reciprocal(_ap(fs2), _ap(fs2)), [fs2], [fs2])
                S_.tt("dve", v6(sq), v6(Y), fs1.un(2).bc([128, 6, 64]), ALU.subtract)
                S_.tt("dve", v6(sq), v6(sq), fs2.un(2).bc([128, 6, 64]), ALU.mult)
                S_.tt("pool", sq, sq, gngb, ALU.mult)
                S_.tt("pool", sq, sq, gnbb, ALU.add)
                S_.tt("pool", sq, sq, st["bv"], ALU.add)
                S_.tt("pool", yob, sq, st["g32"], ALU.mult)
                ps = psum().bitcast(BF16)
                for cc in range(3):
                    S_.tr(ps[:, cc * 128:(cc + 1) * 128], yob[:, cc * 128:(cc + 1) * 128], identb, inc=(cc == 2))
                S_.cp("act", yT_r[:, 0:3, t * 128:(t + 1) * 128], ps[:, 0:384].re("p (k t) -> p k t", k=3))

            def seqfin(t, d, st):
                for q in ((0, 1) if d == 0 else (1, 0)):
                    yield from seq_chunk(t, q, d, st)
                if d == 1:
                    finalize(t, st)
                yield

            def run_interleaved(gens):
                gens = list(gens)
                while gens:
                    for g_ in list(gens):
                        try:
                            next(g_)
                        except StopIteration:
                            gens.remove(g_)

            for d in (0, 1):
                S_.memset("dve", T32, 0.0)
                S_.memset("pool", Tb, 0.0)
                order = list(range(NT)) if d == 0 else list(range(NT - 1, -1, -1))
                run_interleaved([prep(order[0], d, sets[0])])
                for i, t in enumerate(order):
                    st = sets[i % 2]
                    gens = [seqfin(t, d, st)]
                    if i + 1 < NT:
                        gens.append(prep(order[i + 1], d, sets[(i + 1) % 2]))
                    run_interleaved(gens)

    def stage_out(b, yTs):
        with ExitStack() as es5:
            A5 = Alloc(nc, es5)
            Wo = A5.sb([128, 8, D], BF16, "Wo")
            S_.dma("pool", Wo, w_out.re("(k p) c -> p k c", p=128))
            gam = A5.sb([128, D], F32, "gam1"); bet = A5.sb([128, D], F32, "bet1")
            bcast_load(gam, ln1_g); bcast_load(bet, ln1_b)
            Wr32 = A5.sb([128, 8, NE], F32, "Wr32")
            S_.dma("sp", Wr32, w_router.re("(k p) e -> p k e", p=128))
            brb = A5.sb([128, NE], F32, "brb"); bcast_load(brb, b_router)
            h0t = [A5.sb([128, D], F32, "h0t") for _ in range(2)]
            zt = [A5.sb([128, D], F32, "zt") for _ in range(2)]
            h1t = [A5.sb([128, D], F32, "h1t") for _ in range(2)]
            h1T32 = [A5.sb([128, 8, 128], F32, "h1T32") for _ in range(2)]
            h1Tb = [A5.sb([128, 8, 128], BF16, "h1Tb") for _ in range(2)]
            hhi = [A5.sb([128, D], BF16, "hhi") for _ in range(2)]
            hlo = [A5.sb([128, D], BF16, "hlo") for _ in range(2)]
            tp = [dict(st=A5.sb([128, 2, 6], F32, "st"), mv=A5.sb([128, 2], F32, "mv"),
                       rs=A5.sb([128, 1], F32, "rs")) for _ in range(2)]
            lg = A5.sb([128, NE], F32, "lg"); mx8 = A5.sb([128, 8], F32, "mx8"); nmx = A5.sb([128, 1], F32, "nmx")
            ex = A5.sb([128, NE], F32, "ex"); mk = A5.sb([128, NE], F32, "mk"); sm = A5.sb([128, 1], F32, "sm")
            rt = dict(mkb=A5.sb([128, NE], BF16, "mkb"), pos=A5.sb([128, NE], F32, "pos"), vv=A5.sb([128, NE], F32, "vv"),
                      nd=A5.sb([128, NE], F32, "nd"), mx=A5.sb([128, 8], F32, "mxr"), sel=A5.sb([128, NE], F32, "sel"),
                      idf=A5.sb([128, 4], F32, "idf"))
            for t in range(NT):
                i = t % 2
                gt = b * NT + t
                grp, tin = gt // 8, gt % 8
                S_.dma("sp", h0t[i], h0_d[b][t * 128:(t + 1) * 128, :])
                for half in range(2):
                    ps = psum()
                    for k in range(8):
                        S_.mm(ps, yTs[k][:, t * 128:(t + 1) * 128], Wo[:, k, half * 512:(half + 1) * 512],
                              start=(k == 0), stop=(k == 7), inc=(k == 7))
                    S_.stt(zt[i][:, half * 512:(half + 1) * 512], h0t[i][:, half * 512:(half + 1) * 512], ALPHA, ps, ALU.mult, ALU.add)
                layernorm(es5, zt[i], gam, bet, h1t[i], tp[i])
                S_.dma("sp", h1_d[grp][tin * 128:(tin + 1) * 128, :], h1t[i])
                S_.cp("act", hhi[i], h1t[i])
                S_.tt("dve", hlo[i], h1t[i], hhi[i], ALU.subtract)
                for half in range(2):
                    ps = psum()
                    for k in range(4):
                        kk_ = half * 4 + k
                        S_.mm(ps[:, k * 128:(k + 1) * 128], hhi[i][:, kk_ * 128:(kk_ + 1) * 128], identb, start=True, stop=False)
                        S_.mm(ps[:, k * 128:(k + 1) * 128], hlo[i][:, kk_ * 128:(kk_ + 1) * 128], identb, start=False, stop=True, inc=(k == 3))
                    S_.cp("act", h1T32[i][:, half * 4:half * 4 + 4, :], ps.re("p (k t) -> p k t", k=4))
                    S_.cp("dve", h1Tb[i][:, half * 4:half * 4 + 4, :], h1T32[i][:, half * 4:half * 4 + 4, :])
                if "h1Tdma" not in SKIP:
                    S_.dma("sp", h1T_d[grp][:, :, tin * 128:(tin + 1) * 128], h1Tb[i])
                if "router" in SKIP:
                    continue
                ps = psum()
                for k in range(8):
                    S_.mm(ps[:, 0:NE], h1T32[i][:, k, :], Wr32[:, k, :], start=(k == 0), stop=(k == 7), inc=(k == 7))
                S_.tt("dve", lg, ps[:, 0:NE], brb, ALU.add)
                if "top" in SKIP:
                    continue
                S_.op("dve", lambda g: g.max(_ap(mx8), _ap(lg)), [lg], [mx8])
                S_.ts("dve", mk, lg, mx8[:, 3:4], ALU.is_ge)
                S_.ts("dve", nmx, mx8[:, 0:1], -1.0, ALU.mult)
                S_.act(ex, lg, AF.Exp, bias=nmx[:, 0:1])
                S_.tt("dve", ex, ex, mk, ALU.mult)
                S_.red(sm, ex, ALU.add)
                S_.op("dve", lambda g: g.reciprocal(_ap(sm), _ap(sm)), [sm], [sm])
                S_.ts("dve", Gall[:, gt, :], ex, sm[:, 0:1], ALU.mult)
                if SPARSE:
                    route_tile(gt, mk, hhi[i], rt)

    def stage_moe():
        with ExitStack() as es6:
            A6 = Alloc(nc, es6)
            gam = A6.sb([128, D], F32, "gam2"); bet = A6.sb([128, D], F32, "bet2")
            bcast_load(gam, ln2_g); bcast_load(bet, ln2_b)
            bguT = A6.sb([128, 16, NE], F32, "bguT")
            with ExitStack() as esb:
                Ab = Alloc(nc, esb)
                bgu_sb = Ab.sb([NE, 2 * D], F32, "bgu_sb")
                bgu_hi = Ab.sb([NE, 2 * D], BF16, "bgu_hi"); bgu_lo = Ab.sb([NE, 2 * D], BF16, "bgu_lo")
                S_.dma("sp", bgu_sb, b_gate_up)
                S_.cp("act", bgu_hi, bgu_sb)
                S_.tt("dve", bgu_lo, bgu_sb, bgu_hi, ALU.subtract)
                for fc in range(16):
                    ps = psum()
                    S_.mm(ps[:, 0:NE], bgu_hi[0:NE, fc * 128:(fc + 1) * 128], identb[0:NE, 0:NE], start=True, stop=False)
                    S_.mm(ps[:, 0:NE], bgu_lo[0:NE, fc * 128:(fc + 1) * 128], identb[0:NE, 0:NE], start=False, stop=True, inc=True)
                    S_.cp("act", bguT[:, fc, :], ps[:, 0:NE])
                S_.barrier()
            bd_sb = A6.sb([NE, D], F32, "bd_sb")
            S_.dma("sp", bd_sb, b_down)
            h1T = A6.sb([128, 8, 1024], BF16, "h1T")
            acc = A6.sb([128, 8, D], F32, "acc")
            wgu = [A6.sb([128, 8, 2, 256], BF16, "wgu") for _ in range(6)]
            wd = [A6.sb([128, 8, D], BF16, "wd") for _ in range(2)]
            actT = A6.sb([128, 8, 1024], BF16, "actT")
            g1 = [A6.sb([128, 512], F32, "g1") for _ in range(2)]
            s1 = [A6.sb([128, 512], F32, "s1") for _ in range(2)]
            u1 = [A6.sb([128, 512], F32, "u1") for _ in range(2)]
            GT = A6.sb([NE, 128], F32, "GT")
            Ghi = A6.sb([128, NE], BF16, "Ghi"); Glo = A6.sb([128, NE], BF16, "Glo")
            h1t = A6.sb([128, D], F32, "h1t2"); zt = A6.sb([128, D], F32, "zt2"); ot = A6.sb([128, D], F32, "ot2")
            tp = dict(st=A6.sb([128, 2, 6], F32, "st"), mv=A6.sb([128, 2], F32, "mv"), rs=A6.sb([128, 1], F32, "rs"))
            tmpd = [A6.sb([128, 512], F32, "tmpd") for _ in range(2)]
            npiece = 0
            it = 0
            nd = 0
            for g in range(4):
                S_.dma("sp", h1T, h1T_d[g])
                for tl in range(8):
                    gt = g * 8 + tl
                    ps = psum()
                    S_.cp("act", Ghi, Gall[:, gt, :])
                    S_.tt("dve", Glo, Gall[:, gt, :], Ghi, ALU.subtract)
                    S_.mm(ps[0:NE, 0:128], Ghi, identb, start=True, stop=False)
                    S_.mm(ps[0:NE, 0:128], Glo, identb, start=False, stop=True, inc=True)
                    S_.cp("act", GT, ps[0:NE, 0:128])
                    for half in range(2):
                        ps = psum()
                        S_.mm(ps, GT[0:NE, :], bd_sb[0:NE, half * 512:(half + 1) * 512], inc=True)
                        S_.cp("act" if half else "dve", acc[:, tl, half * 512:(half + 1) * 512], ps)
                for e in range(NE):
                    wde = wd[e % 2]
                    if "moe_w" not in SKIP:
                        S_.dma("pool", wde, w_down[e].re("(k p) c -> p k c", p=128))
                    src = w_gate_up[e].re("(k p) (u f) -> p k u f", p=128, u=2)
                    for p in range(4):
                        wp = wgu[npiece % 6]; npiece += 1
                        for u_ in range(2):
                            if "moe_w" not in SKIP:
                                S_.dma("pool", wp[:, :, u_, :], src[:, :, u_, p * 256:(p + 1) * 256])
                        if "moe_gu" in SKIP:
                            continue
                        for sg in range(2):
                            for f2 in range(2):
                                fc = 2 * p + f2
                                i = it % 2; it += 1
                                psg = psum(); psu = psum()
                                for k in range(8):
                                    S_.mm(psg, wp[:, k, 0, f2 * 128:(f2 + 1) * 128], h1T[:, k, sg * 512:(sg + 1) * 512],
                                          start=(k == 0), stop=(k == 7), inc=(k == 7))
                                for k in range(8):
                                    S_.mm(psu, wp[:, k, 1, f2 * 128:(f2 + 1) * 128], h1T[:, k, sg * 512:(sg + 1) * 512],
                                          start=(k == 0), stop=(k == 7), inc=(k == 7))
                                S_.ts("dve", g1[i], psg, bguT[:, fc, e:e + 1], ALU.add, 7.0, ALU.min)
                                S_.act(s1[i], g1[i], AF.Sigmoid, scale=1.702)
                                S_.act(u1[i], psu, AF.Identity, bias=bguT[:, 8 + fc, e:e + 1])
                                S_.ts("pool", u1[i], u1[i], 7.0, ALU.min, -7.0, ALU.max)
                                S_.tt("pool", g1[i], g1[i], s1[i], ALU.mult)
                                S_.stt(actT[:, fc, sg * 512:(sg + 1) * 512], u1[i], 1.0, g1[i], ALU.add, ALU.mult)
                    for tl in range(8):
                        gt = g * 8 + tl
                        if "moe_down" in SKIP:
                            continue
                        for half in range(2):
                            ps = psum()
                            for fc in range(8):
                                S_.mm(ps, actT[:, fc, tl * 128:(tl + 1) * 128], wde[:, fc, half * 512:(half + 1) * 512],
                                      start=(fc == 0), stop=(fc == 7), inc=(fc == 7))
                            a_ = acc[:, tl, half * 512:(half + 1) * 512]
                            td = tmpd[nd % 2]; nd += 1
                            S_.ts("dve", td, ps, Gall[:, gt, e:e + 1], ALU.mult)
                            S_.tt("pool", a_, a_, td, ALU.add)
                for tl in range(8):
                    gt = g * 8 + tl
                    S_.dma("sp", h1t, h1_d[g][tl * 128:(tl + 1) * 128, :])
                    S_.stt(zt, h1t, ALPHA, acc[:, tl, :], ALU.mult, ALU.add)
                    layernorm(es6, zt, gam, bet, ot, tp)
                    S_.dma("sp", out_d[gt * 128:(gt + 1) * 128, :], ot)

    def route_tile(gt, mk, hb_tile, rt):
        mkb = rt["mkb"]; pos = rt["pos"]; vv = rt["vv"]; nd = rt["nd"]; mx = rt["mx"]; sel = rt["sel"]; idf = rt["idf"]
        S_.cp("dve", mkb, mk)
        ps = psum()
        S_.mm(ps[:, 0:NE], utri_b, mkb, inc=True)
        ps2 = psum()
        S_.mm(ps2[:, 0:NE], ones_b, mkb, inc=True)
        S_.tt("dve", pos, ps[:, 0:NE], carry, ALU.add)
        S_.tt("dve", carry, ps2[:, 0:NE], carry, ALU.add)
        S_.ts("dve", vv, pos, float(CAP), ALU.is_lt)
        S_.tt("dve", vv, vv, mk, ALU.mult)
        S_.tt("dve", pos, pos, erow, ALU.add)
        S_.ts("dve", nd, pos, -1.0, ALU.mult, BIGF, ALU.add)
        S_.tt("dve", nd, nd, vv, ALU.mult)
        S_.ts("dve", nd, nd, -BIGF, ALU.add)
        S_.op("dve", lambda g: g.max(_ap(mx), _ap(nd)), [nd], [mx])
        S_.ts("dve", idf, mx[:, 0:4], -1.0, ALU.mult, float(NE * CAP), ALU.min)
        S_.cp("dve", idx_all[:, gt, :], idf)
        for k in range(4):
            S_.ts("dve", sel, nd, mx[:, k:k + 1], ALU.is_equal)
            S_.tt("dve", sel, sel, Gall[:, gt, :], ALU.mult)
            S_.red(gk_all[:, gt, k:k + 1], sel, ALU.add)
        S_.ts("dve", idf, mx[:, 0:4], -0.5 * BIGF, ALU.is_gt)
        S_.tt("dve", gk_all[:, gt, :], gk_all[:, gt, :], idf, ALU.mult)
        for k in range(4):
            S_.idma_scatter(x_rows, idx_all[:, gt, k:k + 1], hb_tile, NE * CAP - 1)

    def stage_moe_sparse():
        S_.barrier()
        with ExitStack() as es6:
            A6 = Alloc(nc, es6)
            bguT = A6.sb([128, 16, NE], F32, "bguT")
            with ExitStack() as esb:
                Ab = Alloc(nc, esb)
                bgu_sb = Ab.sb([NE, 2 * D], F32, "bgu_sb")
                bgu_hi = Ab.sb([NE, 2 * D], BF16, "bgu_hi"); bgu_lo = Ab.sb([NE, 2 * D], BF16, "bgu_lo")
                S_.dma("sp", bgu_sb, b_gate_up)
                S_.cp("act", bgu_hi, bgu_sb)
                S_.tt("dve", bgu_lo, bgu_sb, bgu_hi, ALU.subtract)
                for fc in range(16):
                    ps = psum()
                    S_.mm(ps[:, 0:NE], bgu_hi[0:NE, fc * 128:(fc + 1) * 128], identb[0:NE, 0:NE], start=True, stop=False)
                    S_.mm(ps[:, 0:NE], bgu_lo[0:NE, fc * 128:(fc + 1) * 128], identb[0:NE, 0:NE], start=False, stop=True, inc=True)
                    S_.cp("act", bguT[:, fc, :], ps[:, 0:NE])
                S_.barrier()
            with ExitStack() as ese:
                Ae = Alloc(nc, ese)
                NS = CAP // 128
                xe = [Ae.sb([128, NS, D], BF16, "xe") for _ in range(2)]
                xT = [Ae.sb([128, 8, CAP], BF16, "xT") for _ in range(2)]
                wgu = [Ae.sb([128, 8, 2 * D], BF16, "wgu") for _ in range(2)]
                wd = [Ae.sb([128, 8, D], BF16, "wd") for _ in range(2)]
                actT = Ae.sb([128, 8, CAP], BF16, "actT")
                g1 = [Ae.sb([128, 512], F32, "g1") for _ in range(2)]
                s1 = [Ae.sb([128, 512], F32, "s1") for _ in range(2)]
                u1 = [Ae.sb([128, 512], F32, "u1") for _ in range(2)]
                yo = [Ae.sb([128, D], F32, "yo") for _ in range(2)]
                blocks = [(0, 512)] + ([(512, CAP - 512)] if CAP > 512 else [])
                issued = [0]

                wds = [Ae.sb([128, 2, D], F32, "wds") for _ in range(2)]

                def wd_src(e, j):
                    return w_down[e].re("(k p) c -> p k c", p=128)[:, 2 * j:2 * j + 2, :]

                def wd_dma(e, j):
                    S_.dma("sp", wds[j % 2], wd_src(e, j))

                def wd_cast(e, j):
                    S_.cp("act", wd[e % 2][:, 2 * j:2 * j + 2, :], wds[j % 2])

                def issue_weights(upto):
                    while issued[0] <= min(upto, NE - 1):
                        e = issued[0]; issued[0] += 1
                        S_.dma("pool", wgu[e % 2], w_gate_up[e].re("(k p) f -> p k f", p=128))
                        if e == 0:
                            for j in range(4):
                                wd_dma(0, j)
                                wd_cast(0, j)
                        else:
                            wd_dma(e, 0)
                            wd_dma(e, 1)

                def load_x(e):
                    S_.dma("sp", xe[e % 2], x_rows[e * CAP:(e + 1) * CAP, :].re("(n p) d -> p n d", p=128))
                    for n in range(NS):
                        ps = psum().bitcast(BF16)
                        for k in range(8):
                            S_.tr(ps[:, k * 128:(k + 1) * 128], xe[e % 2][:, n, k * 128:(k + 1) * 128], identb, inc=(k == 7))
                        S_.cp("act" if n % 2 else "dve", xT[e % 2][:, :, n * 128:(n + 1) * 128], ps.re("p (k t) -> p k t", k=8))
                it = 0
                load_x(0)
                for e in range(NE):
                    if e + 1 < NE:
                        load_x(e + 1)
                    xTe = xT[e % 2]
                    issue_weights(e + 1)
                    wge = wgu[e % 2]
                    for p in range(4):
                        if e + 1 < NE:
                            wd_cast(e + 1, p)
                            if p + 2 < 4:
                                wd_dma(e + 1, p + 2)
                        for (c0, cn) in blocks:
                            for f2 in range(2):
                                fc = 2 * p + f2
                                i = it % 2; it += 1
                                psg = psum(); psu = psum()
                                for k in range(8):
                                    S_.mm(psg[:, 0:cn], wge[:, k, fc * 128:(fc + 1) * 128], xTe[:, k, c0:c0 + cn],
                                          start=(k == 0), stop=(k == 7), inc=(k == 7))
                                for k in range(8):
                                    S_.mm(psu[:, 0:cn], wge[:, k, D + fc * 128:D + (fc + 1) * 128], xTe[:, k, c0:c0 + cn],
                                          start=(k == 0), stop=(k == 7), inc=(k == 7))
                                gg = g1[i][:, 0:cn]; sg_ = s1[i][:, 0:cn]; uu = u1[i][:, 0:cn]
                                S_.ts("dve", gg, psg[:, 0:cn], bguT[:, fc, e:e + 1], ALU.add, 7.0, ALU.min)
                                S_.act(sg_, gg, AF.Sigmoid, scale=1.702)
                                S_.act(uu, psu[:, 0:cn], AF.Identity, bias=bguT[:, 8 + fc, e:e + 1])
                                S_.ts("pool", uu, uu, 7.0, ALU.min, -7.0, ALU.max)
                                S_.tt("pool", gg, gg, sg_, ALU.mult)
                                S_.stt(actT[:, fc, c0:c0 + cn], uu, 1.0, gg, ALU.add, ALU.mult)
                    wde = wd[e % 2]
                    for n in range(NS):
                        yt_ = yo[n % 2]
                        for half in range(2):
                            ps = psum()
                            for fc in range(8):
                                S_.mm(ps, actT[:, fc, n * 128:(n + 1) * 128], wde[:, fc, half * 512:(half + 1) * 512],
                                      start=(fc == 0), stop=(fc == 7), inc=(fc == 7))
                            S_.cp("act" if half else "dve", yt_[:, half * 512:(half + 1) * 512], ps)
                        r0 = e * CAP + n * 128
                        S_.dma("sp", V(y_rows.ap[r0:r0 + 128, :], [Buf("yrow")]), yt_)
            S_.barrier()
            with ExitStack() as esc:
                Ac = Alloc(nc, esc)
                gam = Ac.sb([128, D], F32, "gam2"); bet = Ac.sb([128, D], F32, "bet2")
                bcast_load(gam, ln2_g); bcast_load(bet, ln2_b)
                bd_sb = Ac.sb([NE, D], F32, "bd_sb")
                S_.dma("sp", bd_sb, b_down)
                GT = Ac.sb([NE, 128], F32, "GT")
                Ghi = Ac.sb([128, NE], BF16, "Ghi"); Glo = Ac.sb([128, NE], BF16, "Glo")
                yk = [[Ac.sb([128, D], F32, "yk") for _ in range(4)] for _ in range(2)]
                h1t = [Ac.sb([128, D], F32, "h1t2") for _ in range(2)]
                zt = Ac.sb([128, D], F32, "zt2"); ot = Ac.sb([128, D], F32, "ot2")
                tmpc = [Ac.sb([128, D], F32, "tmpc") for _ in range(2)]
                tp = dict(st=Ac.sb([128, 2, 6], F32, "st"), mv=Ac.sb([128, 2], F32, "mv"), rs=Ac.sb([128, 1], F32, "rs"))

                def fetch(gt):
                    for k in range(4):
                        S_.idma_gather(yk[gt % 2][k], y_rows, idx_all[:, gt, k:k + 1], NE * CAP - 1)
                    S_.dma("sp", h1t[gt % 2], h1_d[gt // 8][(gt % 8) * 128:(gt % 8 + 1) * 128, :])
                fetch(0)
                for gt in range(NB * NT):
                    if gt + 1 < NB * NT:
                        fetch(gt + 1)
                    S_.cp("act", Ghi, Gall[:, gt, :])
                    S_.tt("dve", Glo, Gall[:, gt, :], Ghi, ALU.subtract)
                    ps = psum()
                    S_.mm(ps[0:NE, 0:128], Ghi, identb, start=True, stop=False)
                    S_.mm(ps[0:NE, 0:128], Glo, identb, start=False, stop=True, inc=True)
                    S_.cp("act", GT, ps[0:NE, 0:128])
                    for half in range(2):
                        ps = psum()
                        S_.mm(ps, GT[0:NE, :], bd_sb[0:NE, half * 512:(half + 1) * 512], inc=True)
                        S_.stt(zt[:, half * 512:(half + 1) * 512], h1t[gt % 2][:, half * 512:(half + 1) * 512], ALPHA, ps, ALU.mult, ALU.add)
                    for k in range(4):
                        S_.act(tmpc[k % 2], yk[gt % 2][k], AF.Copy, scale=gk_all[:, gt, k:k + 1])
                        S_.tt("dve", zt, zt, tmpc[k % 2], ALU.add)
                    layernorm(esc, zt, gam, bet, ot, tp, eng2="dve")
                    S_.dma("sp", out_d[gt * 128:(gt + 1) * 128, :], ot)

    msk = G.sb([128, 5 * 64 + 2], F32, "msk")
    S_.dma("sp", msk, c_masks)
    tri32 = G.sb([128, 2, 128], F32, "tri32")
    S_.dma("sp", tri32, c_tri.re("d p t -> p d t"))
    MS = [msk[:, 0:64], msk[:, 128:192]]
    MI = [msk[:, 64:128], msk[:, 192:256]]
    MID = msk[:, 256:320]
    BLK = msk[:, 320:322]

    def dbg_out(name, src, shape, dt):
        if name in debug:
            dbg[name] = V(nc.dram_tensor("dbg_" + name, list(shape), dt, kind="ExternalOutput").ap(), [Buf("dbg_" + name)])
            S_.dma("sp", dbg[name], src)

    for b in range(NB):
        with ExitStack() as esA:
            A = Alloc(nc, esA)
            hT = A.sb([128, 8, S], BF16, "hT")
            stage_s1(b, hT)
            S_.barrier()
            if b == 0:
                dbg_out("hT", hT, [128, 8, S], BF16)
            yT_r = A.sb([128, 3, S], BF16, "yT_r")
            if "rwkv" in stages:
                stage_rwkv(b, hT, yT_r)
                S_.barrier()
            yT_a = A.sb([128, 3, S], BF16, "yT_a")
            if "att" in stages:
                stage_att(b, hT, yT_a)
                S_.barrier()
            yT_m = A.sb([128, 2, S], BF16, "yT_m")
            if "mem" in stages:
                stage_mem(b, hT, yT_m)
                S_.barrier()
            if b == 0:
                dbg_out("yT_r", yT_r, [128, 3, S], BF16)
                dbg_out("yT_a", yT_a, [128, 3, S], BF16)
                dbg_out("yT_m", yT_m, [128, 2, S], BF16)
            if "out" in stages:
                stage_out(b, [yT_r[:, 0], yT_r[:, 1], yT_r[:, 2], yT_a[:, 0], yT_a[:, 1], yT_a[:, 2], yT_m[:, 0], yT_m[:, 1]])
                S_.barrier()
    if "h1" in debug:
        dbg["h1"] = V(nc.dram_tensor("dbg_h1", [1024, D], F32, kind="ExternalOutput").ap(), [Buf("dbg_h1")])
        S_.dma("sp", dbg["h1"], h1_d[0])
        dbg["G"] = V(nc.dram_tensor("dbg_G", [128, NB * NT, NE], F32, kind="ExternalOutput").ap(), [Buf("dbg_G")])
        S_.dma("sp", dbg["G"], Gall)

    if moe:
        if SPARSE:
            stage_moe_sparse()
        else:
            stage_moe()
    S_.barrier()
    es.close()
    return nc, S_


def host_constants():
    ident = np.eye(128, dtype=np.float32)
    slopes = np.exp2(-8.0 * np.arange(1, H + 1, dtype=np.float32) / H).astype(np.float32)
    x = np.arange(STRIP_W)[None, :]
    p = np.arange(128)[:, None]
    delta = x - p - STRIP_C
    ad = np.abs(delta)
    mult = ((ad <= 64).astype(np.float32) + ((delta % 4 == 0) & (ad <= 256)).astype(np.float32)
            + ((delta % 16 == 0) & (ad <= 1024)).astype(np.float32))
    strip = np.stack([mult * np.exp(-(slopes[h] * ad.astype(np.float32))) for h in range(H)]).astype(np.float32)
    masks = np.zeros((128, 5 * 64 + 2), np.float32)
    si = np.arange(64)[:, None]; ti = np.arange(64)[None, :]
    for q in range(2):
        r = slice(64 * q, 64 * q + 64)
        masks[r, 0:64] = (si < ti); masks[r, 64:128] = (si <= ti)
        masks[r, 128:192] = (si > ti); masks[r, 192:256] = (si >= ti)
        masks[r, 256:320] = (si == ti)
        masks[r, 320 + q] = 1.0
    tri = np.zeros((2, 128, 128), np.float32)
    for q in range(2):
        r = slice(64 * q, 64 * q + 64)
        tri[0, r, r] = (si <= ti); tri[1, r, r] = (si >= ti)
    utri = (np.arange(128)[:, None] < np.arange(128)[None, :]).astype(np.float32)
    erow = np.tile((np.arange(NE, dtype=np.float32) * CAP)[None, :], (128, 1))
    return dict(c_ident=ident, c_strip=strip, c_masks=masks, c_tri=tri, c_utri=utri, c_erow=erow)


def make_in_maps(inputs):
    consts = host_constants()
    maps = []
    sq = lambda a: np.ascontiguousarray(a)
    for i in range(8):
        m = dict(consts)
        m["x"] = sq(inputs["x"][2 * i:2 * i + 2].reshape(NB * S, D))
        m["mem"] = sq(inputs["mem"][2 * i:2 * i + 2].reshape(NB * 256, D))
        for k in ("ln_in_g", "ln_in_b"):
            m[k] = sq(inputs[k])
        m["w_in"] = sq(inputs["w_in"][0])
        m["mu_shift"] = sq(inputs["mu_shift"][0])
        m["w0"] = sq(inputs["w0"][0].reshape(-1)); m["w_up"] = sq(inputs["w_up"][0])
        m["a0"] = sq(inputs["a0"][0].reshape(-1)); m["a_up"] = sq(inputs["a_up"][0])
        m["g_up"] = sq(inputs["g_up"][0])
        for k in ("k_k", "k_a", "gn_g", "gn_b", "ln1_g", "ln1_b", "ln2_g", "ln2_b", "b_router",
                  "w_mem_kv", "w_out", "w_router", "w_gate_up", "b_gate_up", "w_down", "b_down"):
            m[k] = sq(inputs[k][0])
        m["r_k"] = sq(inputs["r_k"][0].reshape(-1))
        maps.append(m)
    return maps


def kernel(**inputs):
    inputs = {k: np.asarray(v) for k, v in inputs.items()}
    nc, _ = build_program()
    maps = make_in_maps(inputs)
    res = run_bass_kernel_spmd(nc, maps, core_ids=list(range(8)))
    out = np.stack([r["out"].reshape(NB, S, D) for r in res.results]).reshape(16, S, D)
    return out.astype(np.float32)
```

```python
import numpy as np
from contextlib import ExitStack
import concourse.bass as bass
import concourse.mybir as mybir
from concourse.bass_utils import run_bass_kernel_spmd

F32 = mybir.dt.float32
BF16 = mybir.dt.bfloat16
U32 = mybir.dt.uint32
AF = mybir.ActivationFunctionType
ALU = mybir.AluOpType
AX = mybir.AxisListType

D = 1024
S = 2048
NB = 2
NT = S // 128
C = 384
H = 6
RWKV_IN = 1344
IN_W = 2752
NE = 32
ALPHA = 2.0 ** 0.25
LN_EPS = 1e-5
GN_EPS = 64e-5
LDS = float(np.exp(-0.5))
STRIP_W = 3200
STRIP_C = 1536
CAP = 640
BIGF = 1.0e6
import os
SKIP = os.environ.get('KSKIP', '')
SPARSE = os.environ.get('KDENSE', '') == ''


class Buf:
    __slots__ = ("name", "w", "rs")

    def __init__(self, name):
        self.name = name
        self.w = None
        self.rs = []


class Ev:
    __slots__ = ("sem", "val", "eng")

    def __init__(self, eng):
        self.sem = None
        self.val = None
        self.eng = eng


class V:
    __slots__ = ("ap", "bufs")

    def __init__(self, ap, bufs):
        self.ap = ap
        self.bufs = bufs if isinstance(bufs, (list, tuple)) else [bufs]

    def __getitem__(self, k):
        return V(self.ap[k], self.bufs)

    def re(self, pat, **kw):
        return V(self.ap.rearrange(pat, **kw), self.bufs)

    def bc(self, shape):
        return V(self.ap.to_broadcast(list(shape)), self.bufs)

    def un(self, axis):
        return V(self.ap.unsqueeze(axis), self.bufs)

    def bitcast(self, dt):
        return V(self.ap.bitcast(dt), self.bufs)

    def on(self, bufs):
        return V(self.ap, bufs)


def _ap(x):
    return x.ap if isinstance(x, V) else x


class Sched:
    ENG = ("pe", "act", "dve", "pool", "sp")
    NQ = 16

    def __init__(self, nc, es):
        self.nc = nc
        self.es = es
        self.eng = dict(pe=nc.tensor, act=nc.scalar, dve=nc.vector, pool=nc.gpsimd, sp=nc.sync)
        self.sem = {k: es.enter_context(nc.semaphore("s_" + k)) for k in self.ENG}
        self.cnt = {k: 0 for k in self.ENG}
        self.dsem = {q: [es.enter_context(nc.semaphore("d_%s%d" % (q, i))) for i in range(self.NQ)]
                     for q in ("sp", "pool")}
        self.dcnt = {q: 0 for q in self.dsem}
        self.seen = {k: {} for k in self.ENG}
        self.pending = {k: [] for k in self.ENG}
        self.all_dma = []
        self.ninst = 0

    def _wait(self, e, ev):
        assert ev.val is not None, "dependency on an unresolved (non-inc) event"
        key = id(ev.sem)
        if self.seen[e].get(key, 0) >= ev.val:
            return
        self.eng[e].wait_ge(ev.sem, ev.val)
        self.seen[e][key] = ev.val

    def _deps(self, e, reads, writes, is_dma):
        for v in reads:
            for b in v.bufs:
                if b.w is not None:
                    if b.w.eng == e and e == "pe" and not is_dma:
                        continue
                    self._wait(e, b.w)
        for v in writes:
            for b in v.bufs:
                if b.w is not None and (is_dma or b.w.eng != e or e != "pe"):
                    self._wait(e, b.w)
                for r in b.rs:
                    if is_dma or r.eng != e or e != "pe":
                        self._wait(e, r)

    def _record(self, ev, reads, writes):
        for v in reads:
            for b in v.bufs:
                if ev.eng is not None:
                    b.rs = [r for r in b.rs if r.eng != ev.eng]
                b.rs.append(ev)
        for v in writes:
            for b in v.bufs:
                b.w = ev
                b.rs = []

    def op(self, e, fn, reads, writes, inc=True):
        reads = [r for r in reads if isinstance(r, V)]
        writes = [w for w in writes if isinstance(w, V)]
        self._deps(e, reads, writes, False)
        ins = fn(self.eng[e])
        self.ninst += 1
        ev = Ev(e)
        if inc:
            self.cnt[e] += 1
            ins.then_inc(self.sem[e], 1)
            ev.sem = self.sem[e]
            ev.val = self.cnt[e]
            for p in self.pending[e]:
                p.sem = ev.sem
                p.val = ev.val
            self.pending[e] = []
        else:
            self.pending[e].append(ev)
        self._record(ev, reads, writes)
        return ev

    def dma(self, q, out, in_, **kw):
        self._deps(q, [in_], [out], True)
        n = self.dcnt[q]
        self.dcnt[q] += 1
        sem = self.dsem[q][n % self.NQ]
        val = 16 * (n // self.NQ + 1)
        if n >= self.NQ:
            pe_ = Ev(None); pe_.sem = sem; pe_.val = val - 16
            self._wait(q, pe_)
        ins = self.eng[q].dma_start(out=_ap(out), in_=_ap(in_), **kw)
        ins.then_inc(sem, 16)
        self.ninst += 1
        ev = Ev(None)
        ev.sem = sem
        ev.val = val
        self._record(ev, [in_], [out])
        self.all_dma.append(ev)
        return ev

    def _idma(self, out, out_off, in_, in_off, reads, writes, bound):
        q = "pool"
        self._deps(q, reads, writes, True)
        n = self.dcnt[q]
        self.dcnt[q] += 1
        sem = self.dsem[q][n % self.NQ]
        val = 16 * (n // self.NQ + 1)
        if n >= self.NQ:
            pe_ = Ev(None); pe_.sem = sem; pe_.val = val - 16
            self._wait(q, pe_)
        ins = self.eng[q].indirect_dma_start(out=_ap(out), out_offset=out_off, in_=_ap(in_), in_offset=in_off)
        ins.then_inc(sem, 16)
        self.ninst += 1
        ev = Ev(None)
        ev.sem = sem
        ev.val = val
        self._record(ev, reads, writes)
        return ev

    def idma_scatter(self, dram, idx, src, bound):
        off = bass.IndirectOffsetOnAxis(ap=_ap(idx), axis=0)
        return self._idma(dram, off, src, None, [src, idx], [V(dram.ap, [Buf("scatter")])], bound)

    def idma_gather(self, dst, dram, idx, bound):
        off = bass.IndirectOffsetOnAxis(ap=_ap(idx), axis=0)
        return self._idma(dst, None, dram, off, [idx], [dst], bound)

    def barrier(self):
        for e in self.ENG:
            for o in self.ENG:
                if self.cnt[o] > 0:
                    assert not self.pending[o]
                    ev = Ev(o)
                    ev.sem = self.sem[o]
                    ev.val = self.cnt[o]
                    self._wait(e, ev)
            for q in self.dsem:
                n = self.dcnt[q]
                for i in range(min(n, self.NQ)):
                    ev = Ev(None)
                    ev.sem = self.dsem[q][i]
                    ev.val = 16 * ((n - 1 - i) // self.NQ + 1)
                    self._wait(e, ev)

    def _pe_rows(self, lhsT):
        a = _ap(lhsT)
        lo = a.base_partition()
        rows = (lo, lo + a.shape[0])
        prev = getattr(self, "_last_pe", None)
        if prev is not None:
            pins, pev, prow = prev
            if rows[0] >= prow[1] or prow[0] >= rows[1]:
                if pev.val is None:
                    self.cnt["pe"] += 1
                    pins.then_inc(self.sem["pe"], 1)
                    for p in self.pending["pe"]:
                        p.sem = self.sem["pe"]
                        p.val = self.cnt["pe"]
                    self.pending["pe"] = []
                self._wait("pe", pev)
        return rows

    def _pe_op(self, fn, lhsT, reads, writes, inc):
        rows = self._pe_rows(lhsT)
        box = []

        def f(e):
            i = fn(e)
            box.append(i)
            return i
        ev = self.op("pe", f, reads, writes, inc=inc)
        self._last_pe = (box[0], ev, rows)
        return ev

    def mm(self, out, lhsT, rhs, start=True, stop=True, inc=False):
        return self._pe_op(lambda e: e.matmul(_ap(out), _ap(lhsT), _ap(rhs), start=start, stop=stop),
                           lhsT, [lhsT, rhs], [out], inc)

    def tr(self, out, in_, ident, inc=False):
        return self._pe_op(lambda e: e.transpose(_ap(out), _ap(in_), _ap(ident)), in_, [in_, ident], [out], inc)

    def act(self, out, in_, func, bias=None, scale=None, accum_out=None):
        kw = {}
        if bias is not None:
            kw["bias"] = _ap(bias)
        if scale is not None:
            kw["scale"] = _ap(scale)
        if accum_out is not None:
            kw["accum_out"] = _ap(accum_out)
        return self.op("act", lambda e: e.activation(_ap(out), _ap(in_), func, **kw),
                       [in_, bias, scale], [out, accum_out])

    def ts(self, e, out, in0, s1, op0, s2=None, op1=None, accum_out=None):
        kw = {}
        if op1 is not None:
            kw["op1"] = op1
        if accum_out is not None:
            kw["accum_out"] = _ap(accum_out)
        return self.op(e, lambda g: g.tensor_scalar(_ap(out), _ap(in0), _ap(s1), _ap(s2), op0, **kw),
                       [in0, s1, s2], [out, accum_out])

    def tt(self, e, out, in0, in1, op):
        return self.op(e, lambda g: g.tensor_tensor(_ap(out), _ap(in0), _ap(in1), op), [in0, in1], [out])

    def stt(self, out, in0, scalar, in1, op0, op1):
        return self.op("dve", lambda g: g.scalar_tensor_tensor(_ap(out), _ap(in0), _ap(scalar), _ap(in1), op0, op1),
                       [in0, scalar, in1], [out])

    def cp(self, e, out, in_):
        if e == "act":
            return self.op("act", lambda g: g.copy(_ap(out), _ap(in_)), [in_], [out])
        return self.op(e, lambda g: g.tensor_copy(_ap(out), _ap(in_)), [in_], [out])

    def red(self, out, in_, op, axis=AX.X):
        return self.op("dve", lambda g: g.tensor_reduce(_ap(out), _ap(in_), axis, op), [in_], [out])

    def memset(self, e, out, val):
        return self.op(e, lambda g: g.memset(_ap(out), val), [], [out])


class Alloc:
    N = [0]

    def __init__(self, nc, es):
        self.nc = nc
        self.es = es

    def sb(self, shape, dt, name=None, nbuf=1):
        Alloc.N[0] += 1
        name = "%s_%d" % (name or "t", Alloc.N[0])
        t = self.es.enter_context(self.nc.sbuf_tensor(name, list(shape), dt))
        return V(t[:], [Buf(name)])

    def dram(self, name, shape, dt, kind="Internal"):
        t = self.nc.dram_tensor(name, list(shape), dt, kind=kind)
        return V(t.ap(), [Buf(name)])


def build_program(debug=(), moe=True, stages=('rwkv', 'att', 'mem', 'out')):
    nc = bass.Bass("TRN2", target_bir_lowering=False)
    es = ExitStack()
    S_ = Sched(nc, es)
    G = Alloc(nc, es)
    dbg = {}

    def din(name, shape, dt=F32):
        return V(nc.dram_tensor(name, list(shape), dt, kind="ExternalInput").ap(), [Buf(name)])

    x_d = din("x", [NB * S, D])
    mem_d = din("mem", [NB * 256, D])
    ln_in_g = din("ln_in_g", [D]); ln_in_b = din("ln_in_b", [D])
    w_in = din("w_in", [D, IN_W])
    mu_d = din("mu_shift", [RWKV_IN])
    w0_d = din("w0", [2 * C]); w_up_d = din("w_up", [2, 32, C])
    a0_d = din("a0", [2 * C]); a_up_d = din("a_up", [2, 32, C])
    g_up_d = din("g_up", [64, C])
    k_k_d = din("k_k", [C]); k_a_d = din("k_a", [C]); r_k_d = din("r_k", [C])
    gn_g_d = din("gn_g", [C]); gn_b_d = din("gn_b", [C])
    w_mem_kv = din("w_mem_kv", [D, 512])
    w_out = din("w_out", [D, D])
    ln1_g = din("ln1_g", [D]); ln1_b = din("ln1_b", [D])
    w_router = din("w_router", [D, NE]); b_router = din("b_router", [NE])
    if moe:
        w_gate_up = din("w_gate_up", [NE, D, 2 * D]); b_gate_up = din("b_gate_up", [NE, 2 * D])
        w_down = din("w_down", [NE, D, D]); b_down = din("b_down", [NE, D])
    ln2_g = din("ln2_g", [D]); ln2_b = din("ln2_b", [D])
    c_ident = din("c_ident", [128, 128])
    c_strip = din("c_strip", [H, 128, STRIP_W])
    c_masks = din("c_masks", [128, 5 * 128 + 2])
    c_tri = din("c_tri", [2, 128, 128])
    c_utri = din("c_utri", [128, 128])
    c_erow = din("c_erow", [128, NE])

    out_d = V(nc.dram_tensor("out", [NB * S, D], F32, kind="ExternalOutput").ap(), [Buf("out")])

    h0_d = [G.dram("h0_%d" % b, [S, D], F32) for b in range(NB)]
    h1_d = [G.dram("h1_%d" % g, [1024, D], F32) for g in range(4)]
    h1T_d = [G.dram("h1T_%d" % g, [128, 8, 1024], BF16) for g in range(4)]

    banks = []
    for i in range(8):
        t = es.enter_context(nc.psum_tensor("ps%d" % i, [128, 512], F32))
        banks.append(V(t[:], [Buf("ps%d" % i)]))
    bank_i = {"gen": 0, "acc": 0}
    bank_grp = {"gen": [2, 3, 4, 5, 6, 7], "acc": [0, 1]}

    def psum(grp="gen"):
        l = bank_grp[grp]
        b = banks[l[bank_i[grp] % len(l)]]
        bank_i[grp] += 1
        return b

    ident32 = G.sb([128, 128], F32, "ident32")
    identb = G.sb([128, 128], BF16, "identb")
    S_.dma("sp", ident32, c_ident)
    S_.cp("dve", identb, ident32)
    Gall = G.sb([128, NB * NT, NE], F32, "Gall")
    ones_b = G.sb([128, 128], BF16, "ones_b")
    S_.memset("dve", ones_b, 1.0)
    zeros_b = G.sb([128, 512], BF16, "zeros_b")
    S_.memset("dve", zeros_b, 0.0)
    carry = G.sb([128, NE], F32, "carry")
    S_.memset("dve", carry, 0.0)
    idx_all = G.sb([128, NB * NT, 4], U32, "idx_all")
    gk_all = G.sb([128, NB * NT, 4], F32, "gk_all")
    utri32 = G.sb([128, 128], F32, "utri32")
    S_.dma("sp", utri32, c_utri)
    utri_b = G.sb([128, 128], BF16, "utri_b")
    S_.cp("dve", utri_b, utri32)
    erow = G.sb([128, NE], F32, "erow")
    S_.dma("sp", erow, c_erow)
    x_rows = G.dram("x_rows", [NE * CAP + 128, D], BF16)
    y_rows = G.dram("y_rows", [NE * CAP + 128, D], F32)
    es_z = ExitStack()
    zeros32 = Alloc(nc, es_z).sb([128, D], F32, "zeros32")
    S_.memset("pool", zeros32, 0.0)
    S_.dma("sp", V(y_rows.ap[NE * CAP:NE * CAP + 128, :], [Buf("ydump")]), zeros32)
    zb = zeros32.bitcast(BF16).re("p (n d) -> p n d", n=2)
    xr = x_rows.re("(n p) d -> p n d", p=128)
    nrow = (NE * CAP + 128) // 128
    for n0 in range(0, nrow, 2):
        nn = min(2, nrow - n0)
        S_.dma("sp", V(xr.ap[:, n0:n0 + nn, :], [Buf("xfill")]), zb[:, 0:nn, :])
    S_.barrier()
    es_z.close()

    def bcast_load(dst, src1d, q="sp"):
        S_.dma(q, dst, V(src1d.ap.partition_broadcast(128), src1d.bufs))

    def layernorm(es2, xin, gam, bet, out, tmp_pool, eng2="pool"):
        st = tmp_pool["st"]; mv = tmp_pool["mv"]; rs = tmp_pool["rs"]
        for i in range(2):
            S_.op("dve", lambda g, i=i: g.bn_stats(_ap(st[:, i, :]), _ap(xin[:, i * 512:(i + 1) * 512])),
                  [xin], [st])
        S_.op("dve", lambda g: g.bn_aggr(_ap(mv), _ap(st)), [st], [mv])
        S_.ts("dve", rs, mv[:, 1:2], LN_EPS, ALU.add)
        S_.act(rs, rs, AF.Sqrt)
        S_.op("dve", lambda g: g.reciprocal(_ap(rs), _ap(rs)), [rs], [rs])
        S_.ts("dve", out, xin, mv[:, 0:1], ALU.subtract, rs[:, 0:1], ALU.mult)
        S_.tt(eng2, out, out, gam, ALU.mult)
        S_.tt(eng2, out, out, bet, ALU.add)

    def stage_s1(b, hT):
        with ExitStack() as es1:
            A1 = Alloc(nc, es1)
            gam = A1.sb([128, D], F32, "gam"); bet = A1.sb([128, D], F32, "bet")
            bcast_load(gam, ln_in_g); bcast_load(bet, ln_in_b)
            xt = [A1.sb([128, D], F32, "xt") for _ in range(2)]
            ht = [A1.sb([128, D], F32, "ht") for _ in range(2)]
            hb = [A1.sb([128, D], BF16, "hb") for _ in range(2)]
            tp = [dict(st=A1.sb([128, 2, 6], F32, "st"), mv=A1.sb([128, 2], F32, "mv"),
                       rs=A1.sb([128, 1], F32, "rs")) for _ in range(2)]
            for t in range(NT):
                i = t % 2
                S_.dma("sp", xt[i], x_d[b * S + t * 128: b * S + (t + 1) * 128, :])
                layernorm(es1, xt[i], gam, bet, ht[i], tp[i])
                S_.dma("sp", h0_d[b][t * 128:(t + 1) * 128, :], ht[i])
                S_.cp("act", hb[i], ht[i])
                ps = psum().bitcast(BF16)
                for k in range(8):
                    S_.tr(ps[:, k * 128:(k + 1) * 128], hb[i][:, k * 128:(k + 1) * 128], identb, inc=(k == 7))
                S_.cp("act", hT[:, :, t * 128:(t + 1) * 128], ps.re("p (k t) -> p k t", k=8))

    def attn_core(nh, KTv, QTv, Vv, ytok, n_st, s_range, strips=None):
        with ExitStack() as esc:
            Ac = Alloc(nc, esc)
            Eb = [Ac.sb([128, 512], BF16, "E") for _ in range(3)]
            Pb = [Ac.sb([128, 512], BF16, "P") for _ in range(3)]
            rec = Ac.sb([128, 4], F32, "rec")
            it = 0
            for h in range(nh):
                c, p0 = h // 2, 64 * (h % 2)
                for tq in range(4):
                    s_lo, s_hi, jvalid = s_range(tq)
                    acc = psum("acc")
                    accv = acc[:, 0:260].re("p (j d) -> p j d", j=4)
                    S_.mm(acc[:, 0:260], zeros_b[:, 0:128], zeros_b[:, 0:260], start=True, stop=False)
                    sts = list(range(s_lo, s_hi + 1))

                    def score(st_):
                        sc = psum()
                        S_.mm(sc, KTv[p0:p0 + 64, c, st_ * 128:(st_ + 1) * 128],
                              QTv[p0:p0 + 64, c, tq * 512:(tq + 1) * 512], inc=True)
                        return sc
                    sc_next = score(sts[0])
                    for n_, st_ in enumerate(sts):
                        sc = sc_next
                        if n_ + 1 < len(sts):
                            sc_next = score(sts[n_ + 1])
                        E = Eb[it % 3]; P = Pb[it % 3]; it += 1
                        S_.act(E, sc, AF.Exp, scale=0.125)
                        if strips is not None:
                            off = tq * 512 - st_ * 128 + STRIP_C
                            S_.tt("dve" if it % 3 else "pool", P, E, strips[:, h, off:off + 512], ALU.mult)
                        else:
                            P = E
                        for j in range(4):
                            ok, last = jvalid(tq, j, st_)
                            if not ok:
                                continue
                            S_.mm(accv[:, j, :], P[:, j * 128:(j + 1) * 128], Vv[:, st_, h, :],
                                  start=False, stop=last, inc=True)
                    S_.op("dve", lambda g: g.reciprocal(_ap(rec), _ap(accv[:, :, 64])), [accv], [rec])
                    for j in range(4):
                        S_.ts("dve", ytok[:, 4 * tq + j, h * 64:(h + 1) * 64], accv[:, j, 0:64], rec[:, j:j + 1], ALU.mult)

    def proj_fm(dst, Wv, hT, nchunk, col0):
        n = 0
        for c in range(nchunk):
            for tg in range(4):
                ps = psum()
                for k in range(8):
                    S_.mm(ps, Wv[:, k, col0 + c * 128: col0 + (c + 1) * 128],
                          hT[:, k, tg * 512:(tg + 1) * 512], start=(k == 0), stop=(k == 7), inc=(k == 7))
                S_.cp("act" if n % 2 else "dve", dst[:, c, tg * 512:(tg + 1) * 512], ps)
                n += 1

    def ytok_to_yT(ytok, yT, nchunk):
        for t in range(NT):
            ps = psum().bitcast(BF16)
            for cc in range(nchunk):
                S_.tr(ps[:, cc * 128:(cc + 1) * 128], ytok[:, t, cc * 128:(cc + 1) * 128], identb, inc=(cc == nchunk - 1))
            S_.cp("act" if t % 2 else "dve", yT[:, 0:nchunk, t * 128:(t + 1) * 128],
                  ps[:, 0:nchunk * 128].re("p (k t) -> p k t", k=nchunk))

    def stage_att(b, hT, yT_a):
        with ExitStack() as es2:
            A2 = Alloc(nc, es2)
            Wqkv = A2.sb([128, 8, 3 * C], BF16, "Wqkv")
            S_.dma("pool", Wqkv, w_in[:, RWKV_IN:RWKV_IN + 3 * C].re("(k p) c -> p k c", p=128))
            strips = A2.sb([128, H, STRIP_W], BF16, "strips")
            for h in range(H):
                S_.dma("pool", strips[:, h, :], c_strip[h])
            QT = A2.sb([128, 3, S], BF16, "QT")
            KT = A2.sb([128, 3, S], BF16, "KT")
            Vt = A2.sb([128, NT, H, 65], BF16, "Vt")
            ytok = A2.sb([128, NT, C], BF16, "ytok")
            S_.memset("pool", Vt[:, :, :, 64:65], 1.0)
            proj_fm(QT, Wqkv, hT, 3, 0)
            proj_fm(KT, Wqkv, hT, 3, C)
            for t in range(NT):
                ps = psum()
                for k in range(8):
                    S_.mm(ps[:, 0:C], hT[:, k, t * 128:(t + 1) * 128], Wqkv[:, k, 2 * C:3 * C],
                          start=(k == 0), stop=(k == 7), inc=(k == 7))
                S_.cp("act" if t % 2 else "dve", Vt[:, t, :, 0:64], ps[:, 0:C].re("p (h d) -> p h d", h=H))

            def s_range(tq):
                s_hi = min(NT - 1, 4 * tq + 3 + 8)

                def jvalid(tq, j, st_):
                    tt_ = 4 * tq + j
                    return abs(tt_ - st_) <= 8, (st_ == s_hi and j == 3)
                return max(0, 4 * tq - 8), s_hi, jvalid
            attn_core(H, KT, QT, Vt, ytok, NT, s_range, strips)
            ytok_to_yT(ytok, yT_a, 3)

    def stage_mem(b, hT, yT_m):
        with ExitStack() as es3:
            A3 = Alloc(nc, es3)
            Wkv = A3.sb([128, 8, 512], BF16, "Wkv")
            S_.dma("pool", Wkv, w_mem_kv.re("(k p) c -> p k c", p=128))
            Wqm = A3.sb([128, 8, 256], BF16, "Wqm")
            S_.dma("pool", Wqm, w_in[:, RWKV_IN + 3 * C:IN_W].re("(k p) c -> p k c", p=128))
            memb = A3.sb([128, 2, D], BF16, "memb")
            S_.dma("pool", memb, mem_d[b * 256:(b + 1) * 256, :].re("(m p) d -> p m d", p=128))
            memT = A3.sb([128, 8, 256], BF16, "memT")
            for m in range(2):
                ps = psum().bitcast(BF16)
                for k in range(8):
                    S_.tr(ps[:, k * 128:(k + 1) * 128], memb[:, m, k * 128:(k + 1) * 128], identb, inc=(k == 7))
                S_.cp("act", memT[:, :, m * 128:(m + 1) * 128], ps.re("p (k t) -> p k t", k=8))
            KmT = A3.sb([128, 2, 256], BF16, "KmT")
            for c in range(2):
                ps = psum()
                for k in range(8):
                    S_.mm(ps[:, 0:256], Wkv[:, k, c * 128:(c + 1) * 128], memT[:, k, :],
                          start=(k == 0), stop=(k == 7), inc=(k == 7))
                S_.cp("dve", KmT[:, c, :], ps[:, 0:256])
            Vm = A3.sb([128, 2, 4, 65], BF16, "Vm")
            S_.memset("pool", Vm[:, :, :, 64:65], 1.0)
            for m in range(2):
                ps = psum()
                for k in range(8):
                    S_.mm(ps[:, 0:256], memT[:, k, m * 128:(m + 1) * 128], Wkv[:, k, 256:512],
                          start=(k == 0), stop=(k == 7), inc=(k == 7))
                S_.cp("dve", Vm[:, m, :, 0:64], ps[:, 0:256].re("p (h d) -> p h d", h=4))
            QmT = A3.sb([128, 2, S], BF16, "QmT")
            proj_fm(QmT, Wqm, hT, 2, 0)
            ytok = A3.sb([128, NT, 256], BF16, "ytokm")

            def s_range(tq):
                return 0, 1, (lambda tq, j, st_: (True, st_ == 1 and j == 3))
            attn_core(4, KmT, QmT, Vm, ytok, 2, s_range, None)
            ytok_to_yT(ytok, yT_m, 2)

    def stage_rwkv(b, hT, yT_r):
        with ExitStack() as es4:
            A4 = Alloc(nc, es4)
            f32t = lambda name, n=C: A4.sb([128, n], F32, name)
            Wr = A4.sb([128, 16, RWKV_IN], BF16, "Wr")
            with ExitStack() as esw:
                Aw = Alloc(nc, esw)
                mub = Aw.sb([128, RWKV_IN], F32, "mub"); bcast_load(mub, mu_d)
                omb = Aw.sb([128, RWKV_IN], F32, "omb"); hmb = Aw.sb([128, RWKV_IN], F32, "hmb")
                S_.ts("dve", omb, mub, -1.0, ALU.mult, 1.0, ALU.add)
                S_.ts("pool", hmb, mub, 0.5, ALU.mult)
                stg = [Aw.sb([128, 8, 336], F32, "stg") for _ in range(2)]
                for pc in range(4):
                    cs = slice(pc * 336, (pc + 1) * 336)
                    S_.dma("sp", stg[pc % 2], w_in[:, cs].re("(k p) c -> p k c", p=128))
                    S_.tt("dve", Wr[:, 0:8, cs], stg[pc % 2], omb[:, cs].un(1).bc([128, 8, 336]), ALU.mult)
                    S_.tt("pool", Wr[:, 8:16, cs], stg[pc % 2], hmb[:, cs].un(1).bc([128, 8, 336]), ALU.mult)
                S_.barrier()
            kkb = f32t("kkb"); bcast_load(kkb, k_k_d)
            kab = f32t("kab"); bcast_load(kab, k_a_d)
            omkab = f32t("omkab"); S_.ts("dve", omkab, kab, -1.0, ALU.mult, 1.0, ALU.add)
            rkb = f32t("rkb"); bcast_load(rkb, r_k_d)
            gngb = f32t("gngb"); bcast_load(gngb, gn_g_d)
            gnbb = f32t("gnbb"); bcast_load(gnbb, gn_b_d)
            w0b = f32t("w0b", 2 * C); bcast_load(w0b, w0_d)
            a0b = f32t("a0b", 2 * C); bcast_load(a0b, a0_d)
            gup = A4.sb([128, C], BF16, "gup"); S_.dma("pool", gup[0:64, :], g_up_d)
            wupbd = A4.sb([128, 2 * C], BF16, "wupbd"); S_.memset("pool", wupbd, 0.0)
            S_.dma("pool", wupbd[64:96, 0:C], w_up_d[0]); S_.dma("pool", wupbd[96:128, C:2 * C], w_up_d[1])
            aupbd = A4.sb([128, 2 * C], BF16, "aupbd"); S_.memset("pool", aupbd, 0.0)
            S_.dma("pool", aupbd[0:32, 0:C], a_up_d[0]); S_.dma("pool", aupbd[32:64, C:2 * C], a_up_d[1])
            Yacc = A4.sb([128, NT, C], F32, "Yacc")
            T32 = A4.sb([128, 3, 64], F32, "T32"); Tb = A4.sb([128, 3, 64], BF16, "Tb"); Ttmp = A4.sb([128, 3, 64], F32, "Ttmp")
            hs = A4.sb([128, 8, 128], BF16, "hs")
            r32 = f32t("r32"); k32 = f32t("k32"); v32 = f32t("v32")
            sgw = f32t("sgw"); a32 = f32t("a32"); a32b = f32t("a32b")
            ecw = f32t("ecw"); encw = f32t("encw")
            kk = f32t("kk"); sq = f32t("sq"); kd = f32t("kd"); bb = f32t("bb")
            ss = A4.sb([128, 6], F32, "ss"); rn = A4.sb([128, 6], F32, "rn")
            lor = A4.sb([128, 3, 128], BF16, "lor")
            tok4 = A4.sb([128, 4, C], BF16, "tok4")
            Pp = [A4.sb([128, 6, 128], BF16, "Pp") for _ in range(2)]
            PTp = [A4.sb([128, 6, 128], BF16, "PTp") for _ in range(2)]
            Zs = A4.sb([128, C], BF16, "Zs"); Us = A4.sb([128, C], BF16, "Us")
            fs1 = A4.sb([128, 6], F32, "fs1"); fs2 = A4.sb([128, 6], F32, "fs2"); fs3 = A4.sb([128, 6], F32, "fs3")
            yob = A4.sb([128, C], BF16, "yob")
            sets = []
            for i in range(2):
                sets.append(dict(
                    FM=A4.sb([128, 4, 3, 128], BF16, "FM"), Vb=A4.sb([128, C], BF16, "Vb"),
                    XT=A4.sb([128, 6, 128], BF16, "XT"),
                    LkT=A4.sb([128, 6, 128], BF16, "LkT"), RbT=A4.sb([128, 6, 128], BF16, "RbT"),
                    RkT=A4.sb([128, 6, 128], BF16, "RkT"), WCe=A4.sb([128, 3, 2], F32, "WCe"),
                    Bt=A4.sb([128, C], BF16, "Bt"), Kt=A4.sb([128, C], BF16, "Kt"),
                    g32=f32t("g32"), bv=f32t("bv")))

            def v6(x):
                return x.re("p (h j) -> p h j", h=6)
            HORD = (0, 2, 4, 1, 3, 5)

            def prep(t, d, st):
                lo = t * 128
                if 0 < t < NT - 1:
                    S_.tt("pool", hs, hT[:, :, lo - 1:lo + 127], hT[:, :, lo + 1:lo + 129], ALU.add)
                elif t == 0:
                    S_.tt("pool", hs[:, :, 1:128], hT[:, :, 0:127], hT[:, :, 2:129], ALU.add)
                    S_.cp("pool", hs[:, :, 0:1], hT[:, :, 1:2])
                else:
                    S_.tt("pool", hs[:, :, 0:127], hT[:, :, lo - 1:lo + 126], hT[:, :, lo + 1:lo + 128], ALU.add)
                    S_.cp("pool", hs[:, :, 127:128], hT[:, :, lo + 126:lo + 127])

                def lhs(kc):
                    return hT[:, kc, lo:lo + 128] if kc < 8 else hs[:, kc - 8, :]
                for part, dst in ((0, r32), (1, k32), (2, v32)):
                    ps = psum()
                    for kc in range(16):
                        S_.mm(ps[:, 0:C], lhs(kc), Wr[:, kc, part * C:(part + 1) * C], start=(kc == 0), stop=(kc == 15), inc=(kc == 15))
                    S_.cp("act", dst, ps[:, 0:C])
                S_.cp("pool", st["Vb"], v32)
                yield
                ps = psum()
                for kc in range(16):
                    S_.mm(ps[:, 0:128], Wr[:, kc, 3 * C:3 * C + 128], lhs(kc), start=(kc == 0), stop=(kc == 15), inc=(kc == 15))
                for kc in range(16):
                    S_.mm(ps[0:64, 128:256], Wr[:, kc, 3 * C + 128:3 * C + 192], lhs(kc), start=(kc == 0), stop=(kc == 15), inc=(kc == 15))
                S_.act(lor[0:64, 0, :], ps[0:64, 0:128], AF.Sigmoid)
                S_.act(lor[64:128, 1, :], ps[64:128, 0:128], AF.Tanh)
                S_.cp("act", lor[0:64, 2, :], ps[0:64, 128:256])
                yield
                psg = psum()
                S_.mm(psg[:, 0:C], lor[0:64, 0, :], gup[0:64, :], inc=True)
                S_.cp("act", st["g32"], psg[:, 0:C])
                psw = psum()
                S_.mm(psw[:, 0:C], lor[64:128, 1, :], wupbd[64:128, d * C:(d + 1) * C], inc=True)
                S_.tt("dve", sgw, psw[:, 0:C], w0b[:, d * C:(d + 1) * C], ALU.add)
                S_.act(sgw, sgw, AF.Sigmoid)
                psa = psum()
                S_.mm(psa[:, 0:C], lor[0:64, 2, :], aupbd[0:64, d * C:(d + 1) * C], inc=True)
                S_.tt("dve", a32, psa[:, 0:C], a0b[:, d * C:(d + 1) * C], ALU.add)
                S_.act(a32, a32, AF.Sigmoid)
                pscs = psum()
                S_.mm(pscs[:, 0:C], tri32[:, d, :], sgw, inc=True)
                pswc = psum()
                for c in range(3):
                    S_.mm(pswc[:, c * 2:(c + 1) * 2], sgw[:, c * 128:(c + 1) * 128], BLK, inc=(c == 2))
                S_.act(st["WCe"].re("p c q -> p (c q)"), pswc[:, 0:6], AF.Exp, scale=-LDS)
                S_.act(ecw, pscs[:, 0:C], AF.Exp, scale=-LDS)
                S_.act(encw, pscs[:, 0:C], AF.Exp, scale=LDS)
                S_.act(sgw, sgw, AF.Exp, scale=LDS)
                S_.tt("dve", sgw, sgw, ecw, ALU.mult)
                yield
                S_.tt("pool", kk, k32, kkb, ALU.mult)
                S_.tt("pool", sq, kk, kk, ALU.mult)
                S_.red(ss, v6(sq), ALU.add)
                S_.act(rn, ss, AF.Sqrt)
                S_.ts("dve", rn, rn, 1e-12, ALU.max)
                S_.op("dve", lambda g: g.reciprocal(_ap(rn), _ap(rn)), [rn], [rn])
                S_.tt("dve", v6(kk), v6(kk), rn.un(2).bc([128, 6, 64]), ALU.mult)
                S_.tt("pool", kd, a32, kab, ALU.mult)
                S_.tt("pool", kd, kd, omkab, ALU.add)
                S_.tt("pool", kd, kd, k32, ALU.mult)
                S_.tt("dve", bb, kk, a32, ALU.mult)
                S_.tt("dve", tok4[:, 0, :], kk, sgw, ALU.mult)
                S_.stt(tok4[:, 1, :], bb, -1.0, encw, ALU.mult, ALU.mult)
                S_.tt("pool", tok4[:, 2, :], kd, encw, ALU.mult)
                S_.tt("pool", tok4[:, 3, :], r32, ecw, ALU.mult)
                S_.cp("act", st["Bt"], tok4[:, 1, :])
                S_.cp("act", st["Kt"], tok4[:, 2, :])
                if d == 1:
                    psa2 = psum()
                    S_.mm(psa2[:, 0:C], lor[0:64, 2, :], aupbd[0:64, 0:C], inc=True)
                    S_.tt("dve", a32b, psa2[:, 0:C], a0b[:, 0:C], ALU.add)
                    S_.act(a32b, a32b, AF.Sigmoid)
                    S_.tt("pool", a32b, a32b, a32, ALU.add)
                    S_.tt("pool", a32b, a32b, kab, ALU.mult)
                    S_.stt(a32b, omkab, 2.0, a32b, ALU.mult, ALU.add)
                    S_.tt("pool", a32b, a32b, k32, ALU.mult)
                    S_.tt("pool", a32b, a32b, rkb, ALU.mult)
                    S_.tt("pool", a32b, a32b, r32, ALU.mult)
                    S_.red(fs3, v6(a32b), ALU.add)
                    S_.tt("dve", v6(st["bv"]), v6(v32), fs3.un(2).bc([128, 6, 64]), ALU.mult)
                yield
                FM = st["FM"]
                for half in range(2):
                    ps = psum().bitcast(BF16)
                    n = 0
                    for o in (2 * half, 2 * half + 1):
                        for c in range(3):
                            S_.tr(ps[:, n * 128:(n + 1) * 128], tok4[:, o, c * 128:(c + 1) * 128], identb, inc=(n == 5))
                            n += 1
                    S_.cp("act" if half else "dve", FM[:, 2 * half:2 * half + 2, :, :].re("p o c t -> p (o c) t"),
                          ps[:, 0:768].re("p (n t) -> p n t", n=6))
                KKi, Bi, Ki, Ri = 0, 1, 2, 3
                yield

                def cc_mm(li, ri):
                    psA = psum(); psB = psum()
                    for par, ps in ((0, psA), (1, psB)):
                        p0 = 64 * par
                        for i in range(3):
                            S_.mm(ps[:, i * 128:(i + 1) * 128], FM[p0:p0 + 64, li, i, :], FM[p0:p0 + 64, ri, i, :], inc=(i == 2))
                    return psA, psB

                def evac_cc(dst, pss, mask):
                    dv = dst.re("p (i two) t -> p i two t", two=2)
                    for par in range(2):
                        S_.tt("dve", dv[:, :, par, :], pss[par][:, 0:384].re("p (i t) -> p i t", i=3),
                              mask.un(1).bc([128, 3, 128]), ALU.mult)
                XT = st["XT"]
                pss = cc_mm(Bi, KKi)
                evac_cc(PTp[0], pss, MS[d])
                S_.tt("pool", XT, PTp[0], MID.un(1).bc([128, 6, 128]), ALU.add)
                yield
                pss = cc_mm(KKi, Bi)
                evac_cc(Pp[0], pss, MS[1 - d])
                yield
                pss = cc_mm(Ki, KKi)
                evac_cc(st["LkT"], pss, MS[d])
                yield
                pss = cc_mm(Bi, Ri)
                evac_cc(st["RbT"], pss, MI[d])
                yield
                pss = cc_mm(Ki, Ri)
                evac_cc(st["RkT"], pss, MI[d])
                def sq_mm(lt, rt):
                    psA = psum(); psB = psum()
                    for h in range(H):
                        ps = psA if h < 3 else psB
                        S_.mm(ps[:, (h % 3) * 128:(h % 3 + 1) * 128], lt[:, h, :], rt[:, h, :], inc=(h % 3 == 2))
                    return psA, psB

                def v3(ps):
                    return ps[:, 0:384].re("p (i t) -> p i t", i=3)
                cur = 0
                for lvl in range(1, 6):
                    nxt = 1 - cur
                    yield
                    pP = sq_mm(PTp[cur], Pp[cur])
                    if lvl < 5:
                        pPT = sq_mm(Pp[cur], PTp[cur])
                    for hh in range(2):
                        S_.cp("act", Pp[nxt][:, 3 * hh:3 * hh + 3, :], v3(pP[hh]))
                    if lvl < 5:
                        for hh in range(2):
                            S_.cp("dve", PTp[nxt][:, 3 * hh:3 * hh + 3, :], v3(pPT[hh]))
                    yield
                    pX = sq_mm(Pp[nxt], XT)
                    for hh in range(2):
                        S_.tt("dve", XT[:, 3 * hh:3 * hh + 3, :], v3(pX[hh]), XT[:, 3 * hh:3 * hh + 3, :], ALU.add)
                    cur = nxt
                st["XTf"] = XT

            def seq_chunk(t, q, d, st):
                q0 = 64 * q
                FM = st["FM"]; Vb = st["Vb"]; XT = st["XTf"]
                hc = lambda h: slice(h * 64, (h + 1) * 64)
                psZ = psum()
                S_.mm(psZ[q0:q0 + 64, 0:C], zeros_b[:, 0:64], zeros_b[:, 0:C], start=True, stop=False)
                for h in HORD:
                    c, p0 = h // 2, 64 * (h % 2)
                    S_.mm(psZ[q0:q0 + 64, hc(h)], FM[p0:p0 + 64, 0, c, q0:q0 + 64], Tb[p0:p0 + 64, c, :], start=False, stop=False)
                for h in range(H):
                    S_.mm(psZ[q0:q0 + 64, hc(h)], st["LkT"][q0:q0 + 64, h, q0:q0 + 64], Vb[q0:q0 + 64, hc(h)], start=False, stop=(h == H - 1), inc=(h == H - 1))
                S_.cp("act", Zs[q0:q0 + 64, :], psZ[q0:q0 + 64, 0:C])
                yield
                psU = psum()
                for h in range(H):
                    S_.mm(psU[q0:q0 + 64, hc(h)], XT[q0:q0 + 64, h, q0:q0 + 64], Zs[q0:q0 + 64, hc(h)], inc=(h == H - 1))
                S_.cp("dve", Us[q0:q0 + 64, :], psU[q0:q0 + 64, 0:C])
                yield
                psY = psum()
                S_.mm(psY[q0:q0 + 64, 0:C], zeros_b[:, 0:64], zeros_b[:, 0:C], start=True, stop=False)
                for h in HORD:
                    c, p0 = h // 2, 64 * (h % 2)
                    S_.mm(psY[q0:q0 + 64, hc(h)], FM[p0:p0 + 64, 3, c, q0:q0 + 64], Tb[p0:p0 + 64, c, :], start=False, stop=False)
                for h in range(H):
                    S_.mm(psY[q0:q0 + 64, hc(h)], st["RbT"][q0:q0 + 64, h, q0:q0 + 64], Us[q0:q0 + 64, hc(h)], start=False, stop=False)
                for h in range(H):
                    S_.mm(psY[q0:q0 + 64, hc(h)], st["RkT"][q0:q0 + 64, h, q0:q0 + 64], Vb[q0:q0 + 64, hc(h)], start=False, stop=(h == H - 1), inc=(h == H - 1))
                psT = psum()
                for h in range(H):
                    c, p0 = h // 2, 64 * (h % 2)
                    S_.mm(psT[p0:p0 + 64, c * 64:(c + 1) * 64], st["Bt"][q0:q0 + 64, hc(h)], Us[q0:q0 + 64, hc(h)], start=True, stop=False)
                    S_.mm(psT[p0:p0 + 64, c * 64:(c + 1) * 64], st["Kt"][q0:q0 + 64, hc(h)], Vb[q0:q0 + 64, hc(h)], start=False, stop=True, inc=(h == H - 1))
                if d == 0:
                    S_.cp("act", Yacc[q0:q0 + 64, t, :], psY[q0:q0 + 64, 0:C])
                else:
                    S_.tt("pool" if False else "dve", Yacc[q0:q0 + 64, t, :], psY[q0:q0 + 64, 0:C], Yacc[q0:q0 + 64, t, :], ALU.add)
                S_.tt("dve", Ttmp, psT[:, 0:192].re("p (c i) -> p c i", c=3), T32, ALU.add)
                S_.tt("dve", T32, Ttmp, st["WCe"][:, :, q:q + 1].bc([128, 3, 64]), ALU.mult)
                S_.cp("act", Tb, T32)
                yield

            def finalize(t, st):
                Y = Yacc[:, t, :]
                S_.red(fs1, v6(Y), ALU.add)
                S_.tt("pool", sq, Y, Y, ALU.mult)
                S_.red(fs2, v6(sq), ALU.add)
                S_.ts("dve", fs1, fs1, 1.0 / 64, ALU.mult)
                S_.tt("dve", fs3, fs1, fs1, ALU.mult)
                S_.stt(fs2, fs2, 1.0 / 64, fs3, ALU.mult, ALU.subtract)
                S_.ts("dve", fs2, fs2, GN_EPS, ALU.add)
                S_.act(fs2, fs2, AF.Sqrt)
                S_.op("dve", lambda g: g.reciprocal(_ap(fs2), _ap(fs2)), [fs2], [fs2])
                S_.tt("dve", v6(sq), v6(Y), fs1.un(2).bc([128, 6, 64]), ALU.subtract)
                S_.tt("dve", v6(sq), v6(sq), fs2.un(2).bc([128, 6, 64]), ALU.mult)
                S_.tt("pool", sq, sq, gngb, ALU.mult)
                S_.tt("pool", sq, sq, gnbb, ALU.add)
                S_.tt("pool", sq, sq, st["bv"], ALU.add)
                S_.tt("pool", yob, sq, st["g32"], ALU.mult)
                ps = psum().bitcast(BF16)
                for cc in range(3):
                    S_.tr(ps[:, cc * 128:(cc + 1) * 128], yob[:, cc * 128:(cc + 1) * 128], identb, inc=(cc == 2))
                S_.cp("act", yT_r[:, 0:3, t * 128:(t + 1) * 128], ps[:, 0:384].re("p (k t) -> p k t", k=3))

            def seqfin(t, d, st):
                for q in ((0, 1) if d == 0 else (1, 0)):
                    yield from seq_chunk(t, q, d, st)
                if d == 1:
                    finalize(t, st)
                yield

            def run_interleaved(gens):
                gens = list(gens)
                while gens:
                    for g_ in list(gens):
                        try:
                            next(g_)
                        except StopIteration:
                            gens.remove(g_)

            for d in (0, 1):
                S_.memset("dve", T32, 0.0)
                S_.memset("pool", Tb, 0.0)
                order = list(range(NT)) if d == 0 else list(range(NT - 1, -1, -1))
                run_interleaved([prep(order[0], d, sets[0])])
                for i, t in enumerate(order):
                    st = sets[i % 2]
                    gens = [seqfin(t, d, st)]
                    if i + 1 < NT:
                        gens.append(prep(order[i + 1], d, sets[(i + 1) % 2]))
                    run_interleaved(gens)

    def stage_out(b, yTs):
        with ExitStack() as es5:
            A5 = Alloc(nc, es5)
            Wo = A5.sb([128, 8, D], BF16, "Wo")
            S_.dma("pool", Wo, w_out.re("(k p) c -> p k c", p=128))
            gam = A5.sb([128, D], F32, "gam1"); bet = A5.sb([128, D], F32, "bet1")
            bcast_load(gam, ln1_g); bcast_load(bet, ln1_b)
            Wr32 = A5.sb([128, 8, NE], F32, "Wr32")
            S_.dma("sp", Wr32, w_router.re("(k p) e -> p k e", p=128))
            brb = A5.sb([128, NE], F32, "brb"); bcast_load(brb, b_router)
            h0t = [A5.sb([128, D], F32, "h0t") for _ in range(2)]
            zt = [A5.sb([128, D], F32, "zt") for _ in range(2)]
            h1t = [A5.sb([128, D], F32, "h1t") for _ in range(2)]
            h1T32 = [A5.sb([128, 8, 128], F32, "h1T32") for _ in range(2)]
            h1Tb = [A5.sb([128, 8, 128], BF16, "h1Tb") for _ in range(2)]
            hhi = [A5.sb([128, D], BF16, "hhi") for _ in range(2)]
            hlo = [A5.sb([128, D], BF16, "hlo") for _ in range(2)]
            tp = [dict(st=A5.sb([128, 2, 6], F32, "st"), mv=A5.sb([128, 2], F32, "mv"),
                       rs=A5.sb([128, 1], F32, "rs")) for _ in range(2)]
            lg = A5.sb([128, NE], F32, "lg"); mx8 = A5.sb([128, 8], F32, "mx8"); nmx = A5.sb([128, 1], F32, "nmx")
            ex = A5.sb([128, NE], F32, "ex"); mk = A5.sb([128, NE], F32, "mk"); sm = A5.sb([128, 1], F32, "sm")
            rt = dict(mkb=A5.sb([128, NE], BF16, "mkb"), pos=A5.sb([128, NE], F32, "pos"), vv=A5.sb([128, NE], F32, "vv"),
                      nd=A5.sb([128, NE], F32, "nd"), mx=A5.sb([128, 8], F32, "mxr"), sel=A5.sb([128, NE], F32, "sel"),
                      idf=A5.sb([128, 4], F32, "idf"))
            for t in range(NT):
                i = t % 2
                gt = b * NT + t
                grp, tin = gt // 8, gt % 8
                S_.dma("sp", h0t[i], h0_d[b][t * 128:(t + 1) * 128, :])
                for half in range(2):
                    ps = psum()
                    for k in range(8):
                        S_.mm(ps, yTs[k][:, t * 128:(t + 1) * 128], Wo[:, k, half * 512:(half + 1) * 512],
                              start=(k == 0), stop=(k == 7), inc=(k == 7))
                    S_.stt(zt[i][:, half * 512:(half + 1) * 512], h0t[i][:, half * 512:(half + 1) * 512], ALPHA, ps, ALU.mult, ALU.add)
                layernorm(es5, zt[i], gam, bet, h1t[i], tp[i])
                S_.dma("sp", h1_d[grp][tin * 128:(tin + 1) * 128, :], h1t[i])
                S_.cp("act", hhi[i], h1t[i])
                S_.tt("dve", hlo[i], h1t[i], hhi[i], ALU.subtract)
                for half in range(2):
                    ps = psum()
                    for k in range(4):
                        kk_ = half * 4 + k
                        S_.mm(ps[:, k * 128:(k + 1) * 128], hhi[i][:, kk_ * 128:(kk_ + 1) * 128], identb, start=True, stop=False)
                        S_.mm(ps[:, k * 128:(k + 1) * 128], hlo[i][:, kk_ * 128:(kk_ + 1) * 128], identb, start=False, stop=True, inc=(k == 3))
                    S_.cp("act", h1T32[i][:, half * 4:half * 4 + 4, :], ps.re("p (k t) -> p k t", k=4))
                    S_.cp("dve", h1Tb[i][:, half * 4:half * 4 + 4, :], h1T32[i][:, half * 4:half * 4 + 4, :])
                if "h1Tdma" not in SKIP:
                    S_.dma("sp", h1T_d[grp][:, :, tin * 128:(tin + 1) * 128], h1Tb[i])
                if "router" in SKIP:
                    continue
                ps = psum()
                for k in range(8):
                    S_.mm(ps[:, 0:NE], h1T32[i][:, k, :], Wr32[:, k, :], start=(k == 0), stop=(k == 7), inc=(k == 7))
                S_.tt("dve", lg, ps[:, 0:NE], brb, ALU.add)
                if "top" in SKIP:
                    continue
                S_.op("dve", lambda g: g.max(_ap(mx8), _ap(lg)), [lg], [mx8])
                S_.ts("dve", mk, lg, mx8[:, 3:4], ALU.is_ge)
                S_.ts("dve", nmx, mx8[:, 0:1], -1.0, ALU.mult)
                S_.act(ex, lg, AF.Exp, bias=nmx[:, 0:1])
                S_.tt("dve", ex, ex, mk, ALU.mult)
                S_.red(sm, ex, ALU.add)
                S_.op("dve", lambda g: g.reciprocal(_ap(sm), _ap(sm)), [sm], [sm])
                S_.ts("dve", Gall[:, gt, :], ex, sm[:, 0:1], ALU.mult)
                if SPARSE:
                    route_tile(gt, mk, hhi[i], rt)

    def stage_moe():
        with ExitStack() as es6:
            A6 = Alloc(nc, es6)
            gam = A6.sb([128, D], F32, "gam2"); bet = A6.sb([128, D], F32, "bet2")
            bcast_load(gam, ln2_g); bcast_load(bet, ln2_b)
            bguT = A6.sb([128, 16, NE], F32, "bguT")
            with ExitStack() as esb:
                Ab = Alloc(nc, esb)
                bgu_sb = Ab.sb([NE, 2 * D], F32, "bgu_sb")
                bgu_hi = Ab.sb([NE, 2 * D], BF16, "bgu_hi"); bgu_lo = Ab.sb([NE, 2 * D], BF16, "bgu_lo")
                S_.dma("sp", bgu_sb, b_gate_up)
                S_.cp("act", bgu_hi, bgu_sb)
                S_.tt("dve", bgu_lo, bgu_sb, bgu_hi, ALU.subtract)
                for fc in range(16):
                    ps = psum()
                    S_.mm(ps[:, 0:NE], bgu_hi[0:NE, fc * 128:(fc + 1) * 128], identb[0:NE, 0:NE], start=True, stop=False)
                    S_.mm(ps[:, 0:NE], bgu_lo[0:NE, fc * 128:(fc + 1) * 128], identb[0:NE, 0:NE], start=False, stop=True, inc=True)
                    S_.cp("act", bguT[:, fc, :], ps[:, 0:NE])
                S_.barrier()
            bd_sb = A6.sb([NE, D], F32, "bd_sb")
            S_.dma("sp", bd_sb, b_down)
            h1T = A6.sb([128, 8, 1024], BF16, "h1T")
            acc = A6.sb([128, 8, D], F32, "acc")
            wgu = [A6.sb([128, 8, 2, 256], BF16, "wgu") for _ in range(6)]
            wd = [A6.sb([128, 8, D], BF16, "wd") for _ in range(2)]
            actT = A6.sb([128, 8, 1024], BF16, "actT")
            g1 = [A6.sb([128, 512], F32, "g1") for _ in range(2)]
            s1 = [A6.sb([128, 512], F32, "s1") for _ in range(2)]
            u1 = [A6.sb([128, 512], F32, "u1") for _ in range(2)]
            GT = A6.sb([NE, 128], F32, "GT")
            Ghi = A6.sb([128, NE], BF16, "Ghi"); Glo = A6.sb([128, NE], BF16, "Glo")
            h1t = A6.sb([128, D], F32, "h1t2"); zt = A6.sb([128, D], F32, "zt2"); ot = A6.sb([128, D], F32, "ot2")
            tp = dict(st=A6.sb([128, 2, 6], F32, "st"), mv=A6.sb([128, 2], F32, "mv"), rs=A6.sb([128, 1], F32, "rs"))
            tmpd = [A6.sb([128, 512], F32, "tmpd") for _ in range(2)]
            npiece = 0
            it = 0
            nd = 0
            for g in range(4):
                S_.dma("sp", h1T, h1T_d[g])
                for tl in range(8):
                    gt = g * 8 + tl
                    ps = psum()
                    S_.cp("act", Ghi, Gall[:, gt, :])
                    S_.tt("dve", Glo, Gall[:, gt, :], Ghi, ALU.subtract)
                    S_.mm(ps[0:NE, 0:128], Ghi, identb, start=True, stop=False)
                    S_.mm(ps[0:NE, 0:128], Glo, identb, start=False, stop=True, inc=True)
                    S_.cp("act", GT, ps[0:NE, 0:128])
                    for half in range(2):
                        ps = psum()
                        S_.mm(ps, GT[0:NE, :], bd_sb[0:NE, half * 512:(half + 1) * 512], inc=True)
                        S_.cp("act" if half else "dve", acc[:, tl, half * 512:(half + 1) * 512], ps)
                for e in range(NE):
                    wde = wd[e % 2]
                    if "moe_w" not in SKIP:
                        S_.dma("pool", wde, w_down[e].re("(k p) c -> p k c", p=128))
                    src = w_gate_up[e].re("(k p) (u f) -> p k u f", p=128, u=2)
                    for p in range(4):
                        wp = wgu[npiece % 6]; npiece += 1
                        for u_ in range(2):
                            if "moe_w" not in SKIP:
                                S_.dma("pool", wp[:, :, u_, :], src[:, :, u_, p * 256:(p + 1) * 256])
                        if "moe_gu" in SKIP:
                            continue
                        for sg in range(2):
                            for f2 in range(2):
                                fc = 2 * p + f2
                                i = it % 2; it += 1
                                psg = psum(); psu = psum()
                                for k in range(8):
                                    S_.mm(psg, wp[:, k, 0, f2 * 128:(f2 + 1) * 128], h1T[:, k, sg * 512:(sg + 1) * 512],
                                          start=(k == 0), stop=(k == 7), inc=(k == 7))
                                for k in range(8):
                                    S_.mm(psu, wp[:, k, 1, f2 * 128:(f2 + 1) * 128], h1T[:, k, sg * 512:(sg + 1) * 512],
                                          start=(k == 0), stop=(k == 7), inc=(k == 7))
                                S_.ts("dve", g1[i], psg, bguT[:, fc, e:e + 1], ALU.add, 7.0, ALU.min)
                                S_.act(s1[i], g1[i], AF.Sigmoid, scale=1.702)
                                S_.act(u1[i], psu, AF.Identity, bias=bguT[:, 8 + fc, e:e + 1])
                                S_.ts("pool", u1[i], u1[i], 7.0, ALU.min, -7.0, ALU.max)
                                S_.tt("pool", g1[i], g1[i], s1[i], ALU.mult)
                                S_.stt(actT[:, fc, sg * 512:(sg + 1) * 512], u1[i], 1.0, g1[i], ALU.add, ALU.mult)
                    for tl in range(8):
                        gt = g * 8 + tl
                        if "moe_down" in SKIP:
                            continue
                        for half in range(2):
                            ps = psum()
                            for fc in range(8):
                                S_.mm(ps, actT[:, fc, tl * 128:(tl + 1) * 128], wde[:, fc, half * 512:(half + 1) * 512],
                                      start=(fc == 0), stop=(fc == 7), inc=(fc == 7))
                            a_ = acc[:, tl, half * 512:(half + 1) * 512]
                            td = tmpd[nd % 2]; nd += 1
                            S_.ts("dve", td, ps, Gall[:, gt, e:e + 1], ALU.mult)
                            S_.tt("pool", a_, a_, td, ALU.add)
                for tl in range(8):
                    gt = g * 8 + tl
                    S_.dma("sp", h1t, h1_d[g][tl * 128:(tl + 1) * 128, :])
                    S_.stt(zt, h1t, ALPHA, acc[:, tl, :], ALU.mult, ALU.add)
                    layernorm(es6, zt, gam, bet, ot, tp)
                    S_.dma("sp", out_d[gt * 128:(gt + 1) * 128, :], ot)

    def route_tile(gt, mk, hb_tile, rt):
        mkb = rt["mkb"]; pos = rt["pos"]; vv = rt["vv"]; nd = rt["nd"]; mx = rt["mx"]; sel = rt["sel"]; idf = rt["idf"]
        S_.cp("dve", mkb, mk)
        ps = psum()
        S_.mm(ps[:, 0:NE], utri_b, mkb, inc=True)
        ps2 = psum()
        S_.mm(ps2[:, 0:NE], ones_b, mkb, inc=True)
        S_.tt("dve", pos, ps[:, 0:NE], carry, ALU.add)
        S_.tt("dve", carry, ps2[:, 0:NE], carry, ALU.add)
        S_.ts("dve", vv, pos, float(CAP), ALU.is_lt)
        S_.tt("dve", vv, vv, mk, ALU.mult)
        S_.tt("dve", pos, pos, erow, ALU.add)
        S_.ts("dve", nd, pos, -1.0, ALU.mult, BIGF, ALU.add)
        S_.tt("dve", nd, nd, vv, ALU.mult)
        S_.ts("dve", nd, nd, -BIGF, ALU.add)
        S_.op("dve", lambda g: g.max(_ap(mx), _ap(nd)), [nd], [mx])
        S_.ts("dve", idf, mx[:, 0:4], -1.0, ALU.mult, float(NE * CAP), ALU.min)
        S_.cp("dve", idx_all[:, gt, :], idf)
        for k in range(4):
            S_.ts("dve", sel, nd, mx[:, k:k + 1], ALU.is_equal)
            S_.tt("dve", sel, sel, Gall[:, gt, :], ALU.mult)
            S_.red(gk_all[:, gt, k:k + 1], sel, ALU.add)
        S_.ts("dve", idf, mx[:, 0:4], -0.5 * BIGF, ALU.is_gt)
        S_.tt("dve", gk_all[:, gt, :], gk_all[:, gt, :], idf, ALU.mult)
        for k in range(4):
            S_.idma_scatter(x_rows, idx_all[:, gt, k:k + 1], hb_tile, NE * CAP - 1)

    def stage_moe_sparse():
        S_.barrier()
        with ExitStack() as es6:
            A6 = Alloc(nc, es6)
            bguT = A6.sb([128, 16, NE], F32, "bguT")
            with ExitStack() as esb:
                Ab = Alloc(nc, esb)
                bgu_sb = Ab.sb([NE, 2 * D], F32, "bgu_sb")
                bgu_hi = Ab.sb([NE, 2 * D], BF16, "bgu_hi"); bgu_lo = Ab.sb([NE, 2 * D], BF16, "bgu_lo")
                S_.dma("sp", bgu_sb, b_gate_up)
                S_.cp("act", bgu_hi, bgu_sb)
                S_.tt("dve", bgu_lo, bgu_sb, bgu_hi, ALU.subtract)
                for fc in range(16):
                    ps = psum()
                    S_.mm(ps[:, 0:NE], bgu_hi[0:NE, fc * 128:(fc + 1) * 128], identb[0:NE, 0:NE], start=True, stop=False)
                    S_.mm(ps[:, 0:NE], bgu_lo[0:NE, fc * 128:(fc + 1) * 128], identb[0:NE, 0:NE], start=False, stop=True, inc=True)
                    S_.cp("act", bguT[:, fc, :], ps[:, 0:NE])
                S_.barrier()
            with ExitStack() as ese:
                Ae = Alloc(nc, ese)
                NS = CAP // 128
                xe = [Ae.sb([128, NS, D], BF16, "xe") for _ in range(2)]
                xT = [Ae.sb([128, 8, CAP], BF16, "xT") for _ in range(2)]
                wgu = [Ae.sb([128, 8, 2 * D], BF16, "wgu") for _ in range(2)]
                wd = [Ae.sb([128, 8, D], BF16, "wd") for _ in range(2)]
                actT = Ae.sb([128, 8, CAP], BF16, "actT")
                g1 = [Ae.sb([128, 512], F32, "g1") for _ in range(2)]
                s1 = [Ae.sb([128, 512], F32, "s1") for _ in range(2)]
                u1 = [Ae.sb([128, 512], F32, "u1") for _ in range(2)]
                yo = [Ae.sb([128, D], F32, "yo") for _ in range(2)]
                blocks = [(0, 512)] + ([(512, CAP - 512)] if CAP > 512 else [])
                issued = [0]

                wds = [Ae.sb([128, 2, D], F32, "wds") for _ in range(2)]

                def wd_src(e, j):
                    return w_down[e].re("(k p) c -> p k c", p=128)[:, 2 * j:2 * j + 2, :]

                def wd_dma(e, j):
                    S_.dma("sp", wds[j % 2], wd_src(e, j))

                def wd_cast(e, j):
                    S_.cp("act", wd[e % 2][:, 2 * j:2 * j + 2, :], wds[j % 2])

                def issue_weights(upto):
                    while issued[0] <= min(upto, NE - 1):
                        e = issued[0]; issued[0] += 1
                        S_.dma("pool", wgu[e % 2], w_gate_up[e].re("(k p) f -> p k f", p=128))
                        if e == 0:
                            for j in range(4):
                                wd_dma(0, j)
                                wd_cast(0, j)
                        else:
                            wd_dma(e, 0)
                            wd_dma(e, 1)

                def load_x(e):
                    S_.dma("sp", xe[e % 2], x_rows[e * CAP:(e + 1) * CAP, :].re("(n p) d -> p n d", p=128))
                    for n in range(NS):
                        ps = psum().bitcast(BF16)
                        for k in range(8):
                            S_.tr(ps[:, k * 128:(k + 1) * 128], xe[e % 2][:, n, k * 128:(k + 1) * 128], identb, inc=(k == 7))
                        S_.cp("act" if n % 2 else "dve", xT[e % 2][:, :, n * 128:(n + 1) * 128], ps.re("p (k t) -> p k t", k=8))
                it = 0
                load_x(0)
                for e in range(NE):
                    if e + 1 < NE:
                        load_x(e + 1)
                    xTe = xT[e % 2]
                    issue_weights(e + 1)
                    wge = wgu[e % 2]
                    for p in range(4):
                        if e + 1 < NE:
                            wd_cast(e + 1, p)
                            if p + 2 < 4:
                                wd_dma(e + 1, p + 2)
                        for (c0, cn) in blocks:
                            for f2 in range(2):
                                fc = 2 * p + f2
                                i = it % 2; it += 1
                                psg = psum(); psu = psum()
                                for k in range(8):
                                    S_.mm(psg[:, 0:cn], wge[:, k, fc * 128:(fc + 1) * 128], xTe[:, k, c0:c0 + cn],
                                          start=(k == 0), stop=(k == 7), inc=(k == 7))
                                for k in range(8):
                                    S_.mm(psu[:, 0:cn], wge[:, k, D + fc * 128:D + (fc + 1) * 128], xTe[:, k, c0:c0 + cn],
                                          start=(k == 0), stop=(k == 7), inc=(k == 7))
                                gg = g1[i][:, 0:cn]; sg_ = s1[i][:, 0:cn]; uu = u1[i][:, 0:cn]
                                S_.ts("dve", gg, psg[:, 0:cn], bguT[:, fc, e:e + 1], ALU.add, 7.0, ALU.min)
                                S_.act(sg_, gg, AF.Sigmoid, scale=1.702)
                                S_.act(uu, psu[:, 0:cn], AF.Identity, bias=bguT[:, 8 + fc, e:e + 1])
                                S_.ts("pool", uu, uu, 7.0, ALU.min, -7.0, ALU.max)
                                S_.tt("pool", gg, gg, sg_, ALU.mult)
                                S_.stt(actT[:, fc, c0:c0 + cn], uu, 1.0, gg, ALU.add, ALU.mult)
                    wde = wd[e % 2]
                    for n in range(NS):
                        yt_ = yo[n % 2]
                        for half in range(2):
                            ps = psum()
                            for fc in range(8):
                                S_.mm(ps, actT[:, fc, n * 128:(n + 1) * 128], wde[:, fc, half * 512:(half + 1) * 512],
                                      start=(fc == 0), stop=(fc == 7), inc=(fc == 7))
                            S_.cp("act" if half else "dve", yt_[:, half * 512:(half + 1) * 512], ps)
                        r0 = e * CAP + n * 128
                        S_.dma("sp", V(y_rows.ap[r0:r0 + 128, :], [Buf("yrow")]), yt_)
            S_.barrier()
            with ExitStack() as esc:
                Ac = Alloc(nc, esc)
                gam = Ac.sb([128, D], F32, "gam2"); bet = Ac.sb([128, D], F32, "bet2")
                bcast_load(gam, ln2_g); bcast_load(bet, ln2_b)
                bd_sb = Ac.sb([NE, D], F32, "bd_sb")
                S_.dma("sp", bd_sb, b_down)
                GT = Ac.sb([NE, 128], F32, "GT")
                Ghi = Ac.sb([128, NE], BF16, "Ghi"); Glo = Ac.sb([128, NE], BF16, "Glo")
                yk = [[Ac.sb([128, D], F32, "yk") for _ in range(4)] for _ in range(2)]
                h1t = [Ac.sb([128, D], F32, "h1t2") for _ in range(2)]
                zt = Ac.sb([128, D], F32, "zt2"); ot = Ac.sb([128, D], F32, "ot2")
                tmpc = [Ac.sb([128, D], F32, "tmpc") for _ in range(2)]
                tp = dict(st=Ac.sb([128, 2, 6], F32, "st"), mv=Ac.sb([128, 2], F32, "mv"), rs=Ac.sb([128, 1], F32, "rs"))

                def fetch(gt):
                    for k in range(4):
                        S_.idma_gather(yk[gt % 2][k], y_rows, idx_all[:, gt, k:k + 1], NE * CAP - 1)
                    S_.dma("sp", h1t[gt % 2], h1_d[gt // 8][(gt % 8) * 128:(gt % 8 + 1) * 128, :])
                fetch(0)
                for gt in range(NB * NT):
                    if gt + 1 < NB * NT:
                        fetch(gt + 1)
                    S_.cp("act", Ghi, Gall[:, gt, :])
                    S_.tt("dve", Glo, Gall[:, gt, :], Ghi, ALU.subtract)
                    ps = psum()
                    S_.mm(ps[0:NE, 0:128], Ghi, identb, start=True, stop=False)
                    S_.mm(ps[0:NE, 0:128], Glo, identb, start=False, stop=True, inc=True)
                    S_.cp("act", GT, ps[0:NE, 0:128])
                    for half in range(2):
                        ps = psum()
                        S_.mm(ps, GT[0:NE, :], bd_sb[0:NE, half * 512:(half + 1) * 512], inc=True)
                        S_.stt(zt[:, half * 512:(half + 1) * 512], h1t[gt % 2][:, half * 512:(half + 1) * 512], ALPHA, ps, ALU.mult, ALU.add)
                    for k in range(4):
                        S_.act(tmpc[k % 2], yk[gt % 2][k], AF.Copy, scale=gk_all[:, gt, k:k + 1])
                        S_.tt("dve", zt, zt, tmpc[k % 2], ALU.add)
                    layernorm(esc, zt, gam, bet, ot, tp, eng2="dve")
                    S_.dma("sp", out_d[gt * 128:(gt + 1) * 128, :], ot)

    msk = G.sb([128, 5 * 128 + 2], F32, "msk")
    S_.dma("sp", msk, c_masks)
    tri32 = G.sb([128, 2, 128], F32, "tri32")
    S_.dma("sp", tri32, c_tri.re("d p t -> p d t"))
    MS = [msk[:, 0:128], msk[:, 256:384]]
    MI = [msk[:, 128:256], msk[:, 384:512]]
    MID = msk[:, 512:640]
    BLK = msk[:, 640:642]

    def dbg_out(name, src, shape, dt):
        if name in debug:
            dbg[name] = V(nc.dram_tensor("dbg_" + name, list(shape), dt, kind="ExternalOutput").ap(), [Buf("dbg_" + name)])
            S_.dma("sp", dbg[name], src)

    for b in range(NB):
        with ExitStack() as esA:
            A = Alloc(nc, esA)
            hT = A.sb([128, 8, S], BF16, "hT")
            stage_s1(b, hT)
            S_.barrier()
            if b == 0:
                dbg_out("hT", hT, [128, 8, S], BF16)
            yT_r = A.sb([128, 3, S], BF16, "yT_r")
            if "rwkv" in stages:
                stage_rwkv(b, hT, yT_r)
                S_.barrier()
            yT_a = A.sb([128, 3, S], BF16, "yT_a")
            if "att" in stages:
                stage_att(b, hT, yT_a)
                S_.barrier()
            yT_m = A.sb([128, 2, S], BF16, "yT_m")
            if "mem" in stages:
                stage_mem(b, hT, yT_m)
                S_.barrier()
            if b == 0:
                dbg_out("yT_r", yT_r, [128, 3, S], BF16)
                dbg_out("yT_a", yT_a, [128, 3, S], BF16)
                dbg_out("yT_m", yT_m, [128, 2, S], BF16)
            if "out" in stages:
                stage_out(b, [yT_r[:, 0], yT_r[:, 1], yT_r[:, 2], yT_a[:, 0], yT_a[:, 1], yT_a[:, 2], yT_m[:, 0], yT_m[:, 1]])
                S_.barrier()
    if "h1" in debug:
        dbg["h1"] = V(nc.dram_tensor("dbg_h1", [1024, D], F32, kind="ExternalOutput").ap(), [Buf("dbg_h1")])
        S_.dma("sp", dbg["h1"], h1_d[0])
        dbg["G"] = V(nc.dram_tensor("dbg_G", [128, NB * NT, NE], F32, kind="ExternalOutput").ap(), [Buf("dbg_G")])
        S_.dma("sp", dbg["G"], Gall)

    if moe:
        if SPARSE:
            stage_moe_sparse()
        else:
            stage_moe()
    S_.barrier()
    es.close()
    return nc, S_


def host_constants():
    ident = np.eye(128, dtype=np.float32)
    slopes = np.exp2(-8.0 * np.arange(1, H + 1, dtype=np.float32) / H).astype(np.float32)
    x = np.arange(STRIP_W)[None, :]
    p = np.arange(128)[:, None]
    delta = x - p - STRIP_C
    ad = np.abs(delta)
    mult = ((ad <= 64).astype(np.float32) + ((delta % 4 == 0) & (ad <= 256)).astype(np.float32)
            + ((delta % 16 == 0) & (ad <= 1024)).astype(np.float32))
    strip = np.stack([mult * np.exp(-(slopes[h] * ad.astype(np.float32))) for h in range(H)]).astype(np.float32)
    masks = np.zeros((128, 5 * 128 + 2), np.float32)
    si = np.arange(64)[:, None]; ti = np.arange(64)[None, :]
    for q in range(2):
        r = slice(64 * q, 64 * q + 64)
        for n_, m_ in enumerate(((si < ti), (si <= ti), (si > ti), (si >= ti), (si == ti))):
            masks[r, n_ * 128 + 64 * q:n_ * 128 + 64 * q + 64] = m_
        masks[r, 640 + q] = 1.0
    tri = np.zeros((2, 128, 128), np.float32)
    for q in range(2):
        r = slice(64 * q, 64 * q + 64)
        tri[0, r, r] = (si <= ti); tri[1, r, r] = (si >= ti)
    utri = (np.arange(128)[:, None] < np.arange(128)[None, :]).astype(np.float32)
    erow = np.tile((np.arange(NE, dtype=np.float32) * CAP)[None, :], (128, 1))
    return dict(c_ident=ident, c_strip=strip, c_masks=masks, c_tri=tri, c_utri=utri, c_erow=erow)


def make_in_maps(inputs):
    consts = host_constants()
    maps = []
    sq = lambda a: np.ascontiguousarray(a)
    for i in range(8):
        m = dict(consts)
        m["x"] = sq(inputs["x"][2 * i:2 * i + 2].reshape(NB * S, D))
        m["mem"] = sq(inputs["mem"][2 * i:2 * i + 2].reshape(NB * 256, D))
        for k in ("ln_in_g", "ln_in_b"):
            m[k] = sq(inputs[k])
        m["w_in"] = sq(inputs["w_in"][0])
        m["mu_shift"] = sq(inputs["mu_shift"][0])
        m["w0"] = sq(inputs["w0"][0].reshape(-1)); m["w_up"] = sq(inputs["w_up"][0])
        m["a0"] = sq(inputs["a0"][0].reshape(-1)); m["a_up"] = sq(inputs["a_up"][0])
        m["g_up"] = sq(inputs["g_up"][0])
        for k in ("k_k", "k_a", "gn_g", "gn_b", "ln1_g", "ln1_b", "ln2_g", "ln2_b", "b_router",
                  "w_mem_kv", "w_out", "w_router", "w_gate_up", "b_gate_up", "w_down", "b_down"):
            m[k] = sq(inputs[k][0])
        m["r_k"] = sq(inputs["r_k"][0].reshape(-1))
        maps.append(m)
    return maps


def kernel(**inputs):
    inputs = {k: np.asarray(v) for k, v in inputs.items()}
    nc, _ = build_program()
    maps = make_in_maps(inputs)
    res = run_bass_kernel_spmd(nc, maps, core_ids=list(range(8)))
    out = np.stack([r["out"].reshape(NB, S, D) for r in res.results]).reshape(16, S, D)
    return out.astype(np.float32)
```

```python
import numpy as np
from contextlib import ExitStack
import concourse.bass as bass
import concourse.mybir as mybir
from concourse.bass_utils import run_bass_kernel_spmd

F32 = mybir.dt.float32
BF16 = mybir.dt.bfloat16
U32 = mybir.dt.uint32
AF = mybir.ActivationFunctionType
ALU = mybir.AluOpType
AX = mybir.AxisListType

D = 1024
S = 2048
NB = 2
NT = S // 128
C = 384
H = 6
RWKV_IN = 1344
IN_W = 2752
NE = 32
ALPHA = 2.0 ** 0.25
LN_EPS = 1e-5
GN_EPS = 64e-5
LDS = float(np.exp(-0.5))
STRIP_W = 3200
STRIP_C = 1536
CAP = 640
BIGF = 1.0e6
import os
SKIP = os.environ.get('KSKIP', '')
SPARSE = os.environ.get('KDENSE', '') == ''


class Buf:
    __slots__ = ("name", "w", "rs")

    def __init__(self, name):
        self.name = name
        self.w = None
        self.rs = []


class Ev:
    __slots__ = ("sem", "val", "eng")

    def __init__(self, eng):
        self.sem = None
        self.val = None
        self.eng = eng


class V:
    __slots__ = ("ap", "bufs")

    def __init__(self, ap, bufs):
        self.ap = ap
        self.bufs = bufs if isinstance(bufs, (list, tuple)) else [bufs]

    def __getitem__(self, k):
        return V(self.ap[k], self.bufs)

    def re(self, pat, **kw):
        return V(self.ap.rearrange(pat, **kw), self.bufs)

    def bc(self, shape):
        return V(self.ap.to_broadcast(list(shape)), self.bufs)

    def un(self, axis):
        return V(self.ap.unsqueeze(axis), self.bufs)

    def bitcast(self, dt):
        return V(self.ap.bitcast(dt), self.bufs)

    def on(self, bufs):
        return V(self.ap, bufs)


def _ap(x):
    return x.ap if isinstance(x, V) else x


class Sched:
    ENG = ("pe", "act", "dve", "pool", "sp")
    NQ = 6

    def __init__(self, nc, es):
        self.nc = nc
        self.es = es
        self.eng = dict(pe=nc.tensor, act=nc.scalar, dve=nc.vector, pool=nc.gpsimd, sp=nc.sync)
        self.sem = {k: es.enter_context(nc.semaphore("s_" + k)) for k in self.ENG}
        self.cnt = {k: 0 for k in self.ENG}
        self.dsem = {q: [es.enter_context(nc.semaphore("d_%s%d" % (q, i))) for i in range(self.NQ)]
                     for q in ("sp", "pool")}
        self.dcnt = {q: 0 for q in self.dsem}
        self.seen = {k: {} for k in self.ENG}
        self.pending = {k: [] for k in self.ENG}
        self.all_dma = []
        self.ninst = 0

    def _wait(self, e, ev):
        assert ev.val is not None, "dependency on an unresolved (non-inc) event"
        key = id(ev.sem)
        if self.seen[e].get(key, 0) >= ev.val:
            return
        self.eng[e].wait_ge(ev.sem, ev.val)
        self.seen[e][key] = ev.val

    def _deps(self, e, reads, writes, is_dma):
        for v in reads:
            for b in v.bufs:
                if b.w is not None:
                    if b.w.eng == e and e == "pe" and not is_dma:
                        continue
                    self._wait(e, b.w)
        for v in writes:
            for b in v.bufs:
                if b.w is not None and (is_dma or b.w.eng != e or e != "pe"):
                    self._wait(e, b.w)
                for r in b.rs:
                    if is_dma or r.eng != e or e != "pe":
                        self._wait(e, r)

    def _record(self, ev, reads, writes):
        for v in reads:
            for b in v.bufs:
                if ev.eng is not None:
                    b.rs = [r for r in b.rs if r.eng != ev.eng]
                b.rs.append(ev)
        for v in writes:
            for b in v.bufs:
                b.w = ev
                b.rs = []

    def op(self, e, fn, reads, writes, inc=True):
        reads = [r for r in reads if isinstance(r, V)]
        writes = [w for w in writes if isinstance(w, V)]
        self._deps(e, reads, writes, False)
        ins = fn(self.eng[e])
        self.ninst += 1
        ev = Ev(e)
        if inc:
            self.cnt[e] += 1
            ins.then_inc(self.sem[e], 1)
            ev.sem = self.sem[e]
            ev.val = self.cnt[e]
            for p in self.pending[e]:
                p.sem = ev.sem
                p.val = ev.val
            self.pending[e] = []
        else:
            self.pending[e].append(ev)
        self._record(ev, reads, writes)
        return ev

    def dma(self, q, out, in_, **kw):
        self._deps(q, [in_], [out], True)
        n = self.dcnt[q]
        self.dcnt[q] += 1
        sem = self.dsem[q][n % self.NQ]
        val = 16 * (n // self.NQ + 1)
        if n >= self.NQ:
            pe_ = Ev(None); pe_.sem = sem; pe_.val = val - 16
            self._wait(q, pe_)
        ins = self.eng[q].dma_start(out=_ap(out), in_=_ap(in_), **kw)
        ins.then_inc(sem, 16)
        self.ninst += 1
        ev = Ev(None)
        ev.sem = sem
        ev.val = val
        self._record(ev, [in_], [out])
        self.all_dma.append(ev)
        return ev

    def _idma(self, out, out_off, in_, in_off, reads, writes, bound):
        q = "pool"
        self._deps(q, reads, writes, True)
        n = self.dcnt[q]
        self.dcnt[q] += 1
        sem = self.dsem[q][n % self.NQ]
        val = 16 * (n // self.NQ + 1)
        if n >= self.NQ:
            pe_ = Ev(None); pe_.sem = sem; pe_.val = val - 16
            self._wait(q, pe_)
        ins = self.eng[q].indirect_dma_start(out=_ap(out), out_offset=out_off, in_=_ap(in_), in_offset=in_off)
        ins.then_inc(sem, 16)
        self.ninst += 1
        ev = Ev(None)
        ev.sem = sem
        ev.val = val
        self._record(ev, reads, writes)
        return ev

    def idma_scatter(self, dram, idx, src, bound):
        off = bass.IndirectOffsetOnAxis(ap=_ap(idx), axis=0)
        return self._idma(dram, off, src, None, [src, idx], [V(dram.ap, [Buf("scatter")])], bound)

    def idma_gather(self, dst, dram, idx, bound):
        off = bass.IndirectOffsetOnAxis(ap=_ap(idx), axis=0)
        return self._idma(dst, None, dram, off, [idx], [dst], bound)

    def barrier(self):
        for e in self.ENG:
            for o in self.ENG:
                if self.cnt[o] > 0:
                    assert not self.pending[o]
                    ev = Ev(o)
                    ev.sem = self.sem[o]
                    ev.val = self.cnt[o]
                    self._wait(e, ev)
            for q in self.dsem:
                n = self.dcnt[q]
                for i in range(min(n, self.NQ)):
                    ev = Ev(None)
                    ev.sem = self.dsem[q][i]
                    ev.val = 16 * ((n - 1 - i) // self.NQ + 1)
                    self._wait(e, ev)

    def _pe_rows(self, lhsT):
        a = _ap(lhsT)
        lo = a.base_partition()
        rows = (lo, lo + a.shape[0])
        prev = getattr(self, "_last_pe", None)
        if prev is not None:
            pins, pev, prow = prev
            if rows[0] >= prow[1] or prow[0] >= rows[1]:
                if pev.val is None:
                    self.cnt["pe"] += 1
                    pins.then_inc(self.sem["pe"], 1)
                    for p in self.pending["pe"]:
                        p.sem = self.sem["pe"]
                        p.val = self.cnt["pe"]
                    self.pending["pe"] = []
                self._wait("pe", pev)
        return rows

    def _pe_op(self, fn, lhsT, reads, writes, inc):
        rows = self._pe_rows(lhsT)
        box = []

        def f(e):
            i = fn(e)
            box.append(i)
            return i
        ev = self.op("pe", f, reads, writes, inc=inc)
        self._last_pe = (box[0], ev, rows)
        return ev

    def mm(self, out, lhsT, rhs, start=True, stop=True, inc=False):
        return self._pe_op(lambda e: e.matmul(_ap(out), _ap(lhsT), _ap(rhs), start=start, stop=stop),
                           lhsT, [lhsT, rhs], [out], inc)

    def tr(self, out, in_, ident, inc=False):
        return self._pe_op(lambda e: e.transpose(_ap(out), _ap(in_), _ap(ident)), in_, [in_, ident], [out], inc)

    def act(self, out, in_, func, bias=None, scale=None, accum_out=None):
        kw = {}
        if bias is not None:
            kw["bias"] = _ap(bias)
        if scale is not None:
            kw["scale"] = _ap(scale)
        if accum_out is not None:
            kw["accum_out"] = _ap(accum_out)
        return self.op("act", lambda e: e.activation(_ap(out), _ap(in_), func, **kw),
                       [in_, bias, scale], [out, accum_out])

    def ts(self, e, out, in0, s1, op0, s2=None, op1=None, accum_out=None):
        kw = {}
        if op1 is not None:
            kw["op1"] = op1
        if accum_out is not None:
            kw["accum_out"] = _ap(accum_out)
        return self.op(e, lambda g: g.tensor_scalar(_ap(out), _ap(in0), _ap(s1), _ap(s2), op0, **kw),
                       [in0, s1, s2], [out, accum_out])

    def tt(self, e, out, in0, in1, op):
        return self.op(e, lambda g: g.tensor_tensor(_ap(out), _ap(in0), _ap(in1), op), [in0, in1], [out])

    def stt(self, out, in0, scalar, in1, op0, op1):
        return self.op("dve", lambda g: g.scalar_tensor_tensor(_ap(out), _ap(in0), _ap(scalar), _ap(in1), op0, op1),
                       [in0, scalar, in1], [out])

    def cp(self, e, out, in_):
        if e == "act":
            return self.op("act", lambda g: g.copy(_ap(out), _ap(in_)), [in_], [out])
        return self.op(e, lambda g: g.tensor_copy(_ap(out), _ap(in_)), [in_], [out])

    def red(self, out, in_, op, axis=AX.X):
        return self.op("dve", lambda g: g.tensor_reduce(_ap(out), _ap(in_), axis, op), [in_], [out])

    def memset(self, e, out, val):
        return self.op(e, lambda g: g.memset(_ap(out), val), [], [out])


class Alloc:
    N = [0]

    def __init__(self, nc, es):
        self.nc = nc
        self.es = es

    def sb(self, shape, dt, name=None, nbuf=1):
        Alloc.N[0] += 1
        name = "%s_%d" % (name or "t", Alloc.N[0])
        t = self.es.enter_context(self.nc.sbuf_tensor(name, list(shape), dt))
        return V(t[:], [Buf(name)])

    def dram(self, name, shape, dt, kind="Internal"):
        t = self.nc.dram_tensor(name, list(shape), dt, kind=kind)
        return V(t.ap(), [Buf(name)])


def build_program(debug=(), moe=True, stages=('rwkv', 'att', 'mem', 'out')):
    nc = bass.Bass("TRN2", target_bir_lowering=False)
    es = ExitStack()
    S_ = Sched(nc, es)
    G = Alloc(nc, es)
    dbg = {}

    def din(name, shape, dt=F32):
        return V(nc.dram_tensor(name, list(shape), dt, kind="ExternalInput").ap(), [Buf(name)])

    x_d = din("x", [NB * S, D])
    mem_d = din("mem", [NB * 256, D])
    ln_in_g = din("ln_in_g", [D]); ln_in_b = din("ln_in_b", [D])
    w_in = din("w_in", [D, IN_W])
    mu_d = din("mu_shift", [RWKV_IN])
    w0_d = din("w0", [2 * C]); w_up_d = din("w_up", [2, 32, C])
    a0_d = din("a0", [2 * C]); a_up_d = din("a_up", [2, 32, C])
    g_up_d = din("g_up", [64, C])
    k_k_d = din("k_k", [C]); k_a_d = din("k_a", [C]); r_k_d = din("r_k", [C])
    gn_g_d = din("gn_g", [C]); gn_b_d = din("gn_b", [C])
    w_mem_kv = din("w_mem_kv", [D, 512])
    w_out = din("w_out", [D, D])
    ln1_g = din("ln1_g", [D]); ln1_b = din("ln1_b", [D])
    w_router = din("w_router", [D, NE]); b_router = din("b_router", [NE])
    if moe:
        w_gate_up = din("w_gate_up", [NE, D, 2 * D]); b_gate_up = din("b_gate_up", [NE, 2 * D])
        w_down = din("w_down", [NE, D, D]); b_down = din("b_down", [NE, D])
    ln2_g = din("ln2_g", [D]); ln2_b = din("ln2_b", [D])
    c_ident = din("c_ident", [128, 128])
    c_strip = din("c_strip", [H, 128, STRIP_W])
    c_masks = din("c_masks", [128, 5 * 128 + 2])
    c_tri = din("c_tri", [2, 128, 128])
    c_utri = din("c_utri", [128, 128])
    c_erow = din("c_erow", [128, NE])

    out_d = V(nc.dram_tensor("out", [NB * S, D], F32, kind="ExternalOutput").ap(), [Buf("out")])

    h0_d = [G.dram("h0_%d" % b, [S, D], F32) for b in range(NB)]
    h1_d = [G.dram("h1_%d" % g, [1024, D], F32) for g in range(4)]
    h1T_d = [G.dram("h1T_%d" % g, [128, 8, 1024], BF16) for g in range(4)]

    banks = []
    for i in range(8):
        t = es.enter_context(nc.psum_tensor("ps%d" % i, [128, 512], F32))
        banks.append(V(t[:], [Buf("ps%d" % i)]))
    bank_i = {"gen": 0, "acc": 0}
    bank_grp = {"gen": [2, 3, 4, 5, 6, 7], "acc": [0, 1]}

    def psum(grp="gen"):
        l = bank_grp[grp]
        b = banks[l[bank_i[grp] % len(l)]]
        bank_i[grp] += 1
        return b

    ident32 = G.sb([128, 128], F32, "ident32")
    identb = G.sb([128, 128], BF16, "identb")
    S_.dma("sp", ident32, c_ident)
    S_.cp("dve", identb, ident32)
    Gall = G.sb([128, NB * NT, NE], F32, "Gall")
    ones_b = G.sb([128, 128], BF16, "ones_b")
    S_.memset("dve", ones_b, 1.0)
    zeros_b = G.sb([128, 512], BF16, "zeros_b")
    S_.memset("dve", zeros_b, 0.0)
    carry = G.sb([128, NE], F32, "carry")
    S_.memset("dve", carry, 0.0)
    idx_all = G.sb([128, NB * NT, 4], U32, "idx_all")
    gk_all = G.sb([128, NB * NT, 4], F32, "gk_all")
    utri32 = G.sb([128, 128], F32, "utri32")
    S_.dma("sp", utri32, c_utri)
    utri_b = G.sb([128, 128], BF16, "utri_b")
    S_.cp("dve", utri_b, utri32)
    erow = G.sb([128, NE], F32, "erow")
    S_.dma("sp", erow, c_erow)
    x_rows = G.dram("x_rows", [NE * CAP + 128, D], BF16)
    y_rows = G.dram("y_rows", [NE * CAP + 128, D], F32)
    es_z = ExitStack()
    zeros32 = Alloc(nc, es_z).sb([128, D], F32, "zeros32")
    S_.memset("pool", zeros32, 0.0)
    S_.dma("sp", V(y_rows.ap[NE * CAP:NE * CAP + 128, :], [Buf("ydump")]), zeros32)
    zb = zeros32.bitcast(BF16).re("p (n d) -> p n d", n=2)
    xr = x_rows.re("(n p) d -> p n d", p=128)
    nrow = (NE * CAP + 128) // 128
    for n0 in range(0, nrow, 2):
        nn = min(2, nrow - n0)
        S_.dma("sp", V(xr.ap[:, n0:n0 + nn, :], [Buf("xfill")]), zb[:, 0:nn, :])
    S_.barrier()
    es_z.close()

    def bcast_load(dst, src1d, q="sp"):
        S_.dma(q, dst, V(src1d.ap.partition_broadcast(128), src1d.bufs))

    def layernorm(es2, xin, gam, bet, out, tmp_pool, eng2="pool"):
        st = tmp_pool["st"]; mv = tmp_pool["mv"]; rs = tmp_pool["rs"]
        for i in range(2):
            S_.op("dve", lambda g, i=i: g.bn_stats(_ap(st[:, i, :]), _ap(xin[:, i * 512:(i + 1) * 512])),
                  [xin], [st])
        S_.op("dve", lambda g: g.bn_aggr(_ap(mv), _ap(st)), [st], [mv])
        S_.ts("dve", rs, mv[:, 1:2], LN_EPS, ALU.add)
        S_.act(rs, rs, AF.Sqrt)
        S_.op("dve", lambda g: g.reciprocal(_ap(rs), _ap(rs)), [rs], [rs])
        S_.ts("dve", out, xin, mv[:, 0:1], ALU.subtract, rs[:, 0:1], ALU.mult)
        S_.tt(eng2, out, out, gam, ALU.mult)
        S_.tt(eng2, out, out, bet, ALU.add)

    def stage_s1(b, hT):
        with ExitStack() as es1:
            A1 = Alloc(nc, es1)
            gam = A1.sb([128, D], F32, "gam"); bet = A1.sb([128, D], F32, "bet")
            bcast_load(gam, ln_in_g); bcast_load(bet, ln_in_b)
            xt = [A1.sb([128, D], F32, "xt") for _ in range(2)]
            ht = [A1.sb([128, D], F32, "ht") for _ in range(2)]
            hb = [A1.sb([128, D], BF16, "hb") for _ in range(2)]
            tp = [dict(st=A1.sb([128, 2, 6], F32, "st"), mv=A1.sb([128, 2], F32, "mv"),
                       rs=A1.sb([128, 1], F32, "rs")) for _ in range(2)]
            for t in range(NT):
                i = t % 2
                S_.dma("sp", xt[i], x_d[b * S + t * 128: b * S + (t + 1) * 128, :])
                layernorm(es1, xt[i], gam, bet, ht[i], tp[i])
                S_.dma("sp", h0_d[b][t * 128:(t + 1) * 128, :], ht[i])
                S_.cp("act", hb[i], ht[i])
                ps = psum().bitcast(BF16)
                for k in range(8):
                    S_.tr(ps[:, k * 128:(k + 1) * 128], hb[i][:, k * 128:(k + 1) * 128], identb, inc=(k == 7))
                S_.cp("act", hT[:, :, t * 128:(t + 1) * 128], ps.re("p (k t) -> p k t", k=8))

    def attn_core(nh, KTv, QTv, Vv, ytok, n_st, s_range, strips=None):
        with ExitStack() as esc:
            Ac = Alloc(nc, esc)
            Eb = [Ac.sb([128, 512], BF16, "E") for _ in range(3)]
            Pb = [Ac.sb([128, 512], BF16, "P") for _ in range(3)]
            rec = Ac.sb([128, 4], F32, "rec")
            it = 0
            for h in range(nh):
                c, p0 = h // 2, 64 * (h % 2)
                for tq in range(4):
                    s_lo, s_hi, jvalid = s_range(tq)
                    acc = psum("acc")
                    accv = acc[:, 0:260].re("p (j d) -> p j d", j=4)
                    S_.mm(acc[:, 0:260], zeros_b[:, 0:128], zeros_b[:, 0:260], start=True, stop=False)
                    sts = list(range(s_lo, s_hi + 1))

                    def score(st_):
                        sc = psum()
                        S_.mm(sc, KTv[p0:p0 + 64, c, st_ * 128:(st_ + 1) * 128],
                              QTv[p0:p0 + 64, c, tq * 512:(tq + 1) * 512], inc=True)
                        return sc
                    sc_next = score(sts[0])
                    for n_, st_ in enumerate(sts):
                        sc = sc_next
                        if n_ + 1 < len(sts):
                            sc_next = score(sts[n_ + 1])
                        E = Eb[it % 3]; P = Pb[it % 3]; it += 1
                        S_.act(E, sc, AF.Exp, scale=0.125)
                        if strips is not None:
                            off = tq * 512 - st_ * 128 + STRIP_C
                            S_.tt("dve" if it % 3 else "pool", P, E, strips[:, h, off:off + 512], ALU.mult)
                        else:
                            P = E
                        for j in range(4):
                            ok, last = jvalid(tq, j, st_)
                            if not ok:
                                continue
                            S_.mm(accv[:, j, :], P[:, j * 128:(j + 1) * 128], Vv[:, st_, h, :],
                                  start=False, stop=last, inc=True)
                    S_.op("dve", lambda g: g.reciprocal(_ap(rec), _ap(accv[:, :, 64])), [accv], [rec])
                    for j in range(4):
                        S_.ts("dve", ytok[:, 4 * tq + j, h * 64:(h + 1) * 64], accv[:, j, 0:64], rec[:, j:j + 1], ALU.mult)

    def proj_fm(dst, Wv, hT, nchunk, col0):
        n = 0
        for c in range(nchunk):
            for tg in range(4):
                ps = psum()
                for k in range(8):
                    S_.mm(ps, Wv[:, k, col0 + c * 128: col0 + (c + 1) * 128],
                          hT[:, k, tg * 512:(tg + 1) * 512], start=(k == 0), stop=(k == 7), inc=(k == 7))
                S_.cp("act" if n % 2 else "dve", dst[:, c, tg * 512:(tg + 1) * 512], ps)
                n += 1

    def ytok_to_yT(ytok, yT, nchunk):
        for t in range(NT):
            ps = psum().bitcast(BF16)
            for cc in range(nchunk):
                S_.tr(ps[:, cc * 128:(cc + 1) * 128], ytok[:, t, cc * 128:(cc + 1) * 128], identb, inc=(cc == nchunk - 1))
            S_.cp("act" if t % 2 else "dve", yT[:, 0:nchunk, t * 128:(t + 1) * 128],
                  ps[:, 0:nchunk * 128].re("p (k t) -> p k t", k=nchunk))

    def stage_att(b, hT, yT_a):
        with ExitStack() as es2:
            A2 = Alloc(nc, es2)
            Wqkv = A2.sb([128, 8, 3 * C], BF16, "Wqkv")
            S_.dma("pool", Wqkv, w_in[:, RWKV_IN:RWKV_IN + 3 * C].re("(k p) c -> p k c", p=128))
            strips = A2.sb([128, H, STRIP_W], BF16, "strips")
            for h in range(H):
                S_.dma("pool", strips[:, h, :], c_strip[h])
            QT = A2.sb([128, 3, S], BF16, "QT")
            KT = A2.sb([128, 3, S], BF16, "KT")
            Vt = A2.sb([128, NT, H, 65], BF16, "Vt")
            ytok = A2.sb([128, NT, C], BF16, "ytok")
            S_.memset("pool", Vt[:, :, :, 64:65], 1.0)
            proj_fm(QT, Wqkv, hT, 3, 0)
            proj_fm(KT, Wqkv, hT, 3, C)
            for t in range(NT):
                ps = psum()
                for k in range(8):
                    S_.mm(ps[:, 0:C], hT[:, k, t * 128:(t + 1) * 128], Wqkv[:, k, 2 * C:3 * C],
                          start=(k == 0), stop=(k == 7), inc=(k == 7))
                S_.cp("act" if t % 2 else "dve", Vt[:, t, :, 0:64], ps[:, 0:C].re("p (h d) -> p h d", h=H))

            def s_range(tq):
                s_hi = min(NT - 1, 4 * tq + 3 + 8)

                def jvalid(tq, j, st_):
                    tt_ = 4 * tq + j
                    return abs(tt_ - st_) <= 8, (st_ == s_hi and j == 3)
                return max(0, 4 * tq - 8), s_hi, jvalid
            attn_core(H, KT, QT, Vt, ytok, NT, s_range, strips)
            ytok_to_yT(ytok, yT_a, 3)

    def stage_mem(b, hT, yT_m):
        with ExitStack() as es3:
            A3 = Alloc(nc, es3)
            Wkv = A3.sb([128, 8, 512], BF16, "Wkv")
            S_.dma("pool", Wkv, w_mem_kv.re("(k p) c -> p k c", p=128))
            Wqm = A3.sb([128, 8, 256], BF16, "Wqm")
            S_.dma("pool", Wqm, w_in[:, RWKV_IN + 3 * C:IN_W].re("(k p) c -> p k c", p=128))
            memb = A3.sb([128, 2, D], BF16, "memb")
            S_.dma("pool", memb, mem_d[b * 256:(b + 1) * 256, :].re("(m p) d -> p m d", p=128))
            memT = A3.sb([128, 8, 256], BF16, "memT")
            for m in range(2):
                ps = psum().bitcast(BF16)
                for k in range(8):
                    S_.tr(ps[:, k * 128:(k + 1) * 128], memb[:, m, k * 128:(k + 1) * 128], identb, inc=(k == 7))
                S_.cp("act", memT[:, :, m * 128:(m + 1) * 128], ps.re("p (k t) -> p k t", k=8))
            KmT = A3.sb([128, 2, 256], BF16, "KmT")
            for c in range(2):
                ps = psum()
                for k in range(8):
                    S_.mm(ps[:, 0:256], Wkv[:, k, c * 128:(c + 1) * 128], memT[:, k, :],
                          start=(k == 0), stop=(k == 7), inc=(k == 7))
                S_.cp("dve", KmT[:, c, :], ps[:, 0:256])
            Vm = A3.sb([128, 2, 4, 65], BF16, "Vm")
            S_.memset("pool", Vm[:, :, :, 64:65], 1.0)
            for m in range(2):
                ps = psum()
                for k in range(8):
                    S_.mm(ps[:, 0:256], memT[:, k, m * 128:(m + 1) * 128], Wkv[:, k, 256:512],
                          start=(k == 0), stop=(k == 7), inc=(k == 7))
                S_.cp("dve", Vm[:, m, :, 0:64], ps[:, 0:256].re("p (h d) -> p h d", h=4))
            QmT = A3.sb([128, 2, S], BF16, "QmT")
            proj_fm(QmT, Wqm, hT, 2, 0)
            ytok = A3.sb([128, NT, 256], BF16, "ytokm")

            def s_range(tq):
                return 0, 1, (lambda tq, j, st_: (True, st_ == 1 and j == 3))
            attn_core(4, KmT, QmT, Vm, ytok, 2, s_range, None)
            ytok_to_yT(ytok, yT_m, 2)

    def stage_rwkv(b, hT, yT_r):
        with ExitStack() as es4:
            A4 = Alloc(nc, es4)
            f32t = lambda name, n=C: A4.sb([128, n], F32, name)
            Wr = A4.sb([128, 16, RWKV_IN], BF16, "Wr")
            with ExitStack() as esw:
                Aw = Alloc(nc, esw)
                mub = Aw.sb([128, RWKV_IN], F32, "mub"); bcast_load(mub, mu_d)
                omb = Aw.sb([128, RWKV_IN], F32, "omb"); hmb = Aw.sb([128, RWKV_IN], F32, "hmb")
                S_.ts("dve", omb, mub, -1.0, ALU.mult, 1.0, ALU.add)
                S_.ts("pool", hmb, mub, 0.5, ALU.mult)
                stg = [Aw.sb([128, 8, 336], F32, "stg") for _ in range(2)]
                for pc in range(4):
                    cs = slice(pc * 336, (pc + 1) * 336)
                    S_.dma("sp", stg[pc % 2], w_in[:, cs].re("(k p) c -> p k c", p=128))
                    S_.tt("dve", Wr[:, 0:8, cs], stg[pc % 2], omb[:, cs].un(1).bc([128, 8, 336]), ALU.mult)
                    S_.tt("pool", Wr[:, 8:16, cs], stg[pc % 2], hmb[:, cs].un(1).bc([128, 8, 336]), ALU.mult)
                S_.barrier()
            kkb = f32t("kkb"); bcast_load(kkb, k_k_d)
            kab = f32t("kab"); bcast_load(kab, k_a_d)
            omkab = f32t("omkab"); S_.ts("dve", omkab, kab, -1.0, ALU.mult, 1.0, ALU.add)
            rkb = f32t("rkb"); bcast_load(rkb, r_k_d)
            gngb = f32t("gngb"); bcast_load(gngb, gn_g_d)
            gnbb = f32t("gnbb"); bcast_load(gnbb, gn_b_d)
            w0b = f32t("w0b", 2 * C); bcast_load(w0b, w0_d)
            a0b = f32t("a0b", 2 * C); bcast_load(a0b, a0_d)
            gup = A4.sb([128, C], BF16, "gup"); S_.dma("pool", gup[0:64, :], g_up_d)
            wupbd = A4.sb([128, 2 * C], BF16, "wupbd"); S_.memset("pool", wupbd, 0.0)
            S_.dma("pool", wupbd[64:96, 0:C], w_up_d[0]); S_.dma("pool", wupbd[96:128, C:2 * C], w_up_d[1])
            aupbd = A4.sb([128, 2 * C], BF16, "aupbd"); S_.memset("pool", aupbd, 0.0)
            S_.dma("pool", aupbd[0:32, 0:C], a_up_d[0]); S_.dma("pool", aupbd[32:64, C:2 * C], a_up_d[1])
            Yacc = A4.sb([128, NT, C], F32, "Yacc")
            T32 = A4.sb([128, 3, 64], F32, "T32"); Tb = A4.sb([128, 3, 64], BF16, "Tb"); Ttmp = A4.sb([128, 3, 64], F32, "Ttmp")
            hs = A4.sb([128, 8, 128], BF16, "hs")
            r32 = f32t("r32"); k32 = f32t("k32"); v32 = f32t("v32")
            sgw = f32t("sgw"); a32 = f32t("a32"); a32b = f32t("a32b")
            ecw = f32t("ecw"); encw = f32t("encw")
            kk = f32t("kk"); sq = f32t("sq"); kd = f32t("kd"); bb = f32t("bb")
            ss = A4.sb([128, 6], F32, "ss"); rn = A4.sb([128, 6], F32, "rn")
            lor = A4.sb([128, 3, 128], BF16, "lor")
            tok4 = A4.sb([128, 4, C], BF16, "tok4")
            Pp = [A4.sb([128, 6, 128], BF16, "Pp") for _ in range(2)]
            PTp = [A4.sb([128, 6, 128], BF16, "PTp") for _ in range(2)]
            Zs = A4.sb([128, C], BF16, "Zs"); Us = A4.sb([128, C], BF16, "Us")
            fs1 = A4.sb([128, 6], F32, "fs1"); fs2 = A4.sb([128, 6], F32, "fs2"); fs3 = A4.sb([128, 6], F32, "fs3")
            yob = A4.sb([128, C], BF16, "yob")
            sets = []
            for i in range(2):
                sets.append(dict(
                    FM=A4.sb([128, 4, 3, 128], BF16, "FM"), Vb=A4.sb([128, C], BF16, "Vb"),
                    XT=A4.sb([128, 6, 128], BF16, "XT"),
                    LkT=A4.sb([128, 6, 128], BF16, "LkT"), RbT=A4.sb([128, 6, 128], BF16, "RbT"),
                    RkT=A4.sb([128, 6, 128], BF16, "RkT"), WCe=A4.sb([128, 3, 2], F32, "WCe"),
                    Bt=A4.sb([128, C], BF16, "Bt"), Kt=A4.sb([128, C], BF16, "Kt"),
                    g32=f32t("g32"), bv=f32t("bv")))

            def v6(x):
                return x.re("p (h j) -> p h j", h=6)
            HORD = (0, 2, 4, 1, 3, 5)

            def prep(t, d, st):
                lo = t * 128
                if 0 < t < NT - 1:
                    S_.tt("pool", hs, hT[:, :, lo - 1:lo + 127], hT[:, :, lo + 1:lo + 129], ALU.add)
                elif t == 0:
                    S_.tt("pool", hs[:, :, 1:128], hT[:, :, 0:127], hT[:, :, 2:129], ALU.add)
                    S_.cp("pool", hs[:, :, 0:1], hT[:, :, 1:2])
                else:
                    S_.tt("pool", hs[:, :, 0:127], hT[:, :, lo - 1:lo + 126], hT[:, :, lo + 1:lo + 128], ALU.add)
                    S_.cp("pool", hs[:, :, 127:128], hT[:, :, lo + 126:lo + 127])

                def lhs(kc):
                    return hT[:, kc, lo:lo + 128] if kc < 8 else hs[:, kc - 8, :]
                for part, dst in ((0, r32), (1, k32), (2, v32)):
                    ps = psum()
                    for kc in range(16):
                        S_.mm(ps[:, 0:C], lhs(kc), Wr[:, kc, part * C:(part + 1) * C], start=(kc == 0), stop=(kc == 15), inc=(kc == 15))
                    S_.cp("act", dst, ps[:, 0:C])
                S_.cp("pool", st["Vb"], v32)
                yield
                ps = psum()
                for kc in range(16):
                    S_.mm(ps[:, 0:128], Wr[:, kc, 3 * C:3 * C + 128], lhs(kc), start=(kc == 0), stop=(kc == 15), inc=(kc == 15))
                for kc in range(16):
                    S_.mm(ps[0:64, 128:256], Wr[:, kc, 3 * C + 128:3 * C + 192], lhs(kc), start=(kc == 0), stop=(kc == 15), inc=(kc == 15))
                S_.act(lor[0:64, 0, :], ps[0:64, 0:128], AF.Sigmoid)
                S_.act(lor[64:128, 1, :], ps[64:128, 0:128], AF.Tanh)
                S_.cp("act", lor[0:64, 2, :], ps[0:64, 128:256])
                yield
                psg = psum()
                S_.mm(psg[:, 0:C], lor[0:64, 0, :], gup[0:64, :], inc=True)
                S_.cp("act", st["g32"], psg[:, 0:C])
                psw = psum()
                S_.mm(psw[:, 0:C], lor[64:128, 1, :], wupbd[64:128, d * C:(d + 1) * C], inc=True)
                S_.tt("dve", sgw, psw[:, 0:C], w0b[:, d * C:(d + 1) * C], ALU.add)
                S_.act(sgw, sgw, AF.Sigmoid)
                psa = psum()
                S_.mm(psa[:, 0:C], lor[0:64, 2, :], aupbd[0:64, d * C:(d + 1) * C], inc=True)
                S_.tt("dve", a32, psa[:, 0:C], a0b[:, d * C:(d + 1) * C], ALU.add)
                S_.act(a32, a32, AF.Sigmoid)
                pscs = psum()
                S_.mm(pscs[:, 0:C], tri32[:, d, :], sgw, inc=True)
                pswc = psum()
                for c in range(3):
                    S_.mm(pswc[:, c * 2:(c + 1) * 2], sgw[:, c * 128:(c + 1) * 128], BLK, inc=(c == 2))
                S_.act(st["WCe"].re("p c q -> p (c q)"), pswc[:, 0:6], AF.Exp, scale=-LDS)
                S_.act(ecw, pscs[:, 0:C], AF.Exp, scale=-LDS)
                S_.act(encw, pscs[:, 0:C], AF.Exp, scale=LDS)
                S_.act(sgw, sgw, AF.Exp, scale=LDS)
                S_.tt("dve", sgw, sgw, ecw, ALU.mult)
                yield
                S_.tt("pool", kk, k32, kkb, ALU.mult)
                S_.tt("pool", sq, kk, kk, ALU.mult)
                S_.red(ss, v6(sq), ALU.add)
                S_.act(rn, ss, AF.Sqrt)
                S_.ts("dve", rn, rn, 1e-12, ALU.max)
                S_.op("dve", lambda g: g.reciprocal(_ap(rn), _ap(rn)), [rn], [rn])
                S_.tt("dve", v6(kk), v6(kk), rn.un(2).bc([128, 6, 64]), ALU.mult)
                S_.tt("pool", kd, a32, kab, ALU.mult)
                S_.tt("pool", kd, kd, omkab, ALU.add)
                S_.tt("pool", kd, kd, k32, ALU.mult)
                S_.tt("dve", bb, kk, a32, ALU.mult)
                S_.tt("dve", tok4[:, 0, :], kk, sgw, ALU.mult)
                S_.stt(tok4[:, 1, :], bb, -1.0, encw, ALU.mult, ALU.mult)
                S_.tt("pool", tok4[:, 2, :], kd, encw, ALU.mult)
                S_.tt("pool", tok4[:, 3, :], r32, ecw, ALU.mult)
                S_.cp("act", st["Bt"], tok4[:, 1, :])
                S_.cp("act", st["Kt"], tok4[:, 2, :])
                if d == 1:
                    psa2 = psum()
                    S_.mm(psa2[:, 0:C], lor[0:64, 2, :], aupbd[0:64, 0:C], inc=True)
                    S_.tt("dve", a32b, psa2[:, 0:C], a0b[:, 0:C], ALU.add)
                    S_.act(a32b, a32b, AF.Sigmoid)
                    S_.tt("pool", a32b, a32b, a32, ALU.add)
                    S_.tt("pool", a32b, a32b, kab, ALU.mult)
                    S_.stt(a32b, omkab, 2.0, a32b, ALU.mult, ALU.add)
                    S_.tt("pool", a32b, a32b, k32, ALU.mult)
                    S_.tt("pool", a32b, a32b, rkb, ALU.mult)
                    S_.tt("pool", a32b, a32b, r32, ALU.mult)
                    S_.red(fs3, v6(a32b), ALU.add)
                    S_.tt("dve", v6(st["bv"]), v6(v32), fs3.un(2).bc([128, 6, 64]), ALU.mult)
                yield
                FM = st["FM"]
                for half in range(2):
                    ps = psum().bitcast(BF16)
                    n = 0
                    for o in (2 * half, 2 * half + 1):
                        for c in range(3):
                            S_.tr(ps[:, n * 128:(n + 1) * 128], tok4[:, o, c * 128:(c + 1) * 128], identb, inc=(n == 5))
                            n += 1
                    S_.cp("act" if half else "dve", FM[:, 2 * half:2 * half + 2, :, :].re("p o c t -> p (o c) t"),
                          ps[:, 0:768].re("p (n t) -> p n t", n=6))
                KKi, Bi, Ki, Ri = 0, 1, 2, 3
                yield

                def cc_mm(li, ri):
                    psA = psum(); psB = psum()
                    for par, ps in ((0, psA), (1, psB)):
                        p0 = 64 * par
                        for i in range(3):
                            S_.mm(ps[:, i * 128:(i + 1) * 128], FM[p0:p0 + 64, li, i, :], FM[p0:p0 + 64, ri, i, :], inc=(i == 2))
                    return psA, psB

                def evac_cc(dst, pss, mask):
                    dv = dst.re("p (i two) t -> p i two t", two=2)
                    for par in range(2):
                        S_.tt("dve", dv[:, :, par, :], pss[par][:, 0:384].re("p (i t) -> p i t", i=3),
                              mask.un(1).bc([128, 3, 128]), ALU.mult)
                XT = st["XT"]
                pss = cc_mm(Bi, KKi)
                evac_cc(PTp[0], pss, MS[d])
                S_.tt("pool", XT, PTp[0], MID.un(1).bc([128, 6, 128]), ALU.add)
                yield
                pss = cc_mm(KKi, Bi)
                evac_cc(Pp[0], pss, MS[1 - d])
                yield
                pss = cc_mm(Ki, KKi)
                evac_cc(st["LkT"], pss, MS[d])
                yield
                pss = cc_mm(Bi, Ri)
                evac_cc(st["RbT"], pss, MI[d])
                yield
                pss = cc_mm(Ki, Ri)
                evac_cc(st["RkT"], pss, MI[d])
                def sq_mm(lt, rt):
                    psA = psum(); psB = psum()
                    for h in range(H):
                        ps = psA if h < 3 else psB
                        S_.mm(ps[:, (h % 3) * 128:(h % 3 + 1) * 128], lt[:, h, :], rt[:, h, :], inc=(h % 3 == 2))
                    return psA, psB

                def v3(ps):
                    return ps[:, 0:384].re("p (i t) -> p i t", i=3)
                cur = 0
                for lvl in range(1, 6):
                    nxt = 1 - cur
                    yield
                    pP = sq_mm(PTp[cur], Pp[cur])
                    if lvl < 5:
                        pPT = sq_mm(Pp[cur], PTp[cur])
                    for hh in range(2):
                        S_.cp("act", Pp[nxt][:, 3 * hh:3 * hh + 3, :], v3(pP[hh]))
                    if lvl < 5:
                        for hh in range(2):
                            S_.cp("dve", PTp[nxt][:, 3 * hh:3 * hh + 3, :], v3(pPT[hh]))
                    yield
                    pX = sq_mm(Pp[nxt], XT)
                    for hh in range(2):
                        S_.tt("dve", XT[:, 3 * hh:3 * hh + 3, :], v3(pX[hh]), XT[:, 3 * hh:3 * hh + 3, :], ALU.add)
                    cur = nxt
                st["XTf"] = XT

            def seq_chunk(t, q, d, st):
                q0 = 64 * q
                FM = st["FM"]; Vb = st["Vb"]; XT = st["XTf"]
                hc = lambda h: slice(h * 64, (h + 1) * 64)
                psZ = psum()
                S_.mm(psZ[q0:q0 + 64, 0:C], zeros_b[:, 0:64], zeros_b[:, 0:C], start=True, stop=False)
                for h in HORD:
                    c, p0 = h // 2, 64 * (h % 2)
                    S_.mm(psZ[q0:q0 + 64, hc(h)], FM[p0:p0 + 64, 0, c, q0:q0 + 64], Tb[p0:p0 + 64, c, :], start=False, stop=False)
                for h in range(H):
                    S_.mm(psZ[q0:q0 + 64, hc(h)], st["LkT"][q0:q0 + 64, h, q0:q0 + 64], Vb[q0:q0 + 64, hc(h)], start=False, stop=(h == H - 1), inc=(h == H - 1))
                S_.cp("act", Zs[q0:q0 + 64, :], psZ[q0:q0 + 64, 0:C])
                yield
                psU = psum()
                for h in range(H):
                    S_.mm(psU[q0:q0 + 64, hc(h)], XT[q0:q0 + 64, h, q0:q0 + 64], Zs[q0:q0 + 64, hc(h)], inc=(h == H - 1))
                S_.cp("dve", Us[q0:q0 + 64, :], psU[q0:q0 + 64, 0:C])
                yield
                psY = psum()
                S_.mm(psY[q0:q0 + 64, 0:C], zeros_b[:, 0:64], zeros_b[:, 0:C], start=True, stop=False)
                for h in HORD:
                    c, p0 = h // 2, 64 * (h % 2)
                    S_.mm(psY[q0:q0 + 64, hc(h)], FM[p0:p0 + 64, 3, c, q0:q0 + 64], Tb[p0:p0 + 64, c, :], start=False, stop=False)
                for h in range(H):
                    S_.mm(psY[q0:q0 + 64, hc(h)], st["RbT"][q0:q0 + 64, h, q0:q0 + 64], Us[q0:q0 + 64, hc(h)], start=False, stop=False)
                for h in range(H):
                    S_.mm(psY[q0:q0 + 64, hc(h)], st["RkT"][q0:q0 + 64, h, q0:q0 + 64], Vb[q0:q0 + 64, hc(h)], start=False, stop=(h == H - 1), inc=(h == H - 1))
                psT = psum()
                for h in range(H):
                    c, p0 = h // 2, 64 * (h % 2)
                    S_.mm(psT[p0:p0 + 64, c * 64:(c + 1) * 64], st["Bt"][q0:q0 + 64, hc(h)], Us[q0:q0 + 64, hc(h)], start=True, stop=False)
                    S_.mm(psT[p0:p0 + 64, c * 64:(c + 1) * 64], st["Kt"][q0:q0 + 64, hc(h)], Vb[q0:q0 + 64, hc(h)], start=False, stop=True, inc=(h == H - 1))
                if d == 0:
                    S_.cp("act", Yacc[q0:q0 + 64, t, :], psY[q0:q0 + 64, 0:C])
                else:
                    S_.tt("pool" if False else "dve", Yacc[q0:q0 + 64, t, :], psY[q0:q0 + 64, 0:C], Yacc[q0:q0 + 64, t, :], ALU.add)
                S_.tt("dve", Ttmp, psT[:, 0:192].re("p (c i) -> p c i", c=3), T32, ALU.add)
                S_.tt("dve", T32, Ttmp, st["WCe"][:, :, q:q + 1].bc([128, 3, 64]), ALU.mult)
                S_.cp("act", Tb, T32)
                yield

            def finalize(t, st):
                Y = Yacc[:, t, :]
                S_.red(fs1, v6(Y), ALU.add)
                S_.tt("pool", sq, Y, Y, ALU.mult)
                S_.red(fs2, v6(sq), ALU.add)
                S_.ts("dve", fs1, fs1, 1.0 / 64, ALU.mult)
                S_.tt("dve", fs3, fs1, fs1, ALU.mult)
                S_.stt(fs2, fs2, 1.0 / 64, fs3, ALU.mult, ALU.subtract)
                S_.ts("dve", fs2, fs2, GN_EPS, ALU.add)
                S_.act(fs2, fs2, AF.Sqrt)
                S_.op("dve", lambda g: g.reciprocal(_ap(fs2), _ap(fs2)), [fs2], [fs2])
                S_.tt("dve", v6(sq), v6(Y), fs1.un(2).bc([128, 6, 64]), ALU.subtract)
                S_.tt("dve", v6(sq), v6(sq), fs2.un(2).bc([128, 6, 64]), ALU.mult)
                S_.tt("pool", sq, sq, gngb, ALU.mult)
                S_.tt("pool", sq, sq, gnbb, ALU.add)
                S_.tt("pool", sq, sq, st["bv"], ALU.add)
                S_.tt("pool", yob, sq, st["g32"], ALU.mult)
                ps = psum().bitcast(BF16)
                for cc in range(3):
                    S_.tr(ps[:, cc * 128:(cc + 1) * 128], yob[:, cc * 128:(cc + 1) * 128], identb, inc=(cc == 2))
                S_.cp("act", yT_r[:, 0:3, t * 128:(t + 1) * 128], ps[:, 0:384].re("p (k t) -> p k t", k=3))

            def seqfin(t, d, st):
                for q in ((0, 1) if d == 0 else (1, 0)):
                    yield from seq_chunk(t, q, d, st)
                if d == 1:
                    finalize(t, st)
                yield

            def run_interleaved(gens):
                gens = list(gens)
                while gens:
                    for g_ in list(gens):
                        try:
                            next(g_)
                        except StopIteration:
                            gens.remove(g_)

            for d in (0, 1):
                S_.memset("dve", T32, 0.0)
                S_.memset("pool", Tb, 0.0)
                order = list(range(NT)) if d == 0 else list(range(NT - 1, -1, -1))
                run_interleaved([prep(order[0], d, sets[0])])
                for i, t in enumerate(order):
                    st = sets[i % 2]
                    gens = [seqfin(t, d, st)]
                    if i + 1 < NT:
                        gens.append(prep(order[i + 1], d, sets[(i + 1) % 2]))
                    run_interleaved(gens)

    def stage_out(b, yTs):
        with ExitStack() as es5:
            A5 = Alloc(nc, es5)
            Wo = A5.sb([128, 8, D], BF16, "Wo")
            S_.dma("pool", Wo, w_out.re("(k p) c -> p k c", p=128))
            gam = A5.sb([128, D], F32, "gam1"); bet = A5.sb([128, D], F32, "bet1")
            bcast_load(gam, ln1_g); bcast_load(bet, ln1_b)
            Wr32 = A5.sb([128, 8, NE], F32, "Wr32")
            S_.dma("sp", Wr32, w_router.re("(k p) e -> p k e", p=128))
            brb = A5.sb([128, NE], F32, "brb"); bcast_load(brb, b_router)
            h0t = [A5.sb([128, D], F32, "h0t") for _ in range(2)]
            zt = [A5.sb([128, D], F32, "zt") for _ in range(2)]
            h1t = [A5.sb([128, D], F32, "h1t") for _ in range(2)]
            h1T32 = [A5.sb([128, 8, 128], F32, "h1T32") for _ in range(2)]
            h1Tb = [A5.sb([128, 8, 128], BF16, "h1Tb") for _ in range(2)]
            hhi = [A5.sb([128, D], BF16, "hhi") for _ in range(2)]
            hlo = [A5.sb([128, D], BF16, "hlo") for _ in range(2)]
            tp = [dict(st=A5.sb([128, 2, 6], F32, "st"), mv=A5.sb([128, 2], F32, "mv"),
                       rs=A5.sb([128, 1], F32, "rs")) for _ in range(2)]
            lg = A5.sb([128, NE], F32, "lg"); mx8 = A5.sb([128, 8], F32, "mx8"); nmx = A5.sb([128, 1], F32, "nmx")
            ex = A5.sb([128, NE], F32, "ex"); mk = A5.sb([128, NE], F32, "mk"); sm = A5.sb([128, 1], F32, "sm")
            rt = dict(mkb=A5.sb([128, NE], BF16, "mkb"), pos=A5.sb([128, NE], F32, "pos"), vv=A5.sb([128, NE], F32, "vv"),
                      nd=A5.sb([128, NE], F32, "nd"), mx=A5.sb([128, 8], F32, "mxr"), sel=A5.sb([128, NE], F32, "sel"),
                      idf=A5.sb([128, 4], F32, "idf"))
            for t in range(NT):
                i = t % 2
                gt = b * NT + t
                grp, tin = gt // 8, gt % 8
                S_.dma("sp", h0t[i], h0_d[b][t * 128:(t + 1) * 128, :])
                for half in range(2):
                    ps = psum()
                    for k in range(8):
                        S_.mm(ps, yTs[k][:, t * 128:(t + 1) * 128], Wo[:, k, half * 512:(half + 1) * 512],
                              start=(k == 0), stop=(k == 7), inc=(k == 7))
                    S_.stt(zt[i][:, half * 512:(half + 1) * 512], h0t[i][:, half * 512:(half + 1) * 512], ALPHA, ps, ALU.mult, ALU.add)
                layernorm(es5, zt[i], gam, bet, h1t[i], tp[i])
                S_.dma("sp", h1_d[grp][tin * 128:(tin + 1) * 128, :], h1t[i])
                S_.cp("act", hhi[i], h1t[i])
                S_.tt("dve", hlo[i], h1t[i], hhi[i], ALU.subtract)
                for half in range(2):
                    ps = psum()
                    for k in range(4):
                        kk_ = half * 4 + k
                        S_.mm(ps[:, k * 128:(k + 1) * 128], hhi[i][:, kk_ * 128:(kk_ + 1) * 128], identb, start=True, stop=False)
                        S_.mm(ps[:, k * 128:(k + 1) * 128], hlo[i][:, kk_ * 128:(kk_ + 1) * 128], identb, start=False, stop=True, inc=(k == 3))
                    S_.cp("act", h1T32[i][:, half * 4:half * 4 + 4, :], ps.re("p (k t) -> p k t", k=4))
                    S_.cp("dve", h1Tb[i][:, half * 4:half * 4 + 4, :], h1T32[i][:, half * 4:half * 4 + 4, :])
                if "h1Tdma" not in SKIP:
                    S_.dma("sp", h1T_d[grp][:, :, tin * 128:(tin + 1) * 128], h1Tb[i])
                if "router" in SKIP:
                    continue
                ps = psum()
                for k in range(8):
                    S_.mm(ps[:, 0:NE], h1T32[i][:, k, :], Wr32[:, k, :], start=(k == 0), stop=(k == 7), inc=(k == 7))
                S_.tt("dve", lg, ps[:, 0:NE], brb, ALU.add)
                if "top" in SKIP:
                    continue
                S_.op("dve", lambda g: g.max(_ap(mx8), _ap(lg)), [lg], [mx8])
                S_.ts("dve", mk, lg, mx8[:, 3:4], ALU.is_ge)
                S_.ts("dve", nmx, mx8[:, 0:1], -1.0, ALU.mult)
                S_.act(ex, lg, AF.Exp, bias=nmx[:, 0:1])
                S_.tt("dve", ex, ex, mk, ALU.mult)
                S_.red(sm, ex, ALU.add)
                S_.op("dve", lambda g: g.reciprocal(_ap(sm), _ap(sm)), [sm], [sm])
                S_.ts("dve", Gall[:, gt, :], ex, sm[:, 0:1], ALU.mult)
                if SPARSE:
                    route_tile(gt, mk, hhi[i], rt)

    def stage_moe():
        with ExitStack() as es6:
            A6 = Alloc(nc, es6)
            gam = A6.sb([128, D], F32, "gam2"); bet = A6.sb([128, D], F32, "bet2")
            bcast_load(gam, ln2_g); bcast_load(bet, ln2_b)
            bguT = A6.sb([128, 16, NE], F32, "bguT")
            with ExitStack() as esb:
                Ab = Alloc(nc, esb)
                bgu_sb = Ab.sb([NE, 2 * D], F32, "bgu_sb")
                bgu_hi = Ab.sb([NE, 2 * D], BF16, "bgu_hi"); bgu_lo = Ab.sb([NE, 2 * D], BF16, "bgu_lo")
                S_.dma("sp", bgu_sb, b_gate_up)
                S_.cp("act", bgu_hi, bgu_sb)
                S_.tt("dve", bgu_lo, bgu_sb, bgu_hi, ALU.subtract)
                for fc in range(16):
                    ps = psum()
                    S_.mm(ps[:, 0:NE], bgu_hi[0:NE, fc * 128:(fc + 1) * 128], identb[0:NE, 0:NE], start=True, stop=False)
                    S_.mm(ps[:, 0:NE], bgu_lo[0:NE, fc * 128:(fc + 1) * 128], identb[0:NE, 0:NE], start=False, stop=True, inc=True)
                    S_.cp("act", bguT[:, fc, :], ps[:, 0:NE])
                S_.barrier()
            bd_sb = A6.sb([NE, D], F32, "bd_sb")
            S_.dma("sp", bd_sb, b_down)
            h1T = A6.sb([128, 8, 1024], BF16, "h1T")
            acc = A6.sb([128, 8, D], F32, "acc")
            wgu = [A6.sb([128, 8, 2, 256], BF16, "wgu") for _ in range(6)]
            wd = [A6.sb([128, 8, D], BF16, "wd") for _ in range(2)]
            actT = A6.sb([128, 8, 1024], BF16, "actT")
            g1 = [A6.sb([128, 512], F32, "g1") for _ in range(2)]
            s1 = [A6.sb([128, 512], F32, "s1") for _ in range(2)]
            u1 = [A6.sb([128, 512], F32, "u1") for _ in range(2)]
            GT = A6.sb([NE, 128], F32, "GT")
            Ghi = A6.sb([128, NE], BF16, "Ghi"); Glo = A6.sb([128, NE], BF16, "Glo")
            h1t = A6.sb([128, D], F32, "h1t2"); zt = A6.sb([128, D], F32, "zt2"); ot = A6.sb([128, D], F32, "ot2")
            tp = dict(st=A6.sb([128, 2, 6], F32, "st"), mv=A6.sb([128, 2], F32, "mv"), rs=A6.sb([128, 1], F32, "rs"))
            tmpd = [A6.sb([128, 512], F32, "tmpd") for _ in range(2)]
            npiece = 0
            it = 0
            nd = 0
            for g in range(4):
                S_.dma("sp", h1T, h1T_d[g])
                for tl in range(8):
                    gt = g * 8 + tl
                    ps = psum()
                    S_.cp("act", Ghi, Gall[:, gt, :])
                    S_.tt("dve", Glo, Gall[:, gt, :], Ghi, ALU.subtract)
                    S_.mm(ps[0:NE, 0:128], Ghi, identb, start=True, stop=False)
                    S_.mm(ps[0:NE, 0:128], Glo, identb, start=False, stop=True, inc=True)
                    S_.cp("act", GT, ps[0:NE, 0:128])
                    for half in range(2):
                        ps = psum()
                        S_.mm(ps, GT[0:NE, :], bd_sb[0:NE, half * 512:(half + 1) * 512], inc=True)
                        S_.cp("act" if half else "dve", acc[:, tl, half * 512:(half + 1) * 512], ps)
                for e in range(NE):
                    wde = wd[e % 2]
                    if "moe_w" not in SKIP:
                        S_.dma("pool", wde, w_down[e].re("(k p) c -> p k c", p=128))
                    src = w_gate_up[e].re("(k p) (u f) -> p k u f", p=128, u=2)
                    for p in range(4):
                        wp = wgu[npiece % 6]; npiece += 1
                        for u_ in range(2):
                            if "moe_w" not in SKIP:
                                S_.dma("pool", wp[:, :, u_, :], src[:, :, u_, p * 256:(p + 1) * 256])
                        if "moe_gu" in SKIP:
                            continue
                        for sg in range(2):
                            for f2 in range(2):
                                fc = 2 * p + f2
                                i = it % 2; it += 1
                                psg = psum(); psu = psum()
                                for k in range(8):
                                    S_.mm(psg, wp[:, k, 0, f2 * 128:(f2 + 1) * 128], h1T[:, k, sg * 512:(sg + 1) * 512],
                                          start=(k == 0), stop=(k == 7), inc=(k == 7))
                                for k in range(8):
                                    S_.mm(psu, wp[:, k, 1, f2 * 128:(f2 + 1) * 128], h1T[:, k, sg * 512:(sg + 1) * 512],
                                          start=(k == 0), stop=(k == 7), inc=(k == 7))
                                S_.ts("dve", g1[i], psg, bguT[:, fc, e:e + 1], ALU.add, 7.0, ALU.min)
                                S_.act(s1[i], g1[i], AF.Sigmoid, scale=1.702)
                                S_.act(u1[i], psu, AF.Identity, bias=bguT[:, 8 + fc, e:e + 1])
                                S_.ts("pool", u1[i], u1[i], 7.0, ALU.min, -7.0, ALU.max)
                                S_.tt("pool", g1[i], g1[i], s1[i], ALU.mult)
                                S_.stt(actT[:, fc, sg * 512:(sg + 1) * 512], u1[i], 1.0, g1[i], ALU.add, ALU.mult)
                    for tl in range(8):
                        gt = g * 8 + tl
                        if "moe_down" in SKIP:
                            continue
                        for half in range(2):
                            ps = psum()
                            for fc in range(8):
                                S_.mm(ps, actT[:, fc, tl * 128:(tl + 1) * 128], wde[:, fc, half * 512:(half + 1) * 512],
                                      start=(fc == 0), stop=(fc == 7), inc=(fc == 7))
                            a_ = acc[:, tl, half * 512:(half + 1) * 512]
                            td = tmpd[nd % 2]; nd += 1
                            S_.ts("dve", td, ps, Gall[:, gt, e:e + 1], ALU.mult)
                            S_.tt("pool", a_, a_, td, ALU.add)
                for tl in range(8):
                    gt = g * 8 + tl
                    S_.dma("sp", h1t, h1_d[g][tl * 128:(tl + 1) * 128, :])
                    S_.stt(zt, h1t, ALPHA, acc[:, tl, :], ALU.mult, ALU.add)
                    layernorm(es6, zt, gam, bet, ot, tp)
                    S_.dma("sp", out_d[gt * 128:(gt + 1) * 128, :], ot)

    def route_tile(gt, mk, hb_tile, rt):
        mkb = rt["mkb"]; pos = rt["pos"]; vv = rt["vv"]; nd = rt["nd"]; mx = rt["mx"]; sel = rt["sel"]; idf = rt["idf"]
        S_.cp("dve", mkb, mk)
        ps = psum()
        S_.mm(ps[:, 0:NE], utri_b, mkb, inc=True)
        ps2 = psum()
        S_.mm(ps2[:, 0:NE], ones_b, mkb, inc=True)
        S_.tt("dve", pos, ps[:, 0:NE], carry, ALU.add)
        S_.tt("dve", carry, ps2[:, 0:NE], carry, ALU.add)
        S_.ts("dve", vv, pos, float(CAP), ALU.is_lt)
        S_.tt("dve", vv, vv, mk, ALU.mult)
        S_.tt("dve", pos, pos, erow, ALU.add)
        S_.ts("dve", nd, pos, -1.0, ALU.mult, BIGF, ALU.add)
        S_.tt("dve", nd, nd, vv, ALU.mult)
        S_.ts("dve", nd, nd, -BIGF, ALU.add)
        S_.op("dve", lambda g: g.max(_ap(mx), _ap(nd)), [nd], [mx])
        S_.ts("dve", idf, mx[:, 0:4], -1.0, ALU.mult, float(NE * CAP), ALU.min)
        S_.cp("dve", idx_all[:, gt, :], idf)
        for k in range(4):
            S_.ts("dve", sel, nd, mx[:, k:k + 1], ALU.is_equal)
            S_.tt("dve", sel, sel, Gall[:, gt, :], ALU.mult)
            S_.red(gk_all[:, gt, k:k + 1], sel, ALU.add)
        S_.ts("dve", idf, mx[:, 0:4], -0.5 * BIGF, ALU.is_gt)
        S_.tt("dve", gk_all[:, gt, :], gk_all[:, gt, :], idf, ALU.mult)
        for k in range(4):
            S_.idma_scatter(x_rows, idx_all[:, gt, k:k + 1], hb_tile, NE * CAP - 1)

    def stage_moe_sparse():
        S_.barrier()
        with ExitStack() as es6:
            A6 = Alloc(nc, es6)
            bguT = A6.sb([128, 16, NE], F32, "bguT")
            with ExitStack() as esb:
                Ab = Alloc(nc, esb)
                bgu_sb = Ab.sb([NE, 2 * D], F32, "bgu_sb")
                bgu_hi = Ab.sb([NE, 2 * D], BF16, "bgu_hi"); bgu_lo = Ab.sb([NE, 2 * D], BF16, "bgu_lo")
                S_.dma("sp", bgu_sb, b_gate_up)
                S_.cp("act", bgu_hi, bgu_sb)
                S_.tt("dve", bgu_lo, bgu_sb, bgu_hi, ALU.subtract)
                for fc in range(16):
                    ps = psum()
                    S_.mm(ps[:, 0:NE], bgu_hi[0:NE, fc * 128:(fc + 1) * 128], identb[0:NE, 0:NE], start=True, stop=False)
                    S_.mm(ps[:, 0:NE], bgu_lo[0:NE, fc * 128:(fc + 1) * 128], identb[0:NE, 0:NE], start=False, stop=True, inc=True)
                    S_.cp("act", bguT[:, fc, :], ps[:, 0:NE])
                S_.barrier()
            with ExitStack() as ese:
                Ae = Alloc(nc, ese)
                NS = CAP // 128
                xe = [Ae.sb([128, NS, D], BF16, "xe") for _ in range(2)]
                xT = [Ae.sb([128, 8, CAP], BF16, "xT") for _ in range(2)]
                wgu = [Ae.sb([128, 8, 2 * D], BF16, "wgu") for _ in range(2)]
                wd = [Ae.sb([128, 8, D], BF16, "wd") for _ in range(2)]
                actT = Ae.sb([128, 8, CAP], BF16, "actT")
                g1 = [Ae.sb([128, 512], F32, "g1") for _ in range(2)]
                s1 = [Ae.sb([128, 512], F32, "s1") for _ in range(2)]
                u1 = [Ae.sb([128, 512], F32, "u1") for _ in range(2)]
                yo = [Ae.sb([128, D], F32, "yo") for _ in range(2)]
                blocks = [(0, 512)] + ([(512, CAP - 512)] if CAP > 512 else [])
                issued = [0]

                wds = [Ae.sb([128, 2, D], F32, "wds") for _ in range(2)]

                def wd_src(e, j):
                    return w_down[e].re("(k p) c -> p k c", p=128)[:, 2 * j:2 * j + 2, :]

                def wd_dma(e, j):
                    S_.dma("sp", wds[j % 2], wd_src(e, j))

                def wd_cast(e, j):
                    S_.cp("act", wd[e % 2][:, 2 * j:2 * j + 2, :], wds[j % 2])

                def issue_weights(upto):
                    while issued[0] <= min(upto, NE - 1):
                        e = issued[0]; issued[0] += 1
                        S_.dma("pool", wgu[e % 2], w_gate_up[e].re("(k p) f -> p k f", p=128))
                        if e == 0:
                            for j in range(4):
                                wd_dma(0, j)
                                wd_cast(0, j)
                        else:
                            wd_dma(e, 0)
                            wd_dma(e, 1)

                def load_x(e):
                    S_.dma("sp", xe[e % 2], x_rows[e * CAP:(e + 1) * CAP, :].re("(n p) d -> p n d", p=128))
                    for n in range(NS):
                        ps = psum().bitcast(BF16)
                        for k in range(8):
                            S_.tr(ps[:, k * 128:(k + 1) * 128], xe[e % 2][:, n, k * 128:(k + 1) * 128], identb, inc=(k == 7))
                        S_.cp("act" if n % 2 else "dve", xT[e % 2][:, :, n * 128:(n + 1) * 128], ps.re("p (k t) -> p k t", k=8))
                it = 0
                load_x(0)
                for e in range(NE):
                    if e + 1 < NE:
                        load_x(e + 1)
                    xTe = xT[e % 2]
                    issue_weights(e + 1)
                    wge = wgu[e % 2]
                    for p in range(4):
                        if e + 1 < NE:
                            wd_cast(e + 1, p)
                            if p + 2 < 4:
                                wd_dma(e + 1, p + 2)
                        for (c0, cn) in blocks:
                            for f2 in range(2):
                                fc = 2 * p + f2
                                i = it % 2; it += 1
                                psg = psum(); psu = psum()
                                for k in range(8):
                                    S_.mm(psg[:, 0:cn], wge[:, k, fc * 128:(fc + 1) * 128], xTe[:, k, c0:c0 + cn],
                                          start=(k == 0), stop=(k == 7), inc=(k == 7))
                                for k in range(8):
                                    S_.mm(psu[:, 0:cn], wge[:, k, D + fc * 128:D + (fc + 1) * 128], xTe[:, k, c0:c0 + cn],
                                          start=(k == 0), stop=(k == 7), inc=(k == 7))
                                gg = g1[i][:, 0:cn]; sg_ = s1[i][:, 0:cn]; uu = u1[i][:, 0:cn]
                                S_.ts("dve", gg, psg[:, 0:cn], bguT[:, fc, e:e + 1], ALU.add, 7.0, ALU.min)
                                S_.act(sg_, gg, AF.Sigmoid, scale=1.702)
                                S_.act(uu, psu[:, 0:cn], AF.Identity, bias=bguT[:, 8 + fc, e:e + 1])
                                S_.ts("pool", uu, uu, 7.0, ALU.min, -7.0, ALU.max)
                                S_.tt("pool", gg, gg, sg_, ALU.mult)
                                S_.stt(actT[:, fc, c0:c0 + cn], uu, 1.0, gg, ALU.add, ALU.mult)
                    wde = wd[e % 2]
                    for n in range(NS):
                        yt_ = yo[n % 2]
                        for half in range(2):
                            ps = psum()
                            for fc in range(8):
                                S_.mm(ps, actT[:, fc, n * 128:(n + 1) * 128], wde[:, fc, half * 512:(half + 1) * 512],
                                      start=(fc == 0), stop=(fc == 7), inc=(fc == 7))
                            S_.cp("act" if half else "dve", yt_[:, half * 512:(half + 1) * 512], ps)
                        r0 = e * CAP + n * 128
                        S_.dma("sp", V(y_rows.ap[r0:r0 + 128, :], [Buf("yrow")]), yt_)
            S_.barrier()
            with ExitStack() as esc:
                Ac = Alloc(nc, esc)
                gam = Ac.sb([128, D], F32, "gam2"); bet = Ac.sb([128, D], F32, "bet2")
                bcast_load(gam, ln2_g); bcast_load(bet, ln2_b)
                bd_sb = Ac.sb([NE, D], F32, "bd_sb")
                S_.dma("sp", bd_sb, b_down)
                GT = Ac.sb([NE, 128], F32, "GT")
                Ghi = Ac.sb([128, NE], BF16, "Ghi"); Glo = Ac.sb([128, NE], BF16, "Glo")
                yk = [[Ac.sb([128, D], F32, "yk") for _ in range(4)] for _ in range(2)]
                h1t = [Ac.sb([128, D], F32, "h1t2") for _ in range(2)]
                zt = Ac.sb([128, D], F32, "zt2"); ot = Ac.sb([128, D], F32, "ot2")
                tmpc = [Ac.sb([128, D], F32, "tmpc") for _ in range(2)]
                tp = dict(st=Ac.sb([128, 2, 6], F32, "st"), mv=Ac.sb([128, 2], F32, "mv"), rs=Ac.sb([128, 1], F32, "rs"))

                def fetch(gt):
                    for k in range(4):
                        S_.idma_gather(yk[gt % 2][k], y_rows, idx_all[:, gt, k:k + 1], NE * CAP - 1)
                    S_.dma("sp", h1t[gt % 2], h1_d[gt // 8][(gt % 8) * 128:(gt % 8 + 1) * 128, :])
                fetch(0)
                for gt in range(NB * NT):
                    if gt + 1 < NB * NT:
                        fetch(gt + 1)
                    S_.cp("act", Ghi, Gall[:, gt, :])
                    S_.tt("dve", Glo, Gall[:, gt, :], Ghi, ALU.subtract)
                    ps = psum()
                    S_.mm(ps[0:NE, 0:128], Ghi, identb, start=True, stop=False)
                    S_.mm(ps[0:NE, 0:128], Glo, identb, start=False, stop=True, inc=True)
                    S_.cp("act", GT, ps[0:NE, 0:128])
                    for half in range(2):
                        ps = psum()
                        S_.mm(ps, GT[0:NE, :], bd_sb[0:NE, half * 512:(half + 1) * 512], inc=True)
                        S_.stt(zt[:, half * 512:(half + 1) * 512], h1t[gt % 2][:, half * 512:(half + 1) * 512], ALPHA, ps, ALU.mult, ALU.add)
                    for k in range(4):
                        S_.act(tmpc[k % 2], yk[gt % 2][k], AF.Copy, scale=gk_all[:, gt, k:k + 1])
                        S_.tt("dve", zt, zt, tmpc[k % 2], ALU.add)
                    layernorm(esc, zt, gam, bet, ot, tp, eng2="dve")
                    S_.dma("sp", out_d[gt * 128:(gt + 1) * 128, :], ot)

    msk = G.sb([128, 5 * 128 + 2], F32, "msk")
    S_.dma("sp", msk, c_masks)
    tri32 = G.sb([128, 2, 128], F32, "tri32")
    S_.dma("sp", tri32, c_tri.re("d p t -> p d t"))
    MS = [msk[:, 0:128], msk[:, 256:384]]
    MI = [msk[:, 128:256], msk[:, 384:512]]
    MID = msk[:, 512:640]
    BLK = msk[:, 640:642]

    def dbg_out(name, src, shape, dt):
        if name in debug:
            dbg[name] = V(nc.dram_tensor("dbg_" + name, list(shape), dt, kind="ExternalOutput").ap(), [Buf("dbg_" + name)])
            S_.dma("sp", dbg[name], src)

    for b in range(NB):
        with ExitStack() as esA:
            A = Alloc(nc, esA)
            hT = A.sb([128, 8, S], BF16, "hT")
            stage_s1(b, hT)
            S_.barrier()
            if b == 0:
                dbg_out("hT", hT, [128, 8, S], BF16)
            yT_r = A.sb([128, 3, S], BF16, "yT_r")
            if "rwkv" in stages:
                stage_rwkv(b, hT, yT_r)
                S_.barrier()
            yT_a = A.sb([128, 3, S], BF16, "yT_a")
            if "att" in stages:
                stage_att(b, hT, yT_a)
                S_.barrier()
            yT_m = A.sb([128, 2, S], BF16, "yT_m")
            if "mem" in stages:
                stage_mem(b, hT, yT_m)
                S_.barrier()
            if b == 0:
                dbg_out("yT_r", yT_r, [128, 3, S], BF16)
                dbg_out("yT_a", yT_a, [128, 3, S], BF16)
                dbg_out("yT_m", yT_m, [128, 2, S], BF16)
            if "out" in stages:
                stage_out(b, [yT_r[:, 0], yT_r[:, 1], yT_r[:, 2], yT_a[:, 0], yT_a[:, 1], yT_a[:, 2], yT_m[:, 0], yT_m[:, 1]])
                S_.barrier()
    if "h1" in debug:
        dbg["h1"] = V(nc.dram_tensor("dbg_h1", [1024, D], F32, kind="ExternalOutput").ap(), [Buf("dbg_h1")])
        S_.dma("sp", dbg["h1"], h1_d[0])
        dbg["G"] = V(nc.dram_tensor("dbg_G", [128, NB * NT, NE], F32, kind="ExternalOutput").ap(), [Buf("dbg_G")])
        S_.dma("sp", dbg["G"], Gall)

    if moe:
        if SPARSE:
            stage_moe_sparse()
        else:
            stage_moe()
    S_.barrier()
    es.close()
    return nc, S_


def host_constants():
    ident = np.eye(128, dtype=np.float32)
    slopes = np.exp2(-8.0 * np.arange(1, H + 1, dtype=np.float32) / H).astype(np.float32)
    x = np.arange(STRIP_W)[None, :]
    p = np.arange(128)[:, None]
    delta = x - p - STRIP_C
    ad = np.abs(delta)
    mult = ((ad <= 64).astype(np.float32) + ((delta % 4 == 0) & (ad <= 256)).astype(np.float32)
            + ((delta % 16 == 0) & (ad <= 1024)).astype(np.float32))
    strip = np.stack([mult * np.exp(-(slopes[h] * ad.astype(np.float32))) for h in range(H)]).astype(np.float32)
    masks = np.zeros((128, 5 * 128 + 2), np.float32)
    si = np.arange(64)[:, None]; ti = np.arange(64)[None, :]
    for q in range(2):
        r = slice(64 * q, 64 * q + 64)
        for n_, m_ in enumerate(((si < ti), (si <= ti), (si > ti), (si >= ti), (si == ti))):
            masks[r, n_ * 128 + 64 * q:n_ * 128 + 64 * q + 64] = m_
        masks[r, 640 + q] = 1.0
    tri = np.zeros((2, 128, 128), np.float32)
    for q in range(2):
        r = slice(64 * q, 64 * q + 64)
        tri[0, r, r] = (si <= ti); tri[1, r, r] = (si >= ti)
    utri = (np.arange(128)[:, None] < np.arange(128)[None, :]).astype(np.float32)
    erow = np.tile((np.arange(NE, dtype=np.float32) * CAP)[None, :], (128, 1))
    return dict(c_ident=ident, c_strip=strip, c_masks=masks, c_tri=tri, c_utri=utri, c_erow=erow)


def make_in_maps(inputs):
    consts = host_constants()
    maps = []
    sq = lambda a: np.ascontiguousarray(a)
    for i in range(8):
        m = dict(consts)
        m["x"] = sq(inputs["x"][2 * i:2 * i + 2].reshape(NB * S, D))
        m["mem"] = sq(inputs["mem"][2 * i:2 * i + 2].reshape(NB * 256, D))
        for k in ("ln_in_g", "ln_in_b"):
            m[k] = sq(inputs[k])
        m["w_in"] = sq(inputs["w_in"][0])
        m["mu_shift"] = sq(inputs["mu_shift"][0])
        m["w0"] = sq(inputs["w0"][0].reshape(-1)); m["w_up"] = sq(inputs["w_up"][0])
        m["a0"] = sq(inputs["a0"][0].reshape(-1)); m["a_up"] = sq(inputs["a_up"][0])
        m["g_up"] = sq(inputs["g_up"][0])
        for k in ("k_k", "k_a", "gn_g", "gn_b", "ln1_g", "ln1_b", "ln2_g", "ln2_b", "b_router",
                  "w_mem_kv", "w_out", "w_router", "w_gate_up", "b_gate_up", "w_down", "b_down"):
            m[k] = sq(inputs[k][0])
        m["r_k"] = sq(inputs["r_k"][0].reshape(-1))
        maps.append(m)
    return maps


def kernel(**inputs):
    inputs = {k: np.asarray(v) for k, v in inputs.items()}
    nc, _ = build_program()
    maps = make_in_maps(inputs)
    res = run_bass_kernel_spmd(nc, maps, core_ids=list(range(8)))
    out = np.stack([r["out"].reshape(NB, S, D) for r in res.results]).reshape(16, S, D)
    return out.astype(np.float32)
```

```python
import numpy as np
from contextlib import ExitStack
import concourse.bass as bass
import concourse.mybir as mybir
from concourse.bass_utils import run_bass_kernel_spmd

F32 = mybir.dt.float32
BF16 = mybir.dt.bfloat16
U32 = mybir.dt.uint32
AF = mybir.ActivationFunctionType
ALU = mybir.AluOpType
AX = mybir.AxisListType

D = 1024
S = 2048
NB = 2
NT = S // 128
C = 384
H = 6
RWKV_IN = 1344
IN_W = 2752
NE = 32
ALPHA = 2.0 ** 0.25
LN_EPS = 1e-5
GN_EPS = 64e-5
LDS = float(np.exp(-0.5))
STRIP_W = 3200
STRIP_C = 1536
CAP = 640
BIGF = 1.0e6
import os
SKIP = os.environ.get('KSKIP', '')
SPARSE = os.environ.get('KDENSE', '') == ''


class Buf:
    __slots__ = ("name", "w", "rs")

    def __init__(self, name):
        self.name = name
        self.w = None
        self.rs = []


class Ev:
    __slots__ = ("sem", "val", "eng")

    def __init__(self, eng):
        self.sem = None
        self.val = None
        self.eng = eng


class V:
    __slots__ = ("ap", "bufs")

    def __init__(self, ap, bufs):
        self.ap = ap
        self.bufs = bufs if isinstance(bufs, (list, tuple)) else [bufs]

    def __getitem__(self, k):
        return V(self.ap[k], self.bufs)

    def re(self, pat, **kw):
        return V(self.ap.rearrange(pat, **kw), self.bufs)

    def bc(self, shape):
        return V(self.ap.to_broadcast(list(shape)), self.bufs)

    def un(self, axis):
        return V(self.ap.unsqueeze(axis), self.bufs)

    def bitcast(self, dt):
        return V(self.ap.bitcast(dt), self.bufs)

    def on(self, bufs):
        return V(self.ap, bufs)


def _ap(x):
    return x.ap if isinstance(x, V) else x


class Sched:
    ENG = ("pe", "act", "dve", "pool", "sp")
    NQ = 6

    def __init__(self, nc, es):
        self.nc = nc
        self.es = es
        self.eng = dict(pe=nc.tensor, act=nc.scalar, dve=nc.vector, pool=nc.gpsimd, sp=nc.sync)
        self.sem = {k: es.enter_context(nc.semaphore("s_" + k)) for k in self.ENG}
        self.cnt = {k: 0 for k in self.ENG}
        self.dsem = {q: [es.enter_context(nc.semaphore("d_%s%d" % (q, i))) for i in range(self.NQ)]
                     for q in ("sp", "pool")}
        self.dcnt = {q: 0 for q in self.dsem}
        self.seen = {k: {} for k in self.ENG}
        self.pending = {k: [] for k in self.ENG}
        self.all_dma = []
        self.ninst = 0

    def _wait(self, e, ev):
        assert ev.val is not None, "dependency on an unresolved (non-inc) event"
        key = id(ev.sem)
        if self.seen[e].get(key, 0) >= ev.val:
            return
        self.eng[e].wait_ge(ev.sem, ev.val)
        self.seen[e][key] = ev.val

    def _deps(self, e, reads, writes, is_dma):
        for v in reads:
            for b in v.bufs:
                if b.w is not None:
                    if b.w.eng == e and e == "pe" and not is_dma:
                        continue
                    self._wait(e, b.w)
        for v in writes:
            for b in v.bufs:
                if b.w is not None and (is_dma or b.w.eng != e or e != "pe"):
                    self._wait(e, b.w)
                for r in b.rs:
                    if is_dma or r.eng != e or e != "pe":
                        self._wait(e, r)

    def _record(self, ev, reads, writes):
        for v in reads:
            for b in v.bufs:
                if ev.eng is not None:
                    b.rs = [r for r in b.rs if r.eng != ev.eng]
                b.rs.append(ev)
        for v in writes:
            for b in v.bufs:
                b.w = ev
                b.rs = []

    def op(self, e, fn, reads, writes, inc=True):
        reads = [r for r in reads if isinstance(r, V)]
        writes = [w for w in writes if isinstance(w, V)]
        self._deps(e, reads, writes, False)
        ins = fn(self.eng[e])
        self.ninst += 1
        ev = Ev(e)
        if inc:
            self.cnt[e] += 1
            ins.then_inc(self.sem[e], 1)
            ev.sem = self.sem[e]
            ev.val = self.cnt[e]
            for p in self.pending[e]:
                p.sem = ev.sem
                p.val = ev.val
            self.pending[e] = []
        else:
            self.pending[e].append(ev)
        self._record(ev, reads, writes)
        return ev

    def dma(self, q, out, in_, **kw):
        self._deps(q, [in_], [out], True)
        n = self.dcnt[q]
        self.dcnt[q] += 1
        sem = self.dsem[q][n % self.NQ]
        val = 16 * (n // self.NQ + 1)
        if n >= self.NQ:
            pe_ = Ev(None); pe_.sem = sem; pe_.val = val - 16
            self._wait(q, pe_)
        ins = self.eng[q].dma_start(out=_ap(out), in_=_ap(in_), **kw)
        ins.then_inc(sem, 16)
        self.ninst += 1
        ev = Ev(None)
        ev.sem = sem
        ev.val = val
        self._record(ev, [in_], [out])
        self.all_dma.append(ev)
        return ev

    def _idma(self, out, out_off, in_, in_off, reads, writes, bound):
        q = "pool"
        self._deps(q, reads, writes, True)
        n = self.dcnt[q]
        self.dcnt[q] += 1
        sem = self.dsem[q][n % self.NQ]
        val = 16 * (n // self.NQ + 1)
        if n >= self.NQ:
            pe_ = Ev(None); pe_.sem = sem; pe_.val = val - 16
            self._wait(q, pe_)
        ins = self.eng[q].indirect_dma_start(out=_ap(out), out_offset=out_off, in_=_ap(in_), in_offset=in_off)
        ins.then_inc(sem, 16)
        self.ninst += 1
        ev = Ev(None)
        ev.sem = sem
        ev.val = val
        self._record(ev, reads, writes)
        return ev

    def idma_scatter(self, dram, idx, src, bound):
        off = bass.IndirectOffsetOnAxis(ap=_ap(idx), axis=0)
        return self._idma(dram, off, src, None, [src, idx], [V(dram.ap, [Buf("scatter")])], bound)

    def idma_gather(self, dst, dram, idx, bound):
        off = bass.IndirectOffsetOnAxis(ap=_ap(idx), axis=0)
        return self._idma(dst, None, dram, off, [idx], [dst], bound)

    def barrier(self):
        for e in self.ENG:
            for o in self.ENG:
                if self.cnt[o] > 0:
                    assert not self.pending[o]
                    ev = Ev(o)
                    ev.sem = self.sem[o]
                    ev.val = self.cnt[o]
                    self._wait(e, ev)
            for q in self.dsem:
                n = self.dcnt[q]
                for i in range(min(n, self.NQ)):
                    ev = Ev(None)
                    ev.sem = self.dsem[q][i]
                    ev.val = 16 * ((n - 1 - i) // self.NQ + 1)
                    self._wait(e, ev)

    def _pe_rows(self, lhsT):
        a = _ap(lhsT)
        lo = a.base_partition()
        rows = (lo, lo + a.shape[0])
        prev = getattr(self, "_last_pe", None)
        if prev is not None:
            pins, pev, prow = prev
            if rows[0] >= prow[1] or prow[0] >= rows[1]:
                if pev.val is None:
                    self.cnt["pe"] += 1
                    pins.then_inc(self.sem["pe"], 1)
                    for p in self.pending["pe"]:
                        p.sem = self.sem["pe"]
                        p.val = self.cnt["pe"]
                    self.pending["pe"] = []
                self._wait("pe", pev)
        return rows

    def _pe_op(self, fn, lhsT, reads, writes, inc):
        rows = self._pe_rows(lhsT)
        box = []

        def f(e):
            i = fn(e)
            box.append(i)
            return i
        ev = self.op("pe", f, reads, writes, inc=inc)
        self._last_pe = (box[0], ev, rows)
        return ev

    def mm(self, out, lhsT, rhs, start=True, stop=True, inc=False):
        return self._pe_op(lambda e: e.matmul(_ap(out), _ap(lhsT), _ap(rhs), start=start, stop=stop),
                           lhsT, [lhsT, rhs], [out], inc)

    def tr(self, out, in_, ident, inc=False):
        return self._pe_op(lambda e: e.transpose(_ap(out), _ap(in_), _ap(ident)), in_, [in_, ident], [out], inc)

    def act(self, out, in_, func, bias=None, scale=None, accum_out=None):
        kw = {}
        if bias is not None:
            kw["bias"] = _ap(bias)
        if scale is not None:
            kw["scale"] = _ap(scale)
        if accum_out is not None:
            kw["accum_out"] = _ap(accum_out)
        return self.op("act", lambda e: e.activation(_ap(out), _ap(in_), func, **kw),
                       [in_, bias, scale], [out, accum_out])

    def ts(self, e, out, in0, s1, op0, s2=None, op1=None, accum_out=None):
        kw = {}
        if op1 is not None:
            kw["op1"] = op1
        if accum_out is not None:
            kw["accum_out"] = _ap(accum_out)
        return self.op(e, lambda g: g.tensor_scalar(_ap(out), _ap(in0), _ap(s1), _ap(s2), op0, **kw),
                       [in0, s1, s2], [out, accum_out])

    def tt(self, e, out, in0, in1, op):
        return self.op(e, lambda g: g.tensor_tensor(_ap(out), _ap(in0), _ap(in1), op), [in0, in1], [out])

    def stt(self, out, in0, scalar, in1, op0, op1):
        return self.op("dve", lambda g: g.scalar_tensor_tensor(_ap(out), _ap(in0), _ap(scalar), _ap(in1), op0, op1),
                       [in0, scalar, in1], [out])

    def cp(self, e, out, in_):
        if e == "act":
            return self.op("act", lambda g: g.copy(_ap(out), _ap(in_)), [in_], [out])
        return self.op(e, lambda g: g.tensor_copy(_ap(out), _ap(in_)), [in_], [out])

    def red(self, out, in_, op, axis=AX.X):
        return self.op("dve", lambda g: g.tensor_reduce(_ap(out), _ap(in_), axis, op), [in_], [out])

    def memset(self, e, out, val):
        return self.op(e, lambda g: g.memset(_ap(out), val), [], [out])


class Alloc:
    N = [0]

    def __init__(self, nc, es):
        self.nc = nc
        self.es = es

    def sb(self, shape, dt, name=None, nbuf=1):
        Alloc.N[0] += 1
        name = "%s_%d" % (name or "t", Alloc.N[0])
        t = self.es.enter_context(self.nc.sbuf_tensor(name, list(shape), dt))
        return V(t[:], [Buf(name)])

    def dram(self, name, shape, dt, kind="Internal"):
        t = self.nc.dram_tensor(name, list(shape), dt, kind=kind)
        return V(t.ap(), [Buf(name)])


def build_program(debug=(), moe=True, stages=('rwkv', 'att', 'mem', 'out')):
    nc = bass.Bass("TRN2", target_bir_lowering=False)
    es = ExitStack()
    S_ = Sched(nc, es)
    G = Alloc(nc, es)
    dbg = {}

    def din(name, shape, dt=F32):
        return V(nc.dram_tensor(name, list(shape), dt, kind="ExternalInput").ap(), [Buf(name)])

    x_d = din("x", [NB * S, D])
    mem_d = din("mem", [NB * 256, D])
    ln_in_g = din("ln_in_g", [D]); ln_in_b = din("ln_in_b", [D])
    w_in = din("w_in", [D, IN_W])
    mu_d = din("mu_shift", [RWKV_IN])
    w0_d = din("w0", [2 * C]); w_up_d = din("w_up", [2, 32, C])
    a0_d = din("a0", [2 * C]); a_up_d = din("a_up", [2, 32, C])
    g_up_d = din("g_up", [64, C])
    k_k_d = din("k_k", [C]); k_a_d = din("k_a", [C]); r_k_d = din("r_k", [C])
    gn_g_d = din("gn_g", [C]); gn_b_d = din("gn_b", [C])
    w_mem_kv = din("w_mem_kv", [D, 512])
    w_out = din("w_out", [D, D])
    ln1_g = din("ln1_g", [D]); ln1_b = din("ln1_b", [D])
    w_router = din("w_router", [D, NE]); b_router = din("b_router", [NE])
    if moe:
        w_gate_up = din("w_gate_up", [NE, D, 2 * D]); b_gate_up = din("b_gate_up", [NE, 2 * D])
        w_down = din("w_down", [NE, D, D]); b_down = din("b_down", [NE, D])
    ln2_g = din("ln2_g", [D]); ln2_b = din("ln2_b", [D])
    c_ident = din("c_ident", [128, 128])
    c_strip = din("c_strip", [H, 128, STRIP_W])
    c_masks = din("c_masks", [128, 5 * 128 + 2])
    c_tri = din("c_tri", [2, 128, 128])
    c_utri = din("c_utri", [128, 128])
    c_erow = din("c_erow", [128, NE])

    out_d = V(nc.dram_tensor("out", [NB * S, D], F32, kind="ExternalOutput").ap(), [Buf("out")])

    h0_d = [G.dram("h0_%d" % b, [S, D], F32) for b in range(NB)]
    h1_d = [G.dram("h1_%d" % g, [1024, D], F32) for g in range(4)]
    h1T_d = [G.dram("h1T_%d" % g, [128, 8, 1024], BF16) for g in range(4)]

    banks = []
    for i in range(8):
        t = es.enter_context(nc.psum_tensor("ps%d" % i, [128, 512], F32))
        banks.append(V(t[:], [Buf("ps%d" % i)]))
    bank_i = {"gen": 0, "acc": 0}
    bank_grp = {"gen": [2, 3, 4, 5, 6, 7], "acc": [0, 1]}

    def psum(grp="gen"):
        l = bank_grp[grp]
        b = banks[l[bank_i[grp] % len(l)]]
        bank_i[grp] += 1
        return b

    ident32 = G.sb([128, 128], F32, "ident32")
    identb = G.sb([128, 128], BF16, "identb")
    S_.dma("sp", ident32, c_ident)
    S_.cp("dve", identb, ident32)
    Gall = G.sb([128, NB * NT, NE], F32, "Gall")
    ones_b = G.sb([128, 128], BF16, "ones_b")
    S_.memset("dve", ones_b, 1.0)
    zeros_b = G.sb([128, 512], BF16, "zeros_b")
    S_.memset("dve", zeros_b, 0.0)
    carry = G.sb([128, NE], F32, "carry")
    S_.memset("dve", carry, 0.0)
    idx_all = G.sb([128, NB * NT, 4], U32, "idx_all")
    gk_all = G.sb([128, NB * NT, 4], F32, "gk_all")
    utri32 = G.sb([128, 128], F32, "utri32")
    S_.dma("sp", utri32, c_utri)
    utri_b = G.sb([128, 128], BF16, "utri_b")
    S_.cp("dve", utri_b, utri32)
    erow = G.sb([128, NE], F32, "erow")
    S_.dma("sp", erow, c_erow)
    x_rows = G.dram("x_rows", [NE * CAP + 128, D], BF16)
    y_rows = G.dram("y_rows", [NE * CAP + 128, D], F32)
    es_z = ExitStack()
    zeros32 = Alloc(nc, es_z).sb([128, D], F32, "zeros32")
    S_.memset("pool", zeros32, 0.0)
    S_.dma("sp", V(y_rows.ap[NE * CAP:NE * CAP + 128, :], [Buf("ydump")]), zeros32)
    zb = zeros32.bitcast(BF16).re("p (n d) -> p n d", n=2)
    xr = x_rows.re("(n p) d -> p n d", p=128)
    nrow = (NE * CAP + 128) // 128
    for n0 in range(0, nrow, 2):
        nn = min(2, nrow - n0)
        S_.dma("sp", V(xr.ap[:, n0:n0 + nn, :], [Buf("xfill")]), zb[:, 0:nn, :])
    S_.barrier()
    es_z.close()

    def bcast_load(dst, src1d, q="sp"):
        S_.dma(q, dst, V(src1d.ap.partition_broadcast(128), src1d.bufs))

    def layernorm(es2, xin, gam, bet, out, tmp_pool, eng2="pool"):
        st = tmp_pool["st"]; mv = tmp_pool["mv"]; rs = tmp_pool["rs"]
        for i in range(2):
            S_.op("dve", lambda g, i=i: g.bn_stats(_ap(st[:, i, :]), _ap(xin[:, i * 512:(i + 1) * 512])),
                  [xin], [st])
        S_.op("dve", lambda g: g.bn_aggr(_ap(mv), _ap(st)), [st], [mv])
        S_.ts("dve", rs, mv[:, 1:2], LN_EPS, ALU.add)
        S_.act(rs, rs, AF.Sqrt)
        S_.op("dve", lambda g: g.reciprocal(_ap(rs), _ap(rs)), [rs], [rs])
        S_.ts("dve", out, xin, mv[:, 0:1], ALU.subtract, rs[:, 0:1], ALU.mult)
        S_.tt(eng2, out, out, gam, ALU.mult)
        S_.tt(eng2, out, out, bet, ALU.add)

    def stage_s1(b, hT):
        with ExitStack() as es1:
            A1 = Alloc(nc, es1)
            gam = A1.sb([128, D], F32, "gam"); bet = A1.sb([128, D], F32, "bet")
            bcast_load(gam, ln_in_g); bcast_load(bet, ln_in_b)
            xt = [A1.sb([128, D], F32, "xt") for _ in range(2)]
            ht = [A1.sb([128, D], F32, "ht") for _ in range(2)]
            hb = [A1.sb([128, D], BF16, "hb") for _ in range(2)]
            tp = [dict(st=A1.sb([128, 2, 6], F32, "st"), mv=A1.sb([128, 2], F32, "mv"),
                       rs=A1.sb([128, 1], F32, "rs")) for _ in range(2)]
            for t in range(NT):
                i = t % 2
                S_.dma("sp", xt[i], x_d[b * S + t * 128: b * S + (t + 1) * 128, :])
                layernorm(es1, xt[i], gam, bet, ht[i], tp[i])
                S_.dma("sp", h0_d[b][t * 128:(t + 1) * 128, :], ht[i])
                S_.cp("act", hb[i], ht[i])
                ps = psum().bitcast(BF16)
                for k in range(8):
                    S_.tr(ps[:, k * 128:(k + 1) * 128], hb[i][:, k * 128:(k + 1) * 128], identb, inc=(k == 7))
                S_.cp("act", hT[:, :, t * 128:(t + 1) * 128], ps.re("p (k t) -> p k t", k=8))

    def attn_core(nh, KTv, QTv, Vv, ytok, n_st, s_range, strips=None):
        with ExitStack() as esc:
            Ac = Alloc(nc, esc)
            Eb = [Ac.sb([128, 512], BF16, "E") for _ in range(3)]
            Pb = [Ac.sb([128, 512], BF16, "P") for _ in range(3)]
            rec = Ac.sb([128, 4], F32, "rec")
            it = 0
            for h in range(nh):
                c, p0 = h // 2, 64 * (h % 2)
                for tq in range(4):
                    s_lo, s_hi, jvalid = s_range(tq)
                    acc = psum("acc")
                    accv = acc[:, 0:260].re("p (j d) -> p j d", j=4)
                    S_.mm(acc[:, 0:260], zeros_b[:, 0:128], zeros_b[:, 0:260], start=True, stop=False)
                    sts = list(range(s_lo, s_hi + 1))

                    def score(st_):
                        sc = psum()
                        S_.mm(sc, KTv[p0:p0 + 64, c, st_ * 128:(st_ + 1) * 128],
                              QTv[p0:p0 + 64, c, tq * 512:(tq + 1) * 512], inc=True)
                        return sc
                    sc_next = score(sts[0])
                    for n_, st_ in enumerate(sts):
                        sc = sc_next
                        if n_ + 1 < len(sts):
                            sc_next = score(sts[n_ + 1])
                        E = Eb[it % 3]; P = Pb[it % 3]; it += 1
                        S_.act(E, sc, AF.Exp, scale=0.125)
                        if strips is not None:
                            off = tq * 512 - st_ * 128 + STRIP_C
                            S_.tt("dve" if it % 3 else "pool", P, E, strips[:, h, off:off + 512], ALU.mult)
                        else:
                            P = E
                        for j in range(4):
                            ok, last = jvalid(tq, j, st_)
                            if not ok:
                                continue
                            S_.mm(accv[:, j, :], P[:, j * 128:(j + 1) * 128], Vv[:, st_, h, :],
                                  start=False, stop=last, inc=True)
                    S_.op("dve", lambda g: g.reciprocal(_ap(rec), _ap(accv[:, :, 64])), [accv], [rec])
                    for j in range(4):
                        S_.ts("dve", ytok[:, 4 * tq + j, h * 64:(h + 1) * 64], accv[:, j, 0:64], rec[:, j:j + 1], ALU.mult)

    def proj_fm(dst, Wv, hT, nchunk, col0):
        n = 0
        for c in range(nchunk):
            for tg in range(4):
                ps = psum()
                for k in range(8):
                    S_.mm(ps, Wv[:, k, col0 + c * 128: col0 + (c + 1) * 128],
                          hT[:, k, tg * 512:(tg + 1) * 512], start=(k == 0), stop=(k == 7), inc=(k == 7))
                S_.cp("act" if n % 2 else "dve", dst[:, c, tg * 512:(tg + 1) * 512], ps)
                n += 1

    def ytok_to_yT(ytok, yT, nchunk):
        for t in range(NT):
            ps = psum().bitcast(BF16)
            for cc in range(nchunk):
                S_.tr(ps[:, cc * 128:(cc + 1) * 128], ytok[:, t, cc * 128:(cc + 1) * 128], identb, inc=(cc == nchunk - 1))
            S_.cp("act" if t % 2 else "dve", yT[:, 0:nchunk, t * 128:(t + 1) * 128],
                  ps[:, 0:nchunk * 128].re("p (k t) -> p k t", k=nchunk))

    def stage_att(b, hT, yT_a):
        with ExitStack() as es2:
            A2 = Alloc(nc, es2)
            Wqkv = A2.sb([128, 8, 3 * C], BF16, "Wqkv")
            S_.dma("pool", Wqkv, w_in[:, RWKV_IN:RWKV_IN + 3 * C].re("(k p) c -> p k c", p=128))
            strips = A2.sb([128, H, STRIP_W], BF16, "strips")
            for h in range(H):
                S_.dma("pool", strips[:, h, :], c_strip[h])
            QT = A2.sb([128, 3, S], BF16, "QT")
            KT = A2.sb([128, 3, S], BF16, "KT")
            Vt = A2.sb([128, NT, H, 65], BF16, "Vt")
            ytok = A2.sb([128, NT, C], BF16, "ytok")
            S_.memset("pool", Vt[:, :, :, 64:65], 1.0)
            proj_fm(QT, Wqkv, hT, 3, 0)
            proj_fm(KT, Wqkv, hT, 3, C)
            for t in range(NT):
                ps = psum()
                for k in range(8):
                    S_.mm(ps[:, 0:C], hT[:, k, t * 128:(t + 1) * 128], Wqkv[:, k, 2 * C:3 * C],
                          start=(k == 0), stop=(k == 7), inc=(k == 7))
                S_.cp("act" if t % 2 else "dve", Vt[:, t, :, 0:64], ps[:, 0:C].re("p (h d) -> p h d", h=H))

            def s_range(tq):
                s_hi = min(NT - 1, 4 * tq + 3 + 8)

                def jvalid(tq, j, st_):
                    tt_ = 4 * tq + j
                    return abs(tt_ - st_) <= 8, (st_ == s_hi and j == 3)
                return max(0, 4 * tq - 8), s_hi, jvalid
            attn_core(H, KT, QT, Vt, ytok, NT, s_range, strips)
            ytok_to_yT(ytok, yT_a, 3)

    def stage_mem(b, hT, yT_m):
        with ExitStack() as es3:
            A3 = Alloc(nc, es3)
            Wkv = A3.sb([128, 8, 512], BF16, "Wkv")
            S_.dma("pool", Wkv, w_mem_kv.re("(k p) c -> p k c", p=128))
            Wqm = A3.sb([128, 8, 256], BF16, "Wqm")
            S_.dma("pool", Wqm, w_in[:, RWKV_IN + 3 * C:IN_W].re("(k p) c -> p k c", p=128))
            memb = A3.sb([128, 2, D], BF16, "memb")
            S_.dma("pool", memb, mem_d[b * 256:(b + 1) * 256, :].re("(m p) d -> p m d", p=128))
            memT = A3.sb([128, 8, 256], BF16, "memT")
            for m in range(2):
                ps = psum().bitcast(BF16)
                for k in range(8):
                    S_.tr(ps[:, k * 128:(k + 1) * 128], memb[:, m, k * 128:(k + 1) * 128], identb, inc=(k == 7))
                S_.cp("act", memT[:, :, m * 128:(m + 1) * 128], ps.re("p (k t) -> p k t", k=8))
            KmT = A3.sb([128, 2, 256], BF16, "KmT")
            for c in range(2):
                ps = psum()
                for k in range(8):
                    S_.mm(ps[:, 0:256], Wkv[:, k, c * 128:(c + 1) * 128], memT[:, k, :],
                          start=(k == 0), stop=(k == 7), inc=(k == 7))
                S_.cp("dve", KmT[:, c, :], ps[:, 0:256])
            Vm = A3.sb([128, 2, 4, 65], BF16, "Vm")
            S_.memset("pool", Vm[:, :, :, 64:65], 1.0)
            for m in range(2):
                ps = psum()
                for k in range(8):
                    S_.mm(ps[:, 0:256], memT[:, k, m * 128:(m + 1) * 128], Wkv[:, k, 256:512],
                          start=(k == 0), stop=(k == 7), inc=(k == 7))
                S_.cp("dve", Vm[:, m, :, 0:64], ps[:, 0:256].re("p (h d) -> p h d", h=4))
            QmT = A3.sb([128, 2, S], BF16, "QmT")
            proj_fm(QmT, Wqm, hT, 2, 0)
            ytok = A3.sb([128, NT, 256], BF16, "ytokm")

            def s_range(tq):
                return 0, 1, (lambda tq, j, st_: (True, st_ == 1 and j == 3))
            attn_core(4, KmT, QmT, Vm, ytok, 2, s_range, None)
            ytok_to_yT(ytok, yT_m, 2)

    def stage_rwkv(b, hT, yT_r):
        with ExitStack() as es4:
            A4 = Alloc(nc, es4)
            f32t = lambda name, n=C: A4.sb([128, n], F32, name)
            Wr = A4.sb([128, 16, RWKV_IN], BF16, "Wr")
            with ExitStack() as esw:
                Aw = Alloc(nc, esw)
                mub = Aw.sb([128, RWKV_IN], F32, "mub"); bcast_load(mub, mu_d)
                omb = Aw.sb([128, RWKV_IN], F32, "omb"); hmb = Aw.sb([128, RWKV_IN], F32, "hmb")
                S_.ts("dve", omb, mub, -1.0, ALU.mult, 1.0, ALU.add)
                S_.ts("pool", hmb, mub, 0.5, ALU.mult)
                stg = [Aw.sb([128, 8, 336], F32, "stg") for _ in range(2)]
                for pc in range(4):
                    cs = slice(pc * 336, (pc + 1) * 336)
                    S_.dma("sp", stg[pc % 2], w_in[:, cs].re("(k p) c -> p k c", p=128))
                    S_.tt("dve", Wr[:, 0:8, cs], stg[pc % 2], omb[:, cs].un(1).bc([128, 8, 336]), ALU.mult)
                    S_.tt("pool", Wr[:, 8:16, cs], stg[pc % 2], hmb[:, cs].un(1).bc([128, 8, 336]), ALU.mult)
                S_.barrier()
            kkb = f32t("kkb"); bcast_load(kkb, k_k_d)
            kab = f32t("kab"); bcast_load(kab, k_a_d)
            omkab = f32t("omkab"); S_.ts("dve", omkab, kab, -1.0, ALU.mult, 1.0, ALU.add)
            rkb = f32t("rkb"); bcast_load(rkb, r_k_d)
            gngb = f32t("gngb"); bcast_load(gngb, gn_g_d)
            gnbb = f32t("gnbb"); bcast_load(gnbb, gn_b_d)
            w0b = f32t("w0b", 2 * C); bcast_load(w0b, w0_d)
            a0b = f32t("a0b", 2 * C); bcast_load(a0b, a0_d)
            gup = A4.sb([128, C], BF16, "gup"); S_.dma("pool", gup[0:64, :], g_up_d)
            wupbd = A4.sb([128, 2 * C], BF16, "wupbd"); S_.memset("pool", wupbd, 0.0)
            S_.dma("pool", wupbd[64:96, 0:C], w_up_d[0]); S_.dma("pool", wupbd[96:128, C:2 * C], w_up_d[1])
            aupbd = A4.sb([128, 2 * C], BF16, "aupbd"); S_.memset("pool", aupbd, 0.0)
            S_.dma("pool", aupbd[0:32, 0:C], a_up_d[0]); S_.dma("pool", aupbd[32:64, C:2 * C], a_up_d[1])
            Yacc = A4.sb([128, NT, C], F32, "Yacc")
            T32 = A4.sb([128, 3, 64], F32, "T32"); Tb = A4.sb([128, 3, 64], BF16, "Tb"); Ttmp = A4.sb([128, 3, 64], F32, "Ttmp")
            hs = A4.sb([128, 8, 128], BF16, "hs")
            r32 = f32t("r32"); k32 = f32t("k32"); v32 = f32t("v32")
            sgw = f32t("sgw"); a32 = f32t("a32"); a32b = f32t("a32b")
            ecw = f32t("ecw"); encw = f32t("encw")
            kk = f32t("kk"); sq = f32t("sq"); kd = f32t("kd"); bb = f32t("bb")
            ss = A4.sb([128, 6], F32, "ss"); rn = A4.sb([128, 6], F32, "rn")
            lor = A4.sb([128, 3, 128], BF16, "lor")
            tok4 = A4.sb([128, 4, C], BF16, "tok4")
            Pp = [A4.sb([128, 6, 128], BF16, "Pp") for _ in range(2)]
            PTp = [A4.sb([128, 6, 128], BF16, "PTp") for _ in range(2)]
            Zs = A4.sb([128, C], BF16, "Zs"); Us = A4.sb([128, C], BF16, "Us")
            fs1 = A4.sb([128, 6], F32, "fs1"); fs2 = A4.sb([128, 6], F32, "fs2"); fs3 = A4.sb([128, 6], F32, "fs3")
            yob = A4.sb([128, C], BF16, "yob")
            sets = []
            for i in range(2):
                sets.append(dict(
                    FM=A4.sb([128, 4, 3, 128], BF16, "FM"), Vb=A4.sb([128, C], BF16, "Vb"),
                    XT=A4.sb([128, 6, 128], BF16, "XT"),
                    LkT=A4.sb([128, 6, 128], BF16, "LkT"), RbT=A4.sb([128, 6, 128], BF16, "RbT"),
                    RkT=A4.sb([128, 6, 128], BF16, "RkT"), WCe=A4.sb([128, 3, 2], F32, "WCe"),
                    Bt=A4.sb([128, C], BF16, "Bt"), Kt=A4.sb([128, C], BF16, "Kt"),
                    g32=f32t("g32"), bv=f32t("bv")))

            def v6(x):
                return x.re("p (h j) -> p h j", h=6)
            HORD = (0, 2, 4, 1, 3, 5)

            def prep(t, d, st):
                lo = t * 128
                if 0 < t < NT - 1:
                    S_.tt("pool", hs, hT[:, :, lo - 1:lo + 127], hT[:, :, lo + 1:lo + 129], ALU.add)
                elif t == 0:
                    S_.tt("pool", hs[:, :, 1:128], hT[:, :, 0:127], hT[:, :, 2:129], ALU.add)
                    S_.cp("pool", hs[:, :, 0:1], hT[:, :, 1:2])
                else:
                    S_.tt("pool", hs[:, :, 0:127], hT[:, :, lo - 1:lo + 126], hT[:, :, lo + 1:lo + 128], ALU.add)
                    S_.cp("pool", hs[:, :, 127:128], hT[:, :, lo + 126:lo + 127])

                def lhs(kc):
                    return hT[:, kc, lo:lo + 128] if kc < 8 else hs[:, kc - 8, :]
                for part, dst in ((0, r32), (1, k32), (2, v32)):
                    ps = psum()
                    for kc in range(16):
                        S_.mm(ps[:, 0:C], lhs(kc), Wr[:, kc, part * C:(part + 1) * C], start=(kc == 0), stop=(kc == 15), inc=(kc == 15))
                    S_.cp("act", dst, ps[:, 0:C])
                S_.cp("pool", st["Vb"], v32)
                yield
                ps = psum()
                for kc in range(16):
                    S_.mm(ps[:, 0:128], Wr[:, kc, 3 * C:3 * C + 128], lhs(kc), start=(kc == 0), stop=(kc == 15), inc=(kc == 15))
                for kc in range(16):
                    S_.mm(ps[0:64, 128:256], Wr[:, kc, 3 * C + 128:3 * C + 192], lhs(kc), start=(kc == 0), stop=(kc == 15), inc=(kc == 15))
                S_.act(lor[0:64, 0, :], ps[0:64, 0:128], AF.Sigmoid)
                S_.act(lor[64:128, 1, :], ps[64:128, 0:128], AF.Tanh)
                S_.cp("act", lor[0:64, 2, :], ps[0:64, 128:256])
                yield
                psg = psum()
                S_.mm(psg[:, 0:C], lor[0:64, 0, :], gup[0:64, :], inc=True)
                S_.cp("act", st["g32"], psg[:, 0:C])
                psw = psum()
                S_.mm(psw[:, 0:C], lor[64:128, 1, :], wupbd[64:128, d * C:(d + 1) * C], inc=True)
                S_.tt("dve", sgw, psw[:, 0:C], w0b[:, d * C:(d + 1) * C], ALU.add)
                S_.act(sgw, sgw, AF.Sigmoid)
                psa = psum()
                S_.mm(psa[:, 0:C], lor[0:64, 2, :], aupbd[0:64, d * C:(d + 1) * C], inc=True)
                S_.tt("dve", a32, psa[:, 0:C], a0b[:, d * C:(d + 1) * C], ALU.add)
                S_.act(a32, a32, AF.Sigmoid)
                pscs = psum()
                S_.mm(pscs[:, 0:C], tri32[:, d, :], sgw, inc=True)
                pswc = psum()
                for c in range(3):
                    S_.mm(pswc[:, c * 2:(c + 1) * 2], sgw[:, c * 128:(c + 1) * 128], BLK, inc=(c == 2))
                S_.act(st["WCe"].re("p c q -> p (c q)"), pswc[:, 0:6], AF.Exp, scale=-LDS)
                S_.act(ecw, pscs[:, 0:C], AF.Exp, scale=-LDS)
                S_.act(encw, pscs[:, 0:C], AF.Exp, scale=LDS)
                S_.act(sgw, sgw, AF.Exp, scale=LDS)
                S_.tt("dve", sgw, sgw, ecw, ALU.mult)
                yield
                S_.tt("dve", kk, k32, kkb, ALU.mult)
                S_.tt("dve", sq, kk, kk, ALU.mult)
                S_.red(ss, v6(sq), ALU.add)
                S_.act(rn, ss, AF.Sqrt)
                S_.ts("dve", rn, rn, 1e-12, ALU.max)
                S_.op("dve", lambda g: g.reciprocal(_ap(rn), _ap(rn)), [rn], [rn])
                S_.tt("dve", v6(kk), v6(kk), rn.un(2).bc([128, 6, 64]), ALU.mult)
                S_.tt("dve", kd, a32, kab, ALU.mult)
                S_.tt("dve", kd, kd, omkab, ALU.add)
                S_.tt("dve", kd, kd, k32, ALU.mult)
                S_.tt("dve", bb, kk, a32, ALU.mult)
                S_.tt("dve", tok4[:, 0, :], kk, sgw, ALU.mult)
                S_.stt(tok4[:, 1, :], bb, -1.0, encw, ALU.mult, ALU.mult)
                S_.tt("dve", tok4[:, 2, :], kd, encw, ALU.mult)
                S_.tt("dve", tok4[:, 3, :], r32, ecw, ALU.mult)
                S_.cp("act", st["Bt"], tok4[:, 1, :])
                S_.cp("act", st["Kt"], tok4[:, 2, :])
                if d == 1:
                    psa2 = psum()
                    S_.mm(psa2[:, 0:C], lor[0:64, 2, :], aupbd[0:64, 0:C], inc=True)
                    S_.tt("dve", a32b, psa2[:, 0:C], a0b[:, 0:C], ALU.add)
                    S_.act(a32b, a32b, AF.Sigmoid)
                    S_.tt("dve", a32b, a32b, a32, ALU.add)
                    S_.tt("dve", a32b, a32b, kab, ALU.mult)
                    S_.stt(a32b, omkab, 2.0, a32b, ALU.mult, ALU.add)
                    S_.tt("dve", a32b, a32b, k32, ALU.mult)
                    S_.tt("dve", a32b, a32b, rkb, ALU.mult)
                    S_.tt("dve", a32b, a32b, r32, ALU.mult)
                    S_.red(fs3, v6(a32b), ALU.add)
                    S_.tt("dve", v6(st["bv"]), v6(v32), fs3.un(2).bc([128, 6, 64]), ALU.mult)
                yield
                FM = st["FM"]
                for half in range(2):
                    ps = psum().bitcast(BF16)
                    n = 0
                    for o in (2 * half, 2 * half + 1):
                        for c in range(3):
                            S_.tr(ps[:, n * 128:(n + 1) * 128], tok4[:, o, c * 128:(c + 1) * 128], identb, inc=(n == 5))
                            n += 1
                    S_.cp("act" if half else "dve", FM[:, 2 * half:2 * half + 2, :, :].re("p o c t -> p (o c) t"),
                          ps[:, 0:768].re("p (n t) -> p n t", n=6))
                KKi, Bi, Ki, Ri = 0, 1, 2, 3
                yield

                def cc_mm(li, ri):
                    psA = psum(); psB = psum()
                    for par, ps in ((0, psA), (1, psB)):
                        p0 = 64 * par
                        for i in range(3):
                            S_.mm(ps[:, i * 128:(i + 1) * 128], FM[p0:p0 + 64, li, i, :], FM[p0:p0 + 64, ri, i, :], inc=(i == 2))
                    return psA, psB

                def evac_cc(dst, pss, mask):
                    dv = dst.re("p (i two) t -> p i two t", two=2)
                    for par in range(2):
                        S_.tt("dve", dv[:, :, par, :], pss[par][:, 0:384].re("p (i t) -> p i t", i=3),
                              mask.un(1).bc([128, 3, 128]), ALU.mult)
                XT = st["XT"]
                pss = cc_mm(Bi, KKi)
                evac_cc(PTp[0], pss, MS[d])
                S_.tt("pool", XT, PTp[0], MID.un(1).bc([128, 6, 128]), ALU.add)
                yield
                pss = cc_mm(KKi, Bi)
                evac_cc(Pp[0], pss, MS[1 - d])
                yield
                pss = cc_mm(Ki, KKi)
                evac_cc(st["LkT"], pss, MS[d])
                yield
                pss = cc_mm(Bi, Ri)
                evac_cc(st["RbT"], pss, MI[d])
                yield
                pss = cc_mm(Ki, Ri)
                evac_cc(st["RkT"], pss, MI[d])
                def sq_mm(lt, rt):
                    psA = psum(); psB = psum()
                    for h in range(H):
                        ps = psA if h < 3 else psB
                        S_.mm(ps[:, (h % 3) * 128:(h % 3 + 1) * 128], lt[:, h, :], rt[:, h, :], inc=(h % 3 == 2))
                    return psA, psB

                def v3(ps):
                    return ps[:, 0:384].re("p (i t) -> p i t", i=3)
                cur = 0
                for lvl in range(1, 6):
                    nxt = 1 - cur
                    yield
                    pP = sq_mm(PTp[cur], Pp[cur])
                    if lvl < 5:
                        pPT = sq_mm(Pp[cur], PTp[cur])
                    for hh in range(2):
                        S_.cp("act", Pp[nxt][:, 3 * hh:3 * hh + 3, :], v3(pP[hh]))
                    if lvl < 5:
                        for hh in range(2):
                            S_.cp("dve", PTp[nxt][:, 3 * hh:3 * hh + 3, :], v3(pPT[hh]))
                    yield
                    pX = sq_mm(Pp[nxt], XT)
                    for hh in range(2):
                        S_.tt("dve", XT[:, 3 * hh:3 * hh + 3, :], v3(pX[hh]), XT[:, 3 * hh:3 * hh + 3, :], ALU.add)
                    cur = nxt
                st["XTf"] = XT

            def seq_chunk(t, q, d, st):
                q0 = 64 * q
                FM = st["FM"]; Vb = st["Vb"]; XT = st["XTf"]
                hc = lambda h: slice(h * 64, (h + 1) * 64)
                psZ = psum()
                S_.mm(psZ[q0:q0 + 64, 0:C], zeros_b[:, 0:64], zeros_b[:, 0:C], start=True, stop=False)
                for h in HORD:
                    c, p0 = h // 2, 64 * (h % 2)
                    S_.mm(psZ[q0:q0 + 64, hc(h)], FM[p0:p0 + 64, 0, c, q0:q0 + 64], Tb[p0:p0 + 64, c, :], start=False, stop=False)
                for h in range(H):
                    S_.mm(psZ[q0:q0 + 64, hc(h)], st["LkT"][q0:q0 + 64, h, q0:q0 + 64], Vb[q0:q0 + 64, hc(h)], start=False, stop=(h == H - 1), inc=(h == H - 1))
                S_.cp("act", Zs[q0:q0 + 64, :], psZ[q0:q0 + 64, 0:C])
                yield
                psU = psum()
                for h in range(H):
                    S_.mm(psU[q0:q0 + 64, hc(h)], XT[q0:q0 + 64, h, q0:q0 + 64], Zs[q0:q0 + 64, hc(h)], inc=(h == H - 1))
                S_.cp("dve", Us[q0:q0 + 64, :], psU[q0:q0 + 64, 0:C])
                yield
                psY = psum()
                S_.mm(psY[q0:q0 + 64, 0:C], zeros_b[:, 0:64], zeros_b[:, 0:C], start=True, stop=False)
                for h in HORD:
                    c, p0 = h // 2, 64 * (h % 2)
                    S_.mm(psY[q0:q0 + 64, hc(h)], FM[p0:p0 + 64, 3, c, q0:q0 + 64], Tb[p0:p0 + 64, c, :], start=False, stop=False)
                for h in range(H):
                    S_.mm(psY[q0:q0 + 64, hc(h)], st["RbT"][q0:q0 + 64, h, q0:q0 + 64], Us[q0:q0 + 64, hc(h)], start=False, stop=False)
                for h in range(H):
                    S_.mm(psY[q0:q0 + 64, hc(h)], st["RkT"][q0:q0 + 64, h, q0:q0 + 64], Vb[q0:q0 + 64, hc(h)], start=False, stop=(h == H - 1), inc=(h == H - 1))
                psT = psum()
                for h in range(H):
                    c, p0 = h // 2, 64 * (h % 2)
                    S_.mm(psT[p0:p0 + 64, c * 64:(c + 1) * 64], st["Bt"][q0:q0 + 64, hc(h)], Us[q0:q0 + 64, hc(h)], start=True, stop=False)
                    S_.mm(psT[p0:p0 + 64, c * 64:(c + 1) * 64], st["Kt"][q0:q0 + 64, hc(h)], Vb[q0:q0 + 64, hc(h)], start=False, stop=True, inc=(h == H - 1))
                if d == 0:
                    S_.cp("act", Yacc[q0:q0 + 64, t, :], psY[q0:q0 + 64, 0:C])
                else:
                    S_.tt("pool" if False else "dve", Yacc[q0:q0 + 64, t, :], psY[q0:q0 + 64, 0:C], Yacc[q0:q0 + 64, t, :], ALU.add)
                S_.tt("dve", Ttmp, psT[:, 0:192].re("p (c i) -> p c i", c=3), T32, ALU.add)
                S_.tt("dve", T32, Ttmp, st["WCe"][:, :, q:q + 1].bc([128, 3, 64]), ALU.mult)
                S_.cp("act", Tb, T32)
                yield

            def finalize(t, st):
                Y = Yacc[:, t, :]
                S_.red(fs1, v6(Y), ALU.add)
                S_.tt("dve", sq, Y, Y, ALU.mult)
                S_.red(fs2, v6(sq), ALU.add)
                S_.ts("dve", fs1, fs1, 1.0 / 64, ALU.mult)
                S_.tt("dve", fs3, fs1, fs1, ALU.mult)
                S_.stt(fs2, fs2, 1.0 / 64, fs3, ALU.mult, ALU.subtract)
                S_.ts("dve", fs2, fs2, GN_EPS, ALU.add)
                S_.act(fs2, fs2, AF.Sqrt)
                S_.op("dve", lambda g: g.reciprocal(_ap(fs2), _ap(fs2)), [fs2], [fs2])
                S_.tt("dve", v6(sq), v6(Y), fs1.un(2).bc([128, 6, 64]), ALU.subtract)
                S_.tt("dve", v6(sq), v6(sq), fs2.un(2).bc([128, 6, 64]), ALU.mult)
                S_.tt("dve", sq, sq, gngb, ALU.mult)
                S_.tt("dve", sq, sq, gnbb, ALU.add)
                S_.tt("dve", sq, sq, st["bv"], ALU.add)
                S_.tt("dve", yob, sq, st["g32"], ALU.mult)
                ps = psum().bitcast(BF16)
                for cc in range(3):
                    S_.tr(ps[:, cc * 128:(cc + 1) * 128], yob[:, cc * 128:(cc + 1) * 128], identb, inc=(cc == 2))
                S_.cp("act", yT_r[:, 0:3, t * 128:(t + 1) * 128], ps[:, 0:384].re("p (k t) -> p k t", k=3))

            def seqfin(t, d, st):
                for q in ((0, 1) if d == 0 else (1, 0)):
                    yield from seq_chunk(t, q, d, st)
                if d == 1:
                    finalize(t, st)
                yield

            def run_interleaved(gens):
                gens = list(gens)
                while gens:
                    for g_ in list(gens):
                        try:
                            next(g_)
                        except StopIteration:
                            gens.remove(g_)

            for d in (0, 1):
                S_.memset("dve", T32, 0.0)
                S_.memset("pool", Tb, 0.0)
                order = list(range(NT)) if d == 0 else list(range(NT - 1, -1, -1))
                run_interleaved([prep(order[0], d, sets[0])])
                for i, t in enumerate(order):
                    st = sets[i % 2]
                    gens = [seqfin(t, d, st)]
                    if i + 1 < NT:
                        gens.append(prep(order[i + 1], d, sets[(i + 1) % 2]))
                    run_interleaved(gens)

    def stage_out(b, yTs):
        with ExitStack() as es5:
            A5 = Alloc(nc, es5)
            Wo = A5.sb([128, 8, D], BF16, "Wo")
            S_.dma("pool", Wo, w_out.re("(k p) c -> p k c", p=128))
            gam = A5.sb([128, D], F32, "gam1"); bet = A5.sb([128, D], F32, "bet1")
            bcast_load(gam, ln1_g); bcast_load(bet, ln1_b)
            Wr32 = A5.sb([128, 8, NE], F32, "Wr32")
            S_.dma("sp", Wr32, w_router.re("(k p) e -> p k e", p=128))
            brb = A5.sb([128, NE], F32, "brb"); bcast_load(brb, b_router)
            h0t = [A5.sb([128, D], F32, "h0t") for _ in range(2)]
            zt = [A5.sb([128, D], F32, "zt") for _ in range(2)]
            h1t = [A5.sb([128, D], F32, "h1t") for _ in range(2)]
            h1T32 = [A5.sb([128, 8, 128], F32, "h1T32") for _ in range(2)]
            h1Tb = [A5.sb([128, 8, 128], BF16, "h1Tb") for _ in range(2)]
            hhi = [A5.sb([128, D], BF16, "hhi") for _ in range(2)]
            hlo = [A5.sb([128, D], BF16, "hlo") for _ in range(2)]
            tp = [dict(st=A5.sb([128, 2, 6], F32, "st"), mv=A5.sb([128, 2], F32, "mv"),
                       rs=A5.sb([128, 1], F32, "rs")) for _ in range(2)]
            lg = A5.sb([128, NE], F32, "lg"); mx8 = A5.sb([128, 8], F32, "mx8"); nmx = A5.sb([128, 1], F32, "nmx")
            ex = A5.sb([128, NE], F32, "ex"); mk = A5.sb([128, NE], F32, "mk"); sm = A5.sb([128, 1], F32, "sm")
            rt = dict(mkb=A5.sb([128, NE], BF16, "mkb"), pos=A5.sb([128, NE], F32, "pos"), vv=A5.sb([128, NE], F32, "vv"),
                      nd=A5.sb([128, NE], F32, "nd"), mx=A5.sb([128, 8], F32, "mxr"), sel=A5.sb([128, NE], F32, "sel"),
                      idf=A5.sb([128, 4], F32, "idf"))
            for t in range(NT):
                i = t % 2
                gt = b * NT + t
                grp, tin = gt // 8, gt % 8
                S_.dma("sp", h0t[i], h0_d[b][t * 128:(t + 1) * 128, :])
                for half in range(2):
                    ps = psum()
                    for k in range(8):
                        S_.mm(ps, yTs[k][:, t * 128:(t + 1) * 128], Wo[:, k, half * 512:(half + 1) * 512],
                              start=(k == 0), stop=(k == 7), inc=(k == 7))
                    S_.stt(zt[i][:, half * 512:(half + 1) * 512], h0t[i][:, half * 512:(half + 1) * 512], ALPHA, ps, ALU.mult, ALU.add)
                layernorm(es5, zt[i], gam, bet, h1t[i], tp[i])
                S_.dma("sp", h1_d[grp][tin * 128:(tin + 1) * 128, :], h1t[i])
                S_.cp("act", hhi[i], h1t[i])
                S_.tt("dve", hlo[i], h1t[i], hhi[i], ALU.subtract)
                for half in range(2):
                    ps = psum()
                    for k in range(4):
                        kk_ = half * 4 + k
                        S_.mm(ps[:, k * 128:(k + 1) * 128], hhi[i][:, kk_ * 128:(kk_ + 1) * 128], identb, start=True, stop=False)
                        S_.mm(ps[:, k * 128:(k + 1) * 128], hlo[i][:, kk_ * 128:(kk_ + 1) * 128], identb, start=False, stop=True, inc=(k == 3))
                    S_.cp("act", h1T32[i][:, half * 4:half * 4 + 4, :], ps.re("p (k t) -> p k t", k=4))
                    S_.cp("dve", h1Tb[i][:, half * 4:half * 4 + 4, :], h1T32[i][:, half * 4:half * 4 + 4, :])
                if "h1Tdma" not in SKIP:
                    S_.dma("sp", h1T_d[grp][:, :, tin * 128:(tin + 1) * 128], h1Tb[i])
                if "router" in SKIP:
                    continue
                ps = psum()
                for k in range(8):
                    S_.mm(ps[:, 0:NE], h1T32[i][:, k, :], Wr32[:, k, :], start=(k == 0), stop=(k == 7), inc=(k == 7))
                S_.tt("dve", lg, ps[:, 0:NE], brb, ALU.add)
                if "top" in SKIP:
                    continue
                S_.op("dve", lambda g: g.max(_ap(mx8), _ap(lg)), [lg], [mx8])
                S_.ts("dve", mk, lg, mx8[:, 3:4], ALU.is_ge)
                S_.ts("dve", nmx, mx8[:, 0:1], -1.0, ALU.mult)
                S_.act(ex, lg, AF.Exp, bias=nmx[:, 0:1])
                S_.tt("dve", ex, ex, mk, ALU.mult)
                S_.red(sm, ex, ALU.add)
                S_.op("dve", lambda g: g.reciprocal(_ap(sm), _ap(sm)), [sm], [sm])
                S_.ts("dve", Gall[:, gt, :], ex, sm[:, 0:1], ALU.mult)
                if SPARSE:
                    route_tile(gt, mk, hhi[i], rt)

    def stage_moe():
        with ExitStack() as es6:
            A6 = Alloc(nc, es6)
            gam = A6.sb([128, D], F32, "gam2"); bet = A6.sb([128, D], F32, "bet2")
            bcast_load(gam, ln2_g); bcast_load(bet, ln2_b)
            bguT = A6.sb([128, 16, NE], F32, "bguT")
            with ExitStack() as esb:
                Ab = Alloc(nc, esb)
                bgu_sb = Ab.sb([NE, 2 * D], F32, "bgu_sb")
                bgu_hi = Ab.sb([NE, 2 * D], BF16, "bgu_hi"); bgu_lo = Ab.sb([NE, 2 * D], BF16, "bgu_lo")
                S_.dma("sp", bgu_sb, b_gate_up)
                S_.cp("act", bgu_hi, bgu_sb)
                S_.tt("dve", bgu_lo, bgu_sb, bgu_hi, ALU.subtract)
                for fc in range(16):
                    ps = psum()
                    S_.mm(ps[:, 0:NE], bgu_hi[0:NE, fc * 128:(fc + 1) * 128], identb[0:NE, 0:NE], start=True, stop=False)
                    S_.mm(ps[:, 0:NE], bgu_lo[0:NE, fc * 128:(fc + 1) * 128], identb[0:NE, 0:NE], start=False, stop=True, inc=True)
                    S_.cp("act", bguT[:, fc, :], ps[:, 0:NE])
                S_.barrier()
            bd_sb = A6.sb([NE, D], F32, "bd_sb")
            S_.dma("sp", bd_sb, b_down)
            h1T = A6.sb([128, 8, 1024], BF16, "h1T")
            acc = A6.sb([128, 8, D], F32, "acc")
            wgu = [A6.sb([128, 8, 2, 256], BF16, "wgu") for _ in range(6)]
            wd = [A6.sb([128, 8, D], BF16, "wd") for _ in range(2)]
            actT = A6.sb([128, 8, 1024], BF16, "actT")
            g1 = [A6.sb([128, 512], F32, "g1") for _ in range(2)]
            s1 = [A6.sb([128, 512], F32, "s1") for _ in range(2)]
            u1 = [A6.sb([128, 512], F32, "u1") for _ in range(2)]
            GT = A6.sb([NE, 128], F32, "GT")
            Ghi = A6.sb([128, NE], BF16, "Ghi"); Glo = A6.sb([128, NE], BF16, "Glo")
            h1t = A6.sb([128, D], F32, "h1t2"); zt = A6.sb([128, D], F32, "zt2"); ot = A6.sb([128, D], F32, "ot2")
            tp = dict(st=A6.sb([128, 2, 6], F32, "st"), mv=A6.sb([128, 2], F32, "mv"), rs=A6.sb([128, 1], F32, "rs"))
            tmpd = [A6.sb([128, 512], F32, "tmpd") for _ in range(2)]
            npiece = 0
            it = 0
            nd = 0
            for g in range(4):
                S_.dma("sp", h1T, h1T_d[g])
                for tl in range(8):
                    gt = g * 8 + tl
                    ps = psum()
                    S_.cp("act", Ghi, Gall[:, gt, :])
                    S_.tt("dve", Glo, Gall[:, gt, :], Ghi, ALU.subtract)
                    S_.mm(ps[0:NE, 0:128], Ghi, identb, start=True, stop=False)
                    S_.mm(ps[0:NE, 0:128], Glo, identb, start=False, stop=True, inc=True)
                    S_.cp("act", GT, ps[0:NE, 0:128])
                    for half in range(2):
                        ps = psum()
                        S_.mm(ps, GT[0:NE, :], bd_sb[0:NE, half * 512:(half + 1) * 512], inc=True)
                        S_.cp("act" if half else "dve", acc[:, tl, half * 512:(half + 1) * 512], ps)
                for e in range(NE):
                    wde = wd[e % 2]
                    if "moe_w" not in SKIP:
                        S_.dma("pool", wde, w_down[e].re("(k p) c -> p k c", p=128))
                    src = w_gate_up[e].re("(k p) (u f) -> p k u f", p=128, u=2)
                    for p in range(4):
                        wp = wgu[npiece % 6]; npiece += 1
                        for u_ in range(2):
                            if "moe_w" not in SKIP:
                                S_.dma("pool", wp[:, :, u_, :], src[:, :, u_, p * 256:(p + 1) * 256])
                        if "moe_gu" in SKIP:
                            continue
                        for sg in range(2):
                            for f2 in range(2):
                                fc = 2 * p + f2
                                i = it % 2; it += 1
                                psg = psum(); psu = psum()
                                for k in range(8):
                                    S_.mm(psg, wp[:, k, 0, f2 * 128:(f2 + 1) * 128], h1T[:, k, sg * 512:(sg + 1) * 512],
                                          start=(k == 0), stop=(k == 7), inc=(k == 7))
                                for k in range(8):
                                    S_.mm(psu, wp[:, k, 1, f2 * 128:(f2 + 1) * 128], h1T[:, k, sg * 512:(sg + 1) * 512],
                                          start=(k == 0), stop=(k == 7), inc=(k == 7))
                                S_.ts("dve", g1[i], psg, bguT[:, fc, e:e + 1], ALU.add, 7.0, ALU.min)
                                S_.act(s1[i], g1[i], AF.Sigmoid, scale=1.702)
                                S_.act(u1[i], psu, AF.Identity, bias=bguT[:, 8 + fc, e:e + 1])
                                S_.ts("pool", u1[i], u1[i], 7.0, ALU.min, -7.0, ALU.max)
                                S_.tt("pool", g1[i], g1[i], s1[i], ALU.mult)
                                S_.stt(actT[:, fc, sg * 512:(sg + 1) * 512], u1[i], 1.0, g1[i], ALU.add, ALU.mult)
                    for tl in range(8):
                        gt = g * 8 + tl
                        if "moe_down" in SKIP:
                            continue
                        for half in range(2):
                            ps = psum()
                            for fc in range(8):
                                S_.mm(ps, actT[:, fc, tl * 128:(tl + 1) * 128], wde[:, fc, half * 512:(half + 1) * 512],
                                      start=(fc == 0), stop=(fc == 7), inc=(fc == 7))
                            a_ = acc[:, tl, half * 512:(half + 1) * 512]
                            td = tmpd[nd % 2]; nd += 1
                            S_.ts("dve", td, ps, Gall[:, gt, e:e + 1], ALU.mult)
                            S_.tt("pool", a_, a_, td, ALU.add)
                for tl in range(8):
                    gt = g * 8 + tl
                    S_.dma("sp", h1t, h1_d[g][tl * 128:(tl + 1) * 128, :])
                    S_.stt(zt, h1t, ALPHA, acc[:, tl, :], ALU.mult, ALU.add)
                    layernorm(es6, zt, gam, bet, ot, tp)
                    S_.dma("sp", out_d[gt * 128:(gt + 1) * 128, :], ot)

    def route_tile(gt, mk, hb_tile, rt):
        mkb = rt["mkb"]; pos = rt["pos"]; vv = rt["vv"]; nd = rt["nd"]; mx = rt["mx"]; sel = rt["sel"]; idf = rt["idf"]
        S_.cp("dve", mkb, mk)
        ps = psum()
        S_.mm(ps[:, 0:NE], utri_b, mkb, inc=True)
        ps2 = psum()
        S_.mm(ps2[:, 0:NE], ones_b, mkb, inc=True)
        S_.tt("dve", pos, ps[:, 0:NE], carry, ALU.add)
        S_.tt("dve", carry, ps2[:, 0:NE], carry, ALU.add)
        S_.ts("dve", vv, pos, float(CAP), ALU.is_lt)
        S_.tt("dve", vv, vv, mk, ALU.mult)
        S_.tt("dve", pos, pos, erow, ALU.add)
        S_.ts("dve", nd, pos, -1.0, ALU.mult, BIGF, ALU.add)
        S_.tt("dve", nd, nd, vv, ALU.mult)
        S_.ts("dve", nd, nd, -BIGF, ALU.add)
        S_.op("dve", lambda g: g.max(_ap(mx), _ap(nd)), [nd], [mx])
        S_.ts("dve", idf, mx[:, 0:4], -1.0, ALU.mult, float(NE * CAP), ALU.min)
        S_.cp("dve", idx_all[:, gt, :], idf)
        for k in range(4):
            S_.ts("dve", sel, nd, mx[:, k:k + 1], ALU.is_equal)
            S_.tt("dve", sel, sel, Gall[:, gt, :], ALU.mult)
            S_.red(gk_all[:, gt, k:k + 1], sel, ALU.add)
        S_.ts("dve", idf, mx[:, 0:4], -0.5 * BIGF, ALU.is_gt)
        S_.tt("dve", gk_all[:, gt, :], gk_all[:, gt, :], idf, ALU.mult)
        for k in range(4):
            S_.idma_scatter(x_rows, idx_all[:, gt, k:k + 1], hb_tile, NE * CAP - 1)

    def stage_moe_sparse():
        S_.barrier()
        with ExitStack() as es6:
            A6 = Alloc(nc, es6)
            bguT = A6.sb([128, 16, NE], F32, "bguT")
            with ExitStack() as esb:
                Ab = Alloc(nc, esb)
                bgu_sb = Ab.sb([NE, 2 * D], F32, "bgu_sb")
                bgu_hi = Ab.sb([NE, 2 * D], BF16, "bgu_hi"); bgu_lo = Ab.sb([NE, 2 * D], BF16, "bgu_lo")
                S_.dma("sp", bgu_sb, b_gate_up)
                S_.cp("act", bgu_hi, bgu_sb)
                S_.tt("dve", bgu_lo, bgu_sb, bgu_hi, ALU.subtract)
                for fc in range(16):
                    ps = psum()
                    S_.mm(ps[:, 0:NE], bgu_hi[0:NE, fc * 128:(fc + 1) * 128], identb[0:NE, 0:NE], start=True, stop=False)
                    S_.mm(ps[:, 0:NE], bgu_lo[0:NE, fc * 128:(fc + 1) * 128], identb[0:NE, 0:NE], start=False, stop=True, inc=True)
                    S_.cp("act", bguT[:, fc, :], ps[:, 0:NE])
                S_.barrier()
            with ExitStack() as ese:
                Ae = Alloc(nc, ese)
                NS = CAP // 128
                xe = [Ae.sb([128, NS, D], BF16, "xe") for _ in range(2)]
                xT = [Ae.sb([128, 8, CAP], BF16, "xT") for _ in range(2)]
                wgu = [Ae.sb([128, 8, 2 * D], BF16, "wgu") for _ in range(2)]
                wd = [Ae.sb([128, 8, D], BF16, "wd") for _ in range(2)]
                actT = Ae.sb([128, 8, CAP], BF16, "actT")
                g1 = [Ae.sb([128, 512], F32, "g1") for _ in range(2)]
                s1 = [Ae.sb([128, 512], F32, "s1") for _ in range(2)]
                u1 = [Ae.sb([128, 512], F32, "u1") for _ in range(2)]
                yo = [Ae.sb([128, D], F32, "yo") for _ in range(2)]
                blocks = [(0, 512)] + ([(512, CAP - 512)] if CAP > 512 else [])
                issued = [0]

                wds = [Ae.sb([128, 2, D], F32, "wds") for _ in range(2)]

                def wd_src(e, j):
                    return w_down[e].re("(k p) c -> p k c", p=128)[:, 2 * j:2 * j + 2, :]

                def wd_dma(e, j):
                    S_.dma("sp", wds[j % 2], wd_src(e, j))

                def wd_cast(e, j):
                    S_.cp("act", wd[e % 2][:, 2 * j:2 * j + 2, :], wds[j % 2])

                def issue_weights(upto):
                    while issued[0] <= min(upto, NE - 1):
                        e = issued[0]; issued[0] += 1
                        S_.dma("pool", wgu[e % 2], w_gate_up[e].re("(k p) f -> p k f", p=128))
                        if e == 0:
                            for j in range(4):
                                wd_dma(0, j)
                                wd_cast(0, j)
                        else:
                            wd_dma(e, 0)
                            wd_dma(e, 1)

                def load_x(e):
                    S_.dma("sp", xe[e % 2], x_rows[e * CAP:(e + 1) * CAP, :].re("(n p) d -> p n d", p=128))
                    for n in range(NS):
                        ps = psum().bitcast(BF16)
                        for k in range(8):
                            S_.tr(ps[:, k * 128:(k + 1) * 128], xe[e % 2][:, n, k * 128:(k + 1) * 128], identb, inc=(k == 7))
                        S_.cp("act" if n % 2 else "dve", xT[e % 2][:, :, n * 128:(n + 1) * 128], ps.re("p (k t) -> p k t", k=8))
                it = 0
                load_x(0)
                for e in range(NE):
                    if e + 1 < NE:
                        load_x(e + 1)
                    xTe = xT[e % 2]
                    issue_weights(e + 1)
                    wge = wgu[e % 2]
                    for p in range(4):
                        if e + 1 < NE:
                            wd_cast(e + 1, p)
                            if p + 2 < 4:
                                wd_dma(e + 1, p + 2)
                        for (c0, cn) in blocks:
                            for f2 in range(2):
                                fc = 2 * p + f2
                                i = it % 2; it += 1
                                psg = psum(); psu = psum()
                                for k in range(8):
                                    S_.mm(psg[:, 0:cn], wge[:, k, fc * 128:(fc + 1) * 128], xTe[:, k, c0:c0 + cn],
                                          start=(k == 0), stop=(k == 7), inc=(k == 7))
                                for k in range(8):
                                    S_.mm(psu[:, 0:cn], wge[:, k, D + fc * 128:D + (fc + 1) * 128], xTe[:, k, c0:c0 + cn],
                                          start=(k == 0), stop=(k == 7), inc=(k == 7))
                                gg = g1[i][:, 0:cn]; sg_ = s1[i][:, 0:cn]; uu = u1[i][:, 0:cn]
                                S_.ts("dve", gg, psg[:, 0:cn], bguT[:, fc, e:e + 1], ALU.add, 7.0, ALU.min)
                                S_.act(sg_, gg, AF.Sigmoid, scale=1.702)
                                S_.act(uu, psu[:, 0:cn], AF.Identity, bias=bguT[:, 8 + fc, e:e + 1])
                                S_.ts("pool", uu, uu, 7.0, ALU.min, -7.0, ALU.max)
                                S_.tt("pool", gg, gg, sg_, ALU.mult)
                                S_.stt(actT[:, fc, c0:c0 + cn], uu, 1.0, gg, ALU.add, ALU.mult)
                    wde = wd[e % 2]
                    for n in range(NS):
                        yt_ = yo[n % 2]
                        for half in range(2):
                            ps = psum()
                            for fc in range(8):
                                S_.mm(ps, actT[:, fc, n * 128:(n + 1) * 128], wde[:, fc, half * 512:(half + 1) * 512],
                                      start=(fc == 0), stop=(fc == 7), inc=(fc == 7))
                            S_.cp("act" if half else "dve", yt_[:, half * 512:(half + 1) * 512], ps)
                        r0 = e * CAP + n * 128
                        S_.dma("sp", V(y_rows.ap[r0:r0 + 128, :], [Buf("yrow")]), yt_)
            S_.barrier()
            with ExitStack() as esc:
                Ac = Alloc(nc, esc)
                gam = Ac.sb([128, D], F32, "gam2"); bet = Ac.sb([128, D], F32, "bet2")
                bcast_load(gam, ln2_g); bcast_load(bet, ln2_b)
                bd_sb = Ac.sb([NE, D], F32, "bd_sb")
                S_.dma("sp", bd_sb, b_down)
                GT = Ac.sb([NE, 128], F32, "GT")
                Ghi = Ac.sb([128, NE], BF16, "Ghi"); Glo = Ac.sb([128, NE], BF16, "Glo")
                yk = [[Ac.sb([128, D], F32, "yk") for _ in range(4)] for _ in range(2)]
                h1t = [Ac.sb([128, D], F32, "h1t2") for _ in range(2)]
                zt = Ac.sb([128, D], F32, "zt2"); ot = Ac.sb([128, D], F32, "ot2")
                tmpc = [Ac.sb([128, D], F32, "tmpc") for _ in range(2)]
                tp = dict(st=Ac.sb([128, 2, 6], F32, "st"), mv=Ac.sb([128, 2], F32, "mv"), rs=Ac.sb([128, 1], F32, "rs"))

                def fetch(gt):
                    for k in range(4):
                        S_.idma_gather(yk[gt % 2][k], y_rows, idx_all[:, gt, k:k + 1], NE * CAP - 1)
                    S_.dma("sp", h1t[gt % 2], h1_d[gt // 8][(gt % 8) * 128:(gt % 8 + 1) * 128, :])
                fetch(0)
                for gt in range(NB * NT):
                    if gt + 1 < NB * NT:
                        fetch(gt + 1)
                    S_.cp("act", Ghi, Gall[:, gt, :])
                    S_.tt("dve", Glo, Gall[:, gt, :], Ghi, ALU.subtract)
                    ps = psum()
                    S_.mm(ps[0:NE, 0:128], Ghi, identb, start=True, stop=False)
                    S_.mm(ps[0:NE, 0:128], Glo, identb, start=False, stop=True, inc=True)
                    S_.cp("act", GT, ps[0:NE, 0:128])
                    for half in range(2):
                        ps = psum()
                        S_.mm(ps, GT[0:NE, :], bd_sb[0:NE, half * 512:(half + 1) * 512], inc=True)
                        S_.stt(zt[:, half * 512:(half + 1) * 512], h1t[gt % 2][:, half * 512:(half + 1) * 512], ALPHA, ps, ALU.mult, ALU.add)
                    for k in range(4):
                        S_.act(tmpc[k % 2], yk[gt % 2][k], AF.Copy, scale=gk_all[:, gt, k:k + 1])
                        S_.tt("dve", zt, zt, tmpc[k % 2], ALU.add)
                    layernorm(esc, zt, gam, bet, ot, tp, eng2="dve")
                    S_.dma("sp", out_d[gt * 128:(gt + 1) * 128, :], ot)

    msk = G.sb([128, 5 * 128 + 2], F32, "msk")
    S_.dma("sp", msk, c_masks)
    tri32 = G.sb([128, 2, 128], F32, "tri32")
    S_.dma("sp", tri32, c_tri.re("d p t -> p d t"))
    MS = [msk[:, 0:128], msk[:, 256:384]]
    MI = [msk[:, 128:256], msk[:, 384:512]]
    MID = msk[:, 512:640]
    BLK = msk[:, 640:642]

    def dbg_out(name, src, shape, dt):
        if name in debug:
            dbg[name] = V(nc.dram_tensor("dbg_" + name, list(shape), dt, kind="ExternalOutput").ap(), [Buf("dbg_" + name)])
            S_.dma("sp", dbg[name], src)

    for b in range(NB):
        with ExitStack() as esA:
            A = Alloc(nc, esA)
            hT = A.sb([128, 8, S], BF16, "hT")
            stage_s1(b, hT)
            S_.barrier()
            if b == 0:
                dbg_out("hT", hT, [128, 8, S], BF16)
            yT_r = A.sb([128, 3, S], BF16, "yT_r")
            if "rwkv" in stages:
                stage_rwkv(b, hT, yT_r)
                S_.barrier()
            yT_a = A.sb([128, 3, S], BF16, "yT_a")
            if "att" in stages:
                stage_att(b, hT, yT_a)
                S_.barrier()
            yT_m = A.sb([128, 2, S], BF16, "yT_m")
            if "mem" in stages:
                stage_mem(b, hT, yT_m)
                S_.barrier()
            if b == 0:
                dbg_out("yT_r", yT_r, [128, 3, S], BF16)
                dbg_out("yT_a", yT_a, [128, 3, S], BF16)
                dbg_out("yT_m", yT_m, [128, 2, S], BF16)
            if "out" in stages:
                stage_out(b, [yT_r[:, 0], yT_r[:, 1], yT_r[:, 2], yT_a[:, 0], yT_a[:, 1], yT_a[:, 2], yT_m[:, 0], yT_m[:, 1]])
                S_.barrier()
    if "h1" in debug:
        dbg["h1"] = V(nc.dram_tensor("dbg_h1", [1024, D], F32, kind="ExternalOutput").ap(), [Buf("dbg_h1")])
        S_.dma("sp", dbg["h1"], h1_d[0])
        dbg["G"] = V(nc.dram_tensor("dbg_G", [128, NB * NT, NE], F32, kind="ExternalOutput").ap(), [Buf("dbg_G")])
        S_.dma("sp", dbg["G"], Gall)

    if moe:
        if SPARSE:
            stage_moe_sparse()
        else:
            stage_moe()
    S_.barrier()
    es.close()
    return nc, S_


def host_constants():
    ident = np.eye(128, dtype=np.float32)
    slopes = np.exp2(-8.0 * np.arange(1, H + 1, dtype=np.float32) / H).astype(np.float32)
    x = np.arange(STRIP_W)[None, :]
    p = np.arange(128)[:, None]
    delta = x - p - STRIP_C
    ad = np.abs(delta)
    mult = ((ad <= 64).astype(np.float32) + ((delta % 4 == 0) & (ad <= 256)).astype(np.float32)
            + ((delta % 16 == 0) & (ad <= 1024)).astype(np.float32))
    strip = np.stack([mult * np.exp(-(slopes[h] * ad.astype(np.float32))) for h in range(H)]).astype(np.float32)
    masks = np.zeros((128, 5 * 128 + 2), np.float32)
    si = np.arange(64)[:, None]; ti = np.arange(64)[None, :]
    for q in range(2):
        r = slice(64 * q, 64 * q + 64)
        for n_, m_ in enumerate(((si < ti), (si <= ti), (si > ti), (si >= ti), (si == ti))):
            masks[r, n_ * 128 + 64 * q:n_ * 128 + 64 * q + 64] = m_
        masks[r, 640 + q] = 1.0
    tri = np.zeros((2, 128, 128), np.float32)
    for q in range(2):
        r = slice(64 * q, 64 * q + 64)
        tri[0, r, r] = (si <= ti); tri[1, r, r] = (si >= ti)
    utri = (np.arange(128)[:, None] < np.arange(128)[None, :]).astype(np.float32)
    erow = np.tile((np.arange(NE, dtype=np.float32) * CAP)[None, :], (128, 1))
    return dict(c_ident=ident, c_strip=strip, c_masks=masks, c_tri=tri, c_utri=utri, c_erow=erow)


def make_in_maps(inputs):
    consts = host_constants()
    maps = []
    sq = lambda a: np.ascontiguousarray(a)
    for i in range(8):
        m = dict(consts)
        m["x"] = sq(inputs["x"][2 * i:2 * i + 2].reshape(NB * S, D))
        m["mem"] = sq(inputs["mem"][2 * i:2 * i + 2].reshape(NB * 256, D))
        for k in ("ln_in_g", "ln_in_b"):
            m[k] = sq(inputs[k])
        m["w_in"] = sq(inputs["w_in"][0])
        m["mu_shift"] = sq(inputs["mu_shift"][0])
        m["w0"] = sq(inputs["w0"][0].reshape(-1)); m["w_up"] = sq(inputs["w_up"][0])
        m["a0"] = sq(inputs["a0"][0].reshape(-1)); m["a_up"] = sq(inputs["a_up"][0])
        m["g_up"] = sq(inputs["g_up"][0])
        for k in ("k_k", "k_a", "gn_g", "gn_b", "ln1_g", "ln1_b", "ln2_g", "ln2_b", "b_router",
                  "w_mem_kv", "w_out", "w_router", "w_gate_up", "b_gate_up", "w_down", "b_down"):
            m[k] = sq(inputs[k][0])
        m["r_k"] = sq(inputs["r_k"][0].reshape(-1))
        maps.append(m)
    return maps


def kernel(**inputs):
    inputs = {k: np.asarray(v) for k, v in inputs.items()}
    nc, _ = build_program()
    maps = make_in_maps(inputs)
    res = run_bass_kernel_spmd(nc, maps, core_ids=list(range(8)))
    out = np.stack([r["out"].reshape(NB, S, D) for r in res.results]).reshape(16, S, D)
    return out.astype(np.float32)
```
